# Optimizing a Trainium2 kernel written in Bass

```python
import jax, jax.numpy as jnp
from jax import lax
import numpy as np


D_MODEL = 1024
BATCH = 8
SEQ = 4096
DEPTH = 2

N_HEADS_ATTN = 8
HEAD_DIM = 64
ATTN_WIDTH = N_HEADS_ATTN * HEAD_DIM
ROPE_DIM = HEAD_DIM // 4
ROPE_THETA = 500000.0
IDX_HEADS = 8
IDX_DIM = 64
IDX_ROPE_DIM = IDX_DIM // 4
INDEX_TOPK = 256
Q_BLOCK = 64
LRU_WIDTH = 512
LRU_BLOCKS = 8
LRU_BLOCK_DIM = LRU_WIDTH // LRU_BLOCKS
CONV_WIDTH = 4
LRU_C = 8.0
GMLP_WIDTH = 512
GMLP_GROUPS = 8
GMLP_GROUP_DIM = GMLP_WIDTH // GMLP_GROUPS
CHUNK = 128
N_BRANCH = 3
BRANCH_WIDTH = 512
FFN_DIM = 4 * D_MODEL
PLE_DIM = 256
EPS = 1e-6

IN_SPLITS = (ATTN_WIDTH, ATTN_WIDTH, ATTN_WIDTH, IDX_HEADS * IDX_DIM, IDX_DIM, IDX_HEADS,
             LRU_WIDTH, LRU_WIDTH, GMLP_WIDTH, GMLP_WIDTH, N_BRANCH * D_MODEL)
IN_WIDTH = sum(IN_SPLITS)

kernel_name = 'hybrid_dsa_rglru_gmlp_block'


def rms_norm(x, g):
    xf = x.astype(jnp.float32)
    y = xf * lax.rsqrt(jnp.mean(xf * xf, axis=-1, keepdims=True) + EPS)
    return (y * g.astype(jnp.float32)).astype(x.dtype)


def apply_partial_rope(x, positions, rot_dim):
    half = rot_dim // 2
    inv_freq = ROPE_THETA ** (-jnp.arange(half, dtype=jnp.float32) * 2.0 / rot_dim)
    ang = positions.astype(jnp.float32)[..., None] * inv_freq
    cos = jnp.cos(ang)[:, :, None, :].astype(x.dtype)
    sin = jnp.sin(ang)[:, :, None, :].astype(x.dtype)
    x1 = x[..., :half]
    x2 = x[..., half:rot_dim]
    return jnp.concatenate([x1 * cos - x2 * sin, x2 * cos + x1 * sin, x[..., rot_dim:]], axis=-1)


def dsa_attention(q, k, v, qi, ki, wi):
    B, L = q.shape[0], q.shape[1]
    top_k = min(INDEX_TOPK, L // 4)
    nb = L // Q_BLOCK
    key_pos = jnp.arange(L)
    attn_scale = HEAD_DIM ** -0.5
    idx_scale = (IDX_DIM ** -0.5) * (IDX_HEADS ** -0.5)
    ki32 = ki.astype(jnp.float32)

    def to_blocks(a):
        return jnp.moveaxis(a.reshape((B, nb, Q_BLOCK) + a.shape[2:]), 1, 0)

    def one_block(args):
        qb, qib, wib, qpos = args
        dots = jnp.einsum('bqhd,bsd->bqhs', qib.astype(jnp.float32), ki32)
        score = jnp.einsum('bqh,bqhs->bqs', wib.astype(jnp.float32), jax.nn.relu(dots)) * idx_scale
        causal = key_pos[None, :] <= qpos[:, None]
        score = jnp.where(causal[None], score, -jnp.inf)
        _, sel = lax.top_k(score, top_k)
        valid = sel <= qpos[None, :, None]
        kg = jax.vmap(lambda kb, ib: kb[ib])(k, sel)
        vg = jax.vmap(lambda vb, ib: vb[ib])(v, sel)
        logits = jnp.einsum('bqhd,bqkhd->bqhk', qb, kg).astype(jnp.float32) * attn_scale
        logits = jnp.where(valid[:, :, None, :], logits, -jnp.inf)
        prob = jax.nn.softmax(logits, axis=-1).astype(v.dtype)
        return jnp.einsum('bqhk,bqkhd->bqhd', prob, vg)

    qpos_blocks = jnp.arange(L).reshape(nb, Q_BLOCK)
    out = lax.map(one_block, (to_blocks(q), to_blocks(qi), to_blocks(wi), qpos_blocks))
    return jnp.moveaxis(out, 0, 1).reshape(B, L, ATTN_WIDTH)


def causal_depthwise_conv(x, w, b):
    C = x.shape[-1]
    y = lax.conv_general_dilated(x, w.astype(x.dtype)[:, None, :], window_strides=(1,),
                                 padding=((CONV_WIDTH - 1, 0),),
                                 dimension_numbers=('NWC', 'WIO', 'NWC'),
                                 feature_group_count=C)
    return y + b.astype(x.dtype)


def rg_lru(x, w_a, b_a, w_x, b_x, lam):
    B, L, _ = x.shape
    xb = x.reshape(B, L, LRU_BLOCKS, LRU_BLOCK_DIM)
    r = jax.nn.sigmoid(jnp.einsum('bshi,hij->bshj', xb, w_a).reshape(B, L, LRU_WIDTH) + b_a)
    i = jax.nn.sigmoid(jnp.einsum('bshi,hij->bshj', xb, w_x).reshape(B, L, LRU_WIDTH) + b_x)
    log_a = -LRU_C * r.astype(jnp.float32) * jax.nn.softplus(-lam.astype(jnp.float32))
    a = jnp.exp(log_a)
    inp = jnp.sqrt(-jnp.expm1(2.0 * log_a)) * (i * x).astype(jnp.float32)

    def combine(left, right):
        a1, b1 = left
        a2, b2 = right
        return a1 * a2, a2 * b1 + b2

    _, h = lax.associative_scan(combine, (a, inp), axis=1)
    return h.astype(x.dtype)


def chunked_spatial_gating(u, v, w_s, b_s):
    B, L, _ = u.shape
    nc = L // CHUNK
    vg = v.reshape(B, nc, CHUNK, GMLP_GROUPS, GMLP_GROUP_DIM)
    mask = jnp.tril(jnp.ones((CHUNK, CHUNK), dtype=bool))
    w = jnp.where(mask[None], w_s, 0).astype(v.dtype)
    mixed = jnp.einsum('gts,bcsgd->bctgd', w, vg) + jnp.transpose(b_s).astype(v.dtype)[:, :, None]
    return u * mixed.reshape(B, L, GMLP_WIDTH)


def hybrid_layer(x, p_i, positions, g_pre_mix, w_in, conv_w, conv_b, w_rg_a, b_rg_a, w_rg_x, b_rg_x,
                 lru_lambda, g_gmlp_v, w_spatial, b_spatial, w_branch, w_out, g_post_mix,
                 g_pre_ffn, w_ffn_up, w_ffn_down, g_post_ffn, w_ple, w_ple_gate, g_post_ple):
    B, L, D = x.shape
    h = rms_norm(x, g_pre_mix)
    proj = h @ w_in
    offsets = [int(o) for o in np.cumsum(IN_SPLITS)[:-1]]
    q, k, v, qi, ki, wi, xr, gr, zu, zv, gate_logits = jnp.split(proj, offsets, axis=-1)

    q = apply_partial_rope(q.reshape(B, L, N_HEADS_ATTN, HEAD_DIM), positions, ROPE_DIM)
    k = apply_partial_rope(k.reshape(B, L, N_HEADS_ATTN, HEAD_DIM), positions, ROPE_DIM)
    v = v.reshape(B, L, N_HEADS_ATTN, HEAD_DIM)
    qi = apply_partial_rope(qi.reshape(B, L, IDX_HEADS, IDX_DIM), positions, IDX_ROPE_DIM)
    ki = apply_partial_rope(ki[:, :, None, :], positions, IDX_ROPE_DIM)[:, :, 0, :]
    y_a = dsa_attention(q, k, v, qi, ki, wi)

    xr = causal_depthwise_conv(xr, conv_w, conv_b)
    y_b = rg_lru(xr, w_rg_a, b_rg_a, w_rg_x, b_rg_x, lru_lambda) * jax.nn.gelu(gr)

    y_c = chunked_spatial_gating(jax.nn.gelu(zu), rms_norm(jax.nn.gelu(zv), g_gmlp_v), w_spatial, b_spatial)

    ys = jnp.stack([y_a, y_b, y_c], axis=0)
    branch = jnp.einsum('nbsw,nwd->nbsd', ys, w_branch)
    gates = jax.nn.sigmoid(gate_logits.reshape(B, L, N_BRANCH, D))
    merged = jnp.einsum('bsnd,nbsd->bsd', gates, branch)
    x = x + rms_norm(merged @ w_out, g_post_mix)

    h2 = rms_norm(x, g_pre_ffn)
    f = jnp.square(jax.nn.relu(h2 @ w_ffn_up)) @ w_ffn_down
    x = x + rms_norm(f, g_post_ffn)

    ple = (p_i.astype(x.dtype) @ w_ple) * jax.nn.sigmoid(x @ w_ple_gate)
    return x + rms_norm(ple, g_post_ple)


def setup_inputs(seed: int = 0) -> dict:
    key = jax.random.key(seed)
    ks = jax.random.split(key, 32)
    f32 = jnp.float32

    def nrm(k, shape, fan_in):
        return jax.random.normal(k, shape, f32) * (fan_in ** -0.5)

    def gain(k, n):
        return 1.0 + 0.05 * jax.random.normal(k, (DEPTH, n), f32)

    u = jax.random.uniform(ks[10], (DEPTH, LRU_WIDTH), f32, 0.9, 0.999)
    base = u ** (1.0 / LRU_C)
    lru_lambda = jnp.log(base) - jnp.log1p(-base)
    return {
        'x': jax.random.normal(ks[0], (BATCH, SEQ, D_MODEL), f32),
        'p': jax.random.normal(ks[1], (DEPTH, BATCH, SEQ, PLE_DIM), f32),
        'positions': jnp.broadcast_to(jnp.arange(SEQ, dtype=jnp.int32), (BATCH, SEQ)),
        'g_pre_mix': gain(ks[2], D_MODEL),
        'w_in': nrm(ks[3], (DEPTH, D_MODEL, IN_WIDTH), D_MODEL),
        'conv_w': nrm(ks[4], (DEPTH, CONV_WIDTH, LRU_WIDTH), CONV_WIDTH),
        'conv_b': 0.01 * jax.random.normal(ks[5], (DEPTH, LRU_WIDTH), f32),
        'w_rg_a': nrm(ks[6], (DEPTH, LRU_BLOCKS, LRU_BLOCK_DIM, LRU_BLOCK_DIM), LRU_BLOCK_DIM),
        'b_rg_a': 0.01 * jax.random.normal(ks[7], (DEPTH, LRU_WIDTH), f32),
        'w_rg_x': nrm(ks[8], (DEPTH, LRU_BLOCKS, LRU_BLOCK_DIM, LRU_BLOCK_DIM), LRU_BLOCK_DIM),
        'b_rg_x': 0.01 * jax.random.normal(ks[9], (DEPTH, LRU_WIDTH), f32),
        'lru_lambda': lru_lambda,
        'g_gmlp_v': gain(ks[11], GMLP_WIDTH),
        'w_spatial': nrm(ks[12], (DEPTH, GMLP_GROUPS, CHUNK, CHUNK), CHUNK),
        'b_spatial': 1.0 + 0.1 * jax.random.normal(ks[13], (DEPTH, GMLP_GROUPS, CHUNK), f32),
        'w_branch': nrm(ks[14], (DEPTH, N_BRANCH, BRANCH_WIDTH, D_MODEL), BRANCH_WIDTH),
        'w_out': nrm(ks[15], (DEPTH, D_MODEL, D_MODEL), D_MODEL),
        'g_post_mix': gain(ks[16], D_MODEL),
        'g_pre_ffn': gain(ks[17], D_MODEL),
        'w_ffn_up': nrm(ks[18], (DEPTH, D_MODEL, FFN_DIM), D_MODEL),
        'w_ffn_down': nrm(ks[19], (DEPTH, FFN_DIM, D_MODEL), FFN_DIM),
        'g_post_ffn': gain(ks[20], D_MODEL),
        'w_ple': nrm(ks[21], (DEPTH, PLE_DIM, D_MODEL), PLE_DIM),
        'w_ple_gate': nrm(ks[22], (DEPTH, D_MODEL, D_MODEL), D_MODEL),
        'g_post_ple': gain(ks[23], D_MODEL),
    }


def reference(x, p, positions, g_pre_mix, w_in, conv_w, conv_b, w_rg_a, b_rg_a, w_rg_x, b_rg_x,
              lru_lambda, g_gmlp_v, w_spatial, b_spatial, w_branch, w_out, g_post_mix,
              g_pre_ffn, w_ffn_up, w_ffn_down, g_post_ffn, w_ple, w_ple_gate, g_post_ple):
    for i in range(DEPTH):
        x = hybrid_layer(x, p[i], positions, g_pre_mix[i], w_in[i], conv_w[i], conv_b[i],
                         w_rg_a[i], b_rg_a[i], w_rg_x[i], b_rg_x[i], lru_lambda[i], g_gmlp_v[i],
                         w_spatial[i], b_spatial[i], w_branch[i], w_out[i], g_post_mix[i],
                         g_pre_ffn[i], w_ffn_up[i], w_ffn_down[i], g_post_ffn[i],
                         w_ple[i], w_ple_gate[i], g_post_ple[i])
    return x
```

```python
import math
from contextlib import ExitStack
import numpy as np
import ml_dtypes
import concourse.bass as bass
import concourse.mybir as mybir
from concourse.bass_utils import run_bass_kernel_spmd

F32 = mybir.dt.float32
BF16 = mybir.dt.bfloat16
I32 = mybir.dt.int32
AF = mybir.ActivationFunctionType
ALU = mybir.AluOpType
AX = mybir.AxisListType

D = 1024
NH = 8
HD = 64
TOPK = 256
FFN = 4096
PLE = 256
EPS = 1e-6
DEPTH = 2
T = 256
NSUB = T // 128
KG = 512
NSLOT = 3
SLOTB = 4224
NBIS = 16
BIGM = 30000.0
NEG = -1.0e30
IN_OFF = dict(q=0, k=512, v=1024, qi=1536, kiwi=2048, xr=2120, gr=2632, zu=3144, zv=3656, gate=4168)


class Sem:
    def __init__(self, h, name):
        self.h = h
        self.n = 0
        self.name = name


class Tile:
    __slots__ = ("w", "r", "name")

    def __init__(self, name=""):
        self.w = {}
        self.r = {}
        self.name = name


class Buf:
    def __init__(self, t, name=""):
        self.t = t
        self.T = Tile(name)

    def __getitem__(self, k):
        return self.t[k]


class Engine:
    def __init__(self, name, sem):
        self.name = name
        self.sem = sem
        self.ops = []
        self.seen = {}


class FW:
    def __init__(self, nc, stack):
        self.nc = nc
        self.stack = stack
        self.engs = {}
        for n in ("tensor", "vector", "scalar", "gpsimd", "sync"):
            s = Sem(stack.enter_context(nc.semaphore("sem_" + n)), n)
            self.engs[n] = Engine(n, s)
        self.nops = 0

    def dsem(self, name):
        return Sem(self.stack.enter_context(self.nc.semaphore("dsem_" + name)), name)

    def op(self, eng, fn, reads=(), writes=(), dsem=None):
        E = self.engs[eng]
        need = {}
        for b in reads:
            t = b.T if isinstance(b, Buf) else b
            for s, v in t.w.items():
                if need.get(s, 0) < v:
                    need[s] = v
        for b in writes:
            t = b.T if isinstance(b, Buf) else b
            for s, v in t.w.items():
                if need.get(s, 0) < v:
                    need[s] = v
            for s, v in t.r.items():
                if need.get(s, 0) < v:
                    need[s] = v
        raw_self = 0
        for b in reads:
            t = b.T if isinstance(b, Buf) else b
            raw_self = max(raw_self, t.w.get(E.sem, 0))
        waits = []
        for s, v in need.items():
            if s is E.sem:
                if eng != "tensor" and raw_self > E.seen.get(s, 0):
                    E.seen[s] = raw_self
                    waits.append((s, raw_self))
                continue
            if E.seen.get(s, 0) >= v:
                continue
            E.seen[s] = v
            waits.append((s, v))
        if dsem is not None:
            dsem.n += 16
            sig = (dsem, dsem.n, 16)
        else:
            E.sem.n += 1
            sig = (E.sem, E.sem.n, 1)
        E.ops.append((waits, fn, sig))
        self.nops += 1
        s, v = sig[0], sig[1]
        for b in reads:
            t = b.T if isinstance(b, Buf) else b
            if t.r.get(s, 0) < v:
                t.r[s] = v
        for b in writes:
            t = b.T if isinstance(b, Buf) else b
            if t.w.get(s, 0) < v:
                t.w[s] = v

    def finish(self, eng, tiles):
        E = self.engs[eng]
        need = {}
        for b in tiles:
            t = b.T if isinstance(b, Buf) else b
            for d in (t.w, t.r):
                for s, v in d.items():
                    if need.get(s, 0) < v:
                        need[s] = v
        E.ops.append(([(s, v) for s, v in need.items() if s is not E.sem], None, None))

    def emit(self):
        with self.nc.Block() as block:
            for n, E in self.engs.items():
                def body(e, E=E):
                    for waits, fn, sig in E.ops:
                        for s, v in waits:
                            e.wait_ge(s.h, v)
                        if fn is None:
                            continue
                        fn(e).then_inc(sig[0].h, sig[2])
                getattr(block, n)(body)


def panel_defs():
    P = []
    for nm in ("q", "k", "qi"):
        P.append((nm, "w_in", 0, 1024, IN_OFF[nm], 512))
    P.append(("kiwi", "w_in", 0, 1024, IN_OFF["kiwi"], 72))
    for nm in ("xr", "gr", "zu", "v", "zv"):
        P.append((nm, "w_in", 0, 1024, IN_OFF[nm], 512))
    for n in range(3):
        for hf in range(2):
            P.append(("gate%d_%d" % (n, hf), "w_in", 0, 1024, IN_OFF["gate"] + n * 1024 + hf * 512, 512))
    for n in range(3):
        for hf in range(2):
            P.append(("br%d_%d" % (n, hf), "w_branch%d" % n, 0, 512, hf * 512, 512))
    for hf in range(2):
        P.append(("out_%d" % hf, "w_out", 0, 1024, hf * 512, 512))
    for g in range(8):
        P.append(("up_%d" % g, "w_ffn_up", 0, 1024, g * 512, 512))
        P.append(("dn_%d" % g, "w_ffn_down", g * 512, 512, 0, 1024))
    P.append(("ple", "w_ple", 0, 256, 0, 1024))
    for hf in range(2):
        P.append(("pg_%d" % hf, "w_ple_gate", 0, 1024, hf * 512, 512))
    return P


def build_program(L, depth=DEPTH, dbg=None):
    NT = L // T
    nc = bass.Bass("TRN2", target_bir_lowering=False)
    dram = lambda name, shape, dt, kind="ExternalInput": nc.dram_tensor(name, shape, dt, kind=kind).ap()
    x_in = dram("x", [L, D], F32)
    p_in = dram("p", [depth, L, PLE], F32)
    pos_in = dram("pos", [128, L], I32)
    wsrc = {
        "w_in": dram("w_in", [depth, D, 7240], F32),
        "w_branch": dram("w_branch", [depth, 3, 512, D], F32),
        "w_out": dram("w_out", [depth, D, D], F32),
        "w_ffn_up": dram("w_ffn_up", [depth, D, FFN], F32),
        "w_ffn_down": dram("w_ffn_down", [depth, FFN, D], F32),
        "w_ple": dram("w_ple", [depth, PLE, D], F32),
        "w_ple_gate": dram("w_ple_gate", [depth, D, D], F32),
    }
    cols_in = dram("cols", [depth, 128, 48], F32)
    gb_in = dram("gb", [depth, 128, 3 * D + 512], F32)
    wbd_in = dram("wbd", [depth, 128, 8, 128], F32)
    wsp_in = dram("wsp", [depth, 128, 8, 128], F32)
    bsp_in = dram("bsp", [depth, 8, 128], F32)
    cb_in = dram("cb", [128, 256 + 512], BF16)
    cf_in = dram("cf", [128, 4 * 128 + 1], F32)
    y_out = dram("y", [L, D], F32, kind="ExternalOutput")
    xbuf = dram("xbuf", [L, D], F32, kind="Internal")
    KTc = [dram("ktc%d" % l, [4, 128, L], BF16, kind="Internal") for l in range(depth)]
    Vc = [dram("vc%d" % l, [L, 520], BF16, kind="Internal") for l in range(depth)]
    pdefs = panel_defs()
    Wp = [{nm: dram("wp%d_%s" % (l, nm), [nr, ncol], BF16, kind="Internal") for (nm, _, _, nr, _, ncol) in pdefs}
          for l in range(depth)]
    dbg_out = {}
    if dbg:
        for k, shp in dbg.items():
            dbg_out[k] = dram("dbg_" + k, list(shp), F32, kind="ExternalOutput")

    with ExitStack() as st:
        fw = FW(nc, st)
        op = fw.op

        def sb(name, shape, dt):
            return Buf(st.enter_context(nc.sbuf_tensor("s_" + name, shape, dt)), name)

        PS = [Buf(st.enter_context(nc.psum_tensor("ps%d" % i, [128, 512], F32)), "ps%d" % i) for i in range(8)]

        def mm(out, lhsT, rhs, start, stop, R, W):
            op("tensor", lambda e: e.matmul(out, lhsT=lhsT, rhs=rhs, start=start, stop=stop), R, W)

        def tr(out, in_, ident, R, W):
            op("tensor", lambda e: e.transpose(out=out, in_=in_, identity=ident), R, W)

        def act(out, in_, func, R, W, **kw):
            op("scalar", lambda e: e.activation(out=out, in_=in_, func=func, **kw), R, W)

        def ts(out, in0, s1, s2, op0, op1, R, W, eng="vector"):
            if op1 is None:
                op(eng, lambda e: e.tensor_scalar(out=out, in0=in0, scalar1=s1, scalar2=None, op0=op0), R, W)
            else:
                op(eng, lambda e: e.tensor_scalar(out=out, in0=in0, scalar1=s1, scalar2=s2, op0=op0, op1=op1), R, W)

        def tt(out, in0, in1, o, R, W, eng="vector"):
            op(eng, lambda e: e.tensor_tensor(out=out, in0=in0, in1=in1, op=o), R, W)

        def stt(out, in0, s, in1, op0, op1, R, W):
            op("vector", lambda e: e.scalar_tensor_tensor(out=out, in0=in0, scalar=s, in1=in1, op0=op0, op1=op1), R, W)

        def cp(out, in_, R, W, eng="vector"):
            op(eng, lambda e: e.tensor_copy(out=out, in_=in_), R, W)

        def dma(out, in_, R, W, ds, eng="sync"):
            op(eng, lambda e: e.dma_start(out=out, in_=in_), R, W, dsem=ds)

        def memset(ap, val, W, eng="vector"):
            op(eng, lambda e: e.memset(ap, val), [], W)

        def reduce(out, in_, o, R, W):
            op("vector", lambda e: e.tensor_reduce(out=out, in_=in_, axis=AX.X, op=o), R, W)

        def recip(out, in_, R, W):
            op("vector", lambda e: e.reciprocal(out=out, in_=in_), R, W)

        def scan(out, d0, d1, init, R, W):
            op("vector", lambda e: e.tensor_tensor_scan(out=out, data0=d0, data1=d1, initial=init,
                                                        op0=ALU.mult, op1=ALU.add), R, W)

        def count_ge(out, in0, thr, cnt, R, W):
            op("vector", lambda e: e.tensor_scalar(out=out, in0=in0, scalar1=thr, scalar2=None, op0=ALU.is_ge,
                                                   op1=ALU.add, accum_out=cnt), R, W)

        def cpred(out, mask, data, R, W):
            op("vector", lambda e: e.copy_predicated(out=out, mask=mask, data=data), R, W)

        dbg_sem = fw.dsem("dbg")
        dbg_tiles = []

        def dump(name, ap, R):
            if name in dbg_out:
                t = Tile("dbg")
                dma(dbg_out[name], ap, R, [t], dbg_sem, eng="gpsimd")
                dbg_tiles.append(t)
                del dbg_out[name]

        cb = sb("cb", [128, 768], BF16)
        cf = sb("cf", [128, 513], F32)
        dma(cb[:], cb_in, [], [cb], fw.dsem("c0"))
        dma(cf[:], cf_in, [], [cf], fw.dsem("c1"))
        identb = cb[:, 0:128]
        Rm = cb[:, 128:256]
        esel = cb[0:8, 256:768].rearrange("p (c f) -> p c f", c=4)
        identf = cf[:, 0:128]
        negmask = cf[:, 128:256]
        posfill = cf[:, 256:384]
        tril01 = cf[:, 384:512]
        invf = cf[:, 512:513]

        WT = [dict() for _ in range(depth)]
        for l in range(depth):
            wcs = fw.dsem("wcast%d" % l)
            for (nm, src, r0, nr, c0, ncol) in pdefs:
                if src.startswith("w_branch"):
                    s_ap = wsrc["w_branch"][l, int(src[-1]), r0:r0 + nr, c0:c0 + ncol]
                else:
                    s_ap = wsrc[src][l, r0:r0 + nr, c0:c0 + ncol]
                t = Tile("wp")
                WT[l][nm] = t
                step = 512
                for rr in range(0, nr, step):
                    n2 = min(step, nr - rr)
                    dma(Wp[l][nm][rr:rr + n2, :], s_ap[rr:rr + n2, :], [], [t], wcs, eng="gpsimd")
            for t in WT[l].values():
                t.w = {wcs: wcs.n}

        ring = [sb("ring%d" % i, [128, SLOTB], BF16) for i in range(NSLOT)]
        ring_sem = [fw.dsem("ring%d" % i) for i in range(NSLOT)]
        kiT = sb("kiT", [128, L], BF16)
        xt = sb("xt", [128, NSUB, D], F32)
        pt = sb("pt", [128, NSUB, PLE], F32)
        ptb = sb("ptb", [128, NSUB, PLE], BF16)
        posi = sb("posi", [128, T], I32)
        hT = sb("hT", [128, 8, T], BF16)
        pT = sb("pT", [128, 2, T], BF16)
        qT = sb("qT", [128, 4, T], BF16)
        qiT = sb("qiT", [128, 4, T], BF16)
        KTs = sb("KTs", [128, 4, T], BF16)
        Vs = sb("Vs", [128, NSUB, 520], BF16)
        cosT = sb("cosT", [128, T], F32)
        sinT = sb("sinT", [128, T], F32)
        rtmp = sb("rtmp", [128, 3, T], F32)
        xb16 = sb("xb16", [128, T], BF16)
        xrbuf = sb("xrbuf", [128, 4, 3 + T], F32)
        grT = sb("grT", [128, 4, T], BF16)
        zuT = sb("zuT", [128, 4, T], BF16)
        gz = sb("gz", [128, 512], F32)
        vn = sb("vn", [128, NSUB, 512], BF16)
        lt = sb("lt", [128, 5, T], F32)
        xcb = sb("xcb", [128, T], BF16)
        hst = sb("hst", [128, 4], F32)
        ybT = sb("ybT", [128, 4, T], BF16)
        ycT = sb("ycT", [128, 4, T], BF16)
        yaT = sb("yaT", [128, 4, T], BF16)
        yaTt = sb("yaTt", [64, T], BF16)
        wis = sb("wis", [128, NSUB, 8], F32)
        diag = sb("diag", [128, 8, 128], BF16)
        small = sb("small", [128, 32], F32)
        smalli = sb("smalli", [128, 4], I32)
        bis = sb("bis", [128, 16], F32)
        big = sb("big", [128, 6144], F32)
        TS_ = big.T
        TR_ = Tile("bigR")
        score_ap = big[:, 0:L]
        Rrelu = big[:, 4096:6144].bitcast(BF16).rearrange("p (h w) -> p h w", h=8)
        masks = [sb("mask%d" % s, [128, L], BF16) for s in range(NSUB)]
        maskTk = sb("maskTk", [128, 4, T], BF16)
        PT = [sb("PT%d" % i, [128, 2, T], BF16) for i in range(2)]
        xs = sb("xs", [128, D], BF16)
        acc = sb("acc", [65, 8, T], F32)
        rhl = sb("rhl", [65, 2, T], BF16)
        onesb = sb("onesb", [65, 64], BF16)
        gt = sb("gt", [128, T], F32)
        tmpf = sb("tmpf", [128, 512], F32)
        gsig = sb("gsig", [128, 8, T], BF16)
        merged = big[:, 0:8 * T].rearrange("p (c t) -> p c t", c=8)
        o = 8 * T
        mergedT = big[:, o:o + 4 * T].bitcast(BF16).rearrange("p (c t) -> p c t", c=8)
        o += 4 * T
        fT = [big[:, o + i * 2 * T:o + (i + 1) * 2 * T].bitcast(BF16).rearrange("p (c t) -> p c t", c=4) for i in range(2)]
        o += 4 * T
        assert o <= 4096
        o = 4096
        rl = [big[:, o + i * (T // 2):o + (i + 1) * (T // 2)].bitcast(BF16) for i in range(2)]
        o += T
        sg = big[:, o:o + 512]
        o += 512
        ple_t = big[:, o:o + 1024]
        o += 1024
        assert o <= 6144
        wbdf = big[:, 4096:4096 + 1024].rearrange("p (c j) -> p c j", c=8)
        cols = sb("cols", [128, 48], F32)
        gb = sb("gb", [128, 3 * D + 512], F32)
        wbd = sb("wbd", [128, 8, 128], BF16)
        wspT = sb("wspT", [128, 8, 128], BF16)
        bsp = sb("bsp", [8, 128], F32)
        bsph = sb("bsph", [8, 2, 128], BF16)
        c8 = sb("c8", [128, 8], F32)
        epsc = sb("epsc", [128, 4], F32)
        psem = [fw.dsem("par%d" % i) for i in range(5)]
        xsem = fw.dsem("xload")
        ptsem = fw.dsem("pload")
        possem = fw.dsem("posload")
        ssem = fw.dsem("store")
        kvsem = fw.dsem("kvstore")
        out_tile = Tile("out")

        class Stream:
            def __init__(self):
                self.plan = []
                self.issued = 0
                self.pos = 0

            def add(self, name, loader, deps, hoist=True):
                self.plan.append((name, loader, deps, hoist))

            def next(self, name, look=NSLOT - 1):
                i = self.pos
                assert self.plan[i][0] == name, (self.plan[i][0], name)
                while self.issued < len(self.plan) and (
                        self.issued <= i or (self.issued <= i + look and self.plan[self.issued][3])):
                    k = self.issued
                    _, loader, deps, _ = self.plan[k]
                    loader(ring[k % NSLOT], ring_sem[k % NSLOT], deps)
                    self.issued += 1
                self.pos += 1
                return ring[i % NSLOT]

        stream = Stream()

        def wloader(l, nm, nr, ncol):
            kc = nr // 128

            def f(slot, sem, deps):
                dst = slot[:, 0:kc * ncol].rearrange("p (k w) -> p k w", k=kc)
                src = Wp[l][nm].rearrange("(k p) w -> p k w", p=128)
                dma(dst, src, deps, [slot], sem)
            return f

        KVT = [[Tile("kv") for _ in range((L + KG - 1) // KG)] for _ in range(depth)]

        def kvloader(l, g, wd):
            def f(slot, sem, deps):
                dstk = slot[:, 0:4 * wd].rearrange("p (c w) -> p c w", c=4)
                dma(dstk, KTc[l][:, :, g * KG:g * KG + wd].rearrange("c p w -> p c w"), deps, [slot], sem)
                nb = wd // 128
                dstv = slot[:, 2048:2048 + nb * 520].rearrange("p (b f) -> p b f", b=nb)
                dma(dstv, Vc[l][g * KG:g * KG + wd, :].rearrange("(b p) f -> p b f", p=128), deps, [slot], sem)
            return f

        pinfo = {nm: (nr, ncol) for (nm, _, _, nr, _, ncol) in pdefs}

        def plan_w(l, nm):
            nr, ncol = pinfo[nm]
            stream.add("%d_%s" % (l, nm), wloader(l, nm, nr, ncol), [WT[l][nm]])

        def kv_groups(j):
            nkeys = (j + 1) * T
            out = []
            g = 0
            while g * KG < nkeys:
                out.append((g, min(KG, nkeys - g * KG)))
                g += 1
            return out

        for l in range(depth):
            for j in range(NT):
                for nm in ("q", "k", "qi", "kiwi", "xr", "gr", "zu", "v", "zv"):
                    plan_w(l, nm)
                grps = kv_groups(j)
                for (g, wd) in grps:
                    last = (g == grps[-1][0])
                    stream.add("%d_kv%d_%d" % (l, j, g), kvloader(l, g, wd), [KVT[l][g]], hoist=not last)
                for n in (2, 1, 0):
                    plan_w(l, "gate%d_0" % n)
                    plan_w(l, "gate%d_1" % n)
                    plan_w(l, "br%d_0" % n)
                    plan_w(l, "br%d_1" % n)
                plan_w(l, "out_0")
                plan_w(l, "out_1")
                for g in range(8):
                    plan_w(l, "up_%d" % g)
                    plan_w(l, "dn_%d" % g)
                plan_w(l, "ple")
                plan_w(l, "pg_0")
                plan_w(l, "pg_1")

        def wview(slot, nm):
            nr, ncol = pinfo[nm]
            kc = nr // 128
            return slot[:, 0:kc * ncol].rearrange("p (k w) -> p k w", k=kc)

        sm_i = [0]

        def sm():
            i = sm_i[0] % 32
            sm_i[0] += 1
            return small[:, i:i + 1]

        memset(epsc[:, 0:1], EPS, [epsc])
        memset(epsc[:, 1:2], math.pi / 2, [epsc])
        memset(epsc[:, 2:3], 1.0, [epsc])
        memset(epsc[:, 3:4], -BIGM, [epsc])
        memset(Vs[:, :, :], 1.0, [Vs])
        memset(onesb[:, :], 1.0, [onesb])

        def rstd_from_ss(ss_ap, n):
            a = sm()
            b = sm()
            act(a, ss_ap, AF.Sqrt, [small, epsc], [small], scale=1.0 / n, bias=epsc[:, 0:1])
            recip(b, a, [small], [small])
            return b

        pi = [0]

        def nps():
            pi[0] += 1
            return PS[pi[0] % 4]

        def norm_transpose(gcol0):
            for s in range(NSUB):
                if gcol0 is not None:
                    ss = sm()
                    act(tmpf[:, 0:512], xt[:, s, 0:512], AF.Square, [xt, small], [tmpf, small], accum_out=ss)
                    ss2 = sm()
                    act(tmpf[:, 0:512], xt[:, s, 512:1024], AF.Square, [xt, small], [tmpf, small], accum_out=ss2)
                    ss3 = sm()
                    tt(ss3, ss, ss2, ALU.add, [small], [small])
                    rs = rstd_from_ss(ss3, D)
                    ts(xs[:], xt[:, s, :], rs, None, ALU.mult, None, [xt, small], [xs])
                else:
                    cp(xs[:], xt[:, s, :], [xt], [xs])
                psb = PS[7][:].bitcast(BF16)
                for c in range(8):
                    tr(psb[:, c * 128:(c + 1) * 128], xs[:, c * 128:(c + 1) * 128], identb, [xs, cb], [PS[7]])
                src = psb[:, 0:1024].rearrange("p (c t) -> p c t", c=8)
                dst = hT[:, :, s * 128:(s + 1) * 128]
                if gcol0 is not None:
                    g_ap = cols[:, gcol0:gcol0 + 8].unsqueeze(2).to_broadcast([128, 8, 128])
                    tt(dst, src, g_ap, ALU.mult, [PS[7], cols], [hT])
                else:
                    cp(dst, src, [PS[7]], [hT])

        def post_norm_residual(banks, gcol, s):
            ssa = []
            for hf in range(2):
                a = sm()
                act(tmpf[:, 0:512], banks[hf][:, 0:512], AF.Square, [banks[hf], small], [tmpf, small], accum_out=a)
                ssa.append(a)
            s3 = sm()
            tt(s3, ssa[0], ssa[1], ALU.add, [small], [small])
            rs = rstd_from_ss(s3, D)
            for hf in range(2):
                stt(tmpf[:, 0:512], banks[hf][:, 0:512], rs, gb[:, gcol + hf * 512:gcol + (hf + 1) * 512],
                    ALU.mult, ALU.mult, [banks[hf], small, gb], [tmpf])
                tt(xt[:, s, hf * 512:(hf + 1) * 512], xt[:, s, hf * 512:(hf + 1) * 512], tmpf[:, 0:512], ALU.add,
                   [xt, tmpf], [xt], eng="gpsimd")

        def fm_chunk(panel, pv, c, ps, M=128):
            for kc in range(8):
                mm(ps[0:M, 0:T], pv[:, kc, c * 128:c * 128 + M], hT[:, kc, :], kc == 0, kc == 7, [panel, hT], [ps])

        def rope_evac(ps, dst, W):
            act(xb16[:], ps[:, 0:T], AF.Copy, [ps], [xb16])
            mm(ps[:, T:2 * T], Rm, xb16[:], True, True, [cb, xb16], [ps])
            tt(rtmp[:, 0, :], ps[:, 0:T], cosT[:], ALU.mult, [ps, cosT], [rtmp])
            tt(rtmp[:, 1, :], ps[:, T:2 * T], sinT[:], ALU.mult, [ps, sinT], [rtmp])
            tt(dst, rtmp[:, 0, :], rtmp[:, 1, :], ALU.add, [rtmp], W)

        XTprev = None
        for l in range(depth):
            src_x = x_in if l == 0 else xbuf
            dst_x = y_out if l == depth - 1 else xbuf
            XT = [Tile("xd") for _ in range(NT)] if l < depth - 1 else None
            dma(cols[:], cols_in[l], [], [cols], psem[0])
            dma(gb[:], gb_in[l], [], [gb], psem[1])
            dma(bsp[:], bsp_in[l], [], [bsp], psem[2])
            cp(bsph[:, 0, :], bsp[:], [bsp], [bsph])
            tt(bsp[:], bsp[:], bsph[:, 0, :], ALU.subtract, [bsph], [bsp])
            cp(bsph[:, 1, :], bsp[:], [bsp], [bsph])
            dma(wbdf, wbd_in[l], [], [TR_], psem[3])
            cp(wbd[:], wbdf, [TR_], [wbd])
            dma(wbdf, wsp_in[l], [], [TR_], psem[4])
            tt(wspT[:], wbdf, tril01.unsqueeze(1).to_broadcast([128, 8, 128]), ALU.mult, [TR_, cf], [wspT])
            act(c8[:, 4:8], cols[:, 44:48], AF.Exp, [cols], [c8], scale=-1.0)
            ts(c8[:, 0:4], c8[:, 4:8], -0.25, 1.0 / 3.0, ALU.mult, ALU.add, [c8], [c8])
            tt(c8[:, 0:4], c8[:, 0:4], c8[:, 4:8], ALU.mult, [c8], [c8])
            ts(c8[:, 0:4], c8[:, 0:4], -1.0, 0.5, ALU.mult, ALU.add, [c8], [c8])
            tt(c8[:, 0:4], c8[:, 0:4], c8[:, 4:8], ALU.mult, [c8], [c8])
            ts(c8[:, 0:4], c8[:, 0:4], -1.0, 1.0, ALU.mult, ALU.add, [c8], [c8])
            tt(c8[:, 0:4], c8[:, 0:4], c8[:, 4:8], ALU.mult, [c8], [c8])
            ts(c8[:, 0:4], c8[:, 0:4], -8.0, None, ALU.mult, None, [c8], [c8])
            ts(c8[:, 4:8], c8[:, 0:4], 2.0, None, ALU.mult, None, [c8], [c8])
            memset(xrbuf[:, :, 0:3], 0.0, [xrbuf])
            memset(hst[:], 0.0, [hst])

            for j in range(NT):
                t0 = j * T
                first_tile = (l == 0 and j == 0)
                rd = [XTprev[j]] if (l > 0) else []
                dma(xt[:], src_x[t0:t0 + T, :].rearrange("(s p) d -> p s d", p=128), rd, [xt], xsem)
                dma(pt[:], p_in[l, t0:t0 + T, :].rearrange("(s p) d -> p s d", p=128), [], [pt], ptsem)
                dma(posi[:], pos_in[:, t0:t0 + T], [], [posi], possem)
                ang = rtmp[:, 0, :]
                u = rtmp[:, 1, :]
                rr = rtmp[:, 2, :]
                cp(u, posi[:], [posi], [rtmp])
                ts(ang, u, invf, None, ALU.mult, None, [rtmp, cf], [rtmp])
                ts(u, ang, 1.0 / (2 * math.pi), 12582912.0, ALU.mult, ALU.add, [rtmp], [rtmp])
                ts(u, u, -12582912.0, None, ALU.add, None, [rtmp], [rtmp])
                C1 = 6.28125
                C2 = float(np.float32(2 * math.pi - C1))
                C3 = float(2 * math.pi - C1 - C2)
                stt(rr, u, -C1, ang, ALU.mult, ALU.add, [rtmp], [rtmp])
                stt(rr, u, -C2, rr, ALU.mult, ALU.add, [rtmp], [rtmp])
                stt(rr, u, -C3, rr, ALU.mult, ALU.add, [rtmp], [rtmp])
                act(sinT[:], rr, AF.Sin, [rtmp], [sinT])
                stt(u, rr, -1.0, rr, ALU.mult, ALU.max, [rtmp], [rtmp])
                act(cosT[:], u, AF.Sin, [rtmp, epsc], [cosT], scale=-1.0, bias=epsc[:, 1:2])
                norm_transpose(0)
                if first_tile:
                    dump("hT", hT[:, 0, :], [hT])
                    dump("cosT", cosT[:], [cosT])
                    dump("sinT", sinT[:], [sinT])

                for nm, dstb in (("q", qT), ("k", KTs), ("qi", qiT)):
                    panel = stream.next("%d_%s" % (l, nm))
                    pv = wview(panel, nm)
                    for c in range(4):
                        ps = nps()
                        fm_chunk(panel, pv, c, ps)
                        rope_evac(ps, dstb[:, c, :], [dstb])
                panel = stream.next("%d_kiwi" % l)
                pv = wview(panel, "kiwi")
                ps = nps()
                for half in range(2):
                    for kc in range(8):
                        mm(ps[half * 64:(half + 1) * 64, 0:T], pv[:, kc, 0:64], hT[:, kc, :], kc == 0, kc == 7,
                           [panel, hT], [ps])
                rope_evac(ps, kiT[:, t0:t0 + T], [kiT])
                for s in range(NSUB):
                    ps = nps()
                    for kc in range(8):
                        mm(ps[:, 0:8], hT[:, kc, s * 128:(s + 1) * 128], pv[:, kc, 64:72], kc == 0, kc == 7,
                           [panel, hT], [ps])
                    cp(wis[:, s, :], ps[:, 0:8], [ps], [wis])
                dma(KTc[l][:, :, t0:t0 + T].rearrange("c p w -> p c w"), KTs[:], [KTs], [KVT[l][t0 // KG]], kvsem, eng="gpsimd")
                if first_tile:
                    dump("qT", qT[:, 0, :], [qT])
                    dump("kiT", kiT[:, 0:T], [kiT])
                    dump("wis", wis[:, 0, :], [wis])
                panel = stream.next("%d_xr" % l)
                pv = wview(panel, "xr")
                for c in range(4):
                    ps = nps()
                    fm_chunk(panel, pv, c, ps)
                    act(xrbuf[:, c, 3:3 + T], ps[:, 0:T], AF.Copy, [ps], [xrbuf])
                for nm, dstb in (("gr", grT), ("zu", zuT)):
                    panel = stream.next("%d_%s" % (l, nm))
                    pv = wview(panel, nm)
                    for c in range(4):
                        ps = nps()
                        fm_chunk(panel, pv, c, ps)
                        act(dstb[:, c, :], ps[:, 0:T], AF.Gelu_apprx_tanh, [ps], [dstb])
                panel = stream.next("%d_v" % l)
                pv = wview(panel, "v")
                for s in range(NSUB):
                    ps = nps()
                    for kc in range(8):
                        mm(ps[:, 0:512], hT[:, kc, s * 128:(s + 1) * 128], pv[:, kc, :], kc == 0, kc == 7, [panel, hT], [ps])
                    dstv = Vs[:, s, :].rearrange("p (h f) -> p h f", h=8)[:, :, 0:64]
                    act(dstv, ps[:, 0:512].rearrange("p (h f) -> p h f", h=8), AF.Copy, [ps], [Vs])
                dma(Vc[l][t0:t0 + T, :].rearrange("(s p) f -> p s f", p=128), Vs[:], [Vs], [KVT[l][t0 // KG]], kvsem, eng="gpsimd")
                panel = stream.next("%d_zv" % l)
                pv = wview(panel, "zv")
                for s in range(NSUB):
                    ps = nps()
                    for kc in range(8):
                        mm(ps[:, 0:512], hT[:, kc, s * 128:(s + 1) * 128], pv[:, kc, :], kc == 0, kc == 7, [panel, hT], [ps])
                    act(gz[:], ps[:, 0:512], AF.Gelu_apprx_tanh, [ps], [gz])
                    ss = sm()
                    act(tmpf[:, 0:512], gz[:], AF.Square, [gz, small], [tmpf, small], accum_out=ss)
                    rs = rstd_from_ss(ss, 512)
                    stt(vn[:, s, :], gz[:], rs, gb[:, 3 * D:3 * D + 512], ALU.mult, ALU.mult, [gz, small, gb], [vn])

                for s in range(NSUB):
                    for cpair in range(4):
                        ps = nps()
                        for gg in range(2):
                            g = cpair * 2 + gg
                            mm(ps[gg * 64:(gg + 1) * 64, 0:128], vn[:, s, g * 64:(g + 1) * 64], wspT[:, g, :], True, False,
                               [vn, wspT], [ps])
                        mm(ps[:, 0:128], esel[:, cpair, :], bsph[:, 0, :], False, False, [cb, bsph], [ps])
                        mm(ps[:, 0:128], esel[:, cpair, :], bsph[:, 1, :], False, True, [cb, bsph], [ps])
                        tt(ycT[:, cpair, s * 128:(s + 1) * 128], ps[:, 0:128], zuT[:, cpair, s * 128:(s + 1) * 128], ALU.mult,
                           [ps, zuT], [ycT])
                if first_tile:
                    dump("ycT", ycT[:, 0, :], [ycT])

                for c in range(4):
                    xc = lt[:, 0, :]
                    ts(xc, xrbuf[:, c, 0:T], cols[:, 16 + c:17 + c], cols[:, 32 + c:33 + c], ALU.mult, ALU.add, [xrbuf, cols], [lt])
                    for jj in range(1, 4):
                        stt(xc, xrbuf[:, c, jj:jj + T], cols[:, 16 + jj * 4 + c:17 + jj * 4 + c], xc, ALU.mult, ALU.add,
                            [xrbuf, cols, lt], [lt])
                    cp(xrbuf[:, c, 0:3], xrbuf[:, c, T:T + 3], [xrbuf], [xrbuf])
                    act(xcb[:], xc, AF.Copy, [lt], [xcb])
                    ps = nps()
                    mm(ps[:, 0:T], wbd[:, c, :], xcb[:], True, True, [wbd, xcb], [ps])
                    mm(ps[:, T:2 * T], wbd[:, 4 + c, :], xcb[:], True, True, [wbd, xcb], [ps])
                    rg = lt[:, 1, :]
                    ig = lt[:, 2, :]
                    av = lt[:, 3, :]
                    sq = lt[:, 4, :]
                    act(rg, ps[:, 0:T], AF.Sigmoid, [ps, cols], [lt], bias=cols[:, 36 + c:37 + c])
                    act(ig, ps[:, T:2 * T], AF.Sigmoid, [ps, cols], [lt], bias=cols[:, 40 + c:41 + c])
                    act(av, rg, AF.Exp, [lt, c8], [lt], scale=c8[:, c:c + 1])
                    act(sq, rg, AF.Exp, [lt, c8], [lt], scale=c8[:, 4 + c:5 + c])
                    act(sq, sq, AF.Sqrt, [lt, epsc], [lt], scale=-1.0, bias=epsc[:, 2:3])
                    tt(ig, ig, xc, ALU.mult, [lt], [lt])
                    tt(ig, ig, sq, ALU.mult, [lt], [lt])
                    hh = lt[:, 1, :]
                    scan(hh, av, ig, hst[:, c:c + 1], [lt, hst], [lt])
                    cp(hst[:, c:c + 1], hh[:, T - 1:T], [lt], [hst])
                    tt(ybT[:, c, :], hh, grT[:, c, :], ALU.mult, [lt, grT], [ybT])
                if first_tile:
                    dump("ybT", ybT[:, 0, :], [ybT])
                if l == 0 and j == 1:
                    dump("ybT1", ybT[:, 0, :], [ybT])
                    dump("ycT1", ycT[:, 0, :], [ycT])

                grps = kv_groups(j)
                nkeys = (j + 1) * T
                for s in range(NSUB):
                    N = t0 + 128 * (s + 1)
                    for h in range(8):
                        ts(diag[:, h, :], identf, wis[:, s, h:h + 1], None, ALU.mult, None, [cf, wis], [diag], eng="gpsimd")
                    ngrp = (N + KG - 1) // KG
                    for kg in range(ngrp):
                        wd = min(KG, N - kg * KG)
                        for h in range(8):
                            ps = PS[h % 4]
                            r0 = (h % 2) * 64
                            mm(ps[:, 0:wd], qiT[r0:r0 + 64, h // 2, s * 128:(s + 1) * 128],
                               kiT[r0:r0 + 64, kg * KG:kg * KG + wd], True, True, [qiT, kiT], [ps])
                            act(Rrelu[:, h, 0:wd], ps[:, 0:wd], AF.Relu, [ps], [TR_])
                        for h in range(8):
                            mm(PS[4][:, 0:wd], diag[:, h, :], Rrelu[:, h, 0:wd], h == 0, h == 7, [diag, TR_], [PS[4]])
                        if kg == ngrp - 1:
                            if wd > 128:
                                cp(score_ap[:, kg * KG:kg * KG + wd - 128], PS[4][:, 0:wd - 128], [PS[4]], [TS_])
                            tt(score_ap[:, N - 128:N], PS[4][:, wd - 128:wd], negmask, ALU.add, [PS[4], cf], [TS_])
                            tt(tmpf[:, 0:128], PS[4][:, wd - 128:wd], posfill, ALU.add, [PS[4], cf], [tmpf])
                        else:
                            cp(score_ap[:, kg * KG:kg * KG + wd], PS[4][:, 0:wd], [PS[4]], [TS_])
                    hi0 = bis[:, 0:1]
                    lo = bis[:, 1:2]
                    w0 = bis[:, 2:3]
                    reduce(hi0, score_ap[:, 0:N], ALU.max, [TS_, bis], [bis])
                    reduce(lo, tmpf[:, 0:128], ALU.min, [tmpf, bis], [bis])
                    if N > 128:
                        m1 = bis[:, 3:4]
                        reduce(m1, score_ap[:, 0:N - 128], ALU.min, [TS_, bis], [bis])
                        tt(lo, lo, m1, ALU.min, [bis], [bis])
                    tt(w0, hi0, lo, ALU.subtract, [bis], [bis])
                    mk = masks[s]
                    if N > TOPK:
                        for it in range(NBIS):
                            mid = bis[:, 4 + 2 * (it % 4):5 + 2 * (it % 4)]
                            cnt = bis[:, 5 + 2 * (it % 4):6 + 2 * (it % 4)]
                            stt(mid, w0, 0.5 ** (it + 1), lo, ALU.mult, ALU.add, [bis], [bis])
                            count_ge(mk[:, 0:N], score_ap[:, 0:N], mid, cnt, [TS_, bis], [mk, bis])
                            ge = smalli[:, it % 4:it % 4 + 1]
                            ts(ge, cnt, float(TOPK), None, ALU.is_ge, None, [bis], [smalli])
                            cpred(lo, ge, mid, [bis, smalli], [bis])
                    ts(mk[:, 0:N], score_ap[:, 0:N], lo, None, ALU.is_ge, None, [TS_, bis], [mk])
                    if N < nkeys:
                        memset(mk[:, N:nkeys], 0.0, [mk], eng="gpsimd")
                    if first_tile and s == 0:
                        dump("score", score_ap[:, 0:128], [TS_])
                    if l == 0 and j == 1 and s == 0:
                        dump("score1", score_ap[:, 0:384], [TS_])
                        dump("mask1", mk[:, 0:384], [mk])
                        dump("thr1", lo, [bis])

                first = True
                for gi, (g, wd) in enumerate(grps):
                    panel = stream.next("%d_kv%d_%d" % (l, j, g))
                    nb = wd // 128
                    KTv = panel[:, 0:4 * wd].rearrange("p (c w) -> p c w", c=4)
                    Vv = panel[:, 2048:2048 + nb * 520].rearrange("p (b f) -> p b f", b=nb)
                    psb = PS[6][:].bitcast(BF16)
                    for s in range(NSUB):
                        for b in range(nb):
                            tr(psb[:, (s * 4 + b) * 128:(s * 4 + b + 1) * 128],
                               masks[s][:, g * KG + b * 128:g * KG + (b + 1) * 128], identb, [masks[s], cb], [PS[6]])
                    for s in range(NSUB):
                        act(maskTk[:, 0:nb, s * 128:(s + 1) * 128],
                            psb[:, s * 512:s * 512 + nb * 128].rearrange("p (b t) -> p b t", b=nb),
                            AF.Identity, [PS[6], epsc], [maskTk], scale=BIGM, bias=epsc[:, 3:4])
                    for h in range(8):
                        r0 = (h % 2) * 64
                        pacc = PS[4 + (h % 2)]
                        for bp in range(0, nb, 2):
                            nbb = min(2, nb - bp)
                            ps = PS[(h * 2 + bp // 2) % 4]
                            ptile = PT[(h * 2 + bp // 2) % 2]
                            for b in range(bp, bp + nbb):
                                o_ap = ps[:, (b - bp) * T:(b - bp + 1) * T]
                                mm(o_ap, KTv[r0:r0 + 64, h // 2, b * 128:(b + 1) * 128], qT[r0:r0 + 64, h // 2, :], True, False,
                                   [panel, qT], [ps])
                                mm(o_ap, identb, maskTk[:, b, :], False, True, [cb, maskTk], [ps])
                            act(ptile[:, 0:nbb, :], ps[:, 0:nbb * T].rearrange("p (b t) -> p b t", b=nbb), AF.Exp,
                                [ps], [ptile], scale=HD ** -0.5)
                            for b in range(bp, bp + nbb):
                                mm(pacc[0:65, 0:T], Vv[:, b, h * 65:(h + 1) * 65], ptile[:, b - bp, :], b == 0, b == nb - 1,
                                   [panel, ptile], [pacc])
                        if first:
                            cp(acc[:, h, :], pacc[0:65, 0:T], [pacc], [acc])
                        else:
                            tt(acc[:, h, :], acc[:, h, :], pacc[0:65, 0:T], ALU.add, [pacc, acc], [acc])
                    first = False
                accf = acc[:].rearrange("p h t -> p (h t)")
                act(accf[64:65, :], accf[64:65, :], AF.Ln, [acc], [acc])
                act(accf[64:65, :], accf[64:65, :], AF.Exp, [acc], [acc], scale=-1.0)
                for h in range(8):
                    cp(rhl[64:65, 0, :], acc[64:65, h, :], [acc], [rhl])
                    tt(rhl[64:65, 1, :], acc[64:65, h, :], rhl[64:65, 0, :], ALU.subtract, [acc, rhl], [rhl])
                    ps = nps()
                    mm(ps[0:64, 0:T], onesb[64:65, 0:64], rhl[64:65, 0, :], True, False, [onesb, rhl], [ps])
                    mm(ps[0:64, 0:T], onesb[64:65, 0:64], rhl[64:65, 1, :], False, True, [onesb, rhl], [ps])
                    if h % 2 == 0:
                        tt(yaT[0:64, h // 2, :], acc[0:64, h, :], ps[0:64, 0:T], ALU.mult, [acc, ps], [yaT])
                    else:
                        tt(yaTt[:, :], acc[0:64, h, :], ps[0:64, 0:T], ALU.mult, [acc, ps], [yaTt])
                        ps2 = nps()
                        mm(ps2[64:128, 0:T], identb[0:64, 0:64], yaTt[:, :], True, True, [cb, yaTt], [ps2])
                        cp(yaT[64:128, h // 2, :], ps2[64:128, 0:T], [ps2], [yaT])
                if first_tile:
                    dump("yaT", yaT[:, 0, :], [yaT])
                if l == 0 and j == 1:
                    dump("yaT1", yaT[:, 0, :], [yaT])
                    dump("acc1", acc[:, 0, :], [acc])

                for ni, n in enumerate((2, 1, 0)):
                    for hf in range(2):
                        gpan = stream.next("%d_gate%d_%d" % (l, n, hf))
                        pv = wview(gpan, "gate0_0")
                        for c in range(4):
                            ps = nps()
                            fm_chunk(gpan, pv, c, ps)
                            act(gsig[:, hf * 4 + c, :], ps[:, 0:T], AF.Sigmoid, [ps], [gsig])
                    ysrc = {2: ycT, 1: ybT, 0: yaT}[n]
                    bp_ = [None, None]
                    for hf in range(2):
                        bp_[hf] = stream.next("%d_br%d_%d" % (l, n, hf))
                        pv = wview(bp_[hf], "br0_0")
                        for c in range(4):
                            dchunk = hf * 4 + c
                            ps = nps()
                            for kc in range(4):
                                mm(ps[:, 0:T], pv[:, kc, c * 128:(c + 1) * 128], ysrc[:, kc, :], kc == 0, kc == 3,
                                   [bp_[hf], ysrc], [ps])
                            if ni == 0:
                                tt(merged[:, dchunk, :], ps[:, 0:T], gsig[:, dchunk, :], ALU.mult, [ps, gsig], [TS_])
                            else:
                                tt(gt[:], ps[:, 0:T], gsig[:, dchunk, :], ALU.mult, [ps, gsig], [gt])
                                dsto = mergedT if ni == 2 else merged
                                tt(dsto[:, dchunk, :], merged[:, dchunk, :], gt[:], ALU.add, [TS_, gt], [TS_], eng="gpsimd")
                op_ = [stream.next("%d_out_%d" % (l, hf), look=NSLOT - 1 - hf) for hf in range(2)]
                for s in range(NSUB):
                    banks = [PS[4], PS[5]]
                    for hf in range(2):
                        pv = wview(op_[hf], "out_0")
                        for kc in range(8):
                            mm(banks[hf][:, 0:512], mergedT[:, kc, s * 128:(s + 1) * 128], pv[:, kc, :], kc == 0, kc == 7,
                               [op_[hf], TS_], [banks[hf]])
                    post_norm_residual(banks, 0, s)
                if first_tile:
                    dump("x1", xt[:, 0, :], [xt])
                norm_transpose(8)
                dbanks = [[PS[4], PS[5]], [PS[6], PS[7]]]
                for g in range(8):
                    up = stream.next("%d_up_%d" % (l, g))
                    pv = wview(up, "up_0")
                    fTg = fT[g % 2]
                    for c in range(4):
                        ps = nps()
                        fm_chunk(up, pv, c, ps)
                        rlb = rl[c % 2]
                        act(rlb, ps[:, 0:T], AF.Relu, [ps], [TR_])
                        tt(fTg[:, c, :], rlb, rlb, ALU.mult, [TR_], [TS_], eng="gpsimd")
                    dn = stream.next("%d_dn_%d" % (l, g))
                    dv = wview(dn, "dn_0")
                    for s in range(NSUB):
                        for hf in range(2):
                            for c in range(4):
                                mm(dbanks[s][hf][:, 0:512], fTg[:, c, s * 128:(s + 1) * 128], dv[:, c, hf * 512:(hf + 1) * 512],
                                   g == 0 and c == 0, g == 7 and c == 3, [dn, TS_], [dbanks[s][hf]])
                for s in range(NSUB):
                    post_norm_residual(dbanks[s], D, s)
                if first_tile:
                    dump("x2", xt[:, 0, :], [xt])
                norm_transpose(None)
                for s in range(NSUB):
                    cp(ptb[:, s, :], pt[:, s, :], [pt], [ptb])
                    psb = PS[3][:].bitcast(BF16)
                    for c in range(2):
                        tr(psb[:, c * 128:(c + 1) * 128], ptb[:, s, c * 128:(c + 1) * 128], identb, [ptb, cb], [PS[3]])
                    cp(pT[:, :, s * 128:(s + 1) * 128], psb[:, 0:256].rearrange("p (c t) -> p c t", c=2), [PS[3]], [pT])
                plp = stream.next("%d_ple" % l)
                plv = wview(plp, "ple")
                pg = [stream.next("%d_pg_%d" % (l, hf), look=NSLOT - 2 - hf) for hf in range(2)]
                for s in range(NSUB):
                    ssp = []
                    for hf in range(2):
                        pa = PS[0 + hf]
                        pgb = PS[2 + hf]
                        for kc in range(2):
                            mm(pa[:, 0:512], pT[:, kc, s * 128:(s + 1) * 128], plv[:, kc, hf * 512:(hf + 1) * 512], kc == 0, kc == 1,
                               [plp, pT], [pa])
                        gv = wview(pg[hf], "pg_0")
                        for kc in range(8):
                            mm(pgb[:, 0:512], hT[:, kc, s * 128:(s + 1) * 128], gv[:, kc, :], kc == 0, kc == 7, [pg[hf], hT], [pgb])
                        act(sg, pgb[:, 0:512], AF.Sigmoid, [pgb], [TR_])
                        tt(ple_t[:, hf * 512:(hf + 1) * 512], pa[:, 0:512], sg, ALU.mult, [pa, TR_], [TR_])
                        a = sm()
                        act(tmpf[:, 0:512], ple_t[:, hf * 512:(hf + 1) * 512], AF.Square, [TR_, small], [tmpf, small], accum_out=a)
                        ssp.append(a)
                    s3 = sm()
                    tt(s3, ssp[0], ssp[1], ALU.add, [small], [small])
                    rs = rstd_from_ss(s3, D)
                    for hf in range(2):
                        stt(tmpf[:, 0:512], ple_t[:, hf * 512:(hf + 1) * 512], rs, gb[:, 2 * D + hf * 512:2 * D + (hf + 1) * 512],
                            ALU.mult, ALU.mult, [TR_, small, gb], [tmpf])
                        tt(xt[:, s, hf * 512:(hf + 1) * 512], xt[:, s, hf * 512:(hf + 1) * 512], tmpf[:, 0:512], ALU.add,
                           [xt, tmpf], [xt], eng="gpsimd")
                wt = [XT[j]] if XT is not None else [out_tile]
                dma(dst_x[t0:t0 + T, :].rearrange("(s p) d -> p s d", p=128), xt[:], [xt], wt, ssem, eng="gpsimd")
            XTprev = XT

        fw.finish("sync", [out_tile] + dbg_tiles)
        fw.emit()
    return nc, fw


def _consts():
    bf = ml_dtypes.bfloat16
    ident = np.eye(128, dtype=np.float32)
    Rm = np.zeros((128, 128), np.float32)
    for base in (0, 64):
        for d in range(8):
            Rm[base + d + 8, base + d] = -1.0
            Rm[base + d, base + d + 8] = 1.0
    esel = np.zeros((128, 4, 128), np.float32)
    for g in range(8):
        esel[g, g // 2, (g % 2) * 64:(g % 2 + 1) * 64] = 1.0
    cb = np.concatenate([ident, Rm, esel.reshape(128, 512)], axis=1).astype(bf)
    tt_, ss_ = np.meshgrid(np.arange(128), np.arange(128), indexing="ij")
    negmask = np.where(ss_ > tt_, np.float32(NEG), np.float32(0.0))
    posfill = np.where(ss_ > tt_, np.float32(-2 * NEG), np.float32(0.0))
    tril01 = (tt_ <= ss_).astype(np.float32)
    half = 8
    inv_freq = (np.float32(500000.0) ** (-np.arange(half, dtype=np.float32) * np.float32(2.0) / np.float32(16))).astype(np.float32)
    invf = np.zeros((128, 1), np.float32)
    for f in range(128):
        d = f % 64
        if d < 16:
            invf[f, 0] = inv_freq[d % 8]
    cf = np.concatenate([ident, negmask, posfill, tril01, invf], axis=1).astype(np.float32)
    return cb, cf


def _layout_params(inp, depth):
    f = np.float32
    cols = np.zeros((depth, 128, 48), f)
    gb = np.zeros((depth, 128, 3 * D + 512), f)
    wbd = np.zeros((depth, 128, 8, 128), f)
    wsp = np.zeros((depth, 128, 8, 128), f)
    for l in range(depth):
        cols[l, :, 0:8] = np.asarray(inp["g_pre_mix"][l], f).reshape(8, 128).T
        cols[l, :, 8:16] = np.asarray(inp["g_pre_ffn"][l], f).reshape(8, 128).T
        cw = np.asarray(inp["conv_w"][l], f)
        for jj in range(4):
            cols[l, :, 16 + jj * 4:20 + jj * 4] = cw[jj].reshape(4, 128).T
        cols[l, :, 32:36] = np.asarray(inp["conv_b"][l], f).reshape(4, 128).T
        cols[l, :, 36:40] = np.asarray(inp["b_rg_a"][l], f).reshape(4, 128).T
        cols[l, :, 40:44] = np.asarray(inp["b_rg_x"][l], f).reshape(4, 128).T
        cols[l, :, 44:48] = np.asarray(inp["lru_lambda"][l], f).reshape(4, 128).T
        row = np.concatenate([np.asarray(inp["g_post_mix"][l], f), np.asarray(inp["g_post_ffn"][l], f),
                              np.asarray(inp["g_post_ple"][l], f), np.asarray(inp["g_gmlp_v"][l], f)])
        gb[l] = np.broadcast_to(row[None, :], (128, row.size))
        for gi, key in enumerate(("w_rg_a", "w_rg_x")):
            w = np.asarray(inp[key][l], f)
            for c in range(4):
                for hh in range(2):
                    wbd[l, hh * 64:(hh + 1) * 64, gi * 4 + c, hh * 64:(hh + 1) * 64] = w[c * 2 + hh]
        ws = np.asarray(inp["w_spatial"][l], f)
        wsp[l] = np.transpose(ws, (2, 0, 1))
    bsp = np.ascontiguousarray(np.asarray(inp["b_spatial"], f))
    return cols, gb, wbd, wsp, bsp


_CACHE = {}


def kernel(**inputs):
    depth = DEPTH
    x = np.asarray(inputs["x"], np.float32)
    B, L, _ = x.shape
    p = np.asarray(inputs["p"], np.float32)
    pos = np.asarray(inputs["positions"], np.int32)
    cb, cf = _consts()
    cols, gb, wbd, wsp, bsp = _layout_params(inputs, depth)
    shared = {
        "w_in": np.ascontiguousarray(np.asarray(inputs["w_in"], np.float32)),
        "w_branch": np.ascontiguousarray(np.asarray(inputs["w_branch"], np.float32)),
        "w_out": np.ascontiguousarray(np.asarray(inputs["w_out"], np.float32)),
        "w_ffn_up": np.ascontiguousarray(np.asarray(inputs["w_ffn_up"], np.float32)),
        "w_ffn_down": np.ascontiguousarray(np.asarray(inputs["w_ffn_down"], np.float32)),
        "w_ple": np.ascontiguousarray(np.asarray(inputs["w_ple"], np.float32)),
        "w_ple_gate": np.ascontiguousarray(np.asarray(inputs["w_ple_gate"], np.float32)),
        "cols": cols, "gb": gb, "wbd": wbd, "wsp": wsp, "bsp": bsp, "cb": cb, "cf": cf,
    }
    if L not in _CACHE:
        _CACHE[L] = build_program(L, depth)[0]
    nc = _CACHE[L]
    in_maps = []
    for b in range(B):
        m = dict(shared)
        m["x"] = np.ascontiguousarray(x[b])
        m["p"] = np.ascontiguousarray(p[:, b])
        m["pos"] = np.ascontiguousarray(np.broadcast_to(pos[b][None, :], (128, L)))
        in_maps.append(m)
    res = run_bass_kernel_spmd(nc, in_maps, core_ids=list(range(B)))
    return np.stack([np.asarray(r["y"], np.float32) for r in res.results], axis=0)
```

```python
import math
from contextlib import ExitStack
import numpy as np
import ml_dtypes
import concourse.bass as bass
import concourse.mybir as mybir
from concourse.bass_utils import run_bass_kernel_spmd

F32 = mybir.dt.float32
BF16 = mybir.dt.bfloat16
I32 = mybir.dt.int32
AF = mybir.ActivationFunctionType
ALU = mybir.AluOpType
AX = mybir.AxisListType

D = 1024
NH = 8
HD = 64
TOPK = 256
FFN = 4096
PLE = 256
EPS = 1e-6
DEPTH = 2
T = 256
NSUB = T // 128
KG = 512
NSLOT = 3
SLOTB = 4224
NBIS = 16
BIGM = 30000.0
NEG = -1.0e30
IN_OFF = dict(q=0, k=512, v=1024, qi=1536, kiwi=2048, xr=2120, gr=2632, zu=3144, zv=3656, gate=4168)


class Sem:
    def __init__(self, h, name):
        self.h = h
        self.n = 0
        self.name = name


class Tile:
    __slots__ = ("w", "r", "name")

    def __init__(self, name=""):
        self.w = {}
        self.r = {}
        self.name = name


class Buf:
    def __init__(self, t, name=""):
        self.t = t
        self.T = Tile(name)

    def __getitem__(self, k):
        return self.t[k]


class Engine:
    def __init__(self, name, sem):
        self.name = name
        self.sem = sem
        self.ops = []
        self.seen = {}


class FW:
    def __init__(self, nc, stack):
        self.nc = nc
        self.stack = stack
        self.engs = {}
        for n in ("tensor", "vector", "scalar", "gpsimd", "sync"):
            s = Sem(stack.enter_context(nc.semaphore("sem_" + n)), n)
            self.engs[n] = Engine(n, s)
        self.nops = 0

    def dsem(self, name):
        return Sem(self.stack.enter_context(self.nc.semaphore("dsem_" + name)), name)

    def op(self, eng, fn, reads=(), writes=(), dsem=None):
        E = self.engs[eng]
        need = {}
        for b in reads:
            t = b.T if isinstance(b, Buf) else b
            for s, v in t.w.items():
                if need.get(s, 0) < v:
                    need[s] = v
        for b in writes:
            t = b.T if isinstance(b, Buf) else b
            for s, v in t.w.items():
                if need.get(s, 0) < v:
                    need[s] = v
            for s, v in t.r.items():
                if need.get(s, 0) < v:
                    need[s] = v
        raw_self = 0
        for b in reads:
            t = b.T if isinstance(b, Buf) else b
            raw_self = max(raw_self, t.w.get(E.sem, 0))
        waits = []
        for s, v in need.items():
            if s is E.sem:
                if eng != "tensor" and raw_self > E.seen.get(s, 0):
                    E.seen[s] = raw_self
                    waits.append((s, raw_self))
                continue
            if E.seen.get(s, 0) >= v:
                continue
            E.seen[s] = v
            waits.append((s, v))
        if dsem is not None:
            dsem.n += 16
            sig = (dsem, dsem.n, 16)
        else:
            E.sem.n += 1
            sig = (E.sem, E.sem.n, 1)
        E.ops.append((waits, fn, sig))
        self.nops += 1
        s, v = sig[0], sig[1]
        for b in reads:
            t = b.T if isinstance(b, Buf) else b
            if t.r.get(s, 0) < v:
                t.r[s] = v
        for b in writes:
            t = b.T if isinstance(b, Buf) else b
            if t.w.get(s, 0) < v:
                t.w[s] = v

    def finish(self, eng, tiles):
        E = self.engs[eng]
        need = {}
        for b in tiles:
            t = b.T if isinstance(b, Buf) else b
            for d in (t.w, t.r):
                for s, v in d.items():
                    if need.get(s, 0) < v:
                        need[s] = v
        E.ops.append(([(s, v) for s, v in need.items() if s is not E.sem], None, None))

    def emit(self):
        with self.nc.Block() as block:
            for n, E in self.engs.items():
                def body(e, E=E):
                    for waits, fn, sig in E.ops:
                        for s, v in waits:
                            e.wait_ge(s.h, v)
                        if fn is None:
                            continue
                        fn(e).then_inc(sig[0].h, sig[2])
                getattr(block, n)(body)


def panel_defs():
    P = []
    for nm in ("q", "k", "qi"):
        P.append((nm, "w_in", 0, 1024, IN_OFF[nm], 512))
    P.append(("kiwi", "w_in", 0, 1024, IN_OFF["kiwi"], 72))
    for nm in ("xr", "gr", "zu", "v", "zv"):
        P.append((nm, "w_in", 0, 1024, IN_OFF[nm], 512))
    for n in range(3):
        for hf in range(2):
            P.append(("gate%d_%d" % (n, hf), "w_in", 0, 1024, IN_OFF["gate"] + n * 1024 + hf * 512, 512))
    for n in range(3):
        for hf in range(2):
            P.append(("br%d_%d" % (n, hf), "w_branch%d" % n, 0, 512, hf * 512, 512))
    for hf in range(2):
        P.append(("out_%d" % hf, "w_out", 0, 1024, hf * 512, 512))
    for g in range(8):
        P.append(("up_%d" % g, "w_ffn_up", 0, 1024, g * 512, 512))
        P.append(("dn_%d" % g, "w_ffn_down", g * 512, 512, 0, 1024))
    P.append(("ple", "w_ple", 0, 256, 0, 1024))
    for hf in range(2):
        P.append(("pg_%d" % hf, "w_ple_gate", 0, 1024, hf * 512, 512))
    return P


def build_program(L, depth=DEPTH, dbg=None):
    NT = L // T
    nc = bass.Bass("TRN2", target_bir_lowering=False)
    dram = lambda name, shape, dt, kind="ExternalInput": nc.dram_tensor(name, shape, dt, kind=kind).ap()
    x_in = dram("x", [L, D], F32)
    p_in = dram("p", [depth, L, PLE], F32)
    pos_in = dram("pos", [128, L], I32)
    wsrc = {
        "w_in": dram("w_in", [depth, D, 7240], F32),
        "w_branch": dram("w_branch", [depth, 3, 512, D], F32),
        "w_out": dram("w_out", [depth, D, D], F32),
        "w_ffn_up": dram("w_ffn_up", [depth, D, FFN], F32),
        "w_ffn_down": dram("w_ffn_down", [depth, FFN, D], F32),
        "w_ple": dram("w_ple", [depth, PLE, D], F32),
        "w_ple_gate": dram("w_ple_gate", [depth, D, D], F32),
    }
    cols_in = dram("cols", [depth, 128, 48], F32)
    gb_in = dram("gb", [depth, 128, 3 * D + 512], F32)
    wbd_in = dram("wbd", [depth, 128, 8, 128], F32)
    wsp_in = dram("wsp", [depth, 128, 8, 128], F32)
    bsp_in = dram("bsp", [depth, 8, 128], F32)
    cb_in = dram("cb", [128, 256 + 512], BF16)
    cf_in = dram("cf", [128, 4 * 128 + 1], F32)
    y_out = dram("y", [L, D], F32, kind="ExternalOutput")
    xbuf = dram("xbuf", [L, D], F32, kind="Internal")
    KTc = [dram("ktc%d" % l, [4, 128, L], BF16, kind="Internal") for l in range(depth)]
    Vc = [dram("vc%d" % l, [L, 520], BF16, kind="Internal") for l in range(depth)]
    pdefs = panel_defs()
    Wp = [{nm: dram("wp%d_%s" % (l, nm), [nr, ncol], BF16, kind="Internal") for (nm, _, _, nr, _, ncol) in pdefs}
          for l in range(depth)]
    dbg_out = {}
    if dbg:
        for k, shp in dbg.items():
            dbg_out[k] = dram("dbg_" + k, list(shp), F32, kind="ExternalOutput")

    with ExitStack() as st:
        fw = FW(nc, st)
        op = fw.op

        def sb(name, shape, dt):
            return Buf(st.enter_context(nc.sbuf_tensor("s_" + name, shape, dt)), name)

        PS = [Buf(st.enter_context(nc.psum_tensor("ps%d" % i, [128, 512], F32)), "ps%d" % i) for i in range(8)]

        def mm(out, lhsT, rhs, start, stop, R, W):
            op("tensor", lambda e: e.matmul(out, lhsT=lhsT, rhs=rhs, start=start, stop=stop), R, W)

        def tr(out, in_, ident, R, W):
            op("tensor", lambda e: e.transpose(out=out, in_=in_, identity=ident), R, W)

        def act(out, in_, func, R, W, **kw):
            op("scalar", lambda e: e.activation(out=out, in_=in_, func=func, **kw), R, W)

        def ts(out, in0, s1, s2, op0, op1, R, W, eng="vector"):
            if op1 is None:
                op(eng, lambda e: e.tensor_scalar(out=out, in0=in0, scalar1=s1, scalar2=None, op0=op0), R, W)
            else:
                op(eng, lambda e: e.tensor_scalar(out=out, in0=in0, scalar1=s1, scalar2=s2, op0=op0, op1=op1), R, W)

        def tt(out, in0, in1, o, R, W, eng="vector"):
            op(eng, lambda e: e.tensor_tensor(out=out, in0=in0, in1=in1, op=o), R, W)

        def stt(out, in0, s, in1, op0, op1, R, W):
            op("vector", lambda e: e.scalar_tensor_tensor(out=out, in0=in0, scalar=s, in1=in1, op0=op0, op1=op1), R, W)

        def cp(out, in_, R, W, eng="vector"):
            op(eng, lambda e: e.tensor_copy(out=out, in_=in_), R, W)

        def dma(out, in_, R, W, ds, eng="sync"):
            op(eng, lambda e: e.dma_start(out=out, in_=in_), R, W, dsem=ds)

        def memset(ap, val, W, eng="vector"):
            op(eng, lambda e: e.memset(ap, val), [], W)

        def reduce(out, in_, o, R, W):
            op("vector", lambda e: e.tensor_reduce(out=out, in_=in_, axis=AX.X, op=o), R, W)

        def recip(out, in_, R, W):
            op("vector", lambda e: e.reciprocal(out=out, in_=in_), R, W)

        def scan(out, d0, d1, init, R, W):
            op("vector", lambda e: e.tensor_tensor_scan(out=out, data0=d0, data1=d1, initial=init,
                                                        op0=ALU.mult, op1=ALU.add), R, W)

        def count_ge(out, in0, thr, cnt, R, W):
            op("vector", lambda e: e.tensor_scalar(out=out, in0=in0, scalar1=thr, scalar2=None, op0=ALU.is_ge,
                                                   op1=ALU.add, accum_out=cnt), R, W)

        def cpred(out, mask, data, R, W):
            op("vector", lambda e: e.copy_predicated(out=out, mask=mask, data=data), R, W)

        dbg_sem = fw.dsem("dbg")
        dbg_tiles = []

        def dump(name, ap, R):
            if name in dbg_out:
                t = Tile("dbg")
                dma(dbg_out[name], ap, R, [t], dbg_sem, eng="gpsimd")
                dbg_tiles.append(t)
                del dbg_out[name]

        cb = sb("cb", [128, 768], BF16)
        cf = sb("cf", [128, 513], F32)
        dma(cb[:], cb_in, [], [cb], fw.dsem("c0"))
        dma(cf[:], cf_in, [], [cf], fw.dsem("c1"))
        identb = cb[:, 0:128]
        Rm = cb[:, 128:256]
        esel = cb[0:8, 256:768].rearrange("p (c f) -> p c f", c=4)
        identf = cf[:, 0:128]
        negmask = cf[:, 128:256]
        posfill = cf[:, 256:384]
        tril01 = cf[:, 384:512]
        invf = cf[:, 512:513]

        WT = [dict() for _ in range(depth)]
        for l in range(depth):
            wcs = fw.dsem("wcast%d" % l)
            for (nm, src, r0, nr, c0, ncol) in pdefs:
                if src.startswith("w_branch"):
                    s_ap = wsrc["w_branch"][l, int(src[-1]), r0:r0 + nr, c0:c0 + ncol]
                else:
                    s_ap = wsrc[src][l, r0:r0 + nr, c0:c0 + ncol]
                t = Tile("wp")
                WT[l][nm] = t
                step = 512
                for rr in range(0, nr, step):
                    n2 = min(step, nr - rr)
                    dma(Wp[l][nm][rr:rr + n2, :], s_ap[rr:rr + n2, :], [], [t], wcs, eng="gpsimd")
            for t in WT[l].values():
                t.w = {wcs: wcs.n}

        ring = [sb("ring%d" % i, [128, SLOTB], BF16) for i in range(NSLOT)]
        ring_sem = [fw.dsem("ring%d" % i) for i in range(NSLOT)]
        kiT = sb("kiT", [128, L], BF16)
        xt = sb("xt", [128, NSUB, D], F32)
        pt = sb("pt", [128, NSUB, PLE], F32)
        ptb = sb("ptb", [128, NSUB, PLE], BF16)
        posi = sb("posi", [128, T], I32)
        hT = sb("hT", [128, 8, T], BF16)
        pT = sb("pT", [128, 2, T], BF16)
        qT = sb("qT", [128, 4, T], BF16)
        qiT = sb("qiT", [128, 4, T], BF16)
        KTs = sb("KTs", [128, 4, T], BF16)
        Vs = sb("Vs", [128, NSUB, 520], BF16)
        cosT = sb("cosT", [128, T], F32)
        sinT = sb("sinT", [128, T], F32)
        rtmp = sb("rtmp", [128, 3, T], F32)
        xb16 = sb("xb16", [128, T], BF16)
        xrbuf = sb("xrbuf", [128, 4, 3 + T], F32)
        grT = sb("grT", [128, 4, T], BF16)
        zuT = sb("zuT", [128, 4, T], BF16)
        gz = sb("gz", [128, 512], F32)
        vn = sb("vn", [128, NSUB, 512], BF16)
        lt = sb("lt", [128, 5, T], F32)
        xcb = sb("xcb", [128, T], BF16)
        hst = sb("hst", [128, 4], F32)
        ybT = sb("ybT", [128, 4, T], BF16)
        ycT = sb("ycT", [128, 4, T], BF16)
        yaT = sb("yaT", [128, 4, T], BF16)
        yaTt = sb("yaTt", [64, T], BF16)
        wis = sb("wis", [128, NSUB, 8], F32)
        diag = sb("diag", [128, 8, 128], BF16)
        small = sb("small", [128, 32], F32)
        smalli = sb("smalli", [128, 4], I32)
        bis = sb("bis", [128, 16], F32)
        bis2 = sb("bis2", [128, 16], F32)
        Tmid = Tile("mid")
        Tcnt = Tile("cnt")
        Tsa = Tile("sa")
        big = sb("big", [128, 6144], F32)
        TS_ = big.T
        TR_ = Tile("bigR")
        score_ap = big[:, 0:L]
        Rrelu = big[:, 4096:6144].bitcast(BF16).rearrange("p (h w) -> p h w", h=8)
        masks = [sb("mask%d" % s, [128, L], BF16) for s in range(NSUB)]
        maskTk = sb("maskTk", [128, 4, T], BF16)
        PT = [sb("PT%d" % i, [128, 2, T], BF16) for i in range(4)]
        xs = sb("xs", [128, D], BF16)
        acc = sb("acc", [65, 8, T], F32)
        rhl = sb("rhl", [65, 2, T], BF16)
        onesb = sb("onesb", [65, 64], BF16)
        gt = sb("gt", [128, T], F32)
        tmpf = sb("tmpf", [128, 512], F32)
        gsig = sb("gsig", [128, 8, T], BF16)
        merged = big[:, 0:8 * T].rearrange("p (c t) -> p c t", c=8)
        o = 8 * T
        mergedT = big[:, o:o + 4 * T].bitcast(BF16).rearrange("p (c t) -> p c t", c=8)
        o += 4 * T
        fT = [big[:, o + i * 2 * T:o + (i + 1) * 2 * T].bitcast(BF16).rearrange("p (c t) -> p c t", c=4) for i in range(2)]
        o += 4 * T
        assert o <= 4096
        o = 4096
        rl = [big[:, o + i * (T // 2):o + (i + 1) * (T // 2)].bitcast(BF16) for i in range(2)]
        o += T
        sg = big[:, o:o + 512]
        o += 512
        ple_t = big[:, o:o + 1024]
        o += 1024
        assert o <= 6144
        wbdf = big[:, 4096:4096 + 1024].rearrange("p (c j) -> p c j", c=8)
        cols = sb("cols", [128, 48], F32)
        gb = sb("gb", [128, 3 * D + 512], F32)
        wbd = sb("wbd", [128, 8, 128], BF16)
        wspT = sb("wspT", [128, 8, 128], BF16)
        bsp = sb("bsp", [8, 128], F32)
        bsph = sb("bsph", [8, 2, 128], BF16)
        c8 = sb("c8", [128, 8], F32)
        epsc = sb("epsc", [128, 4], F32)
        psem = [fw.dsem("par%d" % i) for i in range(5)]
        xsem = fw.dsem("xload")
        ptsem = fw.dsem("pload")
        possem = fw.dsem("posload")
        ssem = fw.dsem("store")
        kvsem = fw.dsem("kvstore")
        out_tile = Tile("out")

        class Stream:
            def __init__(self):
                self.plan = []
                self.issued = 0
                self.pos = 0

            def add(self, name, loader, deps, hoist=True):
                self.plan.append((name, loader, deps, hoist))

            def next(self, name, look=NSLOT - 1):
                i = self.pos
                assert self.plan[i][0] == name, (self.plan[i][0], name)
                while self.issued < len(self.plan) and (
                        self.issued <= i or (self.issued <= i + look and self.plan[self.issued][3])):
                    k = self.issued
                    _, loader, deps, _ = self.plan[k]
                    loader(ring[k % NSLOT], ring_sem[k % NSLOT], deps)
                    self.issued += 1
                self.pos += 1
                return ring[i % NSLOT]

        stream = Stream()

        def wloader(l, nm, nr, ncol):
            kc = nr // 128

            def f(slot, sem, deps):
                dst = slot[:, 0:kc * ncol].rearrange("p (k w) -> p k w", k=kc)
                src = Wp[l][nm].rearrange("(k p) w -> p k w", p=128)
                dma(dst, src, deps, [slot], sem)
            return f

        KVT = [[Tile("kv") for _ in range((L + KG - 1) // KG)] for _ in range(depth)]

        def kvloader(l, g, wd):
            def f(slot, sem, deps):
                dstk = slot[:, 0:4 * wd].rearrange("p (c w) -> p c w", c=4)
                dma(dstk, KTc[l][:, :, g * KG:g * KG + wd].rearrange("c p w -> p c w"), deps, [slot], sem)
                nb = wd // 128
                dstv = slot[:, 2048:2048 + nb * 520].rearrange("p (b f) -> p b f", b=nb)
                dma(dstv, Vc[l][g * KG:g * KG + wd, :].rearrange("(b p) f -> p b f", p=128), deps, [slot], sem)
            return f

        pinfo = {nm: (nr, ncol) for (nm, _, _, nr, _, ncol) in pdefs}

        def plan_w(l, nm):
            nr, ncol = pinfo[nm]
            stream.add("%d_%s" % (l, nm), wloader(l, nm, nr, ncol), [WT[l][nm]])

        def kv_groups(j):
            nkeys = (j + 1) * T
            out = []
            g = 0
            while g * KG < nkeys:
                out.append((g, min(KG, nkeys - g * KG)))
                g += 1
            return out

        for l in range(depth):
            for j in range(NT):
                for nm in ("q", "k", "qi", "kiwi", "xr", "gr", "zu", "v", "zv"):
                    plan_w(l, nm)
                grps = kv_groups(j)
                for (g, wd) in grps:
                    last = (g == grps[-1][0])
                    stream.add("%d_kv%d_%d" % (l, j, g), kvloader(l, g, wd), [KVT[l][g]], hoist=not last)
                for n in (2, 1, 0):
                    plan_w(l, "gate%d_0" % n)
                    plan_w(l, "gate%d_1" % n)
                    plan_w(l, "br%d_0" % n)
                    plan_w(l, "br%d_1" % n)
                plan_w(l, "out_0")
                plan_w(l, "out_1")
                for g in range(8):
                    plan_w(l, "up_%d" % g)
                    plan_w(l, "dn_%d" % g)
                plan_w(l, "ple")
                plan_w(l, "pg_0")
                plan_w(l, "pg_1")

        def wview(slot, nm):
            nr, ncol = pinfo[nm]
            kc = nr // 128
            return slot[:, 0:kc * ncol].rearrange("p (k w) -> p k w", k=kc)

        sm_i = [0]

        def sm():
            i = sm_i[0] % 32
            sm_i[0] += 1
            return small[:, i:i + 1]

        memset(epsc[:, 0:1], EPS, [epsc])
        memset(epsc[:, 1:2], math.pi / 2, [epsc])
        memset(epsc[:, 2:3], 1.0, [epsc])
        memset(epsc[:, 3:4], -BIGM, [epsc])
        memset(Vs[:, :, :], 1.0, [Vs])
        memset(onesb[:, :], 1.0, [onesb])

        def rstd_from_ss(ss_ap, n):
            a = sm()
            b = sm()
            act(a, ss_ap, AF.Sqrt, [small, epsc], [small], scale=1.0 / n, bias=epsc[:, 0:1])
            recip(b, a, [small], [small])
            return b

        pi = [0]

        def nps():
            pi[0] += 1
            return PS[pi[0] % 4]

        def norm_transpose(gcol0):
            for s in range(NSUB):
                if gcol0 is not None:
                    ss = sm()
                    act(tmpf[:, 0:512], xt[:, s, 0:512], AF.Square, [xt, small], [tmpf, small], accum_out=ss)
                    ss2 = sm()
                    act(tmpf[:, 0:512], xt[:, s, 512:1024], AF.Square, [xt, small], [tmpf, small], accum_out=ss2)
                    ss3 = sm()
                    tt(ss3, ss, ss2, ALU.add, [small], [small])
                    rs = rstd_from_ss(ss3, D)
                    ts(xs[:], xt[:, s, :], rs, None, ALU.mult, None, [xt, small], [xs])
                else:
                    cp(xs[:], xt[:, s, :], [xt], [xs])
                psb = PS[7][:].bitcast(BF16)
                for c in range(8):
                    tr(psb[:, c * 128:(c + 1) * 128], xs[:, c * 128:(c + 1) * 128], identb, [xs, cb], [PS[7]])
                src = psb[:, 0:1024].rearrange("p (c t) -> p c t", c=8)
                dst = hT[:, :, s * 128:(s + 1) * 128]
                if gcol0 is not None:
                    g_ap = cols[:, gcol0:gcol0 + 8].unsqueeze(2).to_broadcast([128, 8, 128])
                    tt(dst, src, g_ap, ALU.mult, [PS[7], cols], [hT])
                else:
                    cp(dst, src, [PS[7]], [hT])

        def post_norm_residual(banks, gcol, s):
            ssa = []
            for hf in range(2):
                a = sm()
                act(tmpf[:, 0:512], banks[hf][:, 0:512], AF.Square, [banks[hf], small], [tmpf, small], accum_out=a)
                ssa.append(a)
            s3 = sm()
            tt(s3, ssa[0], ssa[1], ALU.add, [small], [small])
            rs = rstd_from_ss(s3, D)
            for hf in range(2):
                stt(tmpf[:, 0:512], banks[hf][:, 0:512], rs, gb[:, gcol + hf * 512:gcol + (hf + 1) * 512],
                    ALU.mult, ALU.mult, [banks[hf], small, gb], [tmpf])
                tt(xt[:, s, hf * 512:(hf + 1) * 512], xt[:, s, hf * 512:(hf + 1) * 512], tmpf[:, 0:512], ALU.add,
                   [xt, tmpf], [xt], eng="gpsimd")

        def fm_chunk(panel, pv, c, ps, M=128):
            for kc in range(8):
                mm(ps[0:M, 0:T], pv[:, kc, c * 128:c * 128 + M], hT[:, kc, :], kc == 0, kc == 7, [panel, hT], [ps])

        def rope_evac(ps, dst, W):
            act(xb16[:], ps[:, 0:T], AF.Copy, [ps], [xb16])
            mm(ps[:, T:2 * T], Rm, xb16[:], True, True, [cb, xb16], [ps])
            tt(rtmp[:, 0, :], ps[:, 0:T], cosT[:], ALU.mult, [ps, cosT], [rtmp])
            tt(rtmp[:, 1, :], ps[:, T:2 * T], sinT[:], ALU.mult, [ps, sinT], [rtmp])
            tt(dst, rtmp[:, 0, :], rtmp[:, 1, :], ALU.add, [rtmp], W)

        XTprev = None
        for l in range(depth):
            src_x = x_in if l == 0 else xbuf
            dst_x = y_out if l == depth - 1 else xbuf
            XT = [Tile("xd") for _ in range(NT)] if l < depth - 1 else None
            dma(cols[:], cols_in[l], [], [cols], psem[0])
            dma(gb[:], gb_in[l], [], [gb], psem[1])
            dma(bsp[:], bsp_in[l], [], [bsp], psem[2])
            cp(bsph[:, 0, :], bsp[:], [bsp], [bsph])
            tt(bsp[:], bsp[:], bsph[:, 0, :], ALU.subtract, [bsph], [bsp])
            cp(bsph[:, 1, :], bsp[:], [bsp], [bsph])
            dma(wbdf, wbd_in[l], [], [TR_], psem[3])
            cp(wbd[:], wbdf, [TR_], [wbd])
            dma(wbdf, wsp_in[l], [], [TR_], psem[4])
            tt(wspT[:], wbdf, tril01.unsqueeze(1).to_broadcast([128, 8, 128]), ALU.mult, [TR_, cf], [wspT])
            act(c8[:, 4:8], cols[:, 44:48], AF.Exp, [cols], [c8], scale=-1.0)
            ts(c8[:, 0:4], c8[:, 4:8], -0.25, 1.0 / 3.0, ALU.mult, ALU.add, [c8], [c8])
            tt(c8[:, 0:4], c8[:, 0:4], c8[:, 4:8], ALU.mult, [c8], [c8])
            ts(c8[:, 0:4], c8[:, 0:4], -1.0, 0.5, ALU.mult, ALU.add, [c8], [c8])
            tt(c8[:, 0:4], c8[:, 0:4], c8[:, 4:8], ALU.mult, [c8], [c8])
            ts(c8[:, 0:4], c8[:, 0:4], -1.0, 1.0, ALU.mult, ALU.add, [c8], [c8])
            tt(c8[:, 0:4], c8[:, 0:4], c8[:, 4:8], ALU.mult, [c8], [c8])
            ts(c8[:, 0:4], c8[:, 0:4], -8.0, None, ALU.mult, None, [c8], [c8])
            ts(c8[:, 4:8], c8[:, 0:4], 2.0, None, ALU.mult, None, [c8], [c8])
            memset(xrbuf[:, :, 0:3], 0.0, [xrbuf])
            memset(hst[:], 0.0, [hst])

            for j in range(NT):
                t0 = j * T
                first_tile = (l == 0 and j == 0)
                rd = [XTprev[j]] if (l > 0) else []
                dma(xt[:], src_x[t0:t0 + T, :].rearrange("(s p) d -> p s d", p=128), rd, [xt], xsem)
                dma(pt[:], p_in[l, t0:t0 + T, :].rearrange("(s p) d -> p s d", p=128), [], [pt], ptsem)
                dma(posi[:], pos_in[:, t0:t0 + T], [], [posi], possem)
                ang = rtmp[:, 0, :]
                u = rtmp[:, 1, :]
                rr = rtmp[:, 2, :]
                cp(u, posi[:], [posi], [rtmp])
                ts(ang, u, invf, None, ALU.mult, None, [rtmp, cf], [rtmp])
                ts(u, ang, 1.0 / (2 * math.pi), 12582912.0, ALU.mult, ALU.add, [rtmp], [rtmp])
                ts(u, u, -12582912.0, None, ALU.add, None, [rtmp], [rtmp])
                C1 = 6.28125
                C2 = float(np.float32(2 * math.pi - C1))
                C3 = float(2 * math.pi - C1 - C2)
                stt(rr, u, -C1, ang, ALU.mult, ALU.add, [rtmp], [rtmp])
                stt(rr, u, -C2, rr, ALU.mult, ALU.add, [rtmp], [rtmp])
                stt(rr, u, -C3, rr, ALU.mult, ALU.add, [rtmp], [rtmp])
                act(sinT[:], rr, AF.Sin, [rtmp], [sinT])
                stt(u, rr, -1.0, rr, ALU.mult, ALU.max, [rtmp], [rtmp])
                act(cosT[:], u, AF.Sin, [rtmp, epsc], [cosT], scale=-1.0, bias=epsc[:, 1:2])
                norm_transpose(0)
                if first_tile:
                    dump("hT", hT[:, 0, :], [hT])
                    dump("cosT", cosT[:], [cosT])
                    dump("sinT", sinT[:], [sinT])

                for nm, dstb in (("q", qT), ("k", KTs), ("qi", qiT)):
                    panel = stream.next("%d_%s" % (l, nm))
                    pv = wview(panel, nm)
                    for c in range(4):
                        ps = nps()
                        fm_chunk(panel, pv, c, ps)
                        rope_evac(ps, dstb[:, c, :], [dstb])
                panel = stream.next("%d_kiwi" % l)
                pv = wview(panel, "kiwi")
                ps = nps()
                for half in range(2):
                    for kc in range(8):
                        mm(ps[half * 64:(half + 1) * 64, 0:T], pv[:, kc, 0:64], hT[:, kc, :], kc == 0, kc == 7,
                           [panel, hT], [ps])
                rope_evac(ps, kiT[:, t0:t0 + T], [kiT])
                for s in range(NSUB):
                    ps = nps()
                    for kc in range(8):
                        mm(ps[:, 0:8], hT[:, kc, s * 128:(s + 1) * 128], pv[:, kc, 64:72], kc == 0, kc == 7,
                           [panel, hT], [ps])
                    cp(wis[:, s, :], ps[:, 0:8], [ps], [wis])
                dma(KTc[l][:, :, t0:t0 + T].rearrange("c p w -> p c w"), KTs[:], [KTs], [KVT[l][t0 // KG]], kvsem, eng="gpsimd")
                if first_tile:
                    dump("qT", qT[:, 0, :], [qT])
                    dump("kiT", kiT[:, 0:T], [kiT])
                    dump("wis", wis[:, 0, :], [wis])
                panel = stream.next("%d_xr" % l)
                pv = wview(panel, "xr")
                for c in range(4):
                    ps = nps()
                    fm_chunk(panel, pv, c, ps)
                    act(xrbuf[:, c, 3:3 + T], ps[:, 0:T], AF.Copy, [ps], [xrbuf])
                for nm, dstb in (("gr", grT), ("zu", zuT)):
                    panel = stream.next("%d_%s" % (l, nm))
                    pv = wview(panel, nm)
                    for c in range(4):
                        ps = nps()
                        fm_chunk(panel, pv, c, ps)
                        act(dstb[:, c, :], ps[:, 0:T], AF.Gelu_apprx_tanh, [ps], [dstb])
                panel = stream.next("%d_v" % l)
                pv = wview(panel, "v")
                for s in range(NSUB):
                    ps = nps()
                    for kc in range(8):
                        mm(ps[:, 0:512], hT[:, kc, s * 128:(s + 1) * 128], pv[:, kc, :], kc == 0, kc == 7, [panel, hT], [ps])
                    dstv = Vs[:, s, :].rearrange("p (h f) -> p h f", h=8)[:, :, 0:64]
                    act(dstv, ps[:, 0:512].rearrange("p (h f) -> p h f", h=8), AF.Copy, [ps], [Vs])
                dma(Vc[l][t0:t0 + T, :].rearrange("(s p) f -> p s f", p=128), Vs[:], [Vs], [KVT[l][t0 // KG]], kvsem, eng="gpsimd")
                panel = stream.next("%d_zv" % l)
                pv = wview(panel, "zv")
                for s in range(NSUB):
                    ps = nps()
                    for kc in range(8):
                        mm(ps[:, 0:512], hT[:, kc, s * 128:(s + 1) * 128], pv[:, kc, :], kc == 0, kc == 7, [panel, hT], [ps])
                    act(gz[:], ps[:, 0:512], AF.Gelu_apprx_tanh, [ps], [gz])
                    ss = sm()
                    act(tmpf[:, 0:512], gz[:], AF.Square, [gz, small], [tmpf, small], accum_out=ss)
                    rs = rstd_from_ss(ss, 512)
                    stt(vn[:, s, :], gz[:], rs, gb[:, 3 * D:3 * D + 512], ALU.mult, ALU.mult, [gz, small, gb], [vn])

                for s in range(NSUB):
                    for cpair in range(4):
                        ps = nps()
                        for gg in range(2):
                            g = cpair * 2 + gg
                            mm(ps[gg * 64:(gg + 1) * 64, 0:128], vn[:, s, g * 64:(g + 1) * 64], wspT[:, g, :], True, False,
                               [vn, wspT], [ps])
                        mm(ps[:, 0:128], esel[:, cpair, :], bsph[:, 0, :], False, False, [cb, bsph], [ps])
                        mm(ps[:, 0:128], esel[:, cpair, :], bsph[:, 1, :], False, True, [cb, bsph], [ps])
                        tt(ycT[:, cpair, s * 128:(s + 1) * 128], ps[:, 0:128], zuT[:, cpair, s * 128:(s + 1) * 128], ALU.mult,
                           [ps, zuT], [ycT])
                if first_tile:
                    dump("ycT", ycT[:, 0, :], [ycT])

                for c in range(4):
                    xc = lt[:, 0, :]
                    ts(xc, xrbuf[:, c, 0:T], cols[:, 16 + c:17 + c], cols[:, 32 + c:33 + c], ALU.mult, ALU.add, [xrbuf, cols], [lt])
                    for jj in range(1, 4):
                        stt(xc, xrbuf[:, c, jj:jj + T], cols[:, 16 + jj * 4 + c:17 + jj * 4 + c], xc, ALU.mult, ALU.add,
                            [xrbuf, cols, lt], [lt])
                    cp(xrbuf[:, c, 0:3], xrbuf[:, c, T:T + 3], [xrbuf], [xrbuf])
                    act(xcb[:], xc, AF.Copy, [lt], [xcb])
                    ps = nps()
                    mm(ps[:, 0:T], wbd[:, c, :], xcb[:], True, True, [wbd, xcb], [ps])
                    mm(ps[:, T:2 * T], wbd[:, 4 + c, :], xcb[:], True, True, [wbd, xcb], [ps])
                    rg = lt[:, 1, :]
                    ig = lt[:, 2, :]
                    av = lt[:, 3, :]
                    sq = lt[:, 4, :]
                    act(rg, ps[:, 0:T], AF.Sigmoid, [ps, cols], [lt], bias=cols[:, 36 + c:37 + c])
                    act(ig, ps[:, T:2 * T], AF.Sigmoid, [ps, cols], [lt], bias=cols[:, 40 + c:41 + c])
                    act(av, rg, AF.Exp, [lt, c8], [lt], scale=c8[:, c:c + 1])
                    act(sq, rg, AF.Exp, [lt, c8], [lt], scale=c8[:, 4 + c:5 + c])
                    act(sq, sq, AF.Sqrt, [lt, epsc], [lt], scale=-1.0, bias=epsc[:, 2:3])
                    tt(ig, ig, xc, ALU.mult, [lt], [lt])
                    tt(ig, ig, sq, ALU.mult, [lt], [lt])
                    hh = lt[:, 1, :]
                    scan(hh, av, ig, hst[:, c:c + 1], [lt, hst], [lt])
                    cp(hst[:, c:c + 1], hh[:, T - 1:T], [lt], [hst])
                    tt(ybT[:, c, :], hh, grT[:, c, :], ALU.mult, [lt, grT], [ybT])
                if first_tile:
                    dump("ybT", ybT[:, 0, :], [ybT])
                if l == 0 and j == 1:
                    dump("ybT1", ybT[:, 0, :], [ybT])
                    dump("ycT1", ycT[:, 0, :], [ycT])

                grps = kv_groups(j)
                nkeys = (j + 1) * T
                for s in range(NSUB):
                    N = t0 + 128 * (s + 1)
                    for h in range(8):
                        act(diag[:, h, :], identf, AF.Copy, [cf, wis], [diag], scale=wis[:, s, h:h + 1])
                    ngrp = (N + KG - 1) // KG
                    for kg in range(ngrp):
                        wd = min(KG, N - kg * KG)
                        for h in range(8):
                            ps = PS[h % 4]
                            r0 = (h % 2) * 64
                            mm(ps[:, 0:wd], qiT[r0:r0 + 64, h // 2, s * 128:(s + 1) * 128],
                               kiT[r0:r0 + 64, kg * KG:kg * KG + wd], True, True, [qiT, kiT], [ps])
                            act(Rrelu[:, h, 0:wd], ps[:, 0:wd], AF.Relu, [ps], [TR_])
                        for h in range(8):
                            mm(PS[4][:, 0:wd], diag[:, h, :], Rrelu[:, h, 0:wd], h == 0, h == 7, [diag, TR_], [PS[4]])
                        if kg == ngrp - 1:
                            if wd > 128:
                                cp(score_ap[:, kg * KG:kg * KG + wd - 128], PS[4][:, 0:wd - 128], [PS[4]], [TS_])
                            tt(score_ap[:, N - 128:N], PS[4][:, wd - 128:wd], negmask, ALU.add, [PS[4], cf], [TS_])
                            tt(tmpf[:, 0:128], PS[4][:, wd - 128:wd], posfill, ALU.add, [PS[4], cf], [tmpf])
                        else:
                            cp(score_ap[:, kg * KG:kg * KG + wd], PS[4][:, 0:wd], [PS[4]], [TS_])
                    hi0 = bis[:, 0:1]
                    lo = bis[:, 1:2]
                    w0 = bis[:, 2:3]
                    reduce(hi0, score_ap[:, 0:N], ALU.max, [TS_, bis], [bis])
                    reduce(lo, tmpf[:, 0:128], ALU.min, [tmpf, bis], [bis])
                    if N > 128:
                        m1 = bis[:, 3:4]
                        reduce(m1, score_ap[:, 0:N - 128], ALU.min, [TS_, bis], [bis])
                        tt(lo, lo, m1, ALU.min, [bis], [bis])
                    tt(w0, hi0, lo, ALU.subtract, [bis], [bis])
                    mk = masks[s]
                    if N > TOPK:
                        Nd = ((N // 2 + 127) // 128) * 128
                        Na = N - Nd
                        junkA = big[:, 4096:6144].bitcast(BF16)
                        for it in range(NBIS):
                            k4 = it % 4
                            mid = bis2[:, k4:k4 + 1]
                            cnt = bis2[:, 4 + k4:5 + k4]
                            vv = bis2[:, 8 + k4:9 + k4]
                            sA = bis2[:, 12 + k4:13 + k4]
                            stt(mid, w0, 0.5 ** (it + 1), lo, ALU.mult, ALU.add, [bis], [Tmid])
                            act(junkA[:, 0:Na], score_ap[:, Nd:N], AF.Sign, [TS_, Tmid], [TR_, Tsa], scale=-1.0, bias=mid,
                                accum_out=sA)
                            count_ge(mk[:, 0:Nd], score_ap[:, 0:Nd], mid, cnt, [TS_, Tmid], [mk, Tcnt])
                            stt(vv, cnt, 2.0, sA, ALU.mult, ALU.subtract, [Tcnt, Tsa], [Tcnt])
                            ge = smalli[:, k4:k4 + 1]
                            ts(ge, vv, float(2 * TOPK - Na), None, ALU.is_ge, None, [Tcnt], [smalli])
                            cpred(lo, ge, mid, [Tmid, smalli], [bis])
                    ts(mk[:, 0:N], score_ap[:, 0:N], lo, None, ALU.is_ge, None, [TS_, bis], [mk])
                    if N < nkeys:
                        memset(mk[:, N:nkeys], 0.0, [mk], eng="gpsimd")
                    if first_tile and s == 0:
                        dump("score", score_ap[:, 0:128], [TS_])
                    if l == 0 and j == 1 and s == 0:
                        dump("score1", score_ap[:, 0:384], [TS_])
                        dump("mask1", mk[:, 0:384], [mk])
                        dump("thr1", lo, [bis])

                first = True
                ucnt = [0]
                for gi, (g, wd) in enumerate(grps):
                    panel = stream.next("%d_kv%d_%d" % (l, j, g))
                    nb = wd // 128
                    KTv = panel[:, 0:4 * wd].rearrange("p (c w) -> p c w", c=4)
                    Vv = panel[:, 2048:2048 + nb * 520].rearrange("p (b f) -> p b f", b=nb)
                    psb = PS[6][:].bitcast(BF16)
                    for s in range(NSUB):
                        for b in range(nb):
                            tr(psb[:, (s * 4 + b) * 128:(s * 4 + b + 1) * 128],
                               masks[s][:, g * KG + b * 128:g * KG + (b + 1) * 128], identb, [masks[s], cb], [PS[6]])
                    for s in range(NSUB):
                        act(maskTk[:, 0:nb, s * 128:(s + 1) * 128],
                            psb[:, s * 512:s * 512 + nb * 128].rearrange("p (b t) -> p b t", b=nb),
                            AF.Copy, [PS[6]], [maskTk])
                    units = [(h, bp) for h in range(8) for bp in range(0, nb, 2)]

                    def stageA(i):
                        h, bp = units[i]
                        r0 = (h % 2) * 64
                        nbb = min(2, nb - bp)
                        ps = PS[(ucnt[0] + i) % 4]
                        for b in range(bp, bp + nbb):
                            mm(ps[:, (b - bp) * T:(b - bp + 1) * T], KTv[r0:r0 + 64, h // 2, b * 128:(b + 1) * 128],
                               qT[r0:r0 + 64, h // 2, :], True, True, [panel, qT], [ps])

                    def stageB(i):
                        h, bp = units[i]
                        nbb = min(2, nb - bp)
                        ps = PS[(ucnt[0] + i) % 4]
                        ptile = PT[(ucnt[0] + i) % len(PT)]
                        act(ptile[:, 0:nbb, :], ps[:, 0:nbb * T].rearrange("p (b t) -> p b t", b=nbb), AF.Exp,
                            [ps], [ptile], scale=HD ** -0.5)
                        tt(ptile[:, 0:nbb, :], ptile[:, 0:nbb, :], maskTk[:, bp:bp + nbb, :], ALU.mult, [ptile, maskTk], [ptile],
                           eng=("vector" if (ucnt[0] + i) % 2 == 0 else "gpsimd"))

                    def stageD(i):
                        h, bp = units[i]
                        nbb = min(2, nb - bp)
                        pacc = PS[4 + (h % 2)]
                        ptile = PT[(ucnt[0] + i) % len(PT)]
                        for b in range(bp, bp + nbb):
                            mm(pacc[0:65, 0:T], Vv[:, b, h * 65:(h + 1) * 65], ptile[:, b - bp, :], b == 0, b == nb - 1,
                               [panel, ptile], [pacc])
                        if bp + 2 >= nb:
                            if first:
                                cp(acc[:, h, :], pacc[0:65, 0:T], [pacc], [acc])
                            else:
                                tt(acc[:, h, :], acc[:, h, :], pacc[0:65, 0:T], ALU.add, [pacc, acc], [acc])

                    stageA(0)
                    for i in range(len(units)):
                        if i + 1 < len(units):
                            stageA(i + 1)
                        stageB(i)
                        stageD(i)
                    ucnt[0] += len(units)
                    first = False
                accf = acc[:].rearrange("p h t -> p (h t)")
                act(accf[64:65, :], accf[64:65, :], AF.Ln, [acc], [acc])
                act(accf[64:65, :], accf[64:65, :], AF.Exp, [acc], [acc], scale=-1.0)
                for h in range(8):
                    cp(rhl[64:65, 0, :], acc[64:65, h, :], [acc], [rhl])
                    tt(rhl[64:65, 1, :], acc[64:65, h, :], rhl[64:65, 0, :], ALU.subtract, [acc, rhl], [rhl])
                    ps = nps()
                    mm(ps[0:64, 0:T], onesb[64:65, 0:64], rhl[64:65, 0, :], True, False, [onesb, rhl], [ps])
                    mm(ps[0:64, 0:T], onesb[64:65, 0:64], rhl[64:65, 1, :], False, True, [onesb, rhl], [ps])
                    if h % 2 == 0:
                        tt(yaT[0:64, h // 2, :], acc[0:64, h, :], ps[0:64, 0:T], ALU.mult, [acc, ps], [yaT])
                    else:
                        tt(yaTt[:, :], acc[0:64, h, :], ps[0:64, 0:T], ALU.mult, [acc, ps], [yaTt])
                        ps2 = nps()
                        mm(ps2[64:128, 0:T], identb[0:64, 0:64], yaTt[:, :], True, True, [cb, yaTt], [ps2])
                        cp(yaT[64:128, h // 2, :], ps2[64:128, 0:T], [ps2], [yaT])
                if first_tile:
                    dump("yaT", yaT[:, 0, :], [yaT])
                if l == 0 and j == 1:
                    dump("yaT1", yaT[:, 0, :], [yaT])
                    dump("acc1", acc[:, 0, :], [acc])

                for ni, n in enumerate((2, 1, 0)):
                    for hf in range(2):
                        gpan = stream.next("%d_gate%d_%d" % (l, n, hf))
                        pv = wview(gpan, "gate0_0")
                        for c in range(4):
                            ps = nps()
                            fm_chunk(gpan, pv, c, ps)
                            act(gsig[:, hf * 4 + c, :], ps[:, 0:T], AF.Sigmoid, [ps], [gsig])
                    ysrc = {2: ycT, 1: ybT, 0: yaT}[n]
                    bp_ = [None, None]
                    for hf in range(2):
                        bp_[hf] = stream.next("%d_br%d_%d" % (l, n, hf))
                        pv = wview(bp_[hf], "br0_0")
                        for c in range(4):
                            dchunk = hf * 4 + c
                            ps = nps()
                            for kc in range(4):
                                mm(ps[:, 0:T], pv[:, kc, c * 128:(c + 1) * 128], ysrc[:, kc, :], kc == 0, kc == 3,
                                   [bp_[hf], ysrc], [ps])
                            if ni == 0:
                                tt(merged[:, dchunk, :], ps[:, 0:T], gsig[:, dchunk, :], ALU.mult, [ps, gsig], [TS_])
                            else:
                                tt(gt[:], ps[:, 0:T], gsig[:, dchunk, :], ALU.mult, [ps, gsig], [gt])
                                dsto = mergedT if ni == 2 else merged
                                tt(dsto[:, dchunk, :], merged[:, dchunk, :], gt[:], ALU.add, [TS_, gt], [TS_], eng="gpsimd")
                op_ = [stream.next("%d_out_%d" % (l, hf), look=NSLOT - 1 - hf) for hf in range(2)]
                for s in range(NSUB):
                    banks = [PS[4], PS[5]]
                    for hf in range(2):
                        pv = wview(op_[hf], "out_0")
                        for kc in range(8):
                            mm(banks[hf][:, 0:512], mergedT[:, kc, s * 128:(s + 1) * 128], pv[:, kc, :], kc == 0, kc == 7,
                               [op_[hf], TS_], [banks[hf]])
                    post_norm_residual(banks, 0, s)
                if first_tile:
                    dump("x1", xt[:, 0, :], [xt])
                norm_transpose(8)
                dbanks = [[PS[4], PS[5]], [PS[6], PS[7]]]
                for g in range(8):
                    up = stream.next("%d_up_%d" % (l, g))
                    pv = wview(up, "up_0")
                    fTg = fT[g % 2]
                    for c in range(4):
                        ps = nps()
                        fm_chunk(up, pv, c, ps)
                        rlb = rl[c % 2]
                        act(rlb, ps[:, 0:T], AF.Relu, [ps], [TR_])
                        tt(fTg[:, c, :], rlb, rlb, ALU.mult, [TR_], [TS_], eng="gpsimd")
                    dn = stream.next("%d_dn_%d" % (l, g))
                    dv = wview(dn, "dn_0")
                    for s in range(NSUB):
                        for hf in range(2):
                            for c in range(4):
                                mm(dbanks[s][hf][:, 0:512], fTg[:, c, s * 128:(s + 1) * 128], dv[:, c, hf * 512:(hf + 1) * 512],
                                   g == 0 and c == 0, g == 7 and c == 3, [dn, TS_], [dbanks[s][hf]])
                for s in range(NSUB):
                    post_norm_residual(dbanks[s], D, s)
                if first_tile:
                    dump("x2", xt[:, 0, :], [xt])
                norm_transpose(None)
                for s in range(NSUB):
                    cp(ptb[:, s, :], pt[:, s, :], [pt], [ptb])
                    psb = PS[3][:].bitcast(BF16)
                    for c in range(2):
                        tr(psb[:, c * 128:(c + 1) * 128], ptb[:, s, c * 128:(c + 1) * 128], identb, [ptb, cb], [PS[3]])
                    cp(pT[:, :, s * 128:(s + 1) * 128], psb[:, 0:256].rearrange("p (c t) -> p c t", c=2), [PS[3]], [pT])
                plp = stream.next("%d_ple" % l)
                plv = wview(plp, "ple")
                pg = [stream.next("%d_pg_%d" % (l, hf), look=NSLOT - 2 - hf) for hf in range(2)]
                for s in range(NSUB):
                    ssp = []
                    for hf in range(2):
                        pa = PS[0 + hf]
                        pgb = PS[2 + hf]
                        for kc in range(2):
                            mm(pa[:, 0:512], pT[:, kc, s * 128:(s + 1) * 128], plv[:, kc, hf * 512:(hf + 1) * 512], kc == 0, kc == 1,
                               [plp, pT], [pa])
                        gv = wview(pg[hf], "pg_0")
                        for kc in range(8):
                            mm(pgb[:, 0:512], hT[:, kc, s * 128:(s + 1) * 128], gv[:, kc, :], kc == 0, kc == 7, [pg[hf], hT], [pgb])
                        act(sg, pgb[:, 0:512], AF.Sigmoid, [pgb], [TR_])
                        tt(ple_t[:, hf * 512:(hf + 1) * 512], pa[:, 0:512], sg, ALU.mult, [pa, TR_], [TR_])
                        a = sm()
                        act(tmpf[:, 0:512], ple_t[:, hf * 512:(hf + 1) * 512], AF.Square, [TR_, small], [tmpf, small], accum_out=a)
                        ssp.append(a)
                    s3 = sm()
                    tt(s3, ssp[0], ssp[1], ALU.add, [small], [small])
                    rs = rstd_from_ss(s3, D)
                    for hf in range(2):
                        stt(tmpf[:, 0:512], ple_t[:, hf * 512:(hf + 1) * 512], rs, gb[:, 2 * D + hf * 512:2 * D + (hf + 1) * 512],
                            ALU.mult, ALU.mult, [TR_, small, gb], [tmpf])
                        tt(xt[:, s, hf * 512:(hf + 1) * 512], xt[:, s, hf * 512:(hf + 1) * 512], tmpf[:, 0:512], ALU.add,
                           [xt, tmpf], [xt], eng="gpsimd")
                wt = [XT[j]] if XT is not None else [out_tile]
                dma(dst_x[t0:t0 + T, :].rearrange("(s p) d -> p s d", p=128), xt[:], [xt], wt, ssem, eng="gpsimd")
            XTprev = XT

        fw.finish("sync", [out_tile] + dbg_tiles)
        fw.emit()
    return nc, fw


def _consts():
    bf = ml_dtypes.bfloat16
    ident = np.eye(128, dtype=np.float32)
    Rm = np.zeros((128, 128), np.float32)
    for base in (0, 64):
        for d in range(8):
            Rm[base + d + 8, base + d] = -1.0
            Rm[base + d, base + d + 8] = 1.0
    esel = np.zeros((128, 4, 128), np.float32)
    for g in range(8):
        esel[g, g // 2, (g % 2) * 64:(g % 2 + 1) * 64] = 1.0
    cb = np.concatenate([ident, Rm, esel.reshape(128, 512)], axis=1).astype(bf)
    tt_, ss_ = np.meshgrid(np.arange(128), np.arange(128), indexing="ij")
    negmask = np.where(ss_ > tt_, np.float32(NEG), np.float32(0.0))
    posfill = np.where(ss_ > tt_, np.float32(-2 * NEG), np.float32(0.0))
    tril01 = (tt_ <= ss_).astype(np.float32)
    half = 8
    inv_freq = (np.float32(500000.0) ** (-np.arange(half, dtype=np.float32) * np.float32(2.0) / np.float32(16))).astype(np.float32)
    invf = np.zeros((128, 1), np.float32)
    for f in range(128):
        d = f % 64
        if d < 16:
            invf[f, 0] = inv_freq[d % 8]
    cf = np.concatenate([ident, negmask, posfill, tril01, invf], axis=1).astype(np.float32)
    return cb, cf


def _layout_params(inp, depth):
    f = np.float32
    cols = np.zeros((depth, 128, 48), f)
    gb = np.zeros((depth, 128, 3 * D + 512), f)
    wbd = np.zeros((depth, 128, 8, 128), f)
    wsp = np.zeros((depth, 128, 8, 128), f)
    for l in range(depth):
        cols[l, :, 0:8] = np.asarray(inp["g_pre_mix"][l], f).reshape(8, 128).T
        cols[l, :, 8:16] = np.asarray(inp["g_pre_ffn"][l], f).reshape(8, 128).T
        cw = np.asarray(inp["conv_w"][l], f)
        for jj in range(4):
            cols[l, :, 16 + jj * 4:20 + jj * 4] = cw[jj].reshape(4, 128).T
        cols[l, :, 32:36] = np.asarray(inp["conv_b"][l], f).reshape(4, 128).T
        cols[l, :, 36:40] = np.asarray(inp["b_rg_a"][l], f).reshape(4, 128).T
        cols[l, :, 40:44] = np.asarray(inp["b_rg_x"][l], f).reshape(4, 128).T
        cols[l, :, 44:48] = np.asarray(inp["lru_lambda"][l], f).reshape(4, 128).T
        row = np.concatenate([np.asarray(inp["g_post_mix"][l], f), np.asarray(inp["g_post_ffn"][l], f),
                              np.asarray(inp["g_post_ple"][l], f), np.asarray(inp["g_gmlp_v"][l], f)])
        gb[l] = np.broadcast_to(row[None, :], (128, row.size))
        for gi, key in enumerate(("w_rg_a", "w_rg_x")):
            w = np.asarray(inp[key][l], f)
            for c in range(4):
                for hh in range(2):
                    wbd[l, hh * 64:(hh + 1) * 64, gi * 4 + c, hh * 64:(hh + 1) * 64] = w[c * 2 + hh]
        ws = np.asarray(inp["w_spatial"][l], f)
        wsp[l] = np.transpose(ws, (2, 0, 1))
    bsp = np.ascontiguousarray(np.asarray(inp["b_spatial"], f))
    return cols, gb, wbd, wsp, bsp


_CACHE = {}


def kernel(**inputs):
    depth = DEPTH
    x = np.asarray(inputs["x"], np.float32)
    B, L, _ = x.shape
    p = np.asarray(inputs["p"], np.float32)
    pos = np.asarray(inputs["positions"], np.int32)
    cb, cf = _consts()
    cols, gb, wbd, wsp, bsp = _layout_params(inputs, depth)
    shared = {
        "w_in": np.ascontiguousarray(np.asarray(inputs["w_in"], np.float32)),
        "w_branch": np.ascontiguousarray(np.asarray(inputs["w_branch"], np.float32)),
        "w_out": np.ascontiguousarray(np.asarray(inputs["w_out"], np.float32)),
        "w_ffn_up": np.ascontiguousarray(np.asarray(inputs["w_ffn_up"], np.float32)),
        "w_ffn_down": np.ascontiguousarray(np.asarray(inputs["w_ffn_down"], np.float32)),
        "w_ple": np.ascontiguousarray(np.asarray(inputs["w_ple"], np.float32)),
        "w_ple_gate": np.ascontiguousarray(np.asarray(inputs["w_ple_gate"], np.float32)),
        "cols": cols, "gb": gb, "wbd": wbd, "wsp": wsp, "bsp": bsp, "cb": cb, "cf": cf,
    }
    if L not in _CACHE:
        _CACHE[L] = build_program(L, depth)[0]
    nc = _CACHE[L]
    in_maps = []
    for b in range(B):
        m = dict(shared)
        m["x"] = np.ascontiguousarray(x[b])
        m["p"] = np.ascontiguousarray(p[:, b])
        m["pos"] = np.ascontiguousarray(np.broadcast_to(pos[b][None, :], (128, L)))
        in_maps.append(m)
    res = run_bass_kernel_spmd(nc, in_maps, core_ids=list(range(B)))
    return np.stack([np.asarray(r["y"], np.float32) for r in res.results], axis=0)
```

```python
import math
from contextlib import ExitStack
import numpy as np
import ml_dtypes
import concourse.bass as bass
import concourse.mybir as mybir
from concourse.bass_utils import run_bass_kernel_spmd

F32 = mybir.dt.float32
BF16 = mybir.dt.bfloat16
I32 = mybir.dt.int32
AF = mybir.ActivationFunctionType
ALU = mybir.AluOpType
AX = mybir.AxisListType

D = 1024
NH = 8
HD = 64
TOPK = 256
FFN = 4096
PLE = 256
EPS = 1e-6
DEPTH = 2
T = 256
NSUB = T // 128
KG = 512
NSLOT = 3
SLOTB = 4224
NBIS = 13
BIGM = 30000.0
NEG = -1.0e30
IN_OFF = dict(q=0, k=512, v=1024, qi=1536, kiwi=2048, xr=2120, gr=2632, zu=3144, zv=3656, gate=4168)


class Sem:
    def __init__(self, h, name):
        self.h = h
        self.n = 0
        self.name = name


class Tile:
    __slots__ = ("w", "r", "name")

    def __init__(self, name=""):
        self.w = {}
        self.r = {}
        self.name = name


class Buf:
    def __init__(self, t, name=""):
        self.t = t
        self.T = Tile(name)

    def __getitem__(self, k):
        return self.t[k]


class Engine:
    def __init__(self, name, sem):
        self.name = name
        self.sem = sem
        self.ops = []
        self.seen = {}


class FW:
    def __init__(self, nc, stack):
        self.nc = nc
        self.stack = stack
        self.engs = {}
        for n in ("tensor", "vector", "scalar", "gpsimd", "sync"):
            s = Sem(stack.enter_context(nc.semaphore("sem_" + n)), n)
            self.engs[n] = Engine(n, s)
        self.nops = 0

    def dsem(self, name):
        return Sem(self.stack.enter_context(self.nc.semaphore("dsem_" + name)), name)

    def op(self, eng, fn, reads=(), writes=(), dsem=None):
        E = self.engs[eng]
        need = {}
        for b in reads:
            t = b.T if isinstance(b, Buf) else b
            for s, v in t.w.items():
                if need.get(s, 0) < v:
                    need[s] = v
        for b in writes:
            t = b.T if isinstance(b, Buf) else b
            for s, v in t.w.items():
                if need.get(s, 0) < v:
                    need[s] = v
            for s, v in t.r.items():
                if need.get(s, 0) < v:
                    need[s] = v
        raw_self = 0
        for b in reads:
            t = b.T if isinstance(b, Buf) else b
            raw_self = max(raw_self, t.w.get(E.sem, 0))
        waits = []
        for s, v in need.items():
            if s is E.sem:
                if eng != "tensor" and raw_self > E.seen.get(s, 0):
                    E.seen[s] = raw_self
                    waits.append((s, raw_self))
                continue
            if E.seen.get(s, 0) >= v:
                continue
            E.seen[s] = v
            waits.append((s, v))
        if dsem is not None:
            dsem.n += 16
            sig = (dsem, dsem.n, 16)
        else:
            E.sem.n += 1
            sig = (E.sem, E.sem.n, 1)
        E.ops.append((waits, fn, sig))
        self.nops += 1
        s, v = sig[0], sig[1]
        for b in reads:
            t = b.T if isinstance(b, Buf) else b
            if t.r.get(s, 0) < v:
                t.r[s] = v
        for b in writes:
            t = b.T if isinstance(b, Buf) else b
            if t.w.get(s, 0) < v:
                t.w[s] = v

    def finish(self, eng, tiles):
        E = self.engs[eng]
        need = {}
        for b in tiles:
            t = b.T if isinstance(b, Buf) else b
            for d in (t.w, t.r):
                for s, v in d.items():
                    if need.get(s, 0) < v:
                        need[s] = v
        E.ops.append(([(s, v) for s, v in need.items() if s is not E.sem], None, None))

    def emit(self):
        with self.nc.Block() as block:
            for n, E in self.engs.items():
                def body(e, E=E):
                    for waits, fn, sig in E.ops:
                        for s, v in waits:
                            e.wait_ge(s.h, v)
                        if fn is None:
                            continue
                        fn(e).then_inc(sig[0].h, sig[2])
                getattr(block, n)(body)


def panel_defs():
    P = []
    for nm in ("q", "k", "qi"):
        P.append((nm, "w_in", 0, 1024, IN_OFF[nm], 512))
    P.append(("kiwi", "w_in", 0, 1024, IN_OFF["kiwi"], 72))
    for nm in ("xr", "gr", "zu", "v", "zv"):
        P.append((nm, "w_in", 0, 1024, IN_OFF[nm], 512))
    for n in range(3):
        for hf in range(2):
            P.append(("gate%d_%d" % (n, hf), "w_in", 0, 1024, IN_OFF["gate"] + n * 1024 + hf * 512, 512))
    for n in range(3):
        for hf in range(2):
            P.append(("br%d_%d" % (n, hf), "w_branch%d" % n, 0, 512, hf * 512, 512))
    for hf in range(2):
        P.append(("out_%d" % hf, "w_out", 0, 1024, hf * 512, 512))
    for g in range(8):
        P.append(("up_%d" % g, "w_ffn_up", 0, 1024, g * 512, 512))
        P.append(("dn_%d" % g, "w_ffn_down", g * 512, 512, 0, 1024))
    P.append(("ple", "w_ple", 0, 256, 0, 1024))
    for hf in range(2):
        P.append(("pg_%d" % hf, "w_ple_gate", 0, 1024, hf * 512, 512))
    return P


def build_program(L, depth=DEPTH, dbg=None):
    NT = L // T
    nc = bass.Bass("TRN2", target_bir_lowering=False)
    dram = lambda name, shape, dt, kind="ExternalInput": nc.dram_tensor(name, shape, dt, kind=kind).ap()
    x_in = dram("x", [L, D], F32)
    p_in = dram("p", [depth, L, PLE], F32)
    pos_in = dram("pos", [128, L], I32)
    wsrc = {
        "w_in": dram("w_in", [depth, D, 7240], F32),
        "w_branch": dram("w_branch", [depth, 3, 512, D], F32),
        "w_out": dram("w_out", [depth, D, D], F32),
        "w_ffn_up": dram("w_ffn_up", [depth, D, FFN], F32),
        "w_ffn_down": dram("w_ffn_down", [depth, FFN, D], F32),
        "w_ple": dram("w_ple", [depth, PLE, D], F32),
        "w_ple_gate": dram("w_ple_gate", [depth, D, D], F32),
    }
    cols_in = dram("cols", [depth, 128, 48], F32)
    gb_in = dram("gb", [depth, 128, 3 * D + 512], F32)
    wbd_in = dram("wbd", [depth, 128, 8, 128], F32)
    wsp_in = dram("wsp", [depth, 128, 8, 128], F32)
    bsp_in = dram("bsp", [depth, 8, 128], F32)
    cb_in = dram("cb", [128, 256 + 512], BF16)
    cf_in = dram("cf", [128, 4 * 128 + 1], F32)
    y_out = dram("y", [L, D], F32, kind="ExternalOutput")
    xbuf = dram("xbuf", [L, D], F32, kind="Internal")
    KTc = [dram("ktc%d" % l, [4, 128, L], BF16, kind="Internal") for l in range(depth)]
    Vc = [dram("vc%d" % l, [L, 520], BF16, kind="Internal") for l in range(depth)]
    pdefs = panel_defs()
    Wp = [{nm: dram("wp%d_%s" % (l, nm), [nr, ncol], BF16, kind="Internal") for (nm, _, _, nr, _, ncol) in pdefs}
          for l in range(depth)]
    dbg_out = {}
    if dbg:
        for k, shp in dbg.items():
            dbg_out[k] = dram("dbg_" + k, list(shp), F32, kind="ExternalOutput")

    with ExitStack() as st:
        fw = FW(nc, st)
        op = fw.op

        def sb(name, shape, dt):
            return Buf(st.enter_context(nc.sbuf_tensor("s_" + name, shape, dt)), name)

        PS = [Buf(st.enter_context(nc.psum_tensor("ps%d" % i, [128, 512], F32)), "ps%d" % i) for i in range(8)]

        def mm(out, lhsT, rhs, start, stop, R, W):
            op("tensor", lambda e: e.matmul(out, lhsT=lhsT, rhs=rhs, start=start, stop=stop), R, W)

        def tr(out, in_, ident, R, W):
            op("tensor", lambda e: e.transpose(out=out, in_=in_, identity=ident), R, W)

        def act(out, in_, func, R, W, **kw):
            op("scalar", lambda e: e.activation(out=out, in_=in_, func=func, **kw), R, W)

        def ts(out, in0, s1, s2, op0, op1, R, W, eng="vector"):
            if op1 is None:
                op(eng, lambda e: e.tensor_scalar(out=out, in0=in0, scalar1=s1, scalar2=None, op0=op0), R, W)
            else:
                op(eng, lambda e: e.tensor_scalar(out=out, in0=in0, scalar1=s1, scalar2=s2, op0=op0, op1=op1), R, W)

        def tt(out, in0, in1, o, R, W, eng="vector"):
            op(eng, lambda e: e.tensor_tensor(out=out, in0=in0, in1=in1, op=o), R, W)

        def stt(out, in0, s, in1, op0, op1, R, W):
            op("vector", lambda e: e.scalar_tensor_tensor(out=out, in0=in0, scalar=s, in1=in1, op0=op0, op1=op1), R, W)

        def cp(out, in_, R, W, eng="vector"):
            op(eng, lambda e: e.tensor_copy(out=out, in_=in_), R, W)

        def dma(out, in_, R, W, ds, eng="sync"):
            op(eng, lambda e: e.dma_start(out=out, in_=in_), R, W, dsem=ds)

        def memset(ap, val, W, eng="vector"):
            op(eng, lambda e: e.memset(ap, val), [], W)

        def reduce(out, in_, o, R, W):
            op("vector", lambda e: e.tensor_reduce(out=out, in_=in_, axis=AX.X, op=o), R, W)

        def recip(out, in_, R, W):
            op("vector", lambda e: e.reciprocal(out=out, in_=in_), R, W)

        def scan(out, d0, d1, init, R, W):
            op("vector", lambda e: e.tensor_tensor_scan(out=out, data0=d0, data1=d1, initial=init,
                                                        op0=ALU.mult, op1=ALU.add), R, W)

        def count_ge(out, in0, thr, cnt, R, W):
            op("vector", lambda e: e.tensor_scalar(out=out, in0=in0, scalar1=thr, scalar2=None, op0=ALU.is_ge,
                                                   op1=ALU.add, accum_out=cnt), R, W)

        def cpred(out, mask, data, R, W):
            op("vector", lambda e: e.copy_predicated(out=out, mask=mask, data=data), R, W)

        dbg_sem = fw.dsem("dbg")
        dbg_tiles = []

        def dump(name, ap, R):
            if name in dbg_out:
                t = Tile("dbg")
                dma(dbg_out[name], ap, R, [t], dbg_sem, eng="gpsimd")
                dbg_tiles.append(t)
                del dbg_out[name]

        cb = sb("cb", [128, 768], BF16)
        cf = sb("cf", [128, 513], F32)
        dma(cb[:], cb_in, [], [cb], fw.dsem("c0"))
        dma(cf[:], cf_in, [], [cf], fw.dsem("c1"))
        identb = cb[:, 0:128]
        Rm = cb[:, 128:256]
        esel = cb[0:8, 256:768].rearrange("p (c f) -> p c f", c=4)
        identf = cf[:, 0:128]
        negmask = cf[:, 128:256]
        posfill = cf[:, 256:384]
        tril01 = cf[:, 384:512]
        invf = cf[:, 512:513]

        WT = [dict() for _ in range(depth)]
        for l in range(depth):
            wcs = fw.dsem("wcast%d" % l)
            for (nm, src, r0, nr, c0, ncol) in pdefs:
                if src.startswith("w_branch"):
                    s_ap = wsrc["w_branch"][l, int(src[-1]), r0:r0 + nr, c0:c0 + ncol]
                else:
                    s_ap = wsrc[src][l, r0:r0 + nr, c0:c0 + ncol]
                t = Tile("wp")
                WT[l][nm] = t
                step = 512
                for rr in range(0, nr, step):
                    n2 = min(step, nr - rr)
                    dma(Wp[l][nm][rr:rr + n2, :], s_ap[rr:rr + n2, :], [], [t], wcs, eng="gpsimd")
            for t in WT[l].values():
                t.w = {wcs: wcs.n}

        ring = [sb("ring%d" % i, [128, SLOTB], BF16) for i in range(NSLOT)]
        ring_sem = [fw.dsem("ring%d" % i) for i in range(NSLOT)]
        kiT = sb("kiT", [128, L], BF16)
        xt = sb("xt", [128, NSUB, D], F32)
        pt = sb("pt", [128, NSUB, PLE], F32)
        ptb = sb("ptb", [128, NSUB, PLE], BF16)
        posi = sb("posi", [128, T], I32)
        hT = sb("hT", [128, 8, T], BF16)
        pT = sb("pT", [128, 2, T], BF16)
        qT = sb("qT", [128, 4, T], BF16)
        qiT = sb("qiT", [128, 4, T], BF16)
        KTs = sb("KTs", [128, 4, T], BF16)
        Vs = sb("Vs", [128, NSUB, 520], BF16)
        cosT = sb("cosT", [128, T], F32)
        sinT = sb("sinT", [128, T], F32)
        rtmp = sb("rtmp", [128, 3, T], F32)
        xb16 = sb("xb16", [128, T], BF16)
        xrbuf = sb("xrbuf", [128, 4, 3 + T], F32)
        grT = sb("grT", [128, 4, T], BF16)
        zuT = sb("zuT", [128, 4, T], BF16)
        gz = sb("gz", [128, 512], F32)
        vn = sb("vn", [128, NSUB, 512], BF16)
        lt = sb("lt", [128, 5, T], F32)
        xcb = sb("xcb", [128, T], BF16)
        hst = sb("hst", [128, 4], F32)
        ybT = sb("ybT", [128, 4, T], BF16)
        ycT = sb("ycT", [128, 4, T], BF16)
        yaT = sb("yaT", [128, 4, T], BF16)
        yaTt = sb("yaTt", [64, T], BF16)
        wis = sb("wis", [128, NSUB, 8], F32)
        diag = sb("diag", [128, 8, 128], BF16)
        small = sb("small", [128, 32], F32)
        smalli = sb("smalli", [128, 4], I32)
        bis = sb("bis", [128, 16], F32)
        bis2 = sb("bis2", [128, 16], F32)
        Tmid = Tile("mid")
        Tcnt = Tile("cnt")
        Tsa = Tile("sa")
        big = sb("big", [128, 6144], F32)
        TS_ = big.T
        TR_ = Tile("bigR")
        score_ap = big[:, 0:L]
        Rrelu = big[:, 4096:6144].bitcast(BF16).rearrange("p (h w) -> p h w", h=8)
        masks = [sb("mask%d" % s, [128, L], BF16) for s in range(NSUB)]
        maskTk = sb("maskTk", [128, 4, T], BF16)
        PT = [sb("PT%d" % i, [128, 2, T], BF16) for i in range(4)]
        xs = sb("xs", [128, D], BF16)
        acc = sb("acc", [65, 8, T], F32)
        rhl = sb("rhl", [65, 2, T], BF16)
        onesb = sb("onesb", [65, 64], BF16)
        gt = sb("gt", [128, T], F32)
        tmpf = sb("tmpf", [128, 512], F32)
        gsig = sb("gsig", [128, 8, T], BF16)
        merged = big[:, 0:8 * T].rearrange("p (c t) -> p c t", c=8)
        o = 8 * T
        mergedT = big[:, o:o + 4 * T].bitcast(BF16).rearrange("p (c t) -> p c t", c=8)
        o += 4 * T
        fT = [big[:, o + i * 2 * T:o + (i + 1) * 2 * T].bitcast(BF16).rearrange("p (c t) -> p c t", c=4) for i in range(2)]
        o += 4 * T
        assert o <= 4096
        o = 4096
        rl = [big[:, o + i * (T // 2):o + (i + 1) * (T // 2)].bitcast(BF16) for i in range(2)]
        o += T
        sg = big[:, o:o + 512]
        o += 512
        ple_t = big[:, o:o + 1024]
        o += 1024
        assert o <= 6144
        wbdf = big[:, 4096:4096 + 1024].rearrange("p (c j) -> p c j", c=8)
        cols = sb("cols", [128, 48], F32)
        gb = sb("gb", [128, 3 * D + 512], F32)
        wbd = sb("wbd", [128, 8, 128], BF16)
        wspT = sb("wspT", [128, 8, 128], BF16)
        bsp = sb("bsp", [8, 128], F32)
        bsph = sb("bsph", [8, 2, 128], BF16)
        c8 = sb("c8", [128, 8], F32)
        epsc = sb("epsc", [128, 4], F32)
        psem = [fw.dsem("par%d" % i) for i in range(5)]
        xsem = fw.dsem("xload")
        ptsem = fw.dsem("pload")
        possem = fw.dsem("posload")
        ssem = fw.dsem("store")
        kvsem = fw.dsem("kvstore")
        out_tile = Tile("out")

        class Stream:
            def __init__(self):
                self.plan = []
                self.issued = 0
                self.pos = 0

            def add(self, name, loader, deps, hoist=True):
                self.plan.append((name, loader, deps, hoist))

            def next(self, name, look=NSLOT - 1):
                i = self.pos
                assert self.plan[i][0] == name, (self.plan[i][0], name)
                while self.issued < len(self.plan) and (
                        self.issued <= i or (self.issued <= i + look and self.plan[self.issued][3])):
                    k = self.issued
                    _, loader, deps, _ = self.plan[k]
                    loader(ring[k % NSLOT], ring_sem[k % NSLOT], deps)
                    self.issued += 1
                self.pos += 1
                return ring[i % NSLOT]

        stream = Stream()

        def wloader(l, nm, nr, ncol):
            kc = nr // 128

            def f(slot, sem, deps):
                dst = slot[:, 0:kc * ncol].rearrange("p (k w) -> p k w", k=kc)
                src = Wp[l][nm].rearrange("(k p) w -> p k w", p=128)
                dma(dst, src, deps, [slot], sem)
            return f

        KVT = [[Tile("kv") for _ in range((L + KG - 1) // KG)] for _ in range(depth)]

        def kvloader(l, g, wd):
            def f(slot, sem, deps):
                dstk = slot[:, 0:4 * wd].rearrange("p (c w) -> p c w", c=4)
                dma(dstk, KTc[l][:, :, g * KG:g * KG + wd].rearrange("c p w -> p c w"), deps, [slot], sem)
                nb = wd // 128
                dstv = slot[:, 2048:2048 + nb * 520].rearrange("p (b f) -> p b f", b=nb)
                dma(dstv, Vc[l][g * KG:g * KG + wd, :].rearrange("(b p) f -> p b f", p=128), deps, [slot], sem)
            return f

        pinfo = {nm: (nr, ncol) for (nm, _, _, nr, _, ncol) in pdefs}

        def plan_w(l, nm):
            nr, ncol = pinfo[nm]
            stream.add("%d_%s" % (l, nm), wloader(l, nm, nr, ncol), [WT[l][nm]])

        def kv_groups(j):
            nkeys = (j + 1) * T
            out = []
            g = 0
            while g * KG < nkeys:
                out.append((g, min(KG, nkeys - g * KG)))
                g += 1
            return out

        for l in range(depth):
            for j in range(NT):
                for nm in ("q", "k", "qi", "kiwi", "xr", "gr", "zu", "v", "zv"):
                    plan_w(l, nm)
                grps = kv_groups(j)
                for (g, wd) in grps:
                    last = (g == grps[-1][0])
                    stream.add("%d_kv%d_%d" % (l, j, g), kvloader(l, g, wd), [KVT[l][g]], hoist=not last)
                for n in (2, 1, 0):
                    plan_w(l, "gate%d_0" % n)
                    plan_w(l, "gate%d_1" % n)
                    plan_w(l, "br%d_0" % n)
                    plan_w(l, "br%d_1" % n)
                plan_w(l, "out_0")
                plan_w(l, "out_1")
                plan_w(l, "up_0")
                for g in range(8):
                    if g + 1 < 8:
                        plan_w(l, "up_%d" % (g + 1))
                    plan_w(l, "dn_%d" % g)
                plan_w(l, "ple")
                plan_w(l, "pg_0")
                plan_w(l, "pg_1")

        def wview(slot, nm):
            nr, ncol = pinfo[nm]
            kc = nr // 128
            return slot[:, 0:kc * ncol].rearrange("p (k w) -> p k w", k=kc)

        sm_i = [0]

        def sm():
            i = sm_i[0] % 32
            sm_i[0] += 1
            return small[:, i:i + 1]

        memset(epsc[:, 0:1], EPS, [epsc])
        memset(epsc[:, 1:2], math.pi / 2, [epsc])
        memset(epsc[:, 2:3], 1.0, [epsc])
        memset(epsc[:, 3:4], -BIGM, [epsc])
        memset(Vs[:, :, :], 1.0, [Vs])
        memset(onesb[:, :], 1.0, [onesb])

        def rstd_from_ss(ss_ap, n):
            a = sm()
            b = sm()
            act(a, ss_ap, AF.Sqrt, [small, epsc], [small], scale=1.0 / n, bias=epsc[:, 0:1])
            recip(b, a, [small], [small])
            return b

        pi = [0]

        def nps():
            pi[0] += 1
            return PS[pi[0] % 4]

        def norm_transpose(gcol0):
            for s in range(NSUB):
                if gcol0 is not None:
                    ss = sm()
                    act(tmpf[:, 0:512], xt[:, s, 0:512], AF.Square, [xt, small], [tmpf, small], accum_out=ss)
                    ss2 = sm()
                    act(tmpf[:, 0:512], xt[:, s, 512:1024], AF.Square, [xt, small], [tmpf, small], accum_out=ss2)
                    ss3 = sm()
                    tt(ss3, ss, ss2, ALU.add, [small], [small])
                    rs = rstd_from_ss(ss3, D)
                    ts(xs[:], xt[:, s, :], rs, None, ALU.mult, None, [xt, small], [xs])
                else:
                    cp(xs[:], xt[:, s, :], [xt], [xs])
                psb = PS[7][:].bitcast(BF16)
                for c in range(8):
                    tr(psb[:, c * 128:(c + 1) * 128], xs[:, c * 128:(c + 1) * 128], identb, [xs, cb], [PS[7]])
                src = psb[:, 0:1024].rearrange("p (c t) -> p c t", c=8)
                dst = hT[:, :, s * 128:(s + 1) * 128]
                if gcol0 is not None:
                    g_ap = cols[:, gcol0:gcol0 + 8].unsqueeze(2).to_broadcast([128, 8, 128])
                    tt(dst, src, g_ap, ALU.mult, [PS[7], cols], [hT])
                else:
                    cp(dst, src, [PS[7]], [hT])

        def post_norm_residual(banks, gcol, s):
            ssa = []
            for hf in range(2):
                a = sm()
                act(tmpf[:, 0:512], banks[hf][:, 0:512], AF.Square, [banks[hf], small], [tmpf, small], accum_out=a)
                ssa.append(a)
            s3 = sm()
            tt(s3, ssa[0], ssa[1], ALU.add, [small], [small])
            rs = rstd_from_ss(s3, D)
            for hf in range(2):
                stt(tmpf[:, 0:512], banks[hf][:, 0:512], rs, gb[:, gcol + hf * 512:gcol + (hf + 1) * 512],
                    ALU.mult, ALU.mult, [banks[hf], small, gb], [tmpf])
                tt(xt[:, s, hf * 512:(hf + 1) * 512], xt[:, s, hf * 512:(hf + 1) * 512], tmpf[:, 0:512], ALU.add,
                   [xt, tmpf], [xt], eng="gpsimd")

        def fm_chunk(panel, pv, c, ps, M=128):
            for kc in range(8):
                mm(ps[0:M, 0:T], pv[:, kc, c * 128:c * 128 + M], hT[:, kc, :], kc == 0, kc == 7, [panel, hT], [ps])

        def rope_evac(ps, dst, W):
            act(xb16[:], ps[:, 0:T], AF.Copy, [ps], [xb16])
            mm(ps[:, T:2 * T], Rm, xb16[:], True, True, [cb, xb16], [ps])
            tt(rtmp[:, 0, :], ps[:, 0:T], cosT[:], ALU.mult, [ps, cosT], [rtmp])
            tt(rtmp[:, 1, :], ps[:, T:2 * T], sinT[:], ALU.mult, [ps, sinT], [rtmp])
            tt(dst, rtmp[:, 0, :], rtmp[:, 1, :], ALU.add, [rtmp], W)

        XTprev = None
        for l in range(depth):
            src_x = x_in if l == 0 else xbuf
            dst_x = y_out if l == depth - 1 else xbuf
            XT = [Tile("xd") for _ in range(NT)] if l < depth - 1 else None
            dma(cols[:], cols_in[l], [], [cols], psem[0])
            dma(gb[:], gb_in[l], [], [gb], psem[1])
            dma(bsp[:], bsp_in[l], [], [bsp], psem[2])
            cp(bsph[:, 0, :], bsp[:], [bsp], [bsph])
            tt(bsp[:], bsp[:], bsph[:, 0, :], ALU.subtract, [bsph], [bsp])
            cp(bsph[:, 1, :], bsp[:], [bsp], [bsph])
            dma(wbdf, wbd_in[l], [], [TR_], psem[3])
            cp(wbd[:], wbdf, [TR_], [wbd])
            dma(wbdf, wsp_in[l], [], [TR_], psem[4])
            tt(wspT[:], wbdf, tril01.unsqueeze(1).to_broadcast([128, 8, 128]), ALU.mult, [TR_, cf], [wspT])
            act(c8[:, 4:8], cols[:, 44:48], AF.Exp, [cols], [c8], scale=-1.0)
            ts(c8[:, 0:4], c8[:, 4:8], -0.25, 1.0 / 3.0, ALU.mult, ALU.add, [c8], [c8])
            tt(c8[:, 0:4], c8[:, 0:4], c8[:, 4:8], ALU.mult, [c8], [c8])
            ts(c8[:, 0:4], c8[:, 0:4], -1.0, 0.5, ALU.mult, ALU.add, [c8], [c8])
            tt(c8[:, 0:4], c8[:, 0:4], c8[:, 4:8], ALU.mult, [c8], [c8])
            ts(c8[:, 0:4], c8[:, 0:4], -1.0, 1.0, ALU.mult, ALU.add, [c8], [c8])
            tt(c8[:, 0:4], c8[:, 0:4], c8[:, 4:8], ALU.mult, [c8], [c8])
            ts(c8[:, 0:4], c8[:, 0:4], -8.0, None, ALU.mult, None, [c8], [c8])
            ts(c8[:, 4:8], c8[:, 0:4], 2.0, None, ALU.mult, None, [c8], [c8])
            memset(xrbuf[:, :, 0:3], 0.0, [xrbuf])
            memset(hst[:], 0.0, [hst])

            for j in range(NT):
                t0 = j * T
                first_tile = (l == 0 and j == 0)
                rd = [XTprev[j]] if (l > 0) else []
                dma(xt[:], src_x[t0:t0 + T, :].rearrange("(s p) d -> p s d", p=128), rd, [xt], xsem)
                dma(pt[:], p_in[l, t0:t0 + T, :].rearrange("(s p) d -> p s d", p=128), [], [pt], ptsem)
                dma(posi[:], pos_in[:, t0:t0 + T], [], [posi], possem)
                ang = rtmp[:, 0, :]
                u = rtmp[:, 1, :]
                rr = rtmp[:, 2, :]
                cp(u, posi[:], [posi], [rtmp])
                ts(ang, u, invf, None, ALU.mult, None, [rtmp, cf], [rtmp])
                ts(u, ang, 1.0 / (2 * math.pi), 12582912.0, ALU.mult, ALU.add, [rtmp], [rtmp])
                ts(u, u, -12582912.0, None, ALU.add, None, [rtmp], [rtmp])
                C1 = 6.28125
                C2 = float(np.float32(2 * math.pi - C1))
                C3 = float(2 * math.pi - C1 - C2)
                stt(rr, u, -C1, ang, ALU.mult, ALU.add, [rtmp], [rtmp])
                stt(rr, u, -C2, rr, ALU.mult, ALU.add, [rtmp], [rtmp])
                stt(rr, u, -C3, rr, ALU.mult, ALU.add, [rtmp], [rtmp])
                act(sinT[:], rr, AF.Sin, [rtmp], [sinT])
                stt(u, rr, -1.0, rr, ALU.mult, ALU.max, [rtmp], [rtmp])
                act(cosT[:], u, AF.Sin, [rtmp, epsc], [cosT], scale=-1.0, bias=epsc[:, 1:2])
                norm_transpose(0)
                if first_tile:
                    dump("hT", hT[:, 0, :], [hT])
                    dump("cosT", cosT[:], [cosT])
                    dump("sinT", sinT[:], [sinT])

                for nm, dstb in (("q", qT), ("k", KTs), ("qi", qiT)):
                    panel = stream.next("%d_%s" % (l, nm))
                    pv = wview(panel, nm)
                    pss = [nps() for _ in range(4)]
                    fm_chunk(panel, pv, 0, pss[0])
                    for c in range(4):
                        if c + 1 < 4:
                            fm_chunk(panel, pv, c + 1, pss[c + 1])
                        rope_evac(pss[c], dstb[:, c, :], [dstb])
                panel = stream.next("%d_kiwi" % l)
                pv = wview(panel, "kiwi")
                ps = nps()
                for half in range(2):
                    for kc in range(8):
                        mm(ps[half * 64:(half + 1) * 64, 0:T], pv[:, kc, 0:64], hT[:, kc, :], kc == 0, kc == 7,
                           [panel, hT], [ps])
                rope_evac(ps, kiT[:, t0:t0 + T], [kiT])
                for s in range(NSUB):
                    ps = nps()
                    for kc in range(8):
                        mm(ps[:, 0:8], hT[:, kc, s * 128:(s + 1) * 128], pv[:, kc, 64:72], kc == 0, kc == 7,
                           [panel, hT], [ps])
                    cp(wis[:, s, :], ps[:, 0:8], [ps], [wis])
                dma(KTc[l][:, :, t0:t0 + T].rearrange("c p w -> p c w"), KTs[:], [KTs], [KVT[l][t0 // KG]], kvsem, eng="gpsimd")
                if first_tile:
                    dump("qT", qT[:, 0, :], [qT])
                    dump("kiT", kiT[:, 0:T], [kiT])
                    dump("wis", wis[:, 0, :], [wis])
                panel = stream.next("%d_xr" % l)
                pv = wview(panel, "xr")
                for c in range(4):
                    ps = nps()
                    fm_chunk(panel, pv, c, ps)
                    act(xrbuf[:, c, 3:3 + T], ps[:, 0:T], AF.Copy, [ps], [xrbuf])
                for nm, dstb in (("gr", grT), ("zu", zuT)):
                    panel = stream.next("%d_%s" % (l, nm))
                    pv = wview(panel, nm)
                    for c in range(4):
                        ps = nps()
                        fm_chunk(panel, pv, c, ps)
                        act(dstb[:, c, :], ps[:, 0:T], AF.Gelu_apprx_tanh, [ps], [dstb])
                panel = stream.next("%d_v" % l)
                pv = wview(panel, "v")
                for s in range(NSUB):
                    ps = nps()
                    for kc in range(8):
                        mm(ps[:, 0:512], hT[:, kc, s * 128:(s + 1) * 128], pv[:, kc, :], kc == 0, kc == 7, [panel, hT], [ps])
                    dstv = Vs[:, s, :].rearrange("p (h f) -> p h f", h=8)[:, :, 0:64]
                    act(dstv, ps[:, 0:512].rearrange("p (h f) -> p h f", h=8), AF.Copy, [ps], [Vs])
                dma(Vc[l][t0:t0 + T, :].rearrange("(s p) f -> p s f", p=128), Vs[:], [Vs], [KVT[l][t0 // KG]], kvsem, eng="gpsimd")
                panel = stream.next("%d_zv" % l)
                pv = wview(panel, "zv")
                for s in range(NSUB):
                    ps = nps()
                    for kc in range(8):
                        mm(ps[:, 0:512], hT[:, kc, s * 128:(s + 1) * 128], pv[:, kc, :], kc == 0, kc == 7, [panel, hT], [ps])
                    act(gz[:], ps[:, 0:512], AF.Gelu_apprx_tanh, [ps], [gz])
                    ss = sm()
                    act(tmpf[:, 0:512], gz[:], AF.Square, [gz, small], [tmpf, small], accum_out=ss)
                    rs = rstd_from_ss(ss, 512)
                    stt(vn[:, s, :], gz[:], rs, gb[:, 3 * D:3 * D + 512], ALU.mult, ALU.mult, [gz, small, gb], [vn])

                for s in range(NSUB):
                    for cpair in range(4):
                        ps = nps()
                        for gg in range(2):
                            g = cpair * 2 + gg
                            mm(ps[gg * 64:(gg + 1) * 64, 0:128], vn[:, s, g * 64:(g + 1) * 64], wspT[:, g, :], True, False,
                               [vn, wspT], [ps])
                        mm(ps[:, 0:128], esel[:, cpair, :], bsph[:, 0, :], False, False, [cb, bsph], [ps])
                        mm(ps[:, 0:128], esel[:, cpair, :], bsph[:, 1, :], False, True, [cb, bsph], [ps])
                        tt(ycT[:, cpair, s * 128:(s + 1) * 128], ps[:, 0:128], zuT[:, cpair, s * 128:(s + 1) * 128], ALU.mult,
                           [ps, zuT], [ycT])
                if first_tile:
                    dump("ycT", ycT[:, 0, :], [ycT])

                for c in range(4):
                    xc = lt[:, 0, :]
                    ts(xc, xrbuf[:, c, 0:T], cols[:, 16 + c:17 + c], cols[:, 32 + c:33 + c], ALU.mult, ALU.add, [xrbuf, cols], [lt])
                    for jj in range(1, 4):
                        stt(xc, xrbuf[:, c, jj:jj + T], cols[:, 16 + jj * 4 + c:17 + jj * 4 + c], xc, ALU.mult, ALU.add,
                            [xrbuf, cols, lt], [lt])
                    cp(xrbuf[:, c, 0:3], xrbuf[:, c, T:T + 3], [xrbuf], [xrbuf])
                    act(xcb[:], xc, AF.Copy, [lt], [xcb])
                    ps = nps()
                    mm(ps[:, 0:T], wbd[:, c, :], xcb[:], True, True, [wbd, xcb], [ps])
                    mm(ps[:, T:2 * T], wbd[:, 4 + c, :], xcb[:], True, True, [wbd, xcb], [ps])
                    rg = lt[:, 1, :]
                    ig = lt[:, 2, :]
                    av = lt[:, 3, :]
                    sq = lt[:, 4, :]
                    act(rg, ps[:, 0:T], AF.Sigmoid, [ps, cols], [lt], bias=cols[:, 36 + c:37 + c])
                    act(ig, ps[:, T:2 * T], AF.Sigmoid, [ps, cols], [lt], bias=cols[:, 40 + c:41 + c])
                    act(av, rg, AF.Exp, [lt, c8], [lt], scale=c8[:, c:c + 1])
                    act(sq, rg, AF.Exp, [lt, c8], [lt], scale=c8[:, 4 + c:5 + c])
                    act(sq, sq, AF.Sqrt, [lt, epsc], [lt], scale=-1.0, bias=epsc[:, 2:3])
                    tt(ig, ig, xc, ALU.mult, [lt], [lt])
                    tt(ig, ig, sq, ALU.mult, [lt], [lt])
                    hh = lt[:, 1, :]
                    scan(hh, av, ig, hst[:, c:c + 1], [lt, hst], [lt])
                    cp(hst[:, c:c + 1], hh[:, T - 1:T], [lt], [hst])
                    tt(ybT[:, c, :], hh, grT[:, c, :], ALU.mult, [lt, grT], [ybT])
                if first_tile:
                    dump("ybT", ybT[:, 0, :], [ybT])
                if l == 0 and j == 1:
                    dump("ybT1", ybT[:, 0, :], [ybT])
                    dump("ycT1", ycT[:, 0, :], [ycT])

                grps = kv_groups(j)
                nkeys = (j + 1) * T
                for s in range(NSUB):
                    N = t0 + 128 * (s + 1)
                    for h in range(8):
                        act(diag[:, h, :], identf, AF.Copy, [cf, wis], [diag], scale=wis[:, s, h:h + 1])
                    ngrp = (N + KG - 1) // KG
                    for kg in range(ngrp):
                        wd = min(KG, N - kg * KG)
                        for h in range(8):
                            ps = PS[h % 4]
                            r0 = (h % 2) * 64
                            mm(ps[:, 0:wd], qiT[r0:r0 + 64, h // 2, s * 128:(s + 1) * 128],
                               kiT[r0:r0 + 64, kg * KG:kg * KG + wd], True, True, [qiT, kiT], [ps])
                            act(Rrelu[:, h, 0:wd], ps[:, 0:wd], AF.Relu, [ps], [TR_])
                        for h in range(8):
                            mm(PS[4][:, 0:wd], diag[:, h, :], Rrelu[:, h, 0:wd], h == 0, h == 7, [diag, TR_], [PS[4]])
                        if kg == ngrp - 1:
                            if wd > 128:
                                cp(score_ap[:, kg * KG:kg * KG + wd - 128], PS[4][:, 0:wd - 128], [PS[4]], [TS_])
                            tt(score_ap[:, N - 128:N], PS[4][:, wd - 128:wd], negmask, ALU.add, [PS[4], cf], [TS_])
                            tt(tmpf[:, 0:128], PS[4][:, wd - 128:wd], posfill, ALU.add, [PS[4], cf], [tmpf])
                        else:
                            cp(score_ap[:, kg * KG:kg * KG + wd], PS[4][:, 0:wd], [PS[4]], [TS_])
                    hi0 = bis[:, 0:1]
                    lo = bis[:, 1:2]
                    w0 = bis[:, 2:3]
                    reduce(hi0, score_ap[:, 0:N], ALU.max, [TS_, bis], [bis])
                    reduce(lo, tmpf[:, 0:128], ALU.min, [tmpf, bis], [bis])
                    if N > 128:
                        m1 = bis[:, 3:4]
                        reduce(m1, score_ap[:, 0:N - 128], ALU.min, [TS_, bis], [bis])
                        tt(lo, lo, m1, ALU.min, [bis], [bis])
                    tt(w0, hi0, lo, ALU.subtract, [bis], [bis])
                    mk = masks[s]
                    if N > TOPK:
                        Nd = ((N // 2 + 127) // 128) * 128
                        Na = N - Nd
                        junkA = big[:, 4096:6144].bitcast(BF16)
                        for it in range(NBIS):
                            k4 = it % 4
                            mid = bis2[:, k4:k4 + 1]
                            cnt = bis2[:, 4 + k4:5 + k4]
                            vv = bis2[:, 8 + k4:9 + k4]
                            sA = bis2[:, 12 + k4:13 + k4]
                            stt(mid, w0, 0.5 ** (it + 1), lo, ALU.mult, ALU.add, [bis], [Tmid])
                            act(junkA[:, 0:Na], score_ap[:, Nd:N], AF.Sign, [TS_, Tmid], [TR_, Tsa], scale=-1.0, bias=mid,
                                accum_out=sA)
                            count_ge(mk[:, 0:Nd], score_ap[:, 0:Nd], mid, cnt, [TS_, Tmid], [mk, Tcnt])
                            stt(vv, cnt, 2.0, sA, ALU.mult, ALU.subtract, [Tcnt, Tsa], [Tcnt])
                            ge = smalli[:, k4:k4 + 1]
                            ts(ge, vv, float(2 * TOPK - Na), None, ALU.is_ge, None, [Tcnt], [smalli])
                            cpred(lo, ge, mid, [Tmid, smalli], [bis])
                    ts(mk[:, 0:N], score_ap[:, 0:N], lo, None, ALU.is_ge, None, [TS_, bis], [mk])
                    if N < nkeys:
                        memset(mk[:, N:nkeys], 0.0, [mk], eng="gpsimd")
                    if first_tile and s == 0:
                        dump("score", score_ap[:, 0:128], [TS_])
                    if l == 0 and j == 1 and s == 0:
                        dump("score1", score_ap[:, 0:384], [TS_])
                        dump("mask1", mk[:, 0:384], [mk])
                        dump("thr1", lo, [bis])

                first = True
                ucnt = [0]
                for gi, (g, wd) in enumerate(grps):
                    panel = stream.next("%d_kv%d_%d" % (l, j, g))
                    nb = wd // 128
                    KTv = panel[:, 0:4 * wd].rearrange("p (c w) -> p c w", c=4)
                    Vv = panel[:, 2048:2048 + nb * 520].rearrange("p (b f) -> p b f", b=nb)
                    psb = PS[6][:].bitcast(BF16)
                    for s in range(NSUB):
                        for b in range(nb):
                            tr(psb[:, (s * 4 + b) * 128:(s * 4 + b + 1) * 128],
                               masks[s][:, g * KG + b * 128:g * KG + (b + 1) * 128], identb, [masks[s], cb], [PS[6]])
                    for s in range(NSUB):
                        act(maskTk[:, 0:nb, s * 128:(s + 1) * 128],
                            psb[:, s * 512:s * 512 + nb * 128].rearrange("p (b t) -> p b t", b=nb),
                            AF.Copy, [PS[6]], [maskTk])
                    units = [(h, bp) for h in range(8) for bp in range(0, nb, 2)]

                    def stageA(i):
                        h, bp = units[i]
                        r0 = (h % 2) * 64
                        nbb = min(2, nb - bp)
                        ps = PS[(ucnt[0] + i) % 4]
                        for b in range(bp, bp + nbb):
                            mm(ps[:, (b - bp) * T:(b - bp + 1) * T], KTv[r0:r0 + 64, h // 2, b * 128:(b + 1) * 128],
                               qT[r0:r0 + 64, h // 2, :], True, True, [panel, qT], [ps])

                    def stageB(i):
                        h, bp = units[i]
                        nbb = min(2, nb - bp)
                        ps = PS[(ucnt[0] + i) % 4]
                        ptile = PT[(ucnt[0] + i) % len(PT)]
                        act(ptile[:, 0:nbb, :], ps[:, 0:nbb * T].rearrange("p (b t) -> p b t", b=nbb), AF.Exp,
                            [ps], [ptile], scale=HD ** -0.5)
                        tt(ptile[:, 0:nbb, :], ptile[:, 0:nbb, :], maskTk[:, bp:bp + nbb, :], ALU.mult, [ptile, maskTk], [ptile],
                           eng=("vector" if (ucnt[0] + i) % 2 == 0 else "gpsimd"))

                    def stageD(i):
                        h, bp = units[i]
                        nbb = min(2, nb - bp)
                        pacc = PS[4 + (h % 2)]
                        ptile = PT[(ucnt[0] + i) % len(PT)]
                        for b in range(bp, bp + nbb):
                            mm(pacc[0:65, 0:T], Vv[:, b, h * 65:(h + 1) * 65], ptile[:, b - bp, :], b == 0, b == nb - 1,
                               [panel, ptile], [pacc])
                        if bp + 2 >= nb:
                            if first:
                                cp(acc[:, h, :], pacc[0:65, 0:T], [pacc], [acc])
                            else:
                                tt(acc[:, h, :], acc[:, h, :], pacc[0:65, 0:T], ALU.add, [pacc, acc], [acc])

                    stageA(0)
                    if len(units) > 1:
                        stageA(1)
                    for i in range(len(units)):
                        if i + 2 < len(units):
                            stageA(i + 2)
                        stageB(i)
                        stageD(i)
                    ucnt[0] += len(units)
                    first = False
                accf = acc[:].rearrange("p h t -> p (h t)")
                act(accf[64:65, :], accf[64:65, :], AF.Ln, [acc], [acc])
                act(accf[64:65, :], accf[64:65, :], AF.Exp, [acc], [acc], scale=-1.0)
                for h in range(8):
                    cp(rhl[64:65, 0, :], acc[64:65, h, :], [acc], [rhl])
                    tt(rhl[64:65, 1, :], acc[64:65, h, :], rhl[64:65, 0, :], ALU.subtract, [acc, rhl], [rhl])
                    ps = nps()
                    mm(ps[0:64, 0:T], onesb[64:65, 0:64], rhl[64:65, 0, :], True, False, [onesb, rhl], [ps])
                    mm(ps[0:64, 0:T], onesb[64:65, 0:64], rhl[64:65, 1, :], False, True, [onesb, rhl], [ps])
                    if h % 2 == 0:
                        tt(yaT[0:64, h // 2, :], acc[0:64, h, :], ps[0:64, 0:T], ALU.mult, [acc, ps], [yaT])
                    else:
                        tt(yaTt[:, :], acc[0:64, h, :], ps[0:64, 0:T], ALU.mult, [acc, ps], [yaTt])
                        ps2 = nps()
                        mm(ps2[64:128, 0:T], identb[0:64, 0:64], yaTt[:, :], True, True, [cb, yaTt], [ps2])
                        cp(yaT[64:128, h // 2, :], ps2[64:128, 0:T], [ps2], [yaT])
                if first_tile:
                    dump("yaT", yaT[:, 0, :], [yaT])
                if l == 0 and j == 1:
                    dump("yaT1", yaT[:, 0, :], [yaT])
                    dump("acc1", acc[:, 0, :], [acc])

                for ni, n in enumerate((2, 1, 0)):
                    for hf in range(2):
                        gpan = stream.next("%d_gate%d_%d" % (l, n, hf))
                        pv = wview(gpan, "gate0_0")
                        for c in range(4):
                            ps = nps()
                            fm_chunk(gpan, pv, c, ps)
                            act(gsig[:, hf * 4 + c, :], ps[:, 0:T], AF.Sigmoid, [ps], [gsig])
                    ysrc = {2: ycT, 1: ybT, 0: yaT}[n]
                    bp_ = [None, None]
                    for hf in range(2):
                        bp_[hf] = stream.next("%d_br%d_%d" % (l, n, hf))
                        pv = wview(bp_[hf], "br0_0")
                        for c in range(4):
                            dchunk = hf * 4 + c
                            ps = nps()
                            for kc in range(4):
                                mm(ps[:, 0:T], pv[:, kc, c * 128:(c + 1) * 128], ysrc[:, kc, :], kc == 0, kc == 3,
                                   [bp_[hf], ysrc], [ps])
                            if ni == 0:
                                tt(merged[:, dchunk, :], ps[:, 0:T], gsig[:, dchunk, :], ALU.mult, [ps, gsig], [TS_])
                            else:
                                tt(gt[:], ps[:, 0:T], gsig[:, dchunk, :], ALU.mult, [ps, gsig], [gt])
                                dsto = mergedT if ni == 2 else merged
                                tt(dsto[:, dchunk, :], merged[:, dchunk, :], gt[:], ALU.add, [TS_, gt], [TS_], eng="gpsimd")
                op_ = [stream.next("%d_out_%d" % (l, hf), look=NSLOT - 1 - hf) for hf in range(2)]
                for s in range(NSUB):
                    banks = [PS[4], PS[5]]
                    for hf in range(2):
                        pv = wview(op_[hf], "out_0")
                        for kc in range(8):
                            mm(banks[hf][:, 0:512], mergedT[:, kc, s * 128:(s + 1) * 128], pv[:, kc, :], kc == 0, kc == 7,
                               [op_[hf], TS_], [banks[hf]])
                    post_norm_residual(banks, 0, s)
                if first_tile:
                    dump("x1", xt[:, 0, :], [xt])
                norm_transpose(8)
                dbanks = [[PS[4], PS[5]], [PS[6], PS[7]]]
                TF = [Tile("fT0"), Tile("fT1")]
                Trl = [Tile("rl0"), Tile("rl1")]

                def merge_tile(dst, src):
                    for a, b in ((dst.w, src.w), (dst.r, src.r)):
                        for k_, v_ in b.items():
                            if a.get(k_, 0) < v_:
                                a[k_] = v_

                for i_ in range(2):
                    merge_tile(TF[i_], TS_)
                    TF[i_].w.update({k_: max(v_, TF[i_].w.get(k_, 0)) for k_, v_ in TS_.r.items()})
                    merge_tile(Trl[i_], TR_)
                    Trl[i_].w.update({k_: max(v_, Trl[i_].w.get(k_, 0)) for k_, v_ in TR_.r.items()})

                def ffn_up(g):
                    up = stream.next("%d_up_%d" % (l, g))
                    pv = wview(up, "up_0")
                    fTg = fT[g % 2]
                    for c in range(4):
                        ps = nps()
                        fm_chunk(up, pv, c, ps)
                        rlb = rl[c % 2]
                        act(rlb, ps[:, 0:T], AF.Relu, [ps], [Trl[c % 2]])
                        tt(fTg[:, c, :], rlb, rlb, ALU.mult, [Trl[c % 2]], [TF[g % 2]], eng="gpsimd")

                def ffn_dn(g):
                    dn = stream.next("%d_dn_%d" % (l, g))
                    dv = wview(dn, "dn_0")
                    fTg = fT[g % 2]
                    for s in range(NSUB):
                        for hf in range(2):
                            for c in range(4):
                                mm(dbanks[s][hf][:, 0:512], fTg[:, c, s * 128:(s + 1) * 128], dv[:, c, hf * 512:(hf + 1) * 512],
                                   g == 0 and c == 0, g == 7 and c == 3, [dn, TF[g % 2]], [dbanks[s][hf]])

                ffn_up(0)
                for g in range(8):
                    if g + 1 < 8:
                        ffn_up(g + 1)
                    ffn_dn(g)
                for i_ in range(2):
                    merge_tile(TS_, TF[i_])
                    merge_tile(TR_, Trl[i_])
                for s in range(NSUB):
                    post_norm_residual(dbanks[s], D, s)
                if first_tile:
                    dump("x2", xt[:, 0, :], [xt])
                norm_transpose(None)
                for s in range(NSUB):
                    cp(ptb[:, s, :], pt[:, s, :], [pt], [ptb])
                    psb = PS[3][:].bitcast(BF16)
                    for c in range(2):
                        tr(psb[:, c * 128:(c + 1) * 128], ptb[:, s, c * 128:(c + 1) * 128], identb, [ptb, cb], [PS[3]])
                    cp(pT[:, :, s * 128:(s + 1) * 128], psb[:, 0:256].rearrange("p (c t) -> p c t", c=2), [PS[3]], [pT])
                plp = stream.next("%d_ple" % l)
                plv = wview(plp, "ple")
                pg = [stream.next("%d_pg_%d" % (l, hf), look=NSLOT - 2 - hf) for hf in range(2)]
                for s in range(NSUB):
                    ssp = []
                    for hf in range(2):
                        pa = PS[0 + hf]
                        pgb = PS[2 + hf]
                        for kc in range(2):
                            mm(pa[:, 0:512], pT[:, kc, s * 128:(s + 1) * 128], plv[:, kc, hf * 512:(hf + 1) * 512], kc == 0, kc == 1,
                               [plp, pT], [pa])
                        gv = wview(pg[hf], "pg_0")
                        for kc in range(8):
                            mm(pgb[:, 0:512], hT[:, kc, s * 128:(s + 1) * 128], gv[:, kc, :], kc == 0, kc == 7, [pg[hf], hT], [pgb])
                        act(sg, pgb[:, 0:512], AF.Sigmoid, [pgb], [TR_])
                        tt(ple_t[:, hf * 512:(hf + 1) * 512], pa[:, 0:512], sg, ALU.mult, [pa, TR_], [TR_])
                        a = sm()
                        act(tmpf[:, 0:512], ple_t[:, hf * 512:(hf + 1) * 512], AF.Square, [TR_, small], [tmpf, small], accum_out=a)
                        ssp.append(a)
                    s3 = sm()
                    tt(s3, ssp[0], ssp[1], ALU.add, [small], [small])
                    rs = rstd_from_ss(s3, D)
                    for hf in range(2):
                        stt(tmpf[:, 0:512], ple_t[:, hf * 512:(hf + 1) * 512], rs, gb[:, 2 * D + hf * 512:2 * D + (hf + 1) * 512],
                            ALU.mult, ALU.mult, [TR_, small, gb], [tmpf])
                        tt(xt[:, s, hf * 512:(hf + 1) * 512], xt[:, s, hf * 512:(hf + 1) * 512], tmpf[:, 0:512], ALU.add,
                           [xt, tmpf], [xt], eng="gpsimd")
                wt = [XT[j]] if XT is not None else [out_tile]
                dma(dst_x[t0:t0 + T, :].rearrange("(s p) d -> p s d", p=128), xt[:], [xt], wt, ssem, eng="gpsimd")
            XTprev = XT

        fw.finish("sync", [out_tile] + dbg_tiles)
        fw.emit()
    return nc, fw


def _consts():
    bf = ml_dtypes.bfloat16
    ident = np.eye(128, dtype=np.float32)
    Rm = np.zeros((128, 128), np.float32)
    for base in (0, 64):
        for d in range(8):
            Rm[base + d + 8, base + d] = -1.0
            Rm[base + d, base + d + 8] = 1.0
    esel = np.zeros((128, 4, 128), np.float32)
    for g in range(8):
        esel[g, g // 2, (g % 2) * 64:(g % 2 + 1) * 64] = 1.0
    cb = np.concatenate([ident, Rm, esel.reshape(128, 512)], axis=1).astype(bf)
    tt_, ss_ = np.meshgrid(np.arange(128), np.arange(128), indexing="ij")
    negmask = np.where(ss_ > tt_, np.float32(NEG), np.float32(0.0))
    posfill = np.where(ss_ > tt_, np.float32(-2 * NEG), np.float32(0.0))
    tril01 = (tt_ <= ss_).astype(np.float32)
    half = 8
    inv_freq = (np.float32(500000.0) ** (-np.arange(half, dtype=np.float32) * np.float32(2.0) / np.float32(16))).astype(np.float32)
    invf = np.zeros((128, 1), np.float32)
    for f in range(128):
        d = f % 64
        if d < 16:
            invf[f, 0] = inv_freq[d % 8]
    cf = np.concatenate([ident, negmask, posfill, tril01, invf], axis=1).astype(np.float32)
    return cb, cf


def _layout_params(inp, depth):
    f = np.float32
    cols = np.zeros((depth, 128, 48), f)
    gb = np.zeros((depth, 128, 3 * D + 512), f)
    wbd = np.zeros((depth, 128, 8, 128), f)
    wsp = np.zeros((depth, 128, 8, 128), f)
    for l in range(depth):
        cols[l, :, 0:8] = np.asarray(inp["g_pre_mix"][l], f).reshape(8, 128).T
        cols[l, :, 8:16] = np.asarray(inp["g_pre_ffn"][l], f).reshape(8, 128).T
        cw = np.asarray(inp["conv_w"][l], f)
        for jj in range(4):
            cols[l, :, 16 + jj * 4:20 + jj * 4] = cw[jj].reshape(4, 128).T
        cols[l, :, 32:36] = np.asarray(inp["conv_b"][l], f).reshape(4, 128).T
        cols[l, :, 36:40] = np.asarray(inp["b_rg_a"][l], f).reshape(4, 128).T
        cols[l, :, 40:44] = np.asarray(inp["b_rg_x"][l], f).reshape(4, 128).T
        cols[l, :, 44:48] = np.asarray(inp["lru_lambda"][l], f).reshape(4, 128).T
        row = np.concatenate([np.asarray(inp["g_post_mix"][l], f), np.asarray(inp["g_post_ffn"][l], f),
                              np.asarray(inp["g_post_ple"][l], f), np.asarray(inp["g_gmlp_v"][l], f)])
        gb[l] = np.broadcast_to(row[None, :], (128, row.size))
        for gi, key in enumerate(("w_rg_a", "w_rg_x")):
            w = np.asarray(inp[key][l], f)
            for c in range(4):
                for hh in range(2):
                    wbd[l, hh * 64:(hh + 1) * 64, gi * 4 + c, hh * 64:(hh + 1) * 64] = w[c * 2 + hh]
        ws = np.asarray(inp["w_spatial"][l], f)
        wsp[l] = np.transpose(ws, (2, 0, 1))
    bsp = np.ascontiguousarray(np.asarray(inp["b_spatial"], f))
    return cols, gb, wbd, wsp, bsp


_CACHE = {}


def kernel(**inputs):
    depth = DEPTH
    x = np.asarray(inputs["x"], np.float32)
    B, L, _ = x.shape
    p = np.asarray(inputs["p"], np.float32)
    pos = np.asarray(inputs["positions"], np.int32)
    cb, cf = _consts()
    cols, gb, wbd, wsp, bsp = _layout_params(inputs, depth)
    shared = {
        "w_in": np.ascontiguousarray(np.asarray(inputs["w_in"], np.float32)),
        "w_branch": np.ascontiguousarray(np.asarray(inputs["w_branch"], np.float32)),
        "w_out": np.ascontiguousarray(np.asarray(inputs["w_out"], np.float32)),
        "w_ffn_up": np.ascontiguousarray(np.asarray(inputs["w_ffn_up"], np.float32)),
        "w_ffn_down": np.ascontiguousarray(np.asarray(inputs["w_ffn_down"], np.float32)),
        "w_ple": np.ascontiguousarray(np.asarray(inputs["w_ple"], np.float32)),
        "w_ple_gate": np.ascontiguousarray(np.asarray(inputs["w_ple_gate"], np.float32)),
        "cols": cols, "gb": gb, "wbd": wbd, "wsp": wsp, "bsp": bsp, "cb": cb, "cf": cf,
    }
    if L not in _CACHE:
        _CACHE[L] = build_program(L, depth)[0]
    nc = _CACHE[L]
    in_maps = []
    for b in range(B):
        m = dict(shared)
        m["x"] = np.ascontiguousarray(x[b])
        m["p"] = np.ascontiguousarray(p[:, b])
        m["pos"] = np.ascontiguousarray(np.broadcast_to(pos[b][None, :], (128, L)))
        in_maps.append(m)
    res = run_bass_kernel_spmd(nc, in_maps, core_ids=list(range(B)))
    return np.stack([np.asarray(r["y"], np.float32) for r in res.results], axis=0)
```

```python
import math
from contextlib import ExitStack
import numpy as np
import ml_dtypes
import concourse.bass as bass
import concourse.mybir as mybir
from concourse.bass_utils import run_bass_kernel_spmd

F32 = mybir.dt.float32
BF16 = mybir.dt.bfloat16
I32 = mybir.dt.int32
AF = mybir.ActivationFunctionType
ALU = mybir.AluOpType
AX = mybir.AxisListType

D = 1024
NH = 8
HD = 64
TOPK = 256
FFN = 4096
PLE = 256
EPS = 1e-6
DEPTH = 2
T = 256
NSUB = T // 128
KG = 512
NSLOT = 3
SLOTB = 4224
NBIS = 13
BIGM = 30000.0
NEG = -1.0e30
IN_OFF = dict(q=0, k=512, v=1024, qi=1536, kiwi=2048, xr=2120, gr=2632, zu=3144, zv=3656, gate=4168)


class Sem:
    def __init__(self, h, name):
        self.h = h
        self.n = 0
        self.name = name


class Tile:
    __slots__ = ("w", "r", "name")

    def __init__(self, name=""):
        self.w = {}
        self.r = {}
        self.name = name


class Buf:
    def __init__(self, t, name=""):
        self.t = t
        self.T = Tile(name)

    def __getitem__(self, k):
        return self.t[k]


class Engine:
    def __init__(self, name, sem):
        self.name = name
        self.sem = sem
        self.ops = []
        self.seen = {}


class FW:
    def __init__(self, nc, stack):
        self.nc = nc
        self.stack = stack
        self.engs = {}
        for n in ("tensor", "vector", "scalar", "gpsimd", "sync"):
            s = Sem(stack.enter_context(nc.semaphore("sem_" + n)), n)
            self.engs[n] = Engine(n, s)
        self.nops = 0

    def dsem(self, name):
        return Sem(self.stack.enter_context(self.nc.semaphore("dsem_" + name)), name)

    def op(self, eng, fn, reads=(), writes=(), dsem=None):
        E = self.engs[eng]
        need = {}
        for b in reads:
            t = b.T if isinstance(b, Buf) else b
            for s, v in t.w.items():
                if need.get(s, 0) < v:
                    need[s] = v
        for b in writes:
            t = b.T if isinstance(b, Buf) else b
            for s, v in t.w.items():
                if need.get(s, 0) < v:
                    need[s] = v
            for s, v in t.r.items():
                if need.get(s, 0) < v:
                    need[s] = v
        raw_self = 0
        for b in reads:
            t = b.T if isinstance(b, Buf) else b
            raw_self = max(raw_self, t.w.get(E.sem, 0))
        waits = []
        for s, v in need.items():
            if s is E.sem:
                if eng != "tensor" and raw_self > E.seen.get(s, 0):
                    E.seen[s] = raw_self
                    waits.append((s, raw_self))
                continue
            if E.seen.get(s, 0) >= v:
                continue
            E.seen[s] = v
            waits.append((s, v))
        if dsem is not None:
            dsem.n += 16
            sig = (dsem, dsem.n, 16)
        else:
            E.sem.n += 1
            sig = (E.sem, E.sem.n, 1)
        E.ops.append((waits, fn, sig))
        self.nops += 1
        s, v = sig[0], sig[1]
        for b in reads:
            t = b.T if isinstance(b, Buf) else b
            if t.r.get(s, 0) < v:
                t.r[s] = v
        for b in writes:
            t = b.T if isinstance(b, Buf) else b
            if t.w.get(s, 0) < v:
                t.w[s] = v

    def finish(self, eng, tiles):
        E = self.engs[eng]
        need = {}
        for b in tiles:
            t = b.T if isinstance(b, Buf) else b
            for d in (t.w, t.r):
                for s, v in d.items():
                    if need.get(s, 0) < v:
                        need[s] = v
        E.ops.append(([(s, v) for s, v in need.items() if s is not E.sem], None, None))

    def emit(self):
        with self.nc.Block() as block:
            for n, E in self.engs.items():
                def body(e, E=E):
                    for waits, fn, sig in E.ops:
                        for s, v in waits:
                            e.wait_ge(s.h, v)
                        if fn is None:
                            continue
                        fn(e).then_inc(sig[0].h, sig[2])
                getattr(block, n)(body)


def panel_defs():
    P = []
    for nm in ("q", "k", "qi"):
        P.append((nm, "w_in", 0, 1024, IN_OFF[nm], 512))
    P.append(("kiwi", "w_in", 0, 1024, IN_OFF["kiwi"], 72))
    for nm in ("xr", "gr", "zu", "v", "zv"):
        P.append((nm, "w_in", 0, 1024, IN_OFF[nm], 512))
    for n in range(3):
        for hf in range(2):
            P.append(("gate%d_%d" % (n, hf), "w_in", 0, 1024, IN_OFF["gate"] + n * 1024 + hf * 512, 512))
    for n in range(3):
        for hf in range(2):
            P.append(("br%d_%d" % (n, hf), "w_branch%d" % n, 0, 512, hf * 512, 512))
    for hf in range(2):
        P.append(("out_%d" % hf, "w_out", 0, 1024, hf * 512, 512))
    for g in range(8):
        P.append(("up_%d" % g, "w_ffn_up", 0, 1024, g * 512, 512))
        P.append(("dn_%d" % g, "w_ffn_down", g * 512, 512, 0, 1024))
    P.append(("ple", "w_ple", 0, 256, 0, 1024))
    for hf in range(2):
        P.append(("pg_%d" % hf, "w_ple_gate", 0, 1024, hf * 512, 512))
    return P


def build_program(L, depth=DEPTH, dbg=None):
    NT = L // T
    nc = bass.Bass("TRN2", target_bir_lowering=False)
    dram = lambda name, shape, dt, kind="ExternalInput": nc.dram_tensor(name, shape, dt, kind=kind).ap()
    x_in = dram("x", [L, D], F32)
    p_in = dram("p", [depth, L, PLE], F32)
    pos_in = dram("pos", [128, L], I32)
    wsrc = {
        "w_in": dram("w_in", [depth, D, 7240], F32),
        "w_branch": dram("w_branch", [depth, 3, 512, D], F32),
        "w_out": dram("w_out", [depth, D, D], F32),
        "w_ffn_up": dram("w_ffn_up", [depth, D, FFN], F32),
        "w_ffn_down": dram("w_ffn_down", [depth, FFN, D], F32),
        "w_ple": dram("w_ple", [depth, PLE, D], F32),
        "w_ple_gate": dram("w_ple_gate", [depth, D, D], F32),
    }
    cols_in = dram("cols", [depth, 128, 48], F32)
    gb_in = dram("gb", [depth, 128, 3 * D + 512], F32)
    wbd_in = dram("wbd", [depth, 128, 8, 128], F32)
    wsp_in = dram("wsp", [depth, 128, 8, 128], F32)
    bsp_in = dram("bsp", [depth, 8, 128], F32)
    cb_in = dram("cb", [128, 256 + 512], BF16)
    cf_in = dram("cf", [128, 4 * 128 + 1], F32)
    y_out = dram("y", [L, D], F32, kind="ExternalOutput")
    xbuf = dram("xbuf", [L, D], F32, kind="Internal")
    KTc = [dram("ktc%d" % l, [4, 128, L], BF16, kind="Internal") for l in range(depth)]
    Vc = [dram("vc%d" % l, [L, 520], BF16, kind="Internal") for l in range(depth)]
    pdefs = panel_defs()
    Wp = [{nm: dram("wp%d_%s" % (l, nm), [nr, ncol], BF16, kind="Internal") for (nm, _, _, nr, _, ncol) in pdefs}
          for l in range(depth)]
    dbg_out = {}
    if dbg:
        for k, shp in dbg.items():
            dbg_out[k] = dram("dbg_" + k, list(shp), F32, kind="ExternalOutput")

    with ExitStack() as st:
        fw = FW(nc, st)
        op = fw.op

        def sb(name, shape, dt):
            return Buf(st.enter_context(nc.sbuf_tensor("s_" + name, shape, dt)), name)

        PS = [Buf(st.enter_context(nc.psum_tensor("ps%d" % i, [128, 512], F32)), "ps%d" % i) for i in range(8)]

        def mm(out, lhsT, rhs, start, stop, R, W):
            op("tensor", lambda e: e.matmul(out, lhsT=lhsT, rhs=rhs, start=start, stop=stop), R, W)

        def tr(out, in_, ident, R, W):
            op("tensor", lambda e: e.transpose(out=out, in_=in_, identity=ident), R, W)

        def act(out, in_, func, R, W, **kw):
            op("scalar", lambda e: e.activation(out=out, in_=in_, func=func, **kw), R, W)

        def ts(out, in0, s1, s2, op0, op1, R, W, eng="vector"):
            if op1 is None:
                op(eng, lambda e: e.tensor_scalar(out=out, in0=in0, scalar1=s1, scalar2=None, op0=op0), R, W)
            else:
                op(eng, lambda e: e.tensor_scalar(out=out, in0=in0, scalar1=s1, scalar2=s2, op0=op0, op1=op1), R, W)

        def tt(out, in0, in1, o, R, W, eng="vector"):
            op(eng, lambda e: e.tensor_tensor(out=out, in0=in0, in1=in1, op=o), R, W)

        def stt(out, in0, s, in1, op0, op1, R, W):
            op("vector", lambda e: e.scalar_tensor_tensor(out=out, in0=in0, scalar=s, in1=in1, op0=op0, op1=op1), R, W)

        def cp(out, in_, R, W, eng="vector"):
            op(eng, lambda e: e.tensor_copy(out=out, in_=in_), R, W)

        def dma(out, in_, R, W, ds, eng="sync"):
            op(eng, lambda e: e.dma_start(out=out, in_=in_), R, W, dsem=ds)

        def memset(ap, val, W, eng="vector"):
            op(eng, lambda e: e.memset(ap, val), [], W)

        def reduce(out, in_, o, R, W):
            op("vector", lambda e: e.tensor_reduce(out=out, in_=in_, axis=AX.X, op=o), R, W)

        def recip(out, in_, R, W):
            op("vector", lambda e: e.reciprocal(out=out, in_=in_), R, W)

        def scan(out, d0, d1, init, R, W):
            op("vector", lambda e: e.tensor_tensor_scan(out=out, data0=d0, data1=d1, initial=init,
                                                        op0=ALU.mult, op1=ALU.add), R, W)

        def count_ge(out, in0, thr, cnt, R, W):
            op("vector", lambda e: e.tensor_scalar(out=out, in0=in0, scalar1=thr, scalar2=None, op0=ALU.is_ge,
                                                   op1=ALU.add, accum_out=cnt), R, W)

        def cpred(out, mask, data, R, W):
            op("vector", lambda e: e.copy_predicated(out=out, mask=mask, data=data), R, W)

        dbg_sem = fw.dsem("dbg")
        dbg_tiles = []

        def dump(name, ap, R):
            if name in dbg_out:
                t = Tile("dbg")
                dma(dbg_out[name], ap, R, [t], dbg_sem, eng="gpsimd")
                dbg_tiles.append(t)
                del dbg_out[name]

        cb = sb("cb", [128, 768], BF16)
        cf = sb("cf", [128, 513], F32)
        dma(cb[:], cb_in, [], [cb], fw.dsem("c0"))
        dma(cf[:], cf_in, [], [cf], fw.dsem("c1"))
        identb = cb[:, 0:128]
        Rm = cb[:, 128:256]
        esel = cb[0:8, 256:768].rearrange("p (c f) -> p c f", c=4)
        identf = cf[:, 0:128]
        negmask = cf[:, 128:256]
        posfill = cf[:, 256:384]
        tril01 = cf[:, 384:512]
        invf = cf[:, 512:513]

        WT = [dict() for _ in range(depth)]
        for l in range(depth):
            wcs = fw.dsem("wcast%d" % l)
            for (nm, src, r0, nr, c0, ncol) in pdefs:
                if src.startswith("w_branch"):
                    s_ap = wsrc["w_branch"][l, int(src[-1]), r0:r0 + nr, c0:c0 + ncol]
                else:
                    s_ap = wsrc[src][l, r0:r0 + nr, c0:c0 + ncol]
                t = Tile("wp")
                WT[l][nm] = t
                step = 512
                for rr in range(0, nr, step):
                    n2 = min(step, nr - rr)
                    dma(Wp[l][nm][rr:rr + n2, :], s_ap[rr:rr + n2, :], [], [t], wcs, eng="gpsimd")
            for t in WT[l].values():
                t.w = {wcs: wcs.n}

        ring = [sb("ring%d" % i, [128, SLOTB], BF16) for i in range(NSLOT)]
        ring_sem = [fw.dsem("ring%d" % i) for i in range(NSLOT)]
        kiT = sb("kiT", [128, L], BF16)
        xt = sb("xt", [128, NSUB, D], F32)
        pt = sb("pt", [128, NSUB, PLE], F32)
        ptb = sb("ptb", [128, NSUB, PLE], BF16)
        posi = sb("posi", [128, T], I32)
        hT = sb("hT", [128, 8, T], BF16)
        pT = sb("pT", [128, 2, T], BF16)
        qT = sb("qT", [128, 4, T], BF16)
        qiT = sb("qiT", [128, 4, T], BF16)
        KTs = sb("KTs", [128, 4, T], BF16)
        Vs = sb("Vs", [128, NSUB, 520], BF16)
        cosT = sb("cosT", [128, T], F32)
        sinT = sb("sinT", [128, T], F32)
        rtmp = sb("rtmp", [128, 3, T], F32)
        xb16 = sb("xb16", [128, T], BF16)
        xrbuf = sb("xrbuf", [128, 4, 3 + T], F32)
        grT = sb("grT", [128, 4, T], BF16)
        zuT = sb("zuT", [128, 4, T], BF16)
        gz = sb("gz", [128, 512], F32)
        vn = sb("vn", [128, NSUB, 512], BF16)
        lt = sb("lt", [128, 5, T], F32)
        xcb = sb("xcb", [128, T], BF16)
        hst = sb("hst", [128, 4], F32)
        ybT = sb("ybT", [128, 4, T], BF16)
        ycT = sb("ycT", [128, 4, T], BF16)
        yaT = sb("yaT", [128, 4, T], BF16)
        yaTt = sb("yaTt", [64, T], BF16)
        wis = sb("wis", [128, NSUB, 8], F32)
        diag = sb("diag", [128, 8, 128], BF16)
        small = sb("small", [128, 32], F32)
        smalli = sb("smalli", [128, 4], I32)
        bis = sb("bis", [128, 16], F32)
        bis2 = sb("bis2", [128, 16], F32)
        Tmid = Tile("mid")
        Tcnt = Tile("cnt")
        Tsa = Tile("sa")
        big = sb("big", [128, 6144], F32)
        TS_ = big.T
        TR_ = Tile("bigR")
        score_ap = big[:, 0:L]
        Rrelu = big[:, 4096:6144].bitcast(BF16).rearrange("p (h w) -> p h w", h=8)
        masks = [sb("mask%d" % s, [128, L], BF16) for s in range(NSUB)]
        maskTk = sb("maskTk", [128, 4, T], BF16)
        PT = [sb("PT%d" % i, [128, 2, T], BF16) for i in range(4)]
        xs = sb("xs", [128, D], BF16)
        acc = sb("acc", [65, 8, T], F32)
        rhl2 = [sb("rhl%d" % i, [65, 2, T], BF16) for i in range(2)]
        onesb = sb("onesb", [65, 64], BF16)
        gt = sb("gt", [128, T], F32)
        tmpf = sb("tmpf", [128, 512], F32)
        gsig = sb("gsig", [128, 8, T], BF16)
        merged = big[:, 0:8 * T].rearrange("p (c t) -> p c t", c=8)
        o = 8 * T
        mergedT = big[:, o:o + 4 * T].bitcast(BF16).rearrange("p (c t) -> p c t", c=8)
        o += 4 * T
        fT = [big[:, o + i * 2 * T:o + (i + 1) * 2 * T].bitcast(BF16).rearrange("p (c t) -> p c t", c=4) for i in range(2)]
        o += 4 * T
        assert o <= 4096
        o = 4096
        rl = [big[:, o + i * (T // 2):o + (i + 1) * (T // 2)].bitcast(BF16) for i in range(2)]
        o += T
        sg = big[:, o:o + 512]
        o += 512
        ple_t = big[:, o:o + 1024]
        o += 1024
        assert o <= 6144
        wbdf = big[:, 4096:4096 + 1024].rearrange("p (c j) -> p c j", c=8)
        cols = sb("cols", [128, 48], F32)
        gb = sb("gb", [128, 3 * D + 512], F32)
        wbd = sb("wbd", [128, 8, 128], BF16)
        wspT = sb("wspT", [128, 8, 128], BF16)
        bsp = sb("bsp", [8, 128], F32)
        bsph = sb("bsph", [8, 2, 128], BF16)
        c8 = sb("c8", [128, 8], F32)
        epsc = sb("epsc", [128, 4], F32)
        psem = [fw.dsem("par%d" % i) for i in range(5)]
        xsem = fw.dsem("xload")
        ptsem = fw.dsem("pload")
        possem = fw.dsem("posload")
        ssem = fw.dsem("store")
        kvsem = fw.dsem("kvstore")
        out_tile = Tile("out")

        class Stream:
            def __init__(self):
                self.plan = []
                self.issued = 0
                self.pos = 0

            def add(self, name, loader, deps, hoist=True):
                self.plan.append((name, loader, deps, hoist))

            def next(self, name, look=NSLOT - 1):
                i = self.pos
                assert self.plan[i][0] == name, (self.plan[i][0], name)
                while self.issued < len(self.plan) and (
                        self.issued <= i or (self.issued <= i + look and self.plan[self.issued][3])):
                    k = self.issued
                    _, loader, deps, _ = self.plan[k]
                    loader(ring[k % NSLOT], ring_sem[k % NSLOT], deps)
                    self.issued += 1
                self.pos += 1
                return ring[i % NSLOT]

        stream = Stream()

        def wloader(l, nm, nr, ncol):
            kc = nr // 128

            def f(slot, sem, deps):
                dst = slot[:, 0:kc * ncol].rearrange("p (k w) -> p k w", k=kc)
                src = Wp[l][nm].rearrange("(k p) w -> p k w", p=128)
                dma(dst, src, deps, [slot], sem)
            return f

        KVT = [[Tile("kv") for _ in range((L + KG - 1) // KG)] for _ in range(depth)]

        def kvloader(l, g, wd):
            def f(slot, sem, deps):
                dstk = slot[:, 0:4 * wd].rearrange("p (c w) -> p c w", c=4)
                dma(dstk, KTc[l][:, :, g * KG:g * KG + wd].rearrange("c p w -> p c w"), deps, [slot], sem)
                nb = wd // 128
                dstv = slot[:, 2048:2048 + nb * 520].rearrange("p (b f) -> p b f", b=nb)
                dma(dstv, Vc[l][g * KG:g * KG + wd, :].rearrange("(b p) f -> p b f", p=128), deps, [slot], sem)
            return f

        pinfo = {nm: (nr, ncol) for (nm, _, _, nr, _, ncol) in pdefs}

        def plan_w(l, nm):
            nr, ncol = pinfo[nm]
            stream.add("%d_%s" % (l, nm), wloader(l, nm, nr, ncol), [WT[l][nm]])

        def kv_groups(j):
            nkeys = (j + 1) * T
            out = []
            g = 0
            while g * KG < nkeys:
                out.append((g, min(KG, nkeys - g * KG)))
                g += 1
            return out

        for l in range(depth):
            for j in range(NT):
                for nm in ("q", "k", "qi", "kiwi", "xr", "gr", "zu", "v", "zv"):
                    plan_w(l, nm)
                grps = kv_groups(j)
                for (g, wd) in grps:
                    last = (g == grps[-1][0])
                    stream.add("%d_kv%d_%d" % (l, j, g), kvloader(l, g, wd), [KVT[l][g]], hoist=not last)
                for n in (2, 1, 0):
                    plan_w(l, "gate%d_0" % n)
                    plan_w(l, "gate%d_1" % n)
                    plan_w(l, "br%d_0" % n)
                    plan_w(l, "br%d_1" % n)
                plan_w(l, "out_0")
                plan_w(l, "out_1")
                plan_w(l, "up_0")
                for g in range(8):
                    if g + 1 < 8:
                        plan_w(l, "up_%d" % (g + 1))
                    plan_w(l, "dn_%d" % g)
                plan_w(l, "ple")
                plan_w(l, "pg_0")
                plan_w(l, "pg_1")

        def wview(slot, nm):
            nr, ncol = pinfo[nm]
            kc = nr // 128
            return slot[:, 0:kc * ncol].rearrange("p (k w) -> p k w", k=kc)

        sm_i = [0]

        def sm():
            i = sm_i[0] % 32
            sm_i[0] += 1
            return small[:, i:i + 1]

        memset(epsc[:, 0:1], EPS, [epsc])
        memset(epsc[:, 1:2], math.pi / 2, [epsc])
        memset(epsc[:, 2:3], 1.0, [epsc])
        memset(epsc[:, 3:4], -BIGM, [epsc])
        memset(Vs[:, :, :], 1.0, [Vs])
        memset(onesb[:, :], 1.0, [onesb])

        def rstd_from_ss(ss_ap, n):
            a = sm()
            b = sm()
            act(a, ss_ap, AF.Sqrt, [small, epsc], [small], scale=1.0 / n, bias=epsc[:, 0:1])
            recip(b, a, [small], [small])
            return b

        pi = [0]

        def nps():
            pi[0] += 1
            return PS[pi[0] % 4]

        def norm_transpose(gcol0):
            for s in range(NSUB):
                if gcol0 is not None:
                    ss = sm()
                    act(tmpf[:, 0:512], xt[:, s, 0:512], AF.Square, [xt, small], [tmpf, small], accum_out=ss)
                    ss2 = sm()
                    act(tmpf[:, 0:512], xt[:, s, 512:1024], AF.Square, [xt, small], [tmpf, small], accum_out=ss2)
                    ss3 = sm()
                    tt(ss3, ss, ss2, ALU.add, [small], [small])
                    rs = rstd_from_ss(ss3, D)
                    ts(xs[:], xt[:, s, :], rs, None, ALU.mult, None, [xt, small], [xs])
                else:
                    cp(xs[:], xt[:, s, :], [xt], [xs])
                psb = PS[7][:].bitcast(BF16)
                for c in range(8):
                    tr(psb[:, c * 128:(c + 1) * 128], xs[:, c * 128:(c + 1) * 128], identb, [xs, cb], [PS[7]])
                src = psb[:, 0:1024].rearrange("p (c t) -> p c t", c=8)
                dst = hT[:, :, s * 128:(s + 1) * 128]
                if gcol0 is not None:
                    g_ap = cols[:, gcol0:gcol0 + 8].unsqueeze(2).to_broadcast([128, 8, 128])
                    tt(dst, src, g_ap, ALU.mult, [PS[7], cols], [hT])
                else:
                    cp(dst, src, [PS[7]], [hT])

        def post_norm_residual(banks, gcol, s):
            ssa = []
            for hf in range(2):
                a = sm()
                act(tmpf[:, 0:512], banks[hf][:, 0:512], AF.Square, [banks[hf], small], [tmpf, small], accum_out=a)
                ssa.append(a)
            s3 = sm()
            tt(s3, ssa[0], ssa[1], ALU.add, [small], [small])
            rs = rstd_from_ss(s3, D)
            for hf in range(2):
                stt(tmpf[:, 0:512], banks[hf][:, 0:512], rs, gb[:, gcol + hf * 512:gcol + (hf + 1) * 512],
                    ALU.mult, ALU.mult, [banks[hf], small, gb], [tmpf])
                tt(xt[:, s, hf * 512:(hf + 1) * 512], xt[:, s, hf * 512:(hf + 1) * 512], tmpf[:, 0:512], ALU.add,
                   [xt, tmpf], [xt])

        def merge_tile(dst, src, as_write=False):
            for a, b in ((dst.w, src.w), (dst.r, src.r)):
                for k_, v_ in b.items():
                    if a.get(k_, 0) < v_:
                        a[k_] = v_
            if as_write:
                for k_, v_ in src.r.items():
                    if dst.w.get(k_, 0) < v_:
                        dst.w[k_] = v_

        def fm_chunk(panel, pv, c, ps, M=128):
            for kc in range(8):
                mm(ps[0:M, 0:T], pv[:, kc, c * 128:c * 128 + M], hT[:, kc, :], kc == 0, kc == 7, [panel, hT], [ps])

        def rope_evac(ps, dst, W):
            act(xb16[:], ps[:, 0:T], AF.Copy, [ps], [xb16])
            mm(ps[:, T:2 * T], Rm, xb16[:], True, True, [cb, xb16], [ps])
            tt(rtmp[:, 0, :], ps[:, 0:T], cosT[:], ALU.mult, [ps, cosT], [rtmp])
            tt(rtmp[:, 1, :], ps[:, T:2 * T], sinT[:], ALU.mult, [ps, sinT], [rtmp])
            tt(dst, rtmp[:, 0, :], rtmp[:, 1, :], ALU.add, [rtmp], W)

        XTprev = None
        for l in range(depth):
            src_x = x_in if l == 0 else xbuf
            dst_x = y_out if l == depth - 1 else xbuf
            XT = [Tile("xd") for _ in range(NT)] if l < depth - 1 else None
            dma(cols[:], cols_in[l], [], [cols], psem[0])
            dma(gb[:], gb_in[l], [], [gb], psem[1])
            dma(bsp[:], bsp_in[l], [], [bsp], psem[2])
            cp(bsph[:, 0, :], bsp[:], [bsp], [bsph])
            tt(bsp[:], bsp[:], bsph[:, 0, :], ALU.subtract, [bsph], [bsp])
            cp(bsph[:, 1, :], bsp[:], [bsp], [bsph])
            dma(wbdf, wbd_in[l], [], [TR_], psem[3])
            cp(wbd[:], wbdf, [TR_], [wbd])
            dma(wbdf, wsp_in[l], [], [TR_], psem[4])
            tt(wspT[:], wbdf, tril01.unsqueeze(1).to_broadcast([128, 8, 128]), ALU.mult, [TR_, cf], [wspT])
            act(c8[:, 4:8], cols[:, 44:48], AF.Exp, [cols], [c8], scale=-1.0)
            ts(c8[:, 0:4], c8[:, 4:8], -0.25, 1.0 / 3.0, ALU.mult, ALU.add, [c8], [c8])
            tt(c8[:, 0:4], c8[:, 0:4], c8[:, 4:8], ALU.mult, [c8], [c8])
            ts(c8[:, 0:4], c8[:, 0:4], -1.0, 0.5, ALU.mult, ALU.add, [c8], [c8])
            tt(c8[:, 0:4], c8[:, 0:4], c8[:, 4:8], ALU.mult, [c8], [c8])
            ts(c8[:, 0:4], c8[:, 0:4], -1.0, 1.0, ALU.mult, ALU.add, [c8], [c8])
            tt(c8[:, 0:4], c8[:, 0:4], c8[:, 4:8], ALU.mult, [c8], [c8])
            ts(c8[:, 0:4], c8[:, 0:4], -8.0, None, ALU.mult, None, [c8], [c8])
            ts(c8[:, 4:8], c8[:, 0:4], 2.0, None, ALU.mult, None, [c8], [c8])
            memset(xrbuf[:, :, 0:3], 0.0, [xrbuf])
            memset(hst[:], 0.0, [hst])

            for j in range(NT):
                t0 = j * T
                first_tile = (l == 0 and j == 0)
                rd = [XTprev[j]] if (l > 0) else []
                dma(xt[:], src_x[t0:t0 + T, :].rearrange("(s p) d -> p s d", p=128), rd, [xt], xsem)
                dma(pt[:], p_in[l, t0:t0 + T, :].rearrange("(s p) d -> p s d", p=128), [], [pt], ptsem)
                dma(posi[:], pos_in[:, t0:t0 + T], [], [posi], possem)
                ang = rtmp[:, 0, :]
                u = rtmp[:, 1, :]
                rr = rtmp[:, 2, :]
                cp(u, posi[:], [posi], [rtmp])
                ts(ang, u, invf, None, ALU.mult, None, [rtmp, cf], [rtmp])
                ts(u, ang, 1.0 / (2 * math.pi), 12582912.0, ALU.mult, ALU.add, [rtmp], [rtmp])
                ts(u, u, -12582912.0, None, ALU.add, None, [rtmp], [rtmp])
                C1 = 6.28125
                C2 = float(np.float32(2 * math.pi - C1))
                C3 = float(2 * math.pi - C1 - C2)
                stt(rr, u, -C1, ang, ALU.mult, ALU.add, [rtmp], [rtmp])
                stt(rr, u, -C2, rr, ALU.mult, ALU.add, [rtmp], [rtmp])
                stt(rr, u, -C3, rr, ALU.mult, ALU.add, [rtmp], [rtmp])
                act(sinT[:], rr, AF.Sin, [rtmp], [sinT])
                stt(u, rr, -1.0, rr, ALU.mult, ALU.max, [rtmp], [rtmp])
                act(cosT[:], u, AF.Sin, [rtmp, epsc], [cosT], scale=-1.0, bias=epsc[:, 1:2])
                norm_transpose(0)
                if first_tile:
                    dump("hT", hT[:, 0, :], [hT])
                    dump("cosT", cosT[:], [cosT])
                    dump("sinT", sinT[:], [sinT])

                for nm, dstb in (("q", qT), ("k", KTs), ("qi", qiT)):
                    panel = stream.next("%d_%s" % (l, nm))
                    pv = wview(panel, nm)
                    pss = [nps() for _ in range(4)]
                    fm_chunk(panel, pv, 0, pss[0])
                    for c in range(4):
                        if c + 1 < 4:
                            fm_chunk(panel, pv, c + 1, pss[c + 1])
                        rope_evac(pss[c], dstb[:, c, :], [dstb])
                panel = stream.next("%d_kiwi" % l)
                pv = wview(panel, "kiwi")
                ps = nps()
                for half in range(2):
                    for kc in range(8):
                        mm(ps[half * 64:(half + 1) * 64, 0:T], pv[:, kc, 0:64], hT[:, kc, :], kc == 0, kc == 7,
                           [panel, hT], [ps])
                rope_evac(ps, kiT[:, t0:t0 + T], [kiT])
                for s in range(NSUB):
                    ps = nps()
                    for kc in range(8):
                        mm(ps[:, 0:8], hT[:, kc, s * 128:(s + 1) * 128], pv[:, kc, 64:72], kc == 0, kc == 7,
                           [panel, hT], [ps])
                    cp(wis[:, s, :], ps[:, 0:8], [ps], [wis])
                dma(KTc[l][:, :, t0:t0 + T].rearrange("c p w -> p c w"), KTs[:], [KTs], [KVT[l][t0 // KG]], kvsem, eng="gpsimd")
                if first_tile:
                    dump("qT", qT[:, 0, :], [qT])
                    dump("kiT", kiT[:, 0:T], [kiT])
                    dump("wis", wis[:, 0, :], [wis])
                panel = stream.next("%d_xr" % l)
                pv = wview(panel, "xr")
                for c in range(4):
                    ps = nps()
                    fm_chunk(panel, pv, c, ps)
                    act(xrbuf[:, c, 3:3 + T], ps[:, 0:T], AF.Copy, [ps], [xrbuf])
                for nm, dstb in (("gr", grT), ("zu", zuT)):
                    panel = stream.next("%d_%s" % (l, nm))
                    pv = wview(panel, nm)
                    for c in range(4):
                        ps = nps()
                        fm_chunk(panel, pv, c, ps)
                        act(dstb[:, c, :], ps[:, 0:T], AF.Gelu_apprx_tanh, [ps], [dstb])
                panel = stream.next("%d_v" % l)
                pv = wview(panel, "v")
                for s in range(NSUB):
                    ps = nps()
                    for kc in range(8):
                        mm(ps[:, 0:512], hT[:, kc, s * 128:(s + 1) * 128], pv[:, kc, :], kc == 0, kc == 7, [panel, hT], [ps])
                    dstv = Vs[:, s, :].rearrange("p (h f) -> p h f", h=8)[:, :, 0:64]
                    act(dstv, ps[:, 0:512].rearrange("p (h f) -> p h f", h=8), AF.Copy, [ps], [Vs])
                dma(Vc[l][t0:t0 + T, :].rearrange("(s p) f -> p s f", p=128), Vs[:], [Vs], [KVT[l][t0 // KG]], kvsem, eng="gpsimd")
                panel = stream.next("%d_zv" % l)
                pv = wview(panel, "zv")
                for s in range(NSUB):
                    ps = nps()
                    for kc in range(8):
                        mm(ps[:, 0:512], hT[:, kc, s * 128:(s + 1) * 128], pv[:, kc, :], kc == 0, kc == 7, [panel, hT], [ps])
                    act(gz[:], ps[:, 0:512], AF.Gelu_apprx_tanh, [ps], [gz])
                    ss = sm()
                    act(tmpf[:, 0:512], gz[:], AF.Square, [gz, small], [tmpf, small], accum_out=ss)
                    rs = rstd_from_ss(ss, 512)
                    stt(vn[:, s, :], gz[:], rs, gb[:, 3 * D:3 * D + 512], ALU.mult, ALU.mult, [gz, small, gb], [vn])

                mpi = [0]

                def mps():
                    mpi[0] += 1
                    return PS[5 + mpi[0] % 3]

                def gen_mixC():
                    for s in range(NSUB):
                        for cpair in range(4):
                            ps = mps()
                            for gg in range(2):
                                g = cpair * 2 + gg
                                mm(ps[gg * 64:(gg + 1) * 64, 0:128], vn[:, s, g * 64:(g + 1) * 64], wspT[:, g, :], True, False,
                                   [vn, wspT], [ps])
                            mm(ps[:, 0:128], esel[:, cpair, :], bsph[:, 0, :], False, False, [cb, bsph], [ps])
                            mm(ps[:, 0:128], esel[:, cpair, :], bsph[:, 1, :], False, True, [cb, bsph], [ps])
                            tt(ycT[:, cpair, s * 128:(s + 1) * 128], ps[:, 0:128], zuT[:, cpair, s * 128:(s + 1) * 128], ALU.mult,
                               [ps, zuT], [ycT])
                            yield

                def gen_mixB():
                    for c in range(4):
                        xc = lt[:, 0, :]
                        ts(xc, xrbuf[:, c, 0:T], cols[:, 16 + c:17 + c], cols[:, 32 + c:33 + c], ALU.mult, ALU.add, [xrbuf, cols], [lt])
                        for jj in range(1, 4):
                            stt(xc, xrbuf[:, c, jj:jj + T], cols[:, 16 + jj * 4 + c:17 + jj * 4 + c], xc, ALU.mult, ALU.add,
                                [xrbuf, cols, lt], [lt])
                        yield
                        cp(xrbuf[:, c, 0:3], xrbuf[:, c, T:T + 3], [xrbuf], [xrbuf])
                        act(xcb[:], xc, AF.Copy, [lt], [xcb])
                        ps = mps()
                        mm(ps[:, 0:T], wbd[:, c, :], xcb[:], True, True, [wbd, xcb], [ps])
                        mm(ps[:, T:2 * T], wbd[:, 4 + c, :], xcb[:], True, True, [wbd, xcb], [ps])
                        yield
                        rg = lt[:, 1, :]
                        ig = lt[:, 2, :]
                        av = lt[:, 3, :]
                        sq = lt[:, 4, :]
                        act(rg, ps[:, 0:T], AF.Sigmoid, [ps, cols], [lt], bias=cols[:, 36 + c:37 + c])
                        act(ig, ps[:, T:2 * T], AF.Sigmoid, [ps, cols], [lt], bias=cols[:, 40 + c:41 + c])
                        yield
                        act(av, rg, AF.Exp, [lt, c8], [lt], scale=c8[:, c:c + 1])
                        act(sq, rg, AF.Exp, [lt, c8], [lt], scale=c8[:, 4 + c:5 + c])
                        act(sq, sq, AF.Sqrt, [lt, epsc], [lt], scale=-1.0, bias=epsc[:, 2:3])
                        yield
                        tt(ig, ig, xc, ALU.mult, [lt], [lt])
                        tt(ig, ig, sq, ALU.mult, [lt], [lt])
                        hh = lt[:, 1, :]
                        scan(hh, av, ig, hst[:, c:c + 1], [lt, hst], [lt])
                        yield
                        cp(hst[:, c:c + 1], hh[:, T - 1:T], [lt], [hst])
                        tt(ybT[:, c, :], hh, grT[:, c, :], ALU.mult, [lt, grT], [ybT])
                        yield

                grps = kv_groups(j)
                nkeys = (j + 1) * T
                TRh = [Tile("relu%d" % h) for h in range(8)]

                def gen_I(s):
                    N = t0 + 128 * (s + 1)
                    for h in range(8):
                        merge_tile(TRh[h], TR_, as_write=True)
                        act(diag[:, h, :], identf, AF.Copy, [cf, wis], [diag], scale=wis[:, s, h:h + 1])
                    yield
                    ngrp = (N + KG - 1) // KG
                    for kg in range(ngrp):
                        wd = min(KG, N - kg * KG)
                        for h in range(8):
                            ps = PS[h % 4]
                            r0 = (h % 2) * 64
                            mm(ps[:, 0:wd], qiT[r0:r0 + 64, h // 2, s * 128:(s + 1) * 128],
                               kiT[r0:r0 + 64, kg * KG:kg * KG + wd], True, True, [qiT, kiT], [ps])
                            if h % 2 == 0:
                                act(Rrelu[:, h, 0:wd], ps[:, 0:wd], AF.Relu, [ps], [TRh[h]])
                            else:
                                ts(Rrelu[:, h, 0:wd], ps[:, 0:wd], 0.0, None, ALU.max, None, [ps], [TRh[h]])
                            if h % 2 == 1:
                                yield
                        for h in range(8):
                            mm(PS[4][:, 0:wd], diag[:, h, :], Rrelu[:, h, 0:wd], h == 0, h == 7, [diag, TRh[h]], [PS[4]])
                        if kg == ngrp - 1:
                            if wd > 128:
                                cp(score_ap[:, kg * KG:kg * KG + wd - 128], PS[4][:, 0:wd - 128], [PS[4]], [TS_])
                            tt(score_ap[:, N - 128:N], PS[4][:, wd - 128:wd], negmask, ALU.add, [PS[4], cf], [TS_])
                            tt(tmpf[:, 0:128], PS[4][:, wd - 128:wd], posfill, ALU.add, [PS[4], cf], [tmpf])
                        else:
                            cp(score_ap[:, kg * KG:kg * KG + wd], PS[4][:, 0:wd], [PS[4]], [TS_])
                        yield
                    for h in range(8):
                        merge_tile(TR_, TRh[h])

                def gen_B(s):
                    N = t0 + 128 * (s + 1)
                    hi0 = bis[:, 0:1]
                    lo = bis[:, 1:2]
                    w0 = bis[:, 2:3]
                    reduce(hi0, score_ap[:, 0:N], ALU.max, [TS_, bis], [bis])
                    reduce(lo, tmpf[:, 0:128], ALU.min, [tmpf, bis], [bis])
                    if N > 128:
                        m1 = bis[:, 3:4]
                        reduce(m1, score_ap[:, 0:N - 128], ALU.min, [TS_, bis], [bis])
                        tt(lo, lo, m1, ALU.min, [bis], [bis])
                    tt(w0, hi0, lo, ALU.subtract, [bis], [bis])
                    yield
                    mk = masks[s]
                    if N > TOPK:
                        Nd = ((N // 2 + 127) // 128) * 128
                        Na = N - Nd
                        junkA = big[:, 4096:6144].bitcast(BF16)
                        for it in range(NBIS):
                            k4 = it % 4
                            mid = bis2[:, k4:k4 + 1]
                            cnt = bis2[:, 4 + k4:5 + k4]
                            vv = bis2[:, 8 + k4:9 + k4]
                            sA = bis2[:, 12 + k4:13 + k4]
                            stt(mid, w0, 0.5 ** (it + 1), lo, ALU.mult, ALU.add, [bis], [Tmid])
                            act(junkA[:, 0:Na], score_ap[:, Nd:N], AF.Sign, [TS_, Tmid], [TR_, Tsa], scale=-1.0, bias=mid,
                                accum_out=sA)
                            count_ge(mk[:, 0:Nd], score_ap[:, 0:Nd], mid, cnt, [TS_, Tmid], [mk, Tcnt])
                            stt(vv, cnt, 2.0, sA, ALU.mult, ALU.subtract, [Tcnt, Tsa], [Tcnt])
                            ge = smalli[:, k4:k4 + 1]
                            ts(ge, vv, float(2 * TOPK - Na), None, ALU.is_ge, None, [Tcnt], [smalli])
                            cpred(lo, ge, mid, [Tmid, smalli], [bis])
                            yield
                    ts(mk[:, 0:N], score_ap[:, 0:N], lo, None, ALU.is_ge, None, [TS_, bis], [mk])
                    if N < nkeys:
                        memset(mk[:, N:nkeys], 0.0, [mk], eng="gpsimd")
                    yield

                def chain(*gens):
                    for g_ in gens:
                        yield from g_

                def run(*gens):
                    live = list(gens)
                    while live:
                        for g_ in list(live):
                            try:
                                next(g_)
                            except StopIteration:
                                live.remove(g_)

                run(chain(gen_mixC(), gen_mixB()), gen_I(0))
                if first_tile:
                    dump("ycT", ycT[:, 0, :], [ycT])
                    dump("ybT", ybT[:, 0, :], [ybT])
                    dump("score", score_ap[:, 0:128], [TS_])
                run(gen_B(0))
                run(gen_I(1))
                run(gen_B(1))

                first = True
                ucnt = [0]
                for gi, (g, wd) in enumerate(grps):
                    panel = stream.next("%d_kv%d_%d" % (l, j, g))
                    nb = wd // 128
                    KTv = panel[:, 0:4 * wd].rearrange("p (c w) -> p c w", c=4)
                    Vv = panel[:, 2048:2048 + nb * 520].rearrange("p (b f) -> p b f", b=nb)
                    psb = PS[6][:].bitcast(BF16)
                    for s in range(NSUB):
                        for b in range(nb):
                            tr(psb[:, (s * 4 + b) * 128:(s * 4 + b + 1) * 128],
                               masks[s][:, g * KG + b * 128:g * KG + (b + 1) * 128], identb, [masks[s], cb], [PS[6]])
                    for s in range(NSUB):
                        act(maskTk[:, 0:nb, s * 128:(s + 1) * 128],
                            psb[:, s * 512:s * 512 + nb * 128].rearrange("p (b t) -> p b t", b=nb),
                            AF.Copy, [PS[6]], [maskTk])
                    units = [(h, bp) for h in range(8) for bp in range(0, nb, 2)]

                    def stageA(i):
                        h, bp = units[i]
                        r0 = (h % 2) * 64
                        nbb = min(2, nb - bp)
                        ps = PS[(ucnt[0] + i) % 4]
                        for b in range(bp, bp + nbb):
                            mm(ps[:, (b - bp) * T:(b - bp + 1) * T], KTv[r0:r0 + 64, h // 2, b * 128:(b + 1) * 128],
                               qT[r0:r0 + 64, h // 2, :], True, True, [panel, qT], [ps])

                    def stageB(i):
                        h, bp = units[i]
                        nbb = min(2, nb - bp)
                        ps = PS[(ucnt[0] + i) % 4]
                        ptile = PT[(ucnt[0] + i) % len(PT)]
                        act(ptile[:, 0:nbb, :], ps[:, 0:nbb * T].rearrange("p (b t) -> p b t", b=nbb), AF.Exp,
                            [ps], [ptile], scale=HD ** -0.5)
                        tt(ptile[:, 0:nbb, :], ptile[:, 0:nbb, :], maskTk[:, bp:bp + nbb, :], ALU.mult, [ptile, maskTk], [ptile],
                           eng=("vector" if (ucnt[0] + i) % 2 == 0 else "gpsimd"))

                    def stageD(i):
                        h, bp = units[i]
                        nbb = min(2, nb - bp)
                        pacc = PS[4 + (h % 2)]
                        ptile = PT[(ucnt[0] + i) % len(PT)]
                        for b in range(bp, bp + nbb):
                            mm(pacc[0:65, 0:T], Vv[:, b, h * 65:(h + 1) * 65], ptile[:, b - bp, :], b == 0, b == nb - 1,
                               [panel, ptile], [pacc])
                        if bp + 2 >= nb:
                            if first:
                                cp(acc[:, h, :], pacc[0:65, 0:T], [pacc], [acc])
                            else:
                                tt(acc[:, h, :], acc[:, h, :], pacc[0:65, 0:T], ALU.add, [pacc, acc], [acc])

                    stageA(0)
                    if len(units) > 1:
                        stageA(1)
                    for i in range(len(units)):
                        if i + 2 < len(units):
                            stageA(i + 2)
                        stageB(i)
                        stageD(i)
                    ucnt[0] += len(units)
                    first = False
                accf = acc[:].rearrange("p h t -> p (h t)")
                act(accf[64:65, :], accf[64:65, :], AF.Ln, [acc], [acc])
                act(accf[64:65, :], accf[64:65, :], AF.Exp, [acc], [acc], scale=-1.0)
                for h in range(8):
                    rhl = rhl2[h % 2]
                    cp(rhl[64:65, 0, :], acc[64:65, h, :], [acc], [rhl])
                    tt(rhl[64:65, 1, :], acc[64:65, h, :], rhl[64:65, 0, :], ALU.subtract, [acc, rhl], [rhl])
                    ps = nps()
                    mm(ps[0:64, 0:T], onesb[64:65, 0:64], rhl[64:65, 0, :], True, False, [onesb, rhl], [ps])
                    mm(ps[0:64, 0:T], onesb[64:65, 0:64], rhl[64:65, 1, :], False, True, [onesb, rhl], [ps])
                    if h % 2 == 0:
                        tt(yaT[0:64, h // 2, :], acc[0:64, h, :], ps[0:64, 0:T], ALU.mult, [acc, ps], [yaT])
                    else:
                        tt(yaTt[:, :], acc[0:64, h, :], ps[0:64, 0:T], ALU.mult, [acc, ps], [yaTt])
                        ps2 = nps()
                        mm(ps2[64:128, 0:T], identb[0:64, 0:64], yaTt[:, :], True, True, [cb, yaTt], [ps2])
                        cp(yaT[64:128, h // 2, :], ps2[64:128, 0:T], [ps2], [yaT])
                if first_tile:
                    dump("yaT", yaT[:, 0, :], [yaT])
                if l == 0 and j == 1:
                    dump("yaT1", yaT[:, 0, :], [yaT])
                    dump("acc1", acc[:, 0, :], [acc])

                for ni, n in enumerate((2, 1, 0)):
                    for hf in range(2):
                        gpan = stream.next("%d_gate%d_%d" % (l, n, hf))
                        pv = wview(gpan, "gate0_0")
                        for c in range(4):
                            ps = nps()
                            fm_chunk(gpan, pv, c, ps)
                            act(gsig[:, hf * 4 + c, :], ps[:, 0:T], AF.Sigmoid, [ps], [gsig])
                    ysrc = {2: ycT, 1: ybT, 0: yaT}[n]
                    bp_ = [None, None]
                    for hf in range(2):
                        bp_[hf] = stream.next("%d_br%d_%d" % (l, n, hf))
                        pv = wview(bp_[hf], "br0_0")
                        for c in range(4):
                            dchunk = hf * 4 + c
                            ps = nps()
                            for kc in range(4):
                                mm(ps[:, 0:T], pv[:, kc, c * 128:(c + 1) * 128], ysrc[:, kc, :], kc == 0, kc == 3,
                                   [bp_[hf], ysrc], [ps])
                            if ni == 0:
                                tt(merged[:, dchunk, :], ps[:, 0:T], gsig[:, dchunk, :], ALU.mult, [ps, gsig], [TS_])
                            else:
                                tt(gt[:], ps[:, 0:T], gsig[:, dchunk, :], ALU.mult, [ps, gsig], [gt])
                                dsto = mergedT if ni == 2 else merged
                                tt(dsto[:, dchunk, :], merged[:, dchunk, :], gt[:], ALU.add, [TS_, gt], [TS_], eng="gpsimd")
                op_ = [stream.next("%d_out_%d" % (l, hf), look=NSLOT - 1 - hf) for hf in range(2)]
                for s in range(NSUB):
                    banks = [PS[4 + 2 * (s % 2)], PS[5 + 2 * (s % 2)]]
                    for hf in range(2):
                        pv = wview(op_[hf], "out_0")
                        for kc in range(8):
                            mm(banks[hf][:, 0:512], mergedT[:, kc, s * 128:(s + 1) * 128], pv[:, kc, :], kc == 0, kc == 7,
                               [op_[hf], TS_], [banks[hf]])
                    post_norm_residual(banks, 0, s)
                if first_tile:
                    dump("x1", xt[:, 0, :], [xt])
                norm_transpose(8)
                dbanks = [[PS[4], PS[5]], [PS[6], PS[7]]]
                TF = [Tile("fT0"), Tile("fT1")]
                Trl = [Tile("rl0"), Tile("rl1")]

                for i_ in range(2):
                    merge_tile(TF[i_], TS_, as_write=True)
                    merge_tile(Trl[i_], TR_, as_write=True)

                def ffn_up(g):
                    up = stream.next("%d_up_%d" % (l, g))
                    pv = wview(up, "up_0")
                    fTg = fT[g % 2]
                    for c in range(4):
                        ps = nps()
                        fm_chunk(up, pv, c, ps)
                        rlb = rl[c % 2]
                        act(rlb, ps[:, 0:T], AF.Relu, [ps], [Trl[c % 2]])
                        tt(fTg[:, c, :], rlb, rlb, ALU.mult, [Trl[c % 2]], [TF[g % 2]], eng="gpsimd")

                def ffn_dn(g):
                    dn = stream.next("%d_dn_%d" % (l, g))
                    dv = wview(dn, "dn_0")
                    fTg = fT[g % 2]
                    for s in range(NSUB):
                        for hf in range(2):
                            for c in range(4):
                                mm(dbanks[s][hf][:, 0:512], fTg[:, c, s * 128:(s + 1) * 128], dv[:, c, hf * 512:(hf + 1) * 512],
                                   g == 0 and c == 0, g == 7 and c == 3, [dn, TF[g % 2]], [dbanks[s][hf]])

                ffn_up(0)
                for g in range(8):
                    if g + 1 < 8:
                        ffn_up(g + 1)
                    ffn_dn(g)
                for i_ in range(2):
                    merge_tile(TS_, TF[i_])
                    merge_tile(TR_, Trl[i_])
                for s in range(NSUB):
                    post_norm_residual(dbanks[s], D, s)
                if first_tile:
                    dump("x2", xt[:, 0, :], [xt])
                norm_transpose(None)
                for s in range(NSUB):
                    cp(ptb[:, s, :], pt[:, s, :], [pt], [ptb])
                    psb = PS[3][:].bitcast(BF16)
                    for c in range(2):
                        tr(psb[:, c * 128:(c + 1) * 128], ptb[:, s, c * 128:(c + 1) * 128], identb, [ptb, cb], [PS[3]])
                    cp(pT[:, :, s * 128:(s + 1) * 128], psb[:, 0:256].rearrange("p (c t) -> p c t", c=2), [PS[3]], [pT])
                plp = stream.next("%d_ple" % l)
                plv = wview(plp, "ple")
                pg = [stream.next("%d_pg_%d" % (l, hf), look=NSLOT - 2 - hf) for hf in range(2)]
                for s in range(NSUB):
                    ssp = []
                    for hf in range(2):
                        pa = PS[4 * (s % 2) + hf]
                        pgb = PS[4 * (s % 2) + 2 + hf]
                        for kc in range(2):
                            mm(pa[:, 0:512], pT[:, kc, s * 128:(s + 1) * 128], plv[:, kc, hf * 512:(hf + 1) * 512], kc == 0, kc == 1,
                               [plp, pT], [pa])
                        gv = wview(pg[hf], "pg_0")
                        for kc in range(8):
                            mm(pgb[:, 0:512], hT[:, kc, s * 128:(s + 1) * 128], gv[:, kc, :], kc == 0, kc == 7, [pg[hf], hT], [pgb])
                        act(sg, pgb[:, 0:512], AF.Sigmoid, [pgb], [TR_])
                        tt(ple_t[:, hf * 512:(hf + 1) * 512], pa[:, 0:512], sg, ALU.mult, [pa, TR_], [TR_])
                        a = sm()
                        act(tmpf[:, 0:512], ple_t[:, hf * 512:(hf + 1) * 512], AF.Square, [TR_, small], [tmpf, small], accum_out=a)
                        ssp.append(a)
                    s3 = sm()
                    tt(s3, ssp[0], ssp[1], ALU.add, [small], [small])
                    rs = rstd_from_ss(s3, D)
                    for hf in range(2):
                        stt(tmpf[:, 0:512], ple_t[:, hf * 512:(hf + 1) * 512], rs, gb[:, 2 * D + hf * 512:2 * D + (hf + 1) * 512],
                            ALU.mult, ALU.mult, [TR_, small, gb], [tmpf])
                        tt(xt[:, s, hf * 512:(hf + 1) * 512], xt[:, s, hf * 512:(hf + 1) * 512], tmpf[:, 0:512], ALU.add,
                           [xt, tmpf], [xt])
                wt = [XT[j]] if XT is not None else [out_tile]
                dma(dst_x[t0:t0 + T, :].rearrange("(s p) d -> p s d", p=128), xt[:], [xt], wt, ssem, eng="gpsimd")
            XTprev = XT

        fw.finish("sync", [out_tile] + dbg_tiles)
        fw.emit()
    return nc, fw


def _consts():
    bf = ml_dtypes.bfloat16
    ident = np.eye(128, dtype=np.float32)
    Rm = np.zeros((128, 128), np.float32)
    for base in (0, 64):
        for d in range(8):
            Rm[base + d + 8, base + d] = -1.0
            Rm[base + d, base + d + 8] = 1.0
    esel = np.zeros((128, 4, 128), np.float32)
    for g in range(8):
        esel[g, g // 2, (g % 2) * 64:(g % 2 + 1) * 64] = 1.0
    cb = np.concatenate([ident, Rm, esel.reshape(128, 512)], axis=1).astype(bf)
    tt_, ss_ = np.meshgrid(np.arange(128), np.arange(128), indexing="ij")
    negmask = np.where(ss_ > tt_, np.float32(NEG), np.float32(0.0))
    posfill = np.where(ss_ > tt_, np.float32(-2 * NEG), np.float32(0.0))
    tril01 = (tt_ <= ss_).astype(np.float32)
    half = 8
    inv_freq = (np.float32(500000.0) ** (-np.arange(half, dtype=np.float32) * np.float32(2.0) / np.float32(16))).astype(np.float32)
    invf = np.zeros((128, 1), np.float32)
    for f in range(128):
        d = f % 64
        if d < 16:
            invf[f, 0] = inv_freq[d % 8]
    cf = np.concatenate([ident, negmask, posfill, tril01, invf], axis=1).astype(np.float32)
    return cb, cf


def _layout_params(inp, depth):
    f = np.float32
    cols = np.zeros((depth, 128, 48), f)
    gb = np.zeros((depth, 128, 3 * D + 512), f)
    wbd = np.zeros((depth, 128, 8, 128), f)
    wsp = np.zeros((depth, 128, 8, 128), f)
    for l in range(depth):
        cols[l, :, 0:8] = np.asarray(inp["g_pre_mix"][l], f).reshape(8, 128).T
        cols[l, :, 8:16] = np.asarray(inp["g_pre_ffn"][l], f).reshape(8, 128).T
        cw = np.asarray(inp["conv_w"][l], f)
        for jj in range(4):
            cols[l, :, 16 + jj * 4:20 + jj * 4] = cw[jj].reshape(4, 128).T
        cols[l, :, 32:36] = np.asarray(inp["conv_b"][l], f).reshape(4, 128).T
        cols[l, :, 36:40] = np.asarray(inp["b_rg_a"][l], f).reshape(4, 128).T
        cols[l, :, 40:44] = np.asarray(inp["b_rg_x"][l], f).reshape(4, 128).T
        cols[l, :, 44:48] = np.asarray(inp["lru_lambda"][l], f).reshape(4, 128).T
        row = np.concatenate([np.asarray(inp["g_post_mix"][l], f), np.asarray(inp["g_post_ffn"][l], f),
                              np.asarray(inp["g_post_ple"][l], f), np.asarray(inp["g_gmlp_v"][l], f)])
        gb[l] = np.broadcast_to(row[None, :], (128, row.size))
        for gi, key in enumerate(("w_rg_a", "w_rg_x")):
            w = np.asarray(inp[key][l], f)
            for c in range(4):
                for hh in range(2):
                    wbd[l, hh * 64:(hh + 1) * 64, gi * 4 + c, hh * 64:(hh + 1) * 64] = w[c * 2 + hh]
        ws = np.asarray(inp["w_spatial"][l], f)
        wsp[l] = np.transpose(ws, (2, 0, 1))
    bsp = np.ascontiguousarray(np.asarray(inp["b_spatial"], f))
    return cols, gb, wbd, wsp, bsp


_CACHE = {}


def kernel(**inputs):
    depth = DEPTH
    x = np.asarray(inputs["x"], np.float32)
    B, L, _ = x.shape
    p = np.asarray(inputs["p"], np.float32)
    pos = np.asarray(inputs["positions"], np.int32)
    cb, cf = _consts()
    cols, gb, wbd, wsp, bsp = _layout_params(inputs, depth)
    shared = {
        "w_in": np.ascontiguousarray(np.asarray(inputs["w_in"], np.float32)),
        "w_branch": np.ascontiguousarray(np.asarray(inputs["w_branch"], np.float32)),
        "w_out": np.ascontiguousarray(np.asarray(inputs["w_out"], np.float32)),
        "w_ffn_up": np.ascontiguousarray(np.asarray(inputs["w_ffn_up"], np.float32)),
        "w_ffn_down": np.ascontiguousarray(np.asarray(inputs["w_ffn_down"], np.float32)),
        "w_ple": np.ascontiguousarray(np.asarray(inputs["w_ple"], np.float32)),
        "w_ple_gate": np.ascontiguousarray(np.asarray(inputs["w_ple_gate"], np.float32)),
        "cols": cols, "gb": gb, "wbd": wbd, "wsp": wsp, "bsp": bsp, "cb": cb, "cf": cf,
    }
    if L not in _CACHE:
        _CACHE[L] = build_program(L, depth)[0]
    nc = _CACHE[L]
    in_maps = []
    for b in range(B):
        m = dict(shared)
        m["x"] = np.ascontiguousarray(x[b])
        m["p"] = np.ascontiguousarray(p[:, b])
        m["pos"] = np.ascontiguousarray(np.broadcast_to(pos[b][None, :], (128, L)))
        in_maps.append(m)
    res = run_bass_kernel_spmd(nc, in_maps, core_ids=list(range(B)))
    return np.stack([np.asarray(r["y"], np.float32) for r in res.results], axis=0)
```

```python
import math
from contextlib import ExitStack
import numpy as np
import ml_dtypes
import concourse.bass as bass
import concourse.mybir as mybir
from concourse.bass_utils import run_bass_kernel_spmd

F32 = mybir.dt.float32
BF16 = mybir.dt.bfloat16
I32 = mybir.dt.int32
AF = mybir.ActivationFunctionType
ALU = mybir.AluOpType
AX = mybir.AxisListType

D = 1024
NH = 8
HD = 64
TOPK = 256
FFN = 4096
PLE = 256
EPS = 1e-6
DEPTH = 2
T = 256
NSUB = T // 128
KG = 512
NSLOT = 3
SLOTB = 4224
NBIS = 13
BIGM = 30000.0
NEG = -1.0e30
IN_OFF = dict(q=0, k=512, v=1024, qi=1536, kiwi=2048, xr=2120, gr=2632, zu=3144, zv=3656, gate=4168)


class Sem:
    def __init__(self, h, name):
        self.h = h
        self.n = 0
        self.name = name


class Tile:
    __slots__ = ("w", "r", "name")

    def __init__(self, name=""):
        self.w = {}
        self.r = {}
        self.name = name


class Buf:
    def __init__(self, t, name=""):
        self.t = t
        self.T = Tile(name)

    def __getitem__(self, k):
        return self.t[k]


class Engine:
    def __init__(self, name, sem):
        self.name = name
        self.sem = sem
        self.ops = []
        self.seen = {}


class FW:
    def __init__(self, nc, stack):
        self.nc = nc
        self.stack = stack
        self.engs = {}
        for n in ("tensor", "vector", "scalar", "gpsimd", "sync"):
            s = Sem(stack.enter_context(nc.semaphore("sem_" + n)), n)
            self.engs[n] = Engine(n, s)
        self.nops = 0

    def dsem(self, name):
        return Sem(self.stack.enter_context(self.nc.semaphore("dsem_" + name)), name)

    def op(self, eng, fn, reads=(), writes=(), dsem=None):
        E = self.engs[eng]
        need = {}
        for b in reads:
            t = b.T if isinstance(b, Buf) else b
            for s, v in t.w.items():
                if need.get(s, 0) < v:
                    need[s] = v
        for b in writes:
            t = b.T if isinstance(b, Buf) else b
            for s, v in t.w.items():
                if need.get(s, 0) < v:
                    need[s] = v
            for s, v in t.r.items():
                if need.get(s, 0) < v:
                    need[s] = v
        raw_self = 0
        for b in reads:
            t = b.T if isinstance(b, Buf) else b
            raw_self = max(raw_self, t.w.get(E.sem, 0))
        waits = []
        for s, v in need.items():
            if s is E.sem:
                if eng != "tensor" and raw_self > E.seen.get(s, 0):
                    E.seen[s] = raw_self
                    waits.append((s, raw_self))
                continue
            if E.seen.get(s, 0) >= v:
                continue
            E.seen[s] = v
            waits.append((s, v))
        if dsem is not None:
            dsem.n += 16
            sig = (dsem, dsem.n, 16)
        else:
            E.sem.n += 1
            sig = (E.sem, E.sem.n, 1)
        E.ops.append((waits, fn, sig))
        self.nops += 1
        s, v = sig[0], sig[1]
        for b in reads:
            t = b.T if isinstance(b, Buf) else b
            if t.r.get(s, 0) < v:
                t.r[s] = v
        for b in writes:
            t = b.T if isinstance(b, Buf) else b
            if t.w.get(s, 0) < v:
                t.w[s] = v

    def finish(self, eng, tiles):
        E = self.engs[eng]
        need = {}
        for b in tiles:
            t = b.T if isinstance(b, Buf) else b
            for d in (t.w, t.r):
                for s, v in d.items():
                    if need.get(s, 0) < v:
                        need[s] = v
        E.ops.append(([(s, v) for s, v in need.items() if s is not E.sem], None, None))

    def emit(self):
        with self.nc.Block() as block:
            for n, E in self.engs.items():
                def body(e, E=E):
                    for waits, fn, sig in E.ops:
                        for s, v in waits:
                            e.wait_ge(s.h, v)
                        if fn is None:
                            continue
                        fn(e).then_inc(sig[0].h, sig[2])
                getattr(block, n)(body)


def panel_defs():
    P = []
    for nm in ("q", "k", "qi"):
        P.append((nm, "w_in", 0, 1024, IN_OFF[nm], 512))
    P.append(("kiwi", "w_in", 0, 1024, IN_OFF["kiwi"], 72))
    for nm in ("xr", "gr", "zu", "v", "zv"):
        P.append((nm, "w_in", 0, 1024, IN_OFF[nm], 512))
    for n in range(3):
        for hf in range(2):
            P.append(("gate%d_%d" % (n, hf), "w_in", 0, 1024, IN_OFF["gate"] + n * 1024 + hf * 512, 512))
    for n in range(3):
        for hf in range(2):
            P.append(("br%d_%d" % (n, hf), "w_branch%d" % n, 0, 512, hf * 512, 512))
    for hf in range(2):
        P.append(("out_%d" % hf, "w_out", 0, 1024, hf * 512, 512))
    for g in range(8):
        P.append(("up_%d" % g, "w_ffn_up", 0, 1024, g * 512, 512))
        P.append(("dn_%d" % g, "w_ffn_down", g * 512, 512, 0, 1024))
    P.append(("ple", "w_ple", 0, 256, 0, 1024))
    for hf in range(2):
        P.append(("pg_%d" % hf, "w_ple_gate", 0, 1024, hf * 512, 512))
    return P


def build_program(L, depth=DEPTH, dbg=None):
    NT = L // T
    nc = bass.Bass("TRN2", target_bir_lowering=False)
    dram = lambda name, shape, dt, kind="ExternalInput": nc.dram_tensor(name, shape, dt, kind=kind).ap()
    x_in = dram("x", [L, D], F32)
    p_in = dram("p", [depth, L, PLE], F32)
    pos_in = dram("pos", [128, L], I32)
    wsrc = {
        "w_in": dram("w_in", [depth, D, 7240], F32),
        "w_branch": dram("w_branch", [depth, 3, 512, D], F32),
        "w_out": dram("w_out", [depth, D, D], F32),
        "w_ffn_up": dram("w_ffn_up", [depth, D, FFN], F32),
        "w_ffn_down": dram("w_ffn_down", [depth, FFN, D], F32),
        "w_ple": dram("w_ple", [depth, PLE, D], F32),
        "w_ple_gate": dram("w_ple_gate", [depth, D, D], F32),
    }
    cols_in = dram("cols", [depth, 128, 48], F32)
    gb_in = dram("gb", [depth, 128, 3 * D + 512], F32)
    wbd_in = dram("wbd", [depth, 128, 8, 128], F32)
    wsp_in = dram("wsp", [depth, 128, 8, 128], F32)
    bsp_in = dram("bsp", [depth, 8, 128], F32)
    cb_in = dram("cb", [128, 256 + 512], BF16)
    cf_in = dram("cf", [128, 4 * 128 + 1], F32)
    y_out = dram("y", [L, D], F32, kind="ExternalOutput")
    xbuf = dram("xbuf", [L, D], F32, kind="Internal")
    KTc = [dram("ktc%d" % l, [4, 128, L], BF16, kind="Internal") for l in range(depth)]
    Vc = [dram("vc%d" % l, [L, 520], BF16, kind="Internal") for l in range(depth)]
    pdefs = panel_defs()
    Wp = [{nm: dram("wp%d_%s" % (l, nm), [nr, ncol], BF16, kind="Internal") for (nm, _, _, nr, _, ncol) in pdefs}
          for l in range(depth)]
    dbg_out = {}
    if dbg:
        for k, shp in dbg.items():
            dbg_out[k] = dram("dbg_" + k, list(shp), F32, kind="ExternalOutput")

    with ExitStack() as st:
        fw = FW(nc, st)
        op = fw.op

        def sb(name, shape, dt):
            return Buf(st.enter_context(nc.sbuf_tensor("s_" + name, shape, dt)), name)

        PS = [Buf(st.enter_context(nc.psum_tensor("ps%d" % i, [128, 512], F32)), "ps%d" % i) for i in range(8)]

        def mm(out, lhsT, rhs, start, stop, R, W):
            op("tensor", lambda e: e.matmul(out, lhsT=lhsT, rhs=rhs, start=start, stop=stop), R, W)

        def tr(out, in_, ident, R, W):
            op("tensor", lambda e: e.transpose(out=out, in_=in_, identity=ident), R, W)

        def act(out, in_, func, R, W, **kw):
            op("scalar", lambda e: e.activation(out=out, in_=in_, func=func, **kw), R, W)

        def ts(out, in0, s1, s2, op0, op1, R, W, eng="vector"):
            if op1 is None:
                op(eng, lambda e: e.tensor_scalar(out=out, in0=in0, scalar1=s1, scalar2=None, op0=op0), R, W)
            else:
                op(eng, lambda e: e.tensor_scalar(out=out, in0=in0, scalar1=s1, scalar2=s2, op0=op0, op1=op1), R, W)

        def tt(out, in0, in1, o, R, W, eng="vector"):
            op(eng, lambda e: e.tensor_tensor(out=out, in0=in0, in1=in1, op=o), R, W)

        def stt(out, in0, s, in1, op0, op1, R, W):
            op("vector", lambda e: e.scalar_tensor_tensor(out=out, in0=in0, scalar=s, in1=in1, op0=op0, op1=op1), R, W)

        def cp(out, in_, R, W, eng="vector"):
            op(eng, lambda e: e.tensor_copy(out=out, in_=in_), R, W)

        def dma(out, in_, R, W, ds, eng="sync"):
            op(eng, lambda e: e.dma_start(out=out, in_=in_), R, W, dsem=ds)

        def memset(ap, val, W, eng="vector"):
            op(eng, lambda e: e.memset(ap, val), [], W)

        def reduce(out, in_, o, R, W):
            op("vector", lambda e: e.tensor_reduce(out=out, in_=in_, axis=AX.X, op=o), R, W)

        def recip(out, in_, R, W):
            op("vector", lambda e: e.reciprocal(out=out, in_=in_), R, W)

        def scan(out, d0, d1, init, R, W):
            op("vector", lambda e: e.tensor_tensor_scan(out=out, data0=d0, data1=d1, initial=init,
                                                        op0=ALU.mult, op1=ALU.add), R, W)

        def count_ge(out, in0, thr, cnt, R, W):
            op("vector", lambda e: e.tensor_scalar(out=out, in0=in0, scalar1=thr, scalar2=None, op0=ALU.is_ge,
                                                   op1=ALU.add, accum_out=cnt), R, W)

        def cpred(out, mask, data, R, W):
            op("vector", lambda e: e.copy_predicated(out=out, mask=mask, data=data), R, W)

        dbg_sem = fw.dsem("dbg")
        dbg_tiles = []

        def dump(name, ap, R):
            if name in dbg_out:
                t = Tile("dbg")
                dma(dbg_out[name], ap, R, [t], dbg_sem, eng="gpsimd")
                dbg_tiles.append(t)
                del dbg_out[name]

        cb = sb("cb", [128, 768], BF16)
        cf = sb("cf", [128, 513], F32)
        dma(cb[:], cb_in, [], [cb], fw.dsem("c0"))
        dma(cf[:], cf_in, [], [cf], fw.dsem("c1"))
        identb = cb[:, 0:128]
        Rm = cb[:, 128:256]
        esel = cb[0:8, 256:768].rearrange("p (c f) -> p c f", c=4)
        identf = cf[:, 0:128]
        negmask = cf[:, 128:256]
        posfill = cf[:, 256:384]
        tril01 = cf[:, 384:512]
        invf = cf[:, 512:513]

        WT = [dict() for _ in range(depth)]
        for l in range(depth):
            wcs = fw.dsem("wcast%d" % l)
            for (nm, src, r0, nr, c0, ncol) in pdefs:
                if src.startswith("w_branch"):
                    s_ap = wsrc["w_branch"][l, int(src[-1]), r0:r0 + nr, c0:c0 + ncol]
                else:
                    s_ap = wsrc[src][l, r0:r0 + nr, c0:c0 + ncol]
                t = Tile("wp")
                WT[l][nm] = t
                step = 512
                for rr in range(0, nr, step):
                    n2 = min(step, nr - rr)
                    dma(Wp[l][nm][rr:rr + n2, :], s_ap[rr:rr + n2, :], [], [t], wcs, eng="gpsimd")
            for t in WT[l].values():
                t.w = {wcs: wcs.n}

        ring = [sb("ring%d" % i, [128, SLOTB], BF16) for i in range(NSLOT)]
        ring_sem = [fw.dsem("ring%d" % i) for i in range(NSLOT)]
        kiT = sb("kiT", [128, L], BF16)
        xt = sb("xt", [128, NSUB, D], F32)
        pt = sb("pt", [128, NSUB, PLE], F32)
        ptb = sb("ptb", [128, NSUB, PLE], BF16)
        posi = sb("posi", [128, T], I32)
        hT = sb("hT", [128, 8, T], BF16)
        pT = sb("pT", [128, 2, T], BF16)
        qT = sb("qT", [128, 4, T], BF16)
        qiT = sb("qiT", [128, 4, T], BF16)
        KTs = sb("KTs", [128, 4, T], BF16)
        Vs = sb("Vs", [128, NSUB, 520], BF16)
        cosT = sb("cosT", [128, T], F32)
        sinT = sb("sinT", [128, T], F32)
        rtmp = sb("rtmp", [128, 3, T], F32)
        xb16 = sb("xb16", [128, T], BF16)
        xrbuf = sb("xrbuf", [128, 4, 3 + T], F32)
        grT = sb("grT", [128, 4, T], BF16)
        zuT = sb("zuT", [128, 4, T], BF16)
        vn = sb("vn", [128, NSUB, 512], BF16)
        hst = sb("hst", [128, 4], F32)
        ybT = sb("ybT", [128, 4, T], BF16)
        ycT = sb("ycT", [128, 4, T], BF16)
        yaT = sb("yaT", [128, 4, T], BF16)
        yaTt = sb("yaTt", [64, T], BF16)
        wis = sb("wis", [128, NSUB, 8], F32)
        diag = sb("diag", [128, 8, 128], BF16)
        small = sb("small", [128, 32], F32)
        smalli = sb("smalli", [128, 4], I32)
        bis = sb("bis", [128, 16], F32)
        bis2 = sb("bis2", [128, 16], F32)
        Tmid = Tile("mid")
        Tcnt = Tile("cnt")
        Tsa = Tile("sa")
        big = sb("big", [128, 6144], F32)
        TS_ = big.T
        TR_ = Tile("bigR")
        score_ap = big[:, 0:L]
        Rrelu = big[:, 4096:6144].bitcast(BF16).rearrange("p (h w) -> p h w", h=8)
        masks = [sb("mask%d" % s, [128, L], BF16) for s in range(NSUB)]
        maskTk = sb("maskTk", [128, 4, T], BF16)
        PT = [sb("PT%d" % i, [128, 2, T], BF16) for i in range(4)]
        xs = sb("xs", [128, D], BF16)
        accm = sb("accm", [128, 8 * T], F32)

        def view(ap, tile):
            v = Buf.__new__(Buf)
            v.t = ap
            v.T = tile
            return v

        acc = view(accm[0:65, :].rearrange("p (h t) -> p h t", h=8), accm.T)
        lt = view(accm[:, 0:5 * T].rearrange("p (k t) -> p k t", k=5), accm.T)
        gz = view(accm[:, 5 * T:5 * T + 512], accm.T)
        xcb = view(accm[:, 5 * T + 512:5 * T + 512 + T // 2].bitcast(BF16), accm.T)
        sc1 = sb("sc1", [128, L], F32)
        rhl2 = [sb("rhl%d" % i, [65, 2, T], BF16) for i in range(2)]
        onesb = sb("onesb", [65, 64], BF16)
        gt = sb("gt", [128, T], F32)
        tmpf = sb("tmpf", [128, 512], F32)
        gsig = sb("gsig", [128, 8, T], BF16)
        merged = big[:, 0:8 * T].rearrange("p (c t) -> p c t", c=8)
        o = 8 * T
        mergedT = big[:, o:o + 4 * T].bitcast(BF16).rearrange("p (c t) -> p c t", c=8)
        o += 4 * T
        fT = [big[:, o + i * 2 * T:o + (i + 1) * 2 * T].bitcast(BF16).rearrange("p (c t) -> p c t", c=4) for i in range(2)]
        o += 4 * T
        assert o <= 4096
        o = 4096
        rl = [big[:, o + i * (T // 2):o + (i + 1) * (T // 2)].bitcast(BF16) for i in range(2)]
        o += T
        sg = big[:, o:o + 512]
        o += 512
        ple_t = big[:, o:o + 1024]
        o += 1024
        assert o <= 6144
        wbdf = big[:, 4096:4096 + 1024].rearrange("p (c j) -> p c j", c=8)
        cols = sb("cols", [128, 48], F32)
        gbv = sb("gbv", [128, 512], F32)
        gcur = sb("gcur", [128, D], F32)
        gsem = fw.dsem("gain")
        wbd = sb("wbd", [128, 8, 128], BF16)
        wspT = sb("wspT", [128, 8, 128], BF16)
        bsp = sb("bsp", [8, 128], F32)
        bsph = sb("bsph", [8, 2, 128], BF16)
        c8 = sb("c8", [128, 8], F32)
        epsc = sb("epsc", [128, 4], F32)
        psem = [fw.dsem("par%d" % i) for i in range(5)]
        xsem = fw.dsem("xload")
        ptsem = fw.dsem("pload")
        possem = fw.dsem("posload")
        ssem = fw.dsem("store")
        kvsem = fw.dsem("kvstore")
        out_tile = Tile("out")

        class Stream:
            def __init__(self):
                self.plan = []
                self.issued = 0
                self.pos = 0

            def add(self, name, loader, deps, hoist=True):
                self.plan.append((name, loader, deps, hoist))

            def next(self, name, look=NSLOT - 1):
                i = self.pos
                assert self.plan[i][0] == name, (self.plan[i][0], name)
                while self.issued < len(self.plan) and (
                        self.issued <= i or (self.issued <= i + look and self.plan[self.issued][3])):
                    k = self.issued
                    _, loader, deps, _ = self.plan[k]
                    loader(ring[k % NSLOT], ring_sem[k % NSLOT], deps)
                    self.issued += 1
                self.pos += 1
                return ring[i % NSLOT]

        stream = Stream()

        def wloader(l, nm, nr, ncol):
            kc = nr // 128

            def f(slot, sem, deps):
                dst = slot[:, 0:kc * ncol].rearrange("p (k w) -> p k w", k=kc)
                src = Wp[l][nm].rearrange("(k p) w -> p k w", p=128)
                dma(dst, src, deps, [slot], sem)
            return f

        KVT = [[Tile("kv") for _ in range((L + KG - 1) // KG)] for _ in range(depth)]

        def kvloader(l, g, wd):
            def f(slot, sem, deps):
                dstk = slot[:, 0:4 * wd].rearrange("p (c w) -> p c w", c=4)
                dma(dstk, KTc[l][:, :, g * KG:g * KG + wd].rearrange("c p w -> p c w"), deps, [slot], sem)
                nb = wd // 128
                dstv = slot[:, 2048:2048 + nb * 520].rearrange("p (b f) -> p b f", b=nb)
                dma(dstv, Vc[l][g * KG:g * KG + wd, :].rearrange("(b p) f -> p b f", p=128), deps, [slot], sem)
            return f

        pinfo = {nm: (nr, ncol) for (nm, _, _, nr, _, ncol) in pdefs}

        def plan_w(l, nm):
            nr, ncol = pinfo[nm]
            stream.add("%d_%s" % (l, nm), wloader(l, nm, nr, ncol), [WT[l][nm]])

        def kv_groups(j):
            nkeys = (j + 1) * T
            out = []
            g = 0
            while g * KG < nkeys:
                out.append((g, min(KG, nkeys - g * KG)))
                g += 1
            return out

        for l in range(depth):
            for j in range(NT):
                for nm in ("q", "k", "qi", "kiwi", "xr", "gr", "zu", "v", "zv"):
                    plan_w(l, nm)
                grps = kv_groups(j)
                for (g, wd) in grps:
                    last = (g == grps[-1][0])
                    stream.add("%d_kv%d_%d" % (l, j, g), kvloader(l, g, wd), [KVT[l][g]], hoist=not last)
                for n in (2, 1, 0):
                    plan_w(l, "gate%d_0" % n)
                    plan_w(l, "gate%d_1" % n)
                    plan_w(l, "br%d_0" % n)
                    plan_w(l, "br%d_1" % n)
                plan_w(l, "out_0")
                plan_w(l, "out_1")
                plan_w(l, "up_0")
                for g in range(8):
                    if g + 1 < 8:
                        plan_w(l, "up_%d" % (g + 1))
                    plan_w(l, "dn_%d" % g)
                plan_w(l, "ple")
                plan_w(l, "pg_0")
                plan_w(l, "pg_1")

        def wview(slot, nm):
            nr, ncol = pinfo[nm]
            kc = nr // 128
            return slot[:, 0:kc * ncol].rearrange("p (k w) -> p k w", k=kc)

        sm_i = [0]

        def sm():
            i = sm_i[0] % 32
            sm_i[0] += 1
            return small[:, i:i + 1]

        memset(epsc[:, 0:1], EPS, [epsc])
        memset(epsc[:, 1:2], math.pi / 2, [epsc])
        memset(epsc[:, 2:3], 1.0, [epsc])
        memset(epsc[:, 3:4], -BIGM, [epsc])
        memset(Vs[:, :, :], 1.0, [Vs])
        memset(onesb[:, :], 1.0, [onesb])

        def rstd_from_ss(ss_ap, n):
            a = sm()
            b = sm()
            act(a, ss_ap, AF.Sqrt, [small, epsc], [small], scale=1.0 / n, bias=epsc[:, 0:1])
            recip(b, a, [small], [small])
            return b

        pi = [0]

        def nps():
            pi[0] += 1
            return PS[pi[0] % 4]

        def norm_transpose(gcol0):
            for s in range(NSUB):
                if gcol0 is not None:
                    ss = sm()
                    act(tmpf[:, 0:512], xt[:, s, 0:512], AF.Square, [xt, small], [tmpf, small], accum_out=ss)
                    ss2 = sm()
                    act(tmpf[:, 0:512], xt[:, s, 512:1024], AF.Square, [xt, small], [tmpf, small], accum_out=ss2)
                    ss3 = sm()
                    tt(ss3, ss, ss2, ALU.add, [small], [small])
                    rs = rstd_from_ss(ss3, D)
                    ts(xs[:], xt[:, s, :], rs, None, ALU.mult, None, [xt, small], [xs])
                else:
                    cp(xs[:], xt[:, s, :], [xt], [xs])
                psb = PS[7][:].bitcast(BF16)
                for c in range(8):
                    tr(psb[:, c * 128:(c + 1) * 128], xs[:, c * 128:(c + 1) * 128], identb, [xs, cb], [PS[7]])
                src = psb[:, 0:1024].rearrange("p (c t) -> p c t", c=8)
                dst = hT[:, :, s * 128:(s + 1) * 128]
                if gcol0 is not None:
                    g_ap = cols[:, gcol0:gcol0 + 8].unsqueeze(2).to_broadcast([128, 8, 128])
                    tt(dst, src, g_ap, ALU.mult, [PS[7], cols], [hT])
                else:
                    cp(dst, src, [PS[7]], [hT])

        def post_norm_residual(banks, gcol, s):
            ssa = []
            for hf in range(2):
                a = sm()
                act(tmpf[:, 0:512], banks[hf][:, 0:512], AF.Square, [banks[hf], small], [tmpf, small], accum_out=a)
                ssa.append(a)
            s3 = sm()
            tt(s3, ssa[0], ssa[1], ALU.add, [small], [small])
            rs = rstd_from_ss(s3, D)
            for hf in range(2):
                stt(tmpf[:, 0:512], banks[hf][:, 0:512], rs, gcur[:, hf * 512:(hf + 1) * 512],
                    ALU.mult, ALU.mult, [banks[hf], small, gcur], [tmpf])
                tt(xt[:, s, hf * 512:(hf + 1) * 512], xt[:, s, hf * 512:(hf + 1) * 512], tmpf[:, 0:512], ALU.add,
                   [xt, tmpf], [xt])

        def merge_tile(dst, src, as_write=False):
            for a, b in ((dst.w, src.w), (dst.r, src.r)):
                for k_, v_ in b.items():
                    if a.get(k_, 0) < v_:
                        a[k_] = v_
            if as_write:
                for k_, v_ in src.r.items():
                    if dst.w.get(k_, 0) < v_:
                        dst.w[k_] = v_

        def fm_chunk(panel, pv, c, ps, M=128):
            for kc in range(8):
                mm(ps[0:M, 0:T], pv[:, kc, c * 128:c * 128 + M], hT[:, kc, :], kc == 0, kc == 7, [panel, hT], [ps])

        def rope_evac(ps, dst, W):
            act(xb16[:], ps[:, 0:T], AF.Copy, [ps], [xb16])
            mm(ps[:, T:2 * T], Rm, xb16[:], True, True, [cb, xb16], [ps])
            tt(rtmp[:, 0, :], ps[:, 0:T], cosT[:], ALU.mult, [ps, cosT], [rtmp])
            tt(rtmp[:, 1, :], ps[:, T:2 * T], sinT[:], ALU.mult, [ps, sinT], [rtmp])
            tt(dst, rtmp[:, 0, :], rtmp[:, 1, :], ALU.add, [rtmp], W)

        XTprev = None
        for l in range(depth):
            src_x = x_in if l == 0 else xbuf
            dst_x = y_out if l == depth - 1 else xbuf
            XT = [Tile("xd") for _ in range(NT)] if l < depth - 1 else None
            dma(cols[:], cols_in[l], [], [cols], psem[0])
            dma(gbv[:], gb_in[l, :, 3 * D:3 * D + 512], [], [gbv], psem[1])
            dma(gcur[:], gb_in[l, :, 0:D], [], [gcur], gsem)
            dma(bsp[:], bsp_in[l], [], [bsp], psem[2])
            cp(bsph[:, 0, :], bsp[:], [bsp], [bsph])
            tt(bsp[:], bsp[:], bsph[:, 0, :], ALU.subtract, [bsph], [bsp])
            cp(bsph[:, 1, :], bsp[:], [bsp], [bsph])
            dma(wbdf, wbd_in[l], [], [TR_], psem[3])
            cp(wbd[:], wbdf, [TR_], [wbd])
            dma(wbdf, wsp_in[l], [], [TR_], psem[4])
            tt(wspT[:], wbdf, tril01.unsqueeze(1).to_broadcast([128, 8, 128]), ALU.mult, [TR_, cf], [wspT])
            act(c8[:, 4:8], cols[:, 44:48], AF.Exp, [cols], [c8], scale=-1.0)
            ts(c8[:, 0:4], c8[:, 4:8], -0.25, 1.0 / 3.0, ALU.mult, ALU.add, [c8], [c8])
            tt(c8[:, 0:4], c8[:, 0:4], c8[:, 4:8], ALU.mult, [c8], [c8])
            ts(c8[:, 0:4], c8[:, 0:4], -1.0, 0.5, ALU.mult, ALU.add, [c8], [c8])
            tt(c8[:, 0:4], c8[:, 0:4], c8[:, 4:8], ALU.mult, [c8], [c8])
            ts(c8[:, 0:4], c8[:, 0:4], -1.0, 1.0, ALU.mult, ALU.add, [c8], [c8])
            tt(c8[:, 0:4], c8[:, 0:4], c8[:, 4:8], ALU.mult, [c8], [c8])
            ts(c8[:, 0:4], c8[:, 0:4], -8.0, None, ALU.mult, None, [c8], [c8])
            ts(c8[:, 4:8], c8[:, 0:4], 2.0, None, ALU.mult, None, [c8], [c8])
            memset(xrbuf[:, :, 0:3], 0.0, [xrbuf])
            memset(hst[:], 0.0, [hst])

            for j in range(NT):
                t0 = j * T
                first_tile = (l == 0 and j == 0)
                rd = [XTprev[j]] if (l > 0) else []
                dma(xt[:], src_x[t0:t0 + T, :].rearrange("(s p) d -> p s d", p=128), rd, [xt], xsem)
                dma(pt[:], p_in[l, t0:t0 + T, :].rearrange("(s p) d -> p s d", p=128), [], [pt], ptsem)
                dma(posi[:], pos_in[:, t0:t0 + T], [], [posi], possem)
                ang = rtmp[:, 0, :]
                u = rtmp[:, 1, :]
                rr = rtmp[:, 2, :]
                cp(u, posi[:], [posi], [rtmp])
                ts(ang, u, invf, None, ALU.mult, None, [rtmp, cf], [rtmp])
                ts(u, ang, 1.0 / (2 * math.pi), 12582912.0, ALU.mult, ALU.add, [rtmp], [rtmp])
                ts(u, u, -12582912.0, None, ALU.add, None, [rtmp], [rtmp])
                C1 = 6.28125
                C2 = float(np.float32(2 * math.pi - C1))
                C3 = float(2 * math.pi - C1 - C2)
                stt(rr, u, -C1, ang, ALU.mult, ALU.add, [rtmp], [rtmp])
                stt(rr, u, -C2, rr, ALU.mult, ALU.add, [rtmp], [rtmp])
                stt(rr, u, -C3, rr, ALU.mult, ALU.add, [rtmp], [rtmp])
                act(sinT[:], rr, AF.Sin, [rtmp], [sinT])
                stt(u, rr, -1.0, rr, ALU.mult, ALU.max, [rtmp], [rtmp])
                act(cosT[:], u, AF.Sin, [rtmp, epsc], [cosT], scale=-1.0, bias=epsc[:, 1:2])
                norm_transpose(0)
                if first_tile:
                    dump("hT", hT[:, 0, :], [hT])
                    dump("cosT", cosT[:], [cosT])
                    dump("sinT", sinT[:], [sinT])

                for nm, dstb in (("q", qT), ("k", KTs), ("qi", qiT)):
                    panel = stream.next("%d_%s" % (l, nm))
                    pv = wview(panel, nm)
                    pss = [nps() for _ in range(4)]
                    fm_chunk(panel, pv, 0, pss[0])
                    for c in range(4):
                        if c + 1 < 4:
                            fm_chunk(panel, pv, c + 1, pss[c + 1])
                        rope_evac(pss[c], dstb[:, c, :], [dstb])
                panel = stream.next("%d_kiwi" % l)
                pv = wview(panel, "kiwi")
                ps = nps()
                for half in range(2):
                    for kc in range(8):
                        mm(ps[half * 64:(half + 1) * 64, 0:T], pv[:, kc, 0:64], hT[:, kc, :], kc == 0, kc == 7,
                           [panel, hT], [ps])
                rope_evac(ps, kiT[:, t0:t0 + T], [kiT])
                for s in range(NSUB):
                    ps = nps()
                    for kc in range(8):
                        mm(ps[:, 0:8], hT[:, kc, s * 128:(s + 1) * 128], pv[:, kc, 64:72], kc == 0, kc == 7,
                           [panel, hT], [ps])
                    cp(wis[:, s, :], ps[:, 0:8], [ps], [wis])
                dma(KTc[l][:, :, t0:t0 + T].rearrange("c p w -> p c w"), KTs[:], [KTs], [KVT[l][t0 // KG]], kvsem, eng="gpsimd")
                if first_tile:
                    dump("qT", qT[:, 0, :], [qT])
                    dump("kiT", kiT[:, 0:T], [kiT])
                    dump("wis", wis[:, 0, :], [wis])
                panel = stream.next("%d_xr" % l)
                pv = wview(panel, "xr")
                for c in range(4):
                    ps = nps()
                    fm_chunk(panel, pv, c, ps)
                    act(xrbuf[:, c, 3:3 + T], ps[:, 0:T], AF.Copy, [ps], [xrbuf])
                for nm, dstb in (("gr", grT), ("zu", zuT)):
                    panel = stream.next("%d_%s" % (l, nm))
                    pv = wview(panel, nm)
                    for c in range(4):
                        ps = nps()
                        fm_chunk(panel, pv, c, ps)
                        act(dstb[:, c, :], ps[:, 0:T], AF.Gelu_apprx_tanh, [ps], [dstb])
                panel = stream.next("%d_v" % l)
                pv = wview(panel, "v")
                for s in range(NSUB):
                    ps = nps()
                    for kc in range(8):
                        mm(ps[:, 0:512], hT[:, kc, s * 128:(s + 1) * 128], pv[:, kc, :], kc == 0, kc == 7, [panel, hT], [ps])
                    dstv = Vs[:, s, :].rearrange("p (h f) -> p h f", h=8)[:, :, 0:64]
                    act(dstv, ps[:, 0:512].rearrange("p (h f) -> p h f", h=8), AF.Copy, [ps], [Vs])
                dma(Vc[l][t0:t0 + T, :].rearrange("(s p) f -> p s f", p=128), Vs[:], [Vs], [KVT[l][t0 // KG]], kvsem, eng="gpsimd")
                panel = stream.next("%d_zv" % l)
                pv = wview(panel, "zv")
                for s in range(NSUB):
                    ps = nps()
                    for kc in range(8):
                        mm(ps[:, 0:512], hT[:, kc, s * 128:(s + 1) * 128], pv[:, kc, :], kc == 0, kc == 7, [panel, hT], [ps])
                    act(gz[:], ps[:, 0:512], AF.Gelu_apprx_tanh, [ps], [gz])
                    ss = sm()
                    act(tmpf[:, 0:512], gz[:], AF.Square, [gz, small], [tmpf, small], accum_out=ss)
                    rs = rstd_from_ss(ss, 512)
                    stt(vn[:, s, :], gz[:], rs, gbv[:], ALU.mult, ALU.mult, [gz, small, gbv], [vn])

                mpi = [0]

                def mps():
                    mpi[0] += 1
                    return PS[5 + mpi[0] % 3]

                def gen_mixC():
                    for s in range(NSUB):
                        for cpair in range(4):
                            ps = mps()
                            for gg in range(2):
                                g = cpair * 2 + gg
                                mm(ps[gg * 64:(gg + 1) * 64, 0:128], vn[:, s, g * 64:(g + 1) * 64], wspT[:, g, :], True, False,
                                   [vn, wspT], [ps])
                            mm(ps[:, 0:128], esel[:, cpair, :], bsph[:, 0, :], False, False, [cb, bsph], [ps])
                            mm(ps[:, 0:128], esel[:, cpair, :], bsph[:, 1, :], False, True, [cb, bsph], [ps])
                            tt(ycT[:, cpair, s * 128:(s + 1) * 128], ps[:, 0:128], zuT[:, cpair, s * 128:(s + 1) * 128], ALU.mult,
                               [ps, zuT], [ycT])
                            yield

                def gen_mixB():
                    for c in range(4):
                        xc = lt[:, 0, :]
                        ts(xc, xrbuf[:, c, 0:T], cols[:, 16 + c:17 + c], cols[:, 32 + c:33 + c], ALU.mult, ALU.add, [xrbuf, cols], [lt])
                        for jj in range(1, 4):
                            stt(xc, xrbuf[:, c, jj:jj + T], cols[:, 16 + jj * 4 + c:17 + jj * 4 + c], xc, ALU.mult, ALU.add,
                                [xrbuf, cols, lt], [lt])
                        yield
                        cp(xrbuf[:, c, 0:3], xrbuf[:, c, T:T + 3], [xrbuf], [xrbuf])
                        act(xcb[:], xc, AF.Copy, [lt], [xcb])
                        ps = mps()
                        mm(ps[:, 0:T], wbd[:, c, :], xcb[:], True, True, [wbd, xcb], [ps])
                        mm(ps[:, T:2 * T], wbd[:, 4 + c, :], xcb[:], True, True, [wbd, xcb], [ps])
                        yield
                        rg = lt[:, 1, :]
                        ig = lt[:, 2, :]
                        av = lt[:, 3, :]
                        sq = lt[:, 4, :]
                        act(rg, ps[:, 0:T], AF.Sigmoid, [ps, cols], [lt], bias=cols[:, 36 + c:37 + c])
                        act(ig, ps[:, T:2 * T], AF.Sigmoid, [ps, cols], [lt], bias=cols[:, 40 + c:41 + c])
                        yield
                        act(av, rg, AF.Exp, [lt, c8], [lt], scale=c8[:, c:c + 1])
                        act(sq, rg, AF.Exp, [lt, c8], [lt], scale=c8[:, 4 + c:5 + c])
                        act(sq, sq, AF.Sqrt, [lt, epsc], [lt], scale=-1.0, bias=epsc[:, 2:3])
                        yield
                        tt(ig, ig, xc, ALU.mult, [lt], [lt])
                        tt(ig, ig, sq, ALU.mult, [lt], [lt])
                        hh = lt[:, 1, :]
                        scan(hh, av, ig, hst[:, c:c + 1], [lt, hst], [lt])
                        yield
                        cp(hst[:, c:c + 1], hh[:, T - 1:T], [lt], [hst])
                        tt(ybT[:, c, :], hh, grT[:, c, :], ALU.mult, [lt, grT], [ybT])
                        yield

                grps = kv_groups(j)
                nkeys = (j + 1) * T
                TRh = [Tile("relu%d" % h) for h in range(8)]

                SC = [(score_ap, TS_), (sc1[:, 0:L], sc1.T)]

                def gen_I(s):
                    N = t0 + 128 * (s + 1)
                    score_s, TSs = SC[s]
                    for h in range(8):
                        merge_tile(TRh[h], TR_, as_write=True)
                        act(diag[:, h, :], identf, AF.Copy, [cf, wis], [diag], scale=wis[:, s, h:h + 1])
                    yield
                    ngrp = (N + KG - 1) // KG
                    for kg in range(ngrp):
                        wd = min(KG, N - kg * KG)
                        for h in range(8):
                            ps = PS[h % 4]
                            r0 = (h % 2) * 64
                            mm(ps[:, 0:wd], qiT[r0:r0 + 64, h // 2, s * 128:(s + 1) * 128],
                               kiT[r0:r0 + 64, kg * KG:kg * KG + wd], True, True, [qiT, kiT], [ps])
                            if h % 2 == 0:
                                act(Rrelu[:, h, 0:wd], ps[:, 0:wd], AF.Relu, [ps], [TRh[h]])
                            else:
                                ts(Rrelu[:, h, 0:wd], ps[:, 0:wd], 0.0, None, ALU.max, None, [ps], [TRh[h]])
                            if h % 2 == 1:
                                yield
                        for h in range(8):
                            mm(PS[4][:, 0:wd], diag[:, h, :], Rrelu[:, h, 0:wd], h == 0, h == 7, [diag, TRh[h]], [PS[4]])
                        if kg == ngrp - 1:
                            if wd > 128:
                                cp(score_s[:, kg * KG:kg * KG + wd - 128], PS[4][:, 0:wd - 128], [PS[4]], [TSs])
                            tt(score_s[:, N - 128:N], PS[4][:, wd - 128:wd], negmask, ALU.add, [PS[4], cf], [TSs])
                            tt(tmpf[:, 0:128], PS[4][:, wd - 128:wd], posfill, ALU.add, [PS[4], cf], [tmpf])
                        else:
                            cp(score_s[:, kg * KG:kg * KG + wd], PS[4][:, 0:wd], [PS[4]], [TSs])
                        yield
                    for h in range(8):
                        merge_tile(TR_, TRh[h])

                def gen_B(s):
                    N = t0 + 128 * (s + 1)
                    score_s, TSs = SC[s]
                    hi0 = bis[:, 0:1]
                    lo = bis[:, 1:2]
                    w0 = bis[:, 2:3]
                    reduce(hi0, score_s[:, 0:N], ALU.max, [TSs, bis], [bis])
                    reduce(lo, tmpf[:, 0:128], ALU.min, [tmpf, bis], [bis])
                    if N > 128:
                        m1 = bis[:, 3:4]
                        reduce(m1, score_s[:, 0:N - 128], ALU.min, [TSs, bis], [bis])
                        tt(lo, lo, m1, ALU.min, [bis], [bis])
                    tt(w0, hi0, lo, ALU.subtract, [bis], [bis])
                    yield
                    mk = masks[s]
                    if N > TOPK:
                        Nd = ((N // 2 + 127) // 128) * 128
                        Na = N - Nd
                        if s == 0:
                            junkA, TJ = masks[1], masks[1].T
                        else:
                            junkA, TJ = big[:, 4096:6144].bitcast(BF16), TR_
                        for it in range(NBIS):
                            k4 = it % 4
                            mid = bis2[:, k4:k4 + 1]
                            cnt = bis2[:, 4 + k4:5 + k4]
                            vv = bis2[:, 8 + k4:9 + k4]
                            sA = bis2[:, 12 + k4:13 + k4]
                            stt(mid, w0, 0.5 ** (it + 1), lo, ALU.mult, ALU.add, [bis], [Tmid])
                            act(junkA[:, 0:Na], score_s[:, Nd:N], AF.Sign, [TSs, Tmid], [TJ, Tsa], scale=-1.0, bias=mid,
                                accum_out=sA)
                            count_ge(mk[:, 0:Nd], score_s[:, 0:Nd], mid, cnt, [TSs, Tmid], [mk, Tcnt])
                            stt(vv, cnt, 2.0, sA, ALU.mult, ALU.subtract, [Tcnt, Tsa], [Tcnt])
                            ge = smalli[:, k4:k4 + 1]
                            ts(ge, vv, float(2 * TOPK - Na), None, ALU.is_ge, None, [Tcnt], [smalli])
                            cpred(lo, ge, mid, [Tmid, smalli], [bis])
                            yield
                    ts(mk[:, 0:N], score_s[:, 0:N], lo, None, ALU.is_ge, None, [TSs, bis], [mk])
                    if N < nkeys:
                        memset(mk[:, N:nkeys], 0.0, [mk], eng="gpsimd")
                    yield

                def chain(*gens):
                    for g_ in gens:
                        yield from g_

                def run(*gens):
                    live = list(gens)
                    while live:
                        for g_ in list(live):
                            try:
                                next(g_)
                            except StopIteration:
                                live.remove(g_)

                def run_w(ga, gb_, kb):
                    la = lb = True
                    while la or lb:
                        if la:
                            try:
                                next(ga)
                            except StopIteration:
                                la = False
                        for _ in range(kb):
                            if lb:
                                try:
                                    next(gb_)
                                except StopIteration:
                                    lb = False

                run(chain(gen_mixC(), gen_mixB()), gen_I(0))
                if first_tile:
                    dump("ycT", ycT[:, 0, :], [ycT])
                    dump("ybT", ybT[:, 0, :], [ybT])
                    dump("score", score_ap[:, 0:128], [TS_])
                N1 = t0 + 256
                lenI = 1 + ((N1 + KG - 1) // KG) * 5
                lenB = 2 + (NBIS if (t0 + 128) > TOPK else 0)
                run_w(gen_B(0), gen_I(1), max(1, -(-lenI // lenB)))
                run(gen_B(1))

                first = True
                ucnt = [0]
                for gi, (g, wd) in enumerate(grps):
                    panel = stream.next("%d_kv%d_%d" % (l, j, g))
                    nb = wd // 128
                    KTv = panel[:, 0:4 * wd].rearrange("p (c w) -> p c w", c=4)
                    Vv = panel[:, 2048:2048 + nb * 520].rearrange("p (b f) -> p b f", b=nb)
                    psb = PS[6][:].bitcast(BF16)
                    for s in range(NSUB):
                        for b in range(nb):
                            tr(psb[:, (s * 4 + b) * 128:(s * 4 + b + 1) * 128],
                               masks[s][:, g * KG + b * 128:g * KG + (b + 1) * 128], identb, [masks[s], cb], [PS[6]])
                    for s in range(NSUB):
                        act(maskTk[:, 0:nb, s * 128:(s + 1) * 128],
                            psb[:, s * 512:s * 512 + nb * 128].rearrange("p (b t) -> p b t", b=nb),
                            AF.Copy, [PS[6]], [maskTk])
                    units = [(h, bp) for h in range(8) for bp in range(0, nb, 2)]

                    def stageA(i):
                        h, bp = units[i]
                        r0 = (h % 2) * 64
                        nbb = min(2, nb - bp)
                        ps = PS[(ucnt[0] + i) % 4]
                        for b in range(bp, bp + nbb):
                            mm(ps[:, (b - bp) * T:(b - bp + 1) * T], KTv[r0:r0 + 64, h // 2, b * 128:(b + 1) * 128],
                               qT[r0:r0 + 64, h // 2, :], True, True, [panel, qT], [ps])

                    def stageB(i):
                        h, bp = units[i]
                        nbb = min(2, nb - bp)
                        ps = PS[(ucnt[0] + i) % 4]
                        ptile = PT[(ucnt[0] + i) % len(PT)]
                        act(ptile[:, 0:nbb, :], ps[:, 0:nbb * T].rearrange("p (b t) -> p b t", b=nbb), AF.Exp,
                            [ps], [ptile], scale=HD ** -0.5)
                        tt(ptile[:, 0:nbb, :], ptile[:, 0:nbb, :], maskTk[:, bp:bp + nbb, :], ALU.mult, [ptile, maskTk], [ptile],
                           eng=("vector" if (ucnt[0] + i) % 2 == 0 else "gpsimd"))

                    def stageD(i):
                        h, bp = units[i]
                        nbb = min(2, nb - bp)
                        pacc = PS[4 + (h % 2)]
                        ptile = PT[(ucnt[0] + i) % len(PT)]
                        for b in range(bp, bp + nbb):
                            mm(pacc[0:65, 0:T], Vv[:, b, h * 65:(h + 1) * 65], ptile[:, b - bp, :], b == 0, b == nb - 1,
                               [panel, ptile], [pacc])
                        if bp + 2 >= nb:
                            if first:
                                cp(acc[:, h, :], pacc[0:65, 0:T], [pacc], [acc])
                            else:
                                tt(acc[:, h, :], acc[:, h, :], pacc[0:65, 0:T], ALU.add, [pacc, acc], [acc])

                    stageA(0)
                    if len(units) > 1:
                        stageA(1)
                    for i in range(len(units)):
                        if i + 2 < len(units):
                            stageA(i + 2)
                        stageB(i)
                        stageD(i)
                    ucnt[0] += len(units)
                    first = False
                accf = accm[0:65, :]
                act(accf[64:65, :], accf[64:65, :], AF.Ln, [acc], [acc])
                act(accf[64:65, :], accf[64:65, :], AF.Exp, [acc], [acc], scale=-1.0)
                for h in range(8):
                    rhl = rhl2[h % 2]
                    cp(rhl[64:65, 0, :], acc[64:65, h, :], [acc], [rhl])
                    tt(rhl[64:65, 1, :], acc[64:65, h, :], rhl[64:65, 0, :], ALU.subtract, [acc, rhl], [rhl])
                    ps = nps()
                    mm(ps[0:64, 0:T], onesb[64:65, 0:64], rhl[64:65, 0, :], True, False, [onesb, rhl], [ps])
                    mm(ps[0:64, 0:T], onesb[64:65, 0:64], rhl[64:65, 1, :], False, True, [onesb, rhl], [ps])
                    if h % 2 == 0:
                        tt(yaT[0:64, h // 2, :], acc[0:64, h, :], ps[0:64, 0:T], ALU.mult, [acc, ps], [yaT])
                    else:
                        tt(yaTt[:, :], acc[0:64, h, :], ps[0:64, 0:T], ALU.mult, [acc, ps], [yaTt])
                        ps2 = nps()
                        mm(ps2[64:128, 0:T], identb[0:64, 0:64], yaTt[:, :], True, True, [cb, yaTt], [ps2])
                        cp(yaT[64:128, h // 2, :], ps2[64:128, 0:T], [ps2], [yaT])
                if first_tile:
                    dump("yaT", yaT[:, 0, :], [yaT])
                if l == 0 and j == 1:
                    dump("yaT1", yaT[:, 0, :], [yaT])
                    dump("acc1", acc[:, 0, :], [acc])

                for ni, n in enumerate((2, 1, 0)):
                    for hf in range(2):
                        gpan = stream.next("%d_gate%d_%d" % (l, n, hf))
                        pv = wview(gpan, "gate0_0")
                        for c in range(4):
                            ps = nps()
                            fm_chunk(gpan, pv, c, ps)
                            act(gsig[:, hf * 4 + c, :], ps[:, 0:T], AF.Sigmoid, [ps], [gsig])
                    ysrc = {2: ycT, 1: ybT, 0: yaT}[n]
                    bp_ = [None, None]
                    for hf in range(2):
                        bp_[hf] = stream.next("%d_br%d_%d" % (l, n, hf))
                        pv = wview(bp_[hf], "br0_0")
                        for c in range(4):
                            dchunk = hf * 4 + c
                            ps = nps()
                            for kc in range(4):
                                mm(ps[:, 0:T], pv[:, kc, c * 128:(c + 1) * 128], ysrc[:, kc, :], kc == 0, kc == 3,
                                   [bp_[hf], ysrc], [ps])
                            if ni == 0:
                                tt(merged[:, dchunk, :], ps[:, 0:T], gsig[:, dchunk, :], ALU.mult, [ps, gsig], [TS_])
                            else:
                                tt(gt[:], ps[:, 0:T], gsig[:, dchunk, :], ALU.mult, [ps, gsig], [gt])
                                dsto = mergedT if ni == 2 else merged
                                tt(dsto[:, dchunk, :], merged[:, dchunk, :], gt[:], ALU.add, [TS_, gt], [TS_], eng="gpsimd")
                op_ = [stream.next("%d_out_%d" % (l, hf), look=NSLOT - 1 - hf) for hf in range(2)]
                for s in range(NSUB):
                    banks = [PS[4 + 2 * (s % 2)], PS[5 + 2 * (s % 2)]]
                    for hf in range(2):
                        pv = wview(op_[hf], "out_0")
                        for kc in range(8):
                            mm(banks[hf][:, 0:512], mergedT[:, kc, s * 128:(s + 1) * 128], pv[:, kc, :], kc == 0, kc == 7,
                               [op_[hf], TS_], [banks[hf]])
                    post_norm_residual(banks, 0, s)
                dma(gcur[:], gb_in[l, :, D:2 * D], [], [gcur], gsem, eng="gpsimd")
                if first_tile:
                    dump("x1", xt[:, 0, :], [xt])
                norm_transpose(8)
                dbanks = [[PS[4], PS[5]], [PS[6], PS[7]]]
                TF = [Tile("fT0"), Tile("fT1")]
                Trl = [Tile("rl0"), Tile("rl1")]

                for i_ in range(2):
                    merge_tile(TF[i_], TS_, as_write=True)
                    merge_tile(Trl[i_], TR_, as_write=True)

                def ffn_up(g):
                    up = stream.next("%d_up_%d" % (l, g))
                    pv = wview(up, "up_0")
                    fTg = fT[g % 2]
                    for c in range(4):
                        ps = nps()
                        fm_chunk(up, pv, c, ps)
                        rlb = rl[c % 2]
                        act(rlb, ps[:, 0:T], AF.Relu, [ps], [Trl[c % 2]])
                        tt(fTg[:, c, :], rlb, rlb, ALU.mult, [Trl[c % 2]], [TF[g % 2]], eng="gpsimd")

                def ffn_dn(g):
                    dn = stream.next("%d_dn_%d" % (l, g))
                    dv = wview(dn, "dn_0")
                    fTg = fT[g % 2]
                    for s in range(NSUB):
                        for hf in range(2):
                            for c in range(4):
                                mm(dbanks[s][hf][:, 0:512], fTg[:, c, s * 128:(s + 1) * 128], dv[:, c, hf * 512:(hf + 1) * 512],
                                   g == 0 and c == 0, g == 7 and c == 3, [dn, TF[g % 2]], [dbanks[s][hf]])

                ffn_up(0)
                for g in range(8):
                    if g + 1 < 8:
                        ffn_up(g + 1)
                    ffn_dn(g)
                for i_ in range(2):
                    merge_tile(TS_, TF[i_])
                    merge_tile(TR_, Trl[i_])
                for s in range(NSUB):
                    post_norm_residual(dbanks[s], D, s)
                dma(gcur[:], gb_in[l, :, 2 * D:3 * D], [], [gcur], gsem, eng="gpsimd")
                if first_tile:
                    dump("x2", xt[:, 0, :], [xt])
                norm_transpose(None)
                for s in range(NSUB):
                    cp(ptb[:, s, :], pt[:, s, :], [pt], [ptb])
                    psb = PS[3][:].bitcast(BF16)
                    for c in range(2):
                        tr(psb[:, c * 128:(c + 1) * 128], ptb[:, s, c * 128:(c + 1) * 128], identb, [ptb, cb], [PS[3]])
                    cp(pT[:, :, s * 128:(s + 1) * 128], psb[:, 0:256].rearrange("p (c t) -> p c t", c=2), [PS[3]], [pT])
                plp = stream.next("%d_ple" % l)
                plv = wview(plp, "ple")
                pg = [stream.next("%d_pg_%d" % (l, hf), look=NSLOT - 2 - hf) for hf in range(2)]
                for s in range(NSUB):
                    ssp = []
                    for hf in range(2):
                        pa = PS[4 * (s % 2) + hf]
                        pgb = PS[4 * (s % 2) + 2 + hf]
                        for kc in range(2):
                            mm(pa[:, 0:512], pT[:, kc, s * 128:(s + 1) * 128], plv[:, kc, hf * 512:(hf + 1) * 512], kc == 0, kc == 1,
                               [plp, pT], [pa])
                        gv = wview(pg[hf], "pg_0")
                        for kc in range(8):
                            mm(pgb[:, 0:512], hT[:, kc, s * 128:(s + 1) * 128], gv[:, kc, :], kc == 0, kc == 7, [pg[hf], hT], [pgb])
                        act(sg, pgb[:, 0:512], AF.Sigmoid, [pgb], [TR_])
                        tt(ple_t[:, hf * 512:(hf + 1) * 512], pa[:, 0:512], sg, ALU.mult, [pa, TR_], [TR_])
                        a = sm()
                        act(tmpf[:, 0:512], ple_t[:, hf * 512:(hf + 1) * 512], AF.Square, [TR_, small], [tmpf, small], accum_out=a)
                        ssp.append(a)
                    s3 = sm()
                    tt(s3, ssp[0], ssp[1], ALU.add, [small], [small])
                    rs = rstd_from_ss(s3, D)
                    for hf in range(2):
                        stt(tmpf[:, 0:512], ple_t[:, hf * 512:(hf + 1) * 512], rs, gcur[:, hf * 512:(hf + 1) * 512],
                            ALU.mult, ALU.mult, [TR_, small, gcur], [tmpf])
                        tt(xt[:, s, hf * 512:(hf + 1) * 512], xt[:, s, hf * 512:(hf + 1) * 512], tmpf[:, 0:512], ALU.add,
                           [xt, tmpf], [xt])
                if j + 1 < NT:
                    dma(gcur[:], gb_in[l, :, 0:D], [], [gcur], gsem, eng="gpsimd")
                wt = [XT[j]] if XT is not None else [out_tile]
                dma(dst_x[t0:t0 + T, :].rearrange("(s p) d -> p s d", p=128), xt[:], [xt], wt, ssem, eng="gpsimd")
            XTprev = XT

        fw.finish("sync", [out_tile] + dbg_tiles)
        fw.emit()
    return nc, fw


def _consts():
    bf = ml_dtypes.bfloat16
    ident = np.eye(128, dtype=np.float32)
    Rm = np.zeros((128, 128), np.float32)
    for base in (0, 64):
        for d in range(8):
            Rm[base + d + 8, base + d] = -1.0
            Rm[base + d, base + d + 8] = 1.0
    esel = np.zeros((128, 4, 128), np.float32)
    for g in range(8):
        esel[g, g // 2, (g % 2) * 64:(g % 2 + 1) * 64] = 1.0
    cb = np.concatenate([ident, Rm, esel.reshape(128, 512)], axis=1).astype(bf)
    tt_, ss_ = np.meshgrid(np.arange(128), np.arange(128), indexing="ij")
    negmask = np.where(ss_ > tt_, np.float32(NEG), np.float32(0.0))
    posfill = np.where(ss_ > tt_, np.float32(-2 * NEG), np.float32(0.0))
    tril01 = (tt_ <= ss_).astype(np.float32)
    half = 8
    inv_freq = (np.float32(500000.0) ** (-np.arange(half, dtype=np.float32) * np.float32(2.0) / np.float32(16))).astype(np.float32)
    invf = np.zeros((128, 1), np.float32)
    for f in range(128):
        d = f % 64
        if d < 16:
            invf[f, 0] = inv_freq[d % 8]
    cf = np.concatenate([ident, negmask, posfill, tril01, invf], axis=1).astype(np.float32)
    return cb, cf


def _layout_params(inp, depth):
    f = np.float32
    cols = np.zeros((depth, 128, 48), f)
    gb = np.zeros((depth, 128, 3 * D + 512), f)
    wbd = np.zeros((depth, 128, 8, 128), f)
    wsp = np.zeros((depth, 128, 8, 128), f)
    for l in range(depth):
        cols[l, :, 0:8] = np.asarray(inp["g_pre_mix"][l], f).reshape(8, 128).T
        cols[l, :, 8:16] = np.asarray(inp["g_pre_ffn"][l], f).reshape(8, 128).T
        cw = np.asarray(inp["conv_w"][l], f)
        for jj in range(4):
            cols[l, :, 16 + jj * 4:20 + jj * 4] = cw[jj].reshape(4, 128).T
        cols[l, :, 32:36] = np.asarray(inp["conv_b"][l], f).reshape(4, 128).T
        cols[l, :, 36:40] = np.asarray(inp["b_rg_a"][l], f).reshape(4, 128).T
        cols[l, :, 40:44] = np.asarray(inp["b_rg_x"][l], f).reshape(4, 128).T
        cols[l, :, 44:48] = np.asarray(inp["lru_lambda"][l], f).reshape(4, 128).T
        row = np.concatenate([np.asarray(inp["g_post_mix"][l], f), np.asarray(inp["g_post_ffn"][l], f),
                              np.asarray(inp["g_post_ple"][l], f), np.asarray(inp["g_gmlp_v"][l], f)])
        gb[l] = np.broadcast_to(row[None, :], (128, row.size))
        for gi, key in enumerate(("w_rg_a", "w_rg_x")):
            w = np.asarray(inp[key][l], f)
            for c in range(4):
                for hh in range(2):
                    wbd[l, hh * 64:(hh + 1) * 64, gi * 4 + c, hh * 64:(hh + 1) * 64] = w[c * 2 + hh]
        ws = np.asarray(inp["w_spatial"][l], f)
        wsp[l] = np.transpose(ws, (2, 0, 1))
    bsp = np.ascontiguousarray(np.asarray(inp["b_spatial"], f))
    return cols, gb, wbd, wsp, bsp


_CACHE = {}


def kernel(**inputs):
    depth = DEPTH
    x = np.asarray(inputs["x"], np.float32)
    B, L, _ = x.shape
    p = np.asarray(inputs["p"], np.float32)
    pos = np.asarray(inputs["positions"], np.int32)
    cb, cf = _consts()
    cols, gb, wbd, wsp, bsp = _layout_params(inputs, depth)
    shared = {
        "w_in": np.ascontiguousarray(np.asarray(inputs["w_in"], np.float32)),
        "w_branch": np.ascontiguousarray(np.asarray(inputs["w_branch"], np.float32)),
        "w_out": np.ascontiguousarray(np.asarray(inputs["w_out"], np.float32)),
        "w_ffn_up": np.ascontiguousarray(np.asarray(inputs["w_ffn_up"], np.float32)),
        "w_ffn_down": np.ascontiguousarray(np.asarray(inputs["w_ffn_down"], np.float32)),
        "w_ple": np.ascontiguousarray(np.asarray(inputs["w_ple"], np.float32)),
        "w_ple_gate": np.ascontiguousarray(np.asarray(inputs["w_ple_gate"], np.float32)),
        "cols": cols, "gb": gb, "wbd": wbd, "wsp": wsp, "bsp": bsp, "cb": cb, "cf": cf,
    }
    if L not in _CACHE:
        _CACHE[L] = build_program(L, depth)[0]
    nc = _CACHE[L]
    in_maps = []
    for b in range(B):
        m = dict(shared)
        m["x"] = np.ascontiguousarray(x[b])
        m["p"] = np.ascontiguousarray(p[:, b])
        m["pos"] = np.ascontiguousarray(np.broadcast_to(pos[b][None, :], (128, L)))
        in_maps.append(m)
    res = run_bass_kernel_spmd(nc, in_maps, core_ids=list(range(B)))
    return np.stack([np.asarray(r["y"], np.float32) for r in res.results], axis=0)
```

```python
import math
from contextlib import ExitStack
import numpy as np
import ml_dtypes
import concourse.bass as bass
import concourse.mybir as mybir
from concourse.bass_utils import run_bass_kernel_spmd

F32 = mybir.dt.float32
BF16 = mybir.dt.bfloat16
I32 = mybir.dt.int32
AF = mybir.ActivationFunctionType
ALU = mybir.AluOpType
AX = mybir.AxisListType

D = 1024
NH = 8
HD = 64
TOPK = 256
FFN = 4096
PLE = 256
EPS = 1e-6
DEPTH = 2
T = 256
NSUB = T // 128
KG = 512
NSLOT = 3
SLOTB = 4224
NBIS = 13
BIGM = 30000.0
NEG = -1.0e30
IN_OFF = dict(q=0, k=512, v=1024, qi=1536, kiwi=2048, xr=2120, gr=2632, zu=3144, zv=3656, gate=4168)


class Sem:
    def __init__(self, h, name):
        self.h = h
        self.n = 0
        self.name = name


class Tile:
    __slots__ = ("w", "r", "name")

    def __init__(self, name=""):
        self.w = {}
        self.r = {}
        self.name = name


class Buf:
    def __init__(self, t, name=""):
        self.t = t
        self.T = Tile(name)

    def __getitem__(self, k):
        return self.t[k]


class Engine:
    def __init__(self, name, sem):
        self.name = name
        self.sem = sem
        self.ops = []
        self.seen = {}


class FW:
    def __init__(self, nc, stack):
        self.nc = nc
        self.stack = stack
        self.engs = {}
        for n in ("tensor", "vector", "scalar", "gpsimd", "sync"):
            s = Sem(stack.enter_context(nc.semaphore("sem_" + n)), n)
            self.engs[n] = Engine(n, s)
        self.nops = 0

    def dsem(self, name):
        return Sem(self.stack.enter_context(self.nc.semaphore("dsem_" + name)), name)

    def op(self, eng, fn, reads=(), writes=(), dsem=None):
        E = self.engs[eng]
        need = {}
        for b in reads:
            t = b.T if isinstance(b, Buf) else b
            for s, v in t.w.items():
                if need.get(s, 0) < v:
                    need[s] = v
        for b in writes:
            t = b.T if isinstance(b, Buf) else b
            for s, v in t.w.items():
                if need.get(s, 0) < v:
                    need[s] = v
            for s, v in t.r.items():
                if need.get(s, 0) < v:
                    need[s] = v
        raw_self = 0
        for b in reads:
            t = b.T if isinstance(b, Buf) else b
            raw_self = max(raw_self, t.w.get(E.sem, 0))
        waits = []
        for s, v in need.items():
            if s is E.sem:
                if eng != "tensor" and raw_self > E.seen.get(s, 0):
                    E.seen[s] = raw_self
                    waits.append((s, raw_self))
                continue
            if E.seen.get(s, 0) >= v:
                continue
            E.seen[s] = v
            waits.append((s, v))
        if dsem is not None:
            dsem.n += 16
            sig = (dsem, dsem.n, 16)
        else:
            E.sem.n += 1
            sig = (E.sem, E.sem.n, 1)
        E.ops.append((waits, fn, sig))
        self.nops += 1
        s, v = sig[0], sig[1]
        for b in reads:
            t = b.T if isinstance(b, Buf) else b
            if t.r.get(s, 0) < v:
                t.r[s] = v
        for b in writes:
            t = b.T if isinstance(b, Buf) else b
            if t.w.get(s, 0) < v:
                t.w[s] = v

    def finish(self, eng, tiles):
        E = self.engs[eng]
        need = {}
        for b in tiles:
            t = b.T if isinstance(b, Buf) else b
            for d in (t.w, t.r):
                for s, v in d.items():
                    if need.get(s, 0) < v:
                        need[s] = v
        E.ops.append(([(s, v) for s, v in need.items() if s is not E.sem], None, None))

    def emit(self):
        with self.nc.Block() as block:
            for n, E in self.engs.items():
                def body(e, E=E):
                    for waits, fn, sig in E.ops:
                        for s, v in waits:
                            e.wait_ge(s.h, v)
                        if fn is None:
                            continue
                        fn(e).then_inc(sig[0].h, sig[2])
                getattr(block, n)(body)


def panel_defs():
    P = []
    for nm in ("q", "k", "qi"):
        P.append((nm, "w_in", 0, 1024, IN_OFF[nm], 512))
    P.append(("kiwi", "w_in", 0, 1024, IN_OFF["kiwi"], 72))
    for nm in ("xr", "gr", "zu", "v", "zv"):
        P.append((nm, "w_in", 0, 1024, IN_OFF[nm], 512))
    for n in range(3):
        for hf in range(2):
            P.append(("gate%d_%d" % (n, hf), "w_in", 0, 1024, IN_OFF["gate"] + n * 1024 + hf * 512, 512))
    for n in range(3):
        for hf in range(2):
            P.append(("br%d_%d" % (n, hf), "w_branch%d" % n, 0, 512, hf * 512, 512))
    for hf in range(2):
        P.append(("out_%d" % hf, "w_out", 0, 1024, hf * 512, 512))
    for g in range(8):
        P.append(("up_%d" % g, "w_ffn_up", 0, 1024, g * 512, 512))
        P.append(("dn_%d" % g, "w_ffn_down", g * 512, 512, 0, 1024))
    P.append(("ple", "w_ple", 0, 256, 0, 1024))
    for hf in range(2):
        P.append(("pg_%d" % hf, "w_ple_gate", 0, 1024, hf * 512, 512))
    return P


def build_program(L, depth=DEPTH, dbg=None):
    NT = L // T
    nc = bass.Bass("TRN2", target_bir_lowering=False)
    dram = lambda name, shape, dt, kind="ExternalInput": nc.dram_tensor(name, shape, dt, kind=kind).ap()
    x_in = dram("x", [L, D], F32)
    p_in = dram("p", [depth, L, PLE], F32)
    pos_in = dram("pos", [128, L], I32)
    wsrc = {
        "w_in": dram("w_in", [depth, D, 7240], F32),
        "w_branch": dram("w_branch", [depth, 3, 512, D], F32),
        "w_out": dram("w_out", [depth, D, D], F32),
        "w_ffn_up": dram("w_ffn_up", [depth, D, FFN], F32),
        "w_ffn_down": dram("w_ffn_down", [depth, FFN, D], F32),
        "w_ple": dram("w_ple", [depth, PLE, D], F32),
        "w_ple_gate": dram("w_ple_gate", [depth, D, D], F32),
    }
    cols_in = dram("cols", [depth, 128, 48], F32)
    gb_in = dram("gb", [depth, 128, 3 * D + 512], F32)
    wbd_in = dram("wbd", [depth, 128, 8, 128], F32)
    wsp_in = dram("wsp", [depth, 128, 8, 128], F32)
    bsp_in = dram("bsp", [depth, 8, 128], F32)
    cb_in = dram("cb", [128, 256 + 512], BF16)
    cf_in = dram("cf", [128, 4 * 128 + 1], F32)
    y_out = dram("y", [L, D], F32, kind="ExternalOutput")
    xbuf = dram("xbuf", [L, D], F32, kind="Internal")
    KTc = [dram("ktc%d" % l, [4, 128, L], BF16, kind="Internal") for l in range(depth)]
    Vc = [dram("vc%d" % l, [L, 520], BF16, kind="Internal") for l in range(depth)]
    pdefs = panel_defs()
    Wp = [{nm: dram("wp%d_%s" % (l, nm), [nr, ncol], BF16, kind="Internal") for (nm, _, _, nr, _, ncol) in pdefs}
          for l in range(depth)]
    dbg_out = {}
    if dbg:
        for k, shp in dbg.items():
            dbg_out[k] = dram("dbg_" + k, list(shp), F32, kind="ExternalOutput")

    with ExitStack() as st:
        fw = FW(nc, st)
        op = fw.op

        def sb(name, shape, dt):
            return Buf(st.enter_context(nc.sbuf_tensor("s_" + name, shape, dt)), name)

        PS = [Buf(st.enter_context(nc.psum_tensor("ps%d" % i, [128, 512], F32)), "ps%d" % i) for i in range(8)]

        def mm(out, lhsT, rhs, start, stop, R, W):
            op("tensor", lambda e: e.matmul(out, lhsT=lhsT, rhs=rhs, start=start, stop=stop), R, W)

        def tr(out, in_, ident, R, W):
            op("tensor", lambda e: e.transpose(out=out, in_=in_, identity=ident), R, W)

        def act(out, in_, func, R, W, **kw):
            op("scalar", lambda e: e.activation(out=out, in_=in_, func=func, **kw), R, W)

        def ts(out, in0, s1, s2, op0, op1, R, W, eng="vector"):
            if op1 is None:
                op(eng, lambda e: e.tensor_scalar(out=out, in0=in0, scalar1=s1, scalar2=None, op0=op0), R, W)
            else:
                op(eng, lambda e: e.tensor_scalar(out=out, in0=in0, scalar1=s1, scalar2=s2, op0=op0, op1=op1), R, W)

        def tt(out, in0, in1, o, R, W, eng="vector"):
            op(eng, lambda e: e.tensor_tensor(out=out, in0=in0, in1=in1, op=o), R, W)

        def stt(out, in0, s, in1, op0, op1, R, W):
            op("vector", lambda e: e.scalar_tensor_tensor(out=out, in0=in0, scalar=s, in1=in1, op0=op0, op1=op1), R, W)

        def cp(out, in_, R, W, eng="vector"):
            op(eng, lambda e: e.tensor_copy(out=out, in_=in_), R, W)

        def dma(out, in_, R, W, ds, eng="sync"):
            op(eng, lambda e: e.dma_start(out=out, in_=in_), R, W, dsem=ds)

        def memset(ap, val, W, eng="vector"):
            op(eng, lambda e: e.memset(ap, val), [], W)

        def reduce(out, in_, o, R, W):
            op("vector", lambda e: e.tensor_reduce(out=out, in_=in_, axis=AX.X, op=o), R, W)

        def recip(out, in_, R, W):
            op("vector", lambda e: e.reciprocal(out=out, in_=in_), R, W)

        def scan(out, d0, d1, init, R, W):
            op("vector", lambda e: e.tensor_tensor_scan(out=out, data0=d0, data1=d1, initial=init,
                                                        op0=ALU.mult, op1=ALU.add), R, W)

        def count_ge(out, in0, thr, cnt, R, W):
            op("vector", lambda e: e.tensor_scalar(out=out, in0=in0, scalar1=thr, scalar2=None, op0=ALU.is_ge,
                                                   op1=ALU.add, accum_out=cnt), R, W)

        def cpred(out, mask, data, R, W):
            op("vector", lambda e: e.copy_predicated(out=out, mask=mask, data=data), R, W)

        dbg_sem = fw.dsem("dbg")
        dbg_tiles = []

        def dump(name, ap, R):
            if name in dbg_out:
                t = Tile("dbg")
                dma(dbg_out[name], ap, R, [t], dbg_sem, eng="gpsimd")
                dbg_tiles.append(t)
                del dbg_out[name]

        cb = sb("cb", [128, 768], BF16)
        cf = sb("cf", [128, 513], F32)
        dma(cb[:], cb_in, [], [cb], fw.dsem("c0"))
        dma(cf[:], cf_in, [], [cf], fw.dsem("c1"))
        identb = cb[:, 0:128]
        Rm = cb[:, 128:256]
        esel = cb[0:8, 256:768].rearrange("p (c f) -> p c f", c=4)
        identf = cf[:, 0:128]
        negmask = cf[:, 128:256]
        posfill = cf[:, 256:384]
        tril01 = cf[:, 384:512]
        invf = cf[:, 512:513]

        WT = [dict() for _ in range(depth)]
        for l in range(depth):
            wcs = fw.dsem("wcast%d" % l)
            for (nm, src, r0, nr, c0, ncol) in pdefs:
                if src.startswith("w_branch"):
                    s_ap = wsrc["w_branch"][l, int(src[-1]), r0:r0 + nr, c0:c0 + ncol]
                else:
                    s_ap = wsrc[src][l, r0:r0 + nr, c0:c0 + ncol]
                t = Tile("wp")
                WT[l][nm] = t
                step = 512
                for rr in range(0, nr, step):
                    n2 = min(step, nr - rr)
                    dma(Wp[l][nm][rr:rr + n2, :], s_ap[rr:rr + n2, :], [], [t], wcs, eng="gpsimd")
            for t in WT[l].values():
                t.w = {wcs: wcs.n}

        ring = [sb("ring%d" % i, [128, SLOTB], BF16) for i in range(NSLOT)]
        ring_sem = [fw.dsem("ring%d" % i) for i in range(NSLOT)]
        kiT = sb("kiT", [128, L], BF16)
        xt = sb("xt", [128, NSUB, D], F32)
        pt = sb("pt", [128, NSUB, PLE], F32)
        ptb = sb("ptb", [128, NSUB, PLE], BF16)
        posi = sb("posi", [128, T], I32)
        hT = sb("hT", [128, 8, T], BF16)
        pT = sb("pT", [128, 2, T], BF16)
        qT = sb("qT", [128, 4, T], BF16)
        qiT = sb("qiT", [128, 4, T], BF16)
        KTs = sb("KTs", [128, 4, T], BF16)
        Vs = sb("Vs", [128, NSUB, 520], BF16)
        cosT = sb("cosT", [128, T], F32)
        sinT = sb("sinT", [128, T], F32)
        rtmp = sb("rtmp", [128, 3, T], F32)
        xb16 = sb("xb16", [128, T], BF16)
        xrbuf = sb("xrbuf", [128, 4, 3 + T], F32)
        grT = sb("grT", [128, 4, T], BF16)
        zuT = sb("zuT", [128, 4, T], BF16)
        vn = sb("vn", [128, NSUB, 512], BF16)
        hst = sb("hst", [128, 4], F32)
        ybT = sb("ybT", [128, 4, T], BF16)
        ycT = sb("ycT", [128, 4, T], BF16)
        yaT = sb("yaT", [128, 4, T], BF16)
        yaTt = sb("yaTt", [64, T], BF16)
        wis = sb("wis", [128, NSUB, 8], F32)
        diag = sb("diag", [128, 8, 128], BF16)
        small = sb("small", [128, 32], F32)
        smalli = sb("smalli", [128, 4], I32)
        bis = sb("bis", [128, 16], F32)
        bis2 = sb("bis2", [128, 16], F32)
        Tmid = Tile("mid")
        Tcnt = Tile("cnt")
        Tsa = Tile("sa")
        big = sb("big", [128, 6144], F32)
        TS_ = big.T
        TR_ = Tile("bigR")
        score_ap = big[:, 0:L]
        Rrelu = big[:, 4096:6144].bitcast(BF16).rearrange("p (h w) -> p h w", h=8)
        masks = [sb("mask%d" % s, [128, L], BF16) for s in range(NSUB)]
        maskTk = sb("maskTk", [128, 4, T], BF16)
        PT = [sb("PT%d" % i, [128, 2, T], BF16) for i in range(4)]
        xs = sb("xs", [128, D], BF16)
        accm = sb("accm", [128, 8 * T], F32)

        def view(ap, tile):
            v = Buf.__new__(Buf)
            v.t = ap
            v.T = tile
            return v

        acc = view(accm[0:65, :].rearrange("p (h t) -> p h t", h=8), accm.T)
        lt = view(accm[:, 0:5 * T].rearrange("p (k t) -> p k t", k=5), accm.T)
        gz = view(accm[:, 5 * T:5 * T + 512], accm.T)
        xcb = view(accm[:, 5 * T + 512:5 * T + 512 + T // 2].bitcast(BF16), accm.T)
        sc1 = sb("sc1", [128, L], F32)
        rhl2 = [sb("rhl%d" % i, [65, 2, T], BF16) for i in range(2)]
        onesb = sb("onesb", [65, 64], BF16)
        gt = sb("gt", [128, T], F32)
        tmpf = sb("tmpf", [128, 512], F32)
        gsig = sb("gsig", [128, 8, T], BF16)
        merged = big[:, 0:8 * T].rearrange("p (c t) -> p c t", c=8)
        o = 8 * T
        mergedT = big[:, o:o + 4 * T].bitcast(BF16).rearrange("p (c t) -> p c t", c=8)
        o += 4 * T
        fT = [big[:, o + i * 2 * T:o + (i + 1) * 2 * T].bitcast(BF16).rearrange("p (c t) -> p c t", c=4) for i in range(2)]
        o += 4 * T
        assert o <= 4096
        o = 4096
        rl = [big[:, o + i * (T // 2):o + (i + 1) * (T // 2)].bitcast(BF16) for i in range(2)]
        o += T
        sg = big[:, o:o + 512]
        o += 512
        ple_t = big[:, o:o + 1024]
        o += 1024
        assert o <= 6144
        wbdf = big[:, 4096:4096 + 1024].rearrange("p (c j) -> p c j", c=8)
        cols = sb("cols", [128, 48], F32)
        gbv = sb("gbv", [128, 512], F32)
        gcur = sb("gcur", [128, D], F32)
        gsem = fw.dsem("gain")
        wbd = sb("wbd", [128, 8, 128], BF16)
        wspT = sb("wspT", [128, 8, 128], BF16)
        bsp = sb("bsp", [8, 128], F32)
        bsph = sb("bsph", [8, 2, 128], BF16)
        c8 = sb("c8", [128, 8], F32)
        epsc = sb("epsc", [128, 4], F32)
        psem = [fw.dsem("par%d" % i) for i in range(5)]
        xsem = fw.dsem("xload")
        ptsem = fw.dsem("pload")
        possem = fw.dsem("posload")
        ssem = fw.dsem("store")
        kvsem = fw.dsem("kvstore")
        out_tile = Tile("out")

        class Stream:
            def __init__(self):
                self.plan = []
                self.issued = 0
                self.pos = 0

            def add(self, name, loader, deps, hoist=True):
                self.plan.append((name, loader, deps, hoist))

            def next(self, name, look=NSLOT - 1):
                i = self.pos
                assert self.plan[i][0] == name, (self.plan[i][0], name)
                while self.issued < len(self.plan) and (
                        self.issued <= i or (self.issued <= i + look and self.plan[self.issued][3])):
                    k = self.issued
                    _, loader, deps, _ = self.plan[k]
                    loader(ring[k % NSLOT], ring_sem[k % NSLOT], deps)
                    self.issued += 1
                self.pos += 1
                return ring[i % NSLOT]

        stream = Stream()

        def wloader(l, nm, nr, ncol):
            kc = nr // 128

            def f(slot, sem, deps):
                dst = slot[:, 0:kc * ncol].rearrange("p (k w) -> p k w", k=kc)
                src = Wp[l][nm].rearrange("(k p) w -> p k w", p=128)
                dma(dst, src, deps, [slot], sem)
            return f

        KVT = [[Tile("kv") for _ in range((L + KG - 1) // KG)] for _ in range(depth)]

        def kvloader(l, g, wd):
            def f(slot, sem, deps):
                dstk = slot[:, 0:4 * wd].rearrange("p (c w) -> p c w", c=4)
                dma(dstk, KTc[l][:, :, g * KG:g * KG + wd].rearrange("c p w -> p c w"), deps, [slot], sem)
                nb = wd // 128
                dstv = slot[:, 2048:2048 + nb * 520].rearrange("p (b f) -> p b f", b=nb)
                dma(dstv, Vc[l][g * KG:g * KG + wd, :].rearrange("(b p) f -> p b f", p=128), deps, [slot], sem)
            return f

        pinfo = {nm: (nr, ncol) for (nm, _, _, nr, _, ncol) in pdefs}

        def plan_w(l, nm):
            nr, ncol = pinfo[nm]
            stream.add("%d_%s" % (l, nm), wloader(l, nm, nr, ncol), [WT[l][nm]])

        def kv_groups(j):
            nkeys = (j + 1) * T
            out = []
            g = 0
            while g * KG < nkeys:
                out.append((g, min(KG, nkeys - g * KG)))
                g += 1
            return out

        for l in range(depth):
            for j in range(NT):
                for nm in ("q", "k", "qi", "kiwi", "xr", "gr", "zu", "v", "zv"):
                    plan_w(l, nm)
                for n in (2, 1):
                    plan_w(l, "gate%d_0" % n)
                    plan_w(l, "gate%d_1" % n)
                    plan_w(l, "br%d_0" % n)
                    plan_w(l, "br%d_1" % n)
                grps = kv_groups(j)
                for (g, wd) in grps:
                    last = (g == grps[-1][0])
                    stream.add("%d_kv%d_%d" % (l, j, g), kvloader(l, g, wd), [KVT[l][g]], hoist=not last)
                for n in (0,):
                    plan_w(l, "gate%d_0" % n)
                    plan_w(l, "gate%d_1" % n)
                    plan_w(l, "br%d_0" % n)
                    plan_w(l, "br%d_1" % n)
                plan_w(l, "out_0")
                plan_w(l, "out_1")
                plan_w(l, "up_0")
                for g in range(8):
                    if g + 1 < 8:
                        plan_w(l, "up_%d" % (g + 1))
                    plan_w(l, "dn_%d" % g)
                plan_w(l, "ple")
                plan_w(l, "pg_0")
                plan_w(l, "pg_1")

        def wview(slot, nm):
            nr, ncol = pinfo[nm]
            kc = nr // 128
            return slot[:, 0:kc * ncol].rearrange("p (k w) -> p k w", k=kc)

        sm_i = [0]

        def sm():
            i = sm_i[0] % 32
            sm_i[0] += 1
            return small[:, i:i + 1]

        memset(epsc[:, 0:1], EPS, [epsc])
        memset(epsc[:, 1:2], math.pi / 2, [epsc])
        memset(epsc[:, 2:3], 1.0, [epsc])
        memset(epsc[:, 3:4], -BIGM, [epsc])
        memset(Vs[:, :, :], 1.0, [Vs])
        memset(onesb[:, :], 1.0, [onesb])

        def rstd_from_ss(ss_ap, n):
            a = sm()
            b = sm()
            act(a, ss_ap, AF.Sqrt, [small, epsc], [small], scale=1.0 / n, bias=epsc[:, 0:1])
            recip(b, a, [small], [small])
            return b

        pi = [0]

        def nps():
            pi[0] += 1
            return PS[pi[0] % 4]

        def norm_transpose(gcol0):
            for s in range(NSUB):
                if gcol0 is not None:
                    ss = sm()
                    act(tmpf[:, 0:512], xt[:, s, 0:512], AF.Square, [xt, small], [tmpf, small], accum_out=ss)
                    ss2 = sm()
                    act(tmpf[:, 0:512], xt[:, s, 512:1024], AF.Square, [xt, small], [tmpf, small], accum_out=ss2)
                    ss3 = sm()
                    tt(ss3, ss, ss2, ALU.add, [small], [small])
                    rs = rstd_from_ss(ss3, D)
                    ts(xs[:], xt[:, s, :], rs, None, ALU.mult, None, [xt, small], [xs])
                else:
                    cp(xs[:], xt[:, s, :], [xt], [xs])
                psb = PS[7][:].bitcast(BF16)
                for c in range(8):
                    tr(psb[:, c * 128:(c + 1) * 128], xs[:, c * 128:(c + 1) * 128], identb, [xs, cb], [PS[7]])
                src = psb[:, 0:1024].rearrange("p (c t) -> p c t", c=8)
                dst = hT[:, :, s * 128:(s + 1) * 128]
                if gcol0 is not None:
                    g_ap = cols[:, gcol0:gcol0 + 8].unsqueeze(2).to_broadcast([128, 8, 128])
                    tt(dst, src, g_ap, ALU.mult, [PS[7], cols], [hT])
                else:
                    cp(dst, src, [PS[7]], [hT])

        def post_norm_residual(banks, gcol, s):
            ssa = []
            for hf in range(2):
                a = sm()
                act(tmpf[:, 0:512], banks[hf][:, 0:512], AF.Square, [banks[hf], small], [tmpf, small], accum_out=a)
                ssa.append(a)
            s3 = sm()
            tt(s3, ssa[0], ssa[1], ALU.add, [small], [small])
            rs = rstd_from_ss(s3, D)
            for hf in range(2):
                stt(tmpf[:, 0:512], banks[hf][:, 0:512], rs, gcur[:, hf * 512:(hf + 1) * 512],
                    ALU.mult, ALU.mult, [banks[hf], small, gcur], [tmpf])
                tt(xt[:, s, hf * 512:(hf + 1) * 512], xt[:, s, hf * 512:(hf + 1) * 512], tmpf[:, 0:512], ALU.add,
                   [xt, tmpf], [xt])

        def merge_tile(dst, src, as_write=False):
            for a, b in ((dst.w, src.w), (dst.r, src.r)):
                for k_, v_ in b.items():
                    if a.get(k_, 0) < v_:
                        a[k_] = v_
            if as_write:
                for k_, v_ in src.r.items():
                    if dst.w.get(k_, 0) < v_:
                        dst.w[k_] = v_

        def fm_chunk(panel, pv, c, ps, M=128):
            for kc in range(8):
                mm(ps[0:M, 0:T], pv[:, kc, c * 128:c * 128 + M], hT[:, kc, :], kc == 0, kc == 7, [panel, hT], [ps])

        def rope_evac(ps, dst, W):
            act(xb16[:], ps[:, 0:T], AF.Copy, [ps], [xb16])
            mm(ps[:, T:2 * T], Rm, xb16[:], True, True, [cb, xb16], [ps])
            tt(rtmp[:, 0, :], ps[:, 0:T], cosT[:], ALU.mult, [ps, cosT], [rtmp])
            tt(rtmp[:, 1, :], ps[:, T:2 * T], sinT[:], ALU.mult, [ps, sinT], [rtmp])
            tt(dst, rtmp[:, 0, :], rtmp[:, 1, :], ALU.add, [rtmp], W)

        XTprev = None
        for l in range(depth):
            src_x = x_in if l == 0 else xbuf
            dst_x = y_out if l == depth - 1 else xbuf
            XT = [Tile("xd") for _ in range(NT)] if l < depth - 1 else None
            dma(cols[:], cols_in[l], [], [cols], psem[0])
            dma(gbv[:], gb_in[l, :, 3 * D:3 * D + 512], [], [gbv], psem[1])
            dma(gcur[:], gb_in[l, :, 0:D], [], [gcur], gsem)
            dma(bsp[:], bsp_in[l], [], [bsp], psem[2])
            cp(bsph[:, 0, :], bsp[:], [bsp], [bsph])
            tt(bsp[:], bsp[:], bsph[:, 0, :], ALU.subtract, [bsph], [bsp])
            cp(bsph[:, 1, :], bsp[:], [bsp], [bsph])
            dma(wbdf, wbd_in[l], [], [TR_], psem[3])
            cp(wbd[:], wbdf, [TR_], [wbd])
            dma(wbdf, wsp_in[l], [], [TR_], psem[4])
            tt(wspT[:], wbdf, tril01.unsqueeze(1).to_broadcast([128, 8, 128]), ALU.mult, [TR_, cf], [wspT])
            act(c8[:, 4:8], cols[:, 44:48], AF.Exp, [cols], [c8], scale=-1.0)
            ts(c8[:, 0:4], c8[:, 4:8], -0.25, 1.0 / 3.0, ALU.mult, ALU.add, [c8], [c8])
            tt(c8[:, 0:4], c8[:, 0:4], c8[:, 4:8], ALU.mult, [c8], [c8])
            ts(c8[:, 0:4], c8[:, 0:4], -1.0, 0.5, ALU.mult, ALU.add, [c8], [c8])
            tt(c8[:, 0:4], c8[:, 0:4], c8[:, 4:8], ALU.mult, [c8], [c8])
            ts(c8[:, 0:4], c8[:, 0:4], -1.0, 1.0, ALU.mult, ALU.add, [c8], [c8])
            tt(c8[:, 0:4], c8[:, 0:4], c8[:, 4:8], ALU.mult, [c8], [c8])
            ts(c8[:, 0:4], c8[:, 0:4], -8.0, None, ALU.mult, None, [c8], [c8])
            ts(c8[:, 4:8], c8[:, 0:4], 2.0, None, ALU.mult, None, [c8], [c8])
            memset(xrbuf[:, :, 0:3], 0.0, [xrbuf])
            memset(hst[:], 0.0, [hst])

            for j in range(NT):
                t0 = j * T
                first_tile = (l == 0 and j == 0)
                rd = [XTprev[j]] if (l > 0) else []
                dma(xt[:], src_x[t0:t0 + T, :].rearrange("(s p) d -> p s d", p=128), rd, [xt], xsem)
                dma(pt[:], p_in[l, t0:t0 + T, :].rearrange("(s p) d -> p s d", p=128), [], [pt], ptsem)
                dma(posi[:], pos_in[:, t0:t0 + T], [], [posi], possem)
                ang = rtmp[:, 0, :]
                u = rtmp[:, 1, :]
                rr = rtmp[:, 2, :]
                cp(u, posi[:], [posi], [rtmp])
                ts(ang, u, invf, None, ALU.mult, None, [rtmp, cf], [rtmp])
                ts(u, ang, 1.0 / (2 * math.pi), 12582912.0, ALU.mult, ALU.add, [rtmp], [rtmp])
                ts(u, u, -12582912.0, None, ALU.add, None, [rtmp], [rtmp])
                C1 = 6.28125
                C2 = float(np.float32(2 * math.pi - C1))
                C3 = float(2 * math.pi - C1 - C2)
                stt(rr, u, -C1, ang, ALU.mult, ALU.add, [rtmp], [rtmp])
                stt(rr, u, -C2, rr, ALU.mult, ALU.add, [rtmp], [rtmp])
                stt(rr, u, -C3, rr, ALU.mult, ALU.add, [rtmp], [rtmp])
                act(sinT[:], rr, AF.Sin, [rtmp], [sinT])
                stt(u, rr, -1.0, rr, ALU.mult, ALU.max, [rtmp], [rtmp])
                act(cosT[:], u, AF.Sin, [rtmp, epsc], [cosT], scale=-1.0, bias=epsc[:, 1:2])
                norm_transpose(0)
                if first_tile:
                    dump("hT", hT[:, 0, :], [hT])
                    dump("cosT", cosT[:], [cosT])
                    dump("sinT", sinT[:], [sinT])

                for nm, dstb in (("q", qT), ("k", KTs), ("qi", qiT)):
                    panel = stream.next("%d_%s" % (l, nm))
                    pv = wview(panel, nm)
                    pss = [nps() for _ in range(4)]
                    fm_chunk(panel, pv, 0, pss[0])
                    for c in range(4):
                        if c + 1 < 4:
                            fm_chunk(panel, pv, c + 1, pss[c + 1])
                        rope_evac(pss[c], dstb[:, c, :], [dstb])
                panel = stream.next("%d_kiwi" % l)
                pv = wview(panel, "kiwi")
                ps = nps()
                for half in range(2):
                    for kc in range(8):
                        mm(ps[half * 64:(half + 1) * 64, 0:T], pv[:, kc, 0:64], hT[:, kc, :], kc == 0, kc == 7,
                           [panel, hT], [ps])
                rope_evac(ps, kiT[:, t0:t0 + T], [kiT])
                for s in range(NSUB):
                    ps = nps()
                    for kc in range(8):
                        mm(ps[:, 0:8], hT[:, kc, s * 128:(s + 1) * 128], pv[:, kc, 64:72], kc == 0, kc == 7,
                           [panel, hT], [ps])
                    cp(wis[:, s, :], ps[:, 0:8], [ps], [wis])
                dma(KTc[l][:, :, t0:t0 + T].rearrange("c p w -> p c w"), KTs[:], [KTs], [KVT[l][t0 // KG]], kvsem, eng="gpsimd")
                if first_tile:
                    dump("qT", qT[:, 0, :], [qT])
                    dump("kiT", kiT[:, 0:T], [kiT])
                    dump("wis", wis[:, 0, :], [wis])
                panel = stream.next("%d_xr" % l)
                pv = wview(panel, "xr")
                for c in range(4):
                    ps = nps()
                    fm_chunk(panel, pv, c, ps)
                    act(xrbuf[:, c, 3:3 + T], ps[:, 0:T], AF.Copy, [ps], [xrbuf])
                for nm, dstb in (("gr", grT), ("zu", zuT)):
                    panel = stream.next("%d_%s" % (l, nm))
                    pv = wview(panel, nm)
                    for c in range(4):
                        ps = nps()
                        fm_chunk(panel, pv, c, ps)
                        act(dstb[:, c, :], ps[:, 0:T], AF.Gelu_apprx_tanh, [ps], [dstb])
                panel = stream.next("%d_v" % l)
                pv = wview(panel, "v")
                for s in range(NSUB):
                    ps = nps()
                    for kc in range(8):
                        mm(ps[:, 0:512], hT[:, kc, s * 128:(s + 1) * 128], pv[:, kc, :], kc == 0, kc == 7, [panel, hT], [ps])
                    dstv = Vs[:, s, :].rearrange("p (h f) -> p h f", h=8)[:, :, 0:64]
                    act(dstv, ps[:, 0:512].rearrange("p (h f) -> p h f", h=8), AF.Copy, [ps], [Vs])
                dma(Vc[l][t0:t0 + T, :].rearrange("(s p) f -> p s f", p=128), Vs[:], [Vs], [KVT[l][t0 // KG]], kvsem, eng="gpsimd")
                panel = stream.next("%d_zv" % l)
                pv = wview(panel, "zv")
                for s in range(NSUB):
                    ps = nps()
                    for kc in range(8):
                        mm(ps[:, 0:512], hT[:, kc, s * 128:(s + 1) * 128], pv[:, kc, :], kc == 0, kc == 7, [panel, hT], [ps])
                    act(gz[:], ps[:, 0:512], AF.Gelu_apprx_tanh, [ps], [gz])
                    ss = sm()
                    act(tmpf[:, 0:512], gz[:], AF.Square, [gz, small], [tmpf, small], accum_out=ss)
                    rs = rstd_from_ss(ss, 512)
                    stt(vn[:, s, :], gz[:], rs, gbv[:], ALU.mult, ALU.mult, [gz, small, gbv], [vn])

                mpi = [0]

                def mps():
                    mpi[0] += 1
                    return PS[5 + mpi[0] % 3]

                def gen_mixC():
                    for s in range(NSUB):
                        for cpair in range(4):
                            ps = mps()
                            for gg in range(2):
                                g = cpair * 2 + gg
                                mm(ps[gg * 64:(gg + 1) * 64, 0:128], vn[:, s, g * 64:(g + 1) * 64], wspT[:, g, :], True, False,
                                   [vn, wspT], [ps])
                            mm(ps[:, 0:128], esel[:, cpair, :], bsph[:, 0, :], False, False, [cb, bsph], [ps])
                            mm(ps[:, 0:128], esel[:, cpair, :], bsph[:, 1, :], False, True, [cb, bsph], [ps])
                            tt(ycT[:, cpair, s * 128:(s + 1) * 128], ps[:, 0:128], zuT[:, cpair, s * 128:(s + 1) * 128], ALU.mult,
                               [ps, zuT], [ycT])
                            yield

                def gen_mixB():
                    for c in range(4):
                        xc = lt[:, 0, :]
                        ts(xc, xrbuf[:, c, 0:T], cols[:, 16 + c:17 + c], cols[:, 32 + c:33 + c], ALU.mult, ALU.add, [xrbuf, cols], [lt])
                        for jj in range(1, 4):
                            stt(xc, xrbuf[:, c, jj:jj + T], cols[:, 16 + jj * 4 + c:17 + jj * 4 + c], xc, ALU.mult, ALU.add,
                                [xrbuf, cols, lt], [lt])
                        yield
                        cp(xrbuf[:, c, 0:3], xrbuf[:, c, T:T + 3], [xrbuf], [xrbuf])
                        act(xcb[:], xc, AF.Copy, [lt], [xcb])
                        ps = mps()
                        mm(ps[:, 0:T], wbd[:, c, :], xcb[:], True, True, [wbd, xcb], [ps])
                        mm(ps[:, T:2 * T], wbd[:, 4 + c, :], xcb[:], True, True, [wbd, xcb], [ps])
                        yield
                        rg = lt[:, 1, :]
                        ig = lt[:, 2, :]
                        av = lt[:, 3, :]
                        sq = lt[:, 4, :]
                        act(rg, ps[:, 0:T], AF.Sigmoid, [ps, cols], [lt], bias=cols[:, 36 + c:37 + c])
                        act(ig, ps[:, T:2 * T], AF.Sigmoid, [ps, cols], [lt], bias=cols[:, 40 + c:41 + c])
                        yield
                        act(av, rg, AF.Exp, [lt, c8], [lt], scale=c8[:, c:c + 1])
                        act(sq, rg, AF.Exp, [lt, c8], [lt], scale=c8[:, 4 + c:5 + c])
                        act(sq, sq, AF.Sqrt, [lt, epsc], [lt], scale=-1.0, bias=epsc[:, 2:3])
                        yield
                        tt(ig, ig, xc, ALU.mult, [lt], [lt])
                        tt(ig, ig, sq, ALU.mult, [lt], [lt])
                        hh = lt[:, 1, :]
                        scan(hh, av, ig, hst[:, c:c + 1], [lt, hst], [lt])
                        yield
                        cp(hst[:, c:c + 1], hh[:, T - 1:T], [lt], [hst])
                        tt(ybT[:, c, :], hh, grT[:, c, :], ALU.mult, [lt, grT], [ybT])
                        yield

                grps = kv_groups(j)
                nkeys = (j + 1) * T
                TRh = [Tile("relu%d" % h) for h in range(8)]

                SC = [(score_ap, TS_), (sc1[:, 0:L], sc1.T)]

                def gen_I(s):
                    N = t0 + 128 * (s + 1)
                    score_s, TSs = SC[s]
                    for h in range(8):
                        merge_tile(TRh[h], TR_, as_write=True)
                        act(diag[:, h, :], identf, AF.Copy, [cf, wis], [diag], scale=wis[:, s, h:h + 1])
                    yield
                    ngrp = (N + KG - 1) // KG
                    for kg in range(ngrp):
                        wd = min(KG, N - kg * KG)
                        for h in range(8):
                            ps = PS[h % 4]
                            r0 = (h % 2) * 64
                            mm(ps[:, 0:wd], qiT[r0:r0 + 64, h // 2, s * 128:(s + 1) * 128],
                               kiT[r0:r0 + 64, kg * KG:kg * KG + wd], True, True, [qiT, kiT], [ps])
                            if h % 2 == 0:
                                act(Rrelu[:, h, 0:wd], ps[:, 0:wd], AF.Relu, [ps], [TRh[h]])
                            else:
                                ts(Rrelu[:, h, 0:wd], ps[:, 0:wd], 0.0, None, ALU.max, None, [ps], [TRh[h]])
                            if h % 2 == 1:
                                yield
                        for h in range(8):
                            mm(PS[4][:, 0:wd], diag[:, h, :], Rrelu[:, h, 0:wd], h == 0, h == 7, [diag, TRh[h]], [PS[4]])
                        if kg == ngrp - 1:
                            if wd > 128:
                                cp(score_s[:, kg * KG:kg * KG + wd - 128], PS[4][:, 0:wd - 128], [PS[4]], [TSs])
                            tt(score_s[:, N - 128:N], PS[4][:, wd - 128:wd], negmask, ALU.add, [PS[4], cf], [TSs])
                            tt(tmpf[:, 0:128], PS[4][:, wd - 128:wd], posfill, ALU.add, [PS[4], cf], [tmpf])
                        else:
                            cp(score_s[:, kg * KG:kg * KG + wd], PS[4][:, 0:wd], [PS[4]], [TSs])
                        yield
                    for h in range(8):
                        merge_tile(TR_, TRh[h])

                def gen_B(s):
                    N = t0 + 128 * (s + 1)
                    score_s, TSs = SC[s]
                    hi0 = bis[:, 0:1]
                    lo = bis[:, 1:2]
                    w0 = bis[:, 2:3]
                    reduce(hi0, score_s[:, 0:N], ALU.max, [TSs, bis], [bis])
                    reduce(lo, tmpf[:, 0:128], ALU.min, [tmpf, bis], [bis])
                    if N > 128:
                        m1 = bis[:, 3:4]
                        reduce(m1, score_s[:, 0:N - 128], ALU.min, [TSs, bis], [bis])
                        tt(lo, lo, m1, ALU.min, [bis], [bis])
                    tt(w0, hi0, lo, ALU.subtract, [bis], [bis])
                    yield
                    mk = masks[s]
                    if N > TOPK:
                        Nd = max(128, (int(N * 0.42) // 128) * 128)
                        Na = N - Nd
                        if s == 0:
                            junkA, TJ = masks[1], masks[1].T
                        else:
                            junkA, TJ = big[:, 4096:6144].bitcast(BF16), TR_
                        for it in range(NBIS):
                            k4 = it % 4
                            mid = bis2[:, k4:k4 + 1]
                            cnt = bis2[:, 4 + k4:5 + k4]
                            vv = bis2[:, 8 + k4:9 + k4]
                            sA = bis2[:, 12 + k4:13 + k4]
                            stt(mid, w0, 0.5 ** (it + 1), lo, ALU.mult, ALU.add, [bis], [Tmid])
                            act(junkA[:, 0:Na], score_s[:, Nd:N], AF.Sign, [TSs, Tmid], [TJ, Tsa], scale=-1.0, bias=mid,
                                accum_out=sA)
                            count_ge(mk[:, 0:Nd], score_s[:, 0:Nd], mid, cnt, [TSs, Tmid], [mk, Tcnt])
                            stt(vv, cnt, 2.0, sA, ALU.mult, ALU.subtract, [Tcnt, Tsa], [Tcnt])
                            ge = smalli[:, k4:k4 + 1]
                            ts(ge, vv, float(2 * TOPK - Na), None, ALU.is_ge, None, [Tcnt], [smalli])
                            cpred(lo, ge, mid, [Tmid, smalli], [bis])
                            yield
                    ts(mk[:, 0:N], score_s[:, 0:N], lo, None, ALU.is_ge, None, [TSs, bis], [mk])
                    if N < nkeys:
                        memset(mk[:, N:nkeys], 0.0, [mk], eng="gpsimd")
                    yield

                def gen_G(items):
                    for ni, n in items:
                        for hf in range(2):
                            gpan = stream.next("%d_gate%d_%d" % (l, n, hf))
                            pv = wview(gpan, "gate0_0")
                            for c in range(4):
                                ps = nps()
                                fm_chunk(gpan, pv, c, ps)
                                act(gsig[:, hf * 4 + c, :], ps[:, 0:T], AF.Sigmoid, [ps], [gsig])
                                yield
                        ysrc = {2: ycT, 1: ybT, 0: yaT}[n]
                        for hf in range(2):
                            bpan = stream.next("%d_br%d_%d" % (l, n, hf))
                            pv = wview(bpan, "br0_0")
                            for c in range(4):
                                dchunk = hf * 4 + c
                                ps = nps()
                                for kc in range(4):
                                    mm(ps[:, 0:T], pv[:, kc, c * 128:(c + 1) * 128], ysrc[:, kc, :], kc == 0, kc == 3,
                                       [bpan, ysrc], [ps])
                                if ni == 0:
                                    tt(merged[:, dchunk, :], ps[:, 0:T], gsig[:, dchunk, :], ALU.mult, [ps, gsig], [TS_])
                                else:
                                    tt(gt[:], ps[:, 0:T], gsig[:, dchunk, :], ALU.mult, [ps, gsig], [gt])
                                    dsto = mergedT if ni == 2 else merged
                                    tt(dsto[:, dchunk, :], merged[:, dchunk, :], gt[:], ALU.add, [TS_, gt], [TS_], eng="gpsimd")
                                yield

                def chain(*gens):
                    for g_ in gens:
                        yield from g_

                def run(*gens):
                    live = list(gens)
                    while live:
                        for g_ in list(live):
                            try:
                                next(g_)
                            except StopIteration:
                                live.remove(g_)

                def run_w(ga, gb_, kb):
                    la = lb = True
                    while la or lb:
                        if la:
                            try:
                                next(ga)
                            except StopIteration:
                                la = False
                        for _ in range(kb):
                            if lb:
                                try:
                                    next(gb_)
                                except StopIteration:
                                    lb = False

                run(chain(gen_mixC(), gen_mixB()), gen_I(0))
                if first_tile:
                    dump("ycT", ycT[:, 0, :], [ycT])
                    dump("ybT", ybT[:, 0, :], [ybT])
                    dump("score", score_ap[:, 0:128], [TS_])
                N1 = t0 + 256
                lenI = 1 + ((N1 + KG - 1) // KG) * 5
                lenB = 2 + (NBIS if (t0 + 128) > TOPK else 0)
                run_w(gen_B(0), gen_I(1), max(1, -(-lenI // lenB)))
                lenB1 = 2 + (NBIS if (t0 + 256) > TOPK else 0)
                run_w(gen_B(1), gen_G([(0, 2), (1, 1)]), max(1, -(-32 // lenB1)))

                first = True
                ucnt = [0]
                for gi, (g, wd) in enumerate(grps):
                    panel = stream.next("%d_kv%d_%d" % (l, j, g))
                    nb = wd // 128
                    KTv = panel[:, 0:4 * wd].rearrange("p (c w) -> p c w", c=4)
                    Vv = panel[:, 2048:2048 + nb * 520].rearrange("p (b f) -> p b f", b=nb)
                    psb = PS[6][:].bitcast(BF16)
                    for s in range(NSUB):
                        for b in range(nb):
                            tr(psb[:, (s * 4 + b) * 128:(s * 4 + b + 1) * 128],
                               masks[s][:, g * KG + b * 128:g * KG + (b + 1) * 128], identb, [masks[s], cb], [PS[6]])
                    for s in range(NSUB):
                        act(maskTk[:, 0:nb, s * 128:(s + 1) * 128],
                            psb[:, s * 512:s * 512 + nb * 128].rearrange("p (b t) -> p b t", b=nb),
                            AF.Copy, [PS[6]], [maskTk])
                    units = [(h, bp) for h in range(8) for bp in range(0, nb, 2)]

                    def stageA(i):
                        h, bp = units[i]
                        r0 = (h % 2) * 64
                        nbb = min(2, nb - bp)
                        ps = PS[(ucnt[0] + i) % 4]
                        for b in range(bp, bp + nbb):
                            mm(ps[:, (b - bp) * T:(b - bp + 1) * T], KTv[r0:r0 + 64, h // 2, b * 128:(b + 1) * 128],
                               qT[r0:r0 + 64, h // 2, :], True, True, [panel, qT], [ps])

                    def stageB(i):
                        h, bp = units[i]
                        nbb = min(2, nb - bp)
                        ps = PS[(ucnt[0] + i) % 4]
                        ptile = PT[(ucnt[0] + i) % len(PT)]
                        act(ptile[:, 0:nbb, :], ps[:, 0:nbb * T].rearrange("p (b t) -> p b t", b=nbb), AF.Exp,
                            [ps], [ptile], scale=HD ** -0.5)
                        tt(ptile[:, 0:nbb, :], ptile[:, 0:nbb, :], maskTk[:, bp:bp + nbb, :], ALU.mult, [ptile, maskTk], [ptile],
                           eng=("vector" if (ucnt[0] + i) % 2 == 0 else "gpsimd"))

                    def stageD(i):
                        h, bp = units[i]
                        nbb = min(2, nb - bp)
                        pacc = PS[4 + (h % 2)]
                        ptile = PT[(ucnt[0] + i) % len(PT)]
                        for b in range(bp, bp + nbb):
                            mm(pacc[0:65, 0:T], Vv[:, b, h * 65:(h + 1) * 65], ptile[:, b - bp, :], b == 0, b == nb - 1,
                               [panel, ptile], [pacc])
                        if bp + 2 >= nb:
                            if first:
                                cp(acc[:, h, :], pacc[0:65, 0:T], [pacc], [acc])
                            else:
                                tt(acc[:, h, :], acc[:, h, :], pacc[0:65, 0:T], ALU.add, [pacc, acc], [acc])

                    stageA(0)
                    if len(units) > 1:
                        stageA(1)
                    for i in range(len(units)):
                        if i + 2 < len(units):
                            stageA(i + 2)
                        stageB(i)
                        stageD(i)
                    ucnt[0] += len(units)
                    first = False
                accf = accm[0:65, :]
                act(accf[64:65, :], accf[64:65, :], AF.Ln, [acc], [acc])
                act(accf[64:65, :], accf[64:65, :], AF.Exp, [acc], [acc], scale=-1.0)
                for h in range(8):
                    rhl = rhl2[h % 2]
                    cp(rhl[64:65, 0, :], acc[64:65, h, :], [acc], [rhl])
                    tt(rhl[64:65, 1, :], acc[64:65, h, :], rhl[64:65, 0, :], ALU.subtract, [acc, rhl], [rhl])
                    ps = nps()
                    mm(ps[0:64, 0:T], onesb[64:65, 0:64], rhl[64:65, 0, :], True, False, [onesb, rhl], [ps])
                    mm(ps[0:64, 0:T], onesb[64:65, 0:64], rhl[64:65, 1, :], False, True, [onesb, rhl], [ps])
                    if h % 2 == 0:
                        tt(yaT[0:64, h // 2, :], acc[0:64, h, :], ps[0:64, 0:T], ALU.mult, [acc, ps], [yaT])
                    else:
                        tt(yaTt[:, :], acc[0:64, h, :], ps[0:64, 0:T], ALU.mult, [acc, ps], [yaTt])
                        ps2 = nps()
                        mm(ps2[64:128, 0:T], identb[0:64, 0:64], yaTt[:, :], True, True, [cb, yaTt], [ps2])
                        cp(yaT[64:128, h // 2, :], ps2[64:128, 0:T], [ps2], [yaT])
                if first_tile:
                    dump("yaT", yaT[:, 0, :], [yaT])
                if l == 0 and j == 1:
                    dump("yaT1", yaT[:, 0, :], [yaT])
                    dump("acc1", acc[:, 0, :], [acc])

                run(gen_G([(2, 0)]))
                op_ = [stream.next("%d_out_%d" % (l, hf), look=NSLOT - 1 - hf) for hf in range(2)]
                for s in range(NSUB):
                    banks = [PS[4 + 2 * (s % 2)], PS[5 + 2 * (s % 2)]]
                    for hf in range(2):
                        pv = wview(op_[hf], "out_0")
                        for kc in range(8):
                            mm(banks[hf][:, 0:512], mergedT[:, kc, s * 128:(s + 1) * 128], pv[:, kc, :], kc == 0, kc == 7,
                               [op_[hf], TS_], [banks[hf]])
                    post_norm_residual(banks, 0, s)
                dma(gcur[:], gb_in[l, :, D:2 * D], [], [gcur], gsem, eng="gpsimd")
                if first_tile:
                    dump("x1", xt[:, 0, :], [xt])
                norm_transpose(8)
                dbanks = [[PS[4], PS[5]], [PS[6], PS[7]]]
                TF = [Tile("fT0"), Tile("fT1")]
                Trl = [Tile("rl0"), Tile("rl1")]

                for i_ in range(2):
                    merge_tile(TF[i_], TS_, as_write=True)
                    merge_tile(Trl[i_], TR_, as_write=True)

                def ffn_up(g):
                    up = stream.next("%d_up_%d" % (l, g))
                    pv = wview(up, "up_0")
                    fTg = fT[g % 2]
                    for c in range(4):
                        ps = nps()
                        fm_chunk(up, pv, c, ps)
                        rlb = rl[c % 2]
                        act(rlb, ps[:, 0:T], AF.Relu, [ps], [Trl[c % 2]])
                        tt(fTg[:, c, :], rlb, rlb, ALU.mult, [Trl[c % 2]], [TF[g % 2]], eng="gpsimd")

                def ffn_dn(g):
                    dn = stream.next("%d_dn_%d" % (l, g))
                    dv = wview(dn, "dn_0")
                    fTg = fT[g % 2]
                    for s in range(NSUB):
                        for hf in range(2):
                            for c in range(4):
                                mm(dbanks[s][hf][:, 0:512], fTg[:, c, s * 128:(s + 1) * 128], dv[:, c, hf * 512:(hf + 1) * 512],
                                   g == 0 and c == 0, g == 7 and c == 3, [dn, TF[g % 2]], [dbanks[s][hf]])

                ffn_up(0)
                for g in range(8):
                    if g + 1 < 8:
                        ffn_up(g + 1)
                    ffn_dn(g)
                for i_ in range(2):
                    merge_tile(TS_, TF[i_])
                    merge_tile(TR_, Trl[i_])
                for s in range(NSUB):
                    post_norm_residual(dbanks[s], D, s)
                dma(gcur[:], gb_in[l, :, 2 * D:3 * D], [], [gcur], gsem, eng="gpsimd")
                if first_tile:
                    dump("x2", xt[:, 0, :], [xt])
                norm_transpose(None)
                for s in range(NSUB):
                    cp(ptb[:, s, :], pt[:, s, :], [pt], [ptb])
                    psb = PS[3][:].bitcast(BF16)
                    for c in range(2):
                        tr(psb[:, c * 128:(c + 1) * 128], ptb[:, s, c * 128:(c + 1) * 128], identb, [ptb, cb], [PS[3]])
                    cp(pT[:, :, s * 128:(s + 1) * 128], psb[:, 0:256].rearrange("p (c t) -> p c t", c=2), [PS[3]], [pT])
                plp = stream.next("%d_ple" % l)
                plv = wview(plp, "ple")
                pg = [stream.next("%d_pg_%d" % (l, hf), look=NSLOT - 2 - hf) for hf in range(2)]
                for s in range(NSUB):
                    ssp = []
                    for hf in range(2):
                        pa = PS[4 * (s % 2) + hf]
                        pgb = PS[4 * (s % 2) + 2 + hf]
                        for kc in range(2):
                            mm(pa[:, 0:512], pT[:, kc, s * 128:(s + 1) * 128], plv[:, kc, hf * 512:(hf + 1) * 512], kc == 0, kc == 1,
                               [plp, pT], [pa])
                        gv = wview(pg[hf], "pg_0")
                        for kc in range(8):
                            mm(pgb[:, 0:512], hT[:, kc, s * 128:(s + 1) * 128], gv[:, kc, :], kc == 0, kc == 7, [pg[hf], hT], [pgb])
                        act(sg, pgb[:, 0:512], AF.Sigmoid, [pgb], [TR_])
                        tt(ple_t[:, hf * 512:(hf + 1) * 512], pa[:, 0:512], sg, ALU.mult, [pa, TR_], [TR_])
                        a = sm()
                        act(tmpf[:, 0:512], ple_t[:, hf * 512:(hf + 1) * 512], AF.Square, [TR_, small], [tmpf, small], accum_out=a)
                        ssp.append(a)
                    s3 = sm()
                    tt(s3, ssp[0], ssp[1], ALU.add, [small], [small])
                    rs = rstd_from_ss(s3, D)
                    for hf in range(2):
                        stt(tmpf[:, 0:512], ple_t[:, hf * 512:(hf + 1) * 512], rs, gcur[:, hf * 512:(hf + 1) * 512],
                            ALU.mult, ALU.mult, [TR_, small, gcur], [tmpf])
                        tt(xt[:, s, hf * 512:(hf + 1) * 512], xt[:, s, hf * 512:(hf + 1) * 512], tmpf[:, 0:512], ALU.add,
                           [xt, tmpf], [xt])
                if j + 1 < NT:
                    dma(gcur[:], gb_in[l, :, 0:D], [], [gcur], gsem, eng="gpsimd")
                wt = [XT[j]] if XT is not None else [out_tile]
                dma(dst_x[t0:t0 + T, :].rearrange("(s p) d -> p s d", p=128), xt[:], [xt], wt, ssem, eng="gpsimd")
            XTprev = XT

        fw.finish("sync", [out_tile] + dbg_tiles)
        fw.emit()
    return nc, fw


def _consts():
    bf = ml_dtypes.bfloat16
    ident = np.eye(128, dtype=np.float32)
    Rm = np.zeros((128, 128), np.float32)
    for base in (0, 64):
        for d in range(8):
            Rm[base + d + 8, base + d] = -1.0
            Rm[base + d, base + d + 8] = 1.0
    esel = np.zeros((128, 4, 128), np.float32)
    for g in range(8):
        esel[g, g // 2, (g % 2) * 64:(g % 2 + 1) * 64] = 1.0
    cb = np.concatenate([ident, Rm, esel.reshape(128, 512)], axis=1).astype(bf)
    tt_, ss_ = np.meshgrid(np.arange(128), np.arange(128), indexing="ij")
    negmask = np.where(ss_ > tt_, np.float32(NEG), np.float32(0.0))
    posfill = np.where(ss_ > tt_, np.float32(-2 * NEG), np.float32(0.0))
    tril01 = (tt_ <= ss_).astype(np.float32)
    half = 8
    inv_freq = (np.float32(500000.0) ** (-np.arange(half, dtype=np.float32) * np.float32(2.0) / np.float32(16))).astype(np.float32)
    invf = np.zeros((128, 1), np.float32)
    for f in range(128):
        d = f % 64
        if d < 16:
            invf[f, 0] = inv_freq[d % 8]
    cf = np.concatenate([ident, negmask, posfill, tril01, invf], axis=1).astype(np.float32)
    return cb, cf


def _layout_params(inp, depth):
    f = np.float32
    cols = np.zeros((depth, 128, 48), f)
    gb = np.zeros((depth, 128, 3 * D + 512), f)
    wbd = np.zeros((depth, 128, 8, 128), f)
    wsp = np.zeros((depth, 128, 8, 128), f)
    for l in range(depth):
        cols[l, :, 0:8] = np.asarray(inp["g_pre_mix"][l], f).reshape(8, 128).T
        cols[l, :, 8:16] = np.asarray(inp["g_pre_ffn"][l], f).reshape(8, 128).T
        cw = np.asarray(inp["conv_w"][l], f)
        for jj in range(4):
            cols[l, :, 16 + jj * 4:20 + jj * 4] = cw[jj].reshape(4, 128).T
        cols[l, :, 32:36] = np.asarray(inp["conv_b"][l], f).reshape(4, 128).T
        cols[l, :, 36:40] = np.asarray(inp["b_rg_a"][l], f).reshape(4, 128).T
        cols[l, :, 40:44] = np.asarray(inp["b_rg_x"][l], f).reshape(4, 128).T
        cols[l, :, 44:48] = np.asarray(inp["lru_lambda"][l], f).reshape(4, 128).T
        row = np.concatenate([np.asarray(inp["g_post_mix"][l], f), np.asarray(inp["g_post_ffn"][l], f),
                              np.asarray(inp["g_post_ple"][l], f), np.asarray(inp["g_gmlp_v"][l], f)])
        gb[l] = np.broadcast_to(row[None, :], (128, row.size))
        for gi, key in enumerate(("w_rg_a", "w_rg_x")):
            w = np.asarray(inp[key][l], f)
            for c in range(4):
                for hh in range(2):
                    wbd[l, hh * 64:(hh + 1) * 64, gi * 4 + c, hh * 64:(hh + 1) * 64] = w[c * 2 + hh]
        ws = np.asarray(inp["w_spatial"][l], f)
        wsp[l] = np.transpose(ws, (2, 0, 1))
    bsp = np.ascontiguousarray(np.asarray(inp["b_spatial"], f))
    return cols, gb, wbd, wsp, bsp


_CACHE = {}


def kernel(**inputs):
    depth = DEPTH
    x = np.asarray(inputs["x"], np.float32)
    B, L, _ = x.shape
    p = np.asarray(inputs["p"], np.float32)
    pos = np.asarray(inputs["positions"], np.int32)
    cb, cf = _consts()
    cols, gb, wbd, wsp, bsp = _layout_params(inputs, depth)
    shared = {
        "w_in": np.ascontiguousarray(np.asarray(inputs["w_in"], np.float32)),
        "w_branch": np.ascontiguousarray(np.asarray(inputs["w_branch"], np.float32)),
        "w_out": np.ascontiguousarray(np.asarray(inputs["w_out"], np.float32)),
        "w_ffn_up": np.ascontiguousarray(np.asarray(inputs["w_ffn_up"], np.float32)),
        "w_ffn_down": np.ascontiguousarray(np.asarray(inputs["w_ffn_down"], np.float32)),
        "w_ple": np.ascontiguousarray(np.asarray(inputs["w_ple"], np.float32)),
        "w_ple_gate": np.ascontiguousarray(np.asarray(inputs["w_ple_gate"], np.float32)),
        "cols": cols, "gb": gb, "wbd": wbd, "wsp": wsp, "bsp": bsp, "cb": cb, "cf": cf,
    }
    if L not in _CACHE:
        _CACHE[L] = build_program(L, depth)[0]
    nc = _CACHE[L]
    in_maps = []
    for b in range(B):
        m = dict(shared)
        m["x"] = np.ascontiguousarray(x[b])
        m["p"] = np.ascontiguousarray(p[:, b])
        m["pos"] = np.ascontiguousarray(np.broadcast_to(pos[b][None, :], (128, L)))
        in_maps.append(m)
    res = run_bass_kernel_spmd(nc, in_maps, core_ids=list(range(B)))
    return np.stack([np.asarray(r["y"], np.float32) for r in res.results], axis=0)
```

```python
import math
from contextlib import ExitStack
import numpy as np
import ml_dtypes
import concourse.bass as bass
import concourse.mybir as mybir
from concourse.bass_utils import run_bass_kernel_spmd

F32 = mybir.dt.float32
BF16 = mybir.dt.bfloat16
I32 = mybir.dt.int32
AF = mybir.ActivationFunctionType
ALU = mybir.AluOpType
AX = mybir.AxisListType

D = 1024
NH = 8
HD = 64
TOPK = 256
FFN = 4096
PLE = 256
EPS = 1e-6
DEPTH = 2
T = 256
NSUB = T // 128
KG = 512
NSLOT = 3
SLOTB = 4224
NBIS = 12
BIGM = 30000.0
NEG = -1.0e30
IN_OFF = dict(q=0, k=512, v=1024, qi=1536, kiwi=2048, xr=2120, gr=2632, zu=3144, zv=3656, gate=4168)


class Sem:
    def __init__(self, h, name):
        self.h = h
        self.n = 0
        self.name = name


class Tile:
    __slots__ = ("w", "r", "name")

    def __init__(self, name=""):
        self.w = {}
        self.r = {}
        self.name = name


class Buf:
    def __init__(self, t, name=""):
        self.t = t
        self.T = Tile(name)

    def __getitem__(self, k):
        return self.t[k]


class Engine:
    def __init__(self, name, sem):
        self.name = name
        self.sem = sem
        self.ops = []
        self.seen = {}


class FW:
    def __init__(self, nc, stack):
        self.nc = nc
        self.stack = stack
        self.engs = {}
        for n in ("tensor", "vector", "scalar", "gpsimd", "sync"):
            s = Sem(stack.enter_context(nc.semaphore("sem_" + n)), n)
            self.engs[n] = Engine(n, s)
        self.nops = 0

    def dsem(self, name):
        return Sem(self.stack.enter_context(self.nc.semaphore("dsem_" + name)), name)

    def op(self, eng, fn, reads=(), writes=(), dsem=None):
        E = self.engs[eng]
        need = {}
        for b in reads:
            t = b.T if isinstance(b, Buf) else b
            for s, v in t.w.items():
                if need.get(s, 0) < v:
                    need[s] = v
        for b in writes:
            t = b.T if isinstance(b, Buf) else b
            for s, v in t.w.items():
                if need.get(s, 0) < v:
                    need[s] = v
            for s, v in t.r.items():
                if need.get(s, 0) < v:
                    need[s] = v
        raw_self = 0
        for b in reads:
            t = b.T if isinstance(b, Buf) else b
            raw_self = max(raw_self, t.w.get(E.sem, 0))
        waits = []
        for s, v in need.items():
            if s is E.sem:
                if eng != "tensor" and raw_self > E.seen.get(s, 0):
                    E.seen[s] = raw_self
                    waits.append((s, raw_self))
                continue
            if E.seen.get(s, 0) >= v:
                continue
            E.seen[s] = v
            waits.append((s, v))
        if dsem is not None:
            dsem.n += 16
            sig = (dsem, dsem.n, 16)
        else:
            E.sem.n += 1
            sig = (E.sem, E.sem.n, 1)
        E.ops.append((waits, fn, sig))
        self.nops += 1
        s, v = sig[0], sig[1]
        for b in reads:
            t = b.T if isinstance(b, Buf) else b
            if t.r.get(s, 0) < v:
                t.r[s] = v
        for b in writes:
            t = b.T if isinstance(b, Buf) else b
            if t.w.get(s, 0) < v:
                t.w[s] = v

    def finish(self, eng, tiles):
        E = self.engs[eng]
        need = {}
        for b in tiles:
            t = b.T if isinstance(b, Buf) else b
            for d in (t.w, t.r):
                for s, v in d.items():
                    if need.get(s, 0) < v:
                        need[s] = v
        E.ops.append(([(s, v) for s, v in need.items() if s is not E.sem], None, None))

    def emit(self):
        with self.nc.Block() as block:
            for n, E in self.engs.items():
                def body(e, E=E):
                    for waits, fn, sig in E.ops:
                        for s, v in waits:
                            e.wait_ge(s.h, v)
                        if fn is None:
                            continue
                        fn(e).then_inc(sig[0].h, sig[2])
                getattr(block, n)(body)


def panel_defs():
    P = []
    for nm in ("q", "k", "qi"):
        P.append((nm, "w_in", 0, 1024, IN_OFF[nm], 512))
    P.append(("kiwi", "w_in", 0, 1024, IN_OFF["kiwi"], 72))
    for nm in ("xr", "gr", "zu", "v", "zv"):
        P.append((nm, "w_in", 0, 1024, IN_OFF[nm], 512))
    for n in range(3):
        for hf in range(2):
            P.append(("gate%d_%d" % (n, hf), "w_in", 0, 1024, IN_OFF["gate"] + n * 1024 + hf * 512, 512))
    for n in range(3):
        for hf in range(2):
            P.append(("br%d_%d" % (n, hf), "w_branch%d" % n, 0, 512, hf * 512, 512))
    for hf in range(2):
        P.append(("out_%d" % hf, "w_out", 0, 1024, hf * 512, 512))
    for g in range(8):
        P.append(("up_%d" % g, "w_ffn_up", 0, 1024, g * 512, 512))
        P.append(("dn_%d" % g, "w_ffn_down", g * 512, 512, 0, 1024))
    P.append(("ple", "w_ple", 0, 256, 0, 1024))
    for hf in range(2):
        P.append(("pg_%d" % hf, "w_ple_gate", 0, 1024, hf * 512, 512))
    return P


def build_program(L, depth=DEPTH, dbg=None):
    NT = L // T
    nc = bass.Bass("TRN2", target_bir_lowering=False)
    dram = lambda name, shape, dt, kind="ExternalInput": nc.dram_tensor(name, shape, dt, kind=kind).ap()
    x_in = dram("x", [L, D], F32)
    p_in = dram("p", [depth, L, PLE], F32)
    pos_in = dram("pos", [128, L], I32)
    wsrc = {
        "w_in": dram("w_in", [depth, D, 7240], F32),
        "w_branch": dram("w_branch", [depth, 3, 512, D], F32),
        "w_out": dram("w_out", [depth, D, D], F32),
        "w_ffn_up": dram("w_ffn_up", [depth, D, FFN], F32),
        "w_ffn_down": dram("w_ffn_down", [depth, FFN, D], F32),
        "w_ple": dram("w_ple", [depth, PLE, D], F32),
        "w_ple_gate": dram("w_ple_gate", [depth, D, D], F32),
    }
    cols_in = dram("cols", [depth, 128, 48], F32)
    gb_in = dram("gb", [depth, 128, 3 * D + 512], F32)
    wbd_in = dram("wbd", [depth, 128, 8, 128], F32)
    wsp_in = dram("wsp", [depth, 128, 8, 128], F32)
    bsp_in = dram("bsp", [depth, 8, 128], F32)
    cb_in = dram("cb", [128, 256 + 512], BF16)
    cf_in = dram("cf", [128, 4 * 128 + 1], F32)
    y_out = dram("y", [L, D], F32, kind="ExternalOutput")
    xbuf = dram("xbuf", [L, D], F32, kind="Internal")
    KTc = [dram("ktc%d" % l, [4, 128, L], BF16, kind="Internal") for l in range(depth)]
    Vc = [dram("vc%d" % l, [L, 520], BF16, kind="Internal") for l in range(depth)]
    pdefs = panel_defs()
    Wp = [{nm: dram("wp%d_%s" % (l, nm), [nr, ncol], BF16, kind="Internal") for (nm, _, _, nr, _, ncol) in pdefs}
          for l in range(depth)]
    dbg_out = {}
    if dbg:
        for k, shp in dbg.items():
            dbg_out[k] = dram("dbg_" + k, list(shp), F32, kind="ExternalOutput")

    with ExitStack() as st:
        fw = FW(nc, st)
        op = fw.op

        def sb(name, shape, dt):
            return Buf(st.enter_context(nc.sbuf_tensor("s_" + name, shape, dt)), name)

        PS = [Buf(st.enter_context(nc.psum_tensor("ps%d" % i, [128, 512], F32)), "ps%d" % i) for i in range(8)]

        def mm(out, lhsT, rhs, start, stop, R, W):
            op("tensor", lambda e: e.matmul(out, lhsT=lhsT, rhs=rhs, start=start, stop=stop), R, W)

        def tr(out, in_, ident, R, W):
            op("tensor", lambda e: e.transpose(out=out, in_=in_, identity=ident), R, W)

        def act(out, in_, func, R, W, **kw):
            op("scalar", lambda e: e.activation(out=out, in_=in_, func=func, **kw), R, W)

        def ts(out, in0, s1, s2, op0, op1, R, W, eng="vector"):
            if op1 is None:
                op(eng, lambda e: e.tensor_scalar(out=out, in0=in0, scalar1=s1, scalar2=None, op0=op0), R, W)
            else:
                op(eng, lambda e: e.tensor_scalar(out=out, in0=in0, scalar1=s1, scalar2=s2, op0=op0, op1=op1), R, W)

        def tt(out, in0, in1, o, R, W, eng="vector"):
            op(eng, lambda e: e.tensor_tensor(out=out, in0=in0, in1=in1, op=o), R, W)

        def stt(out, in0, s, in1, op0, op1, R, W):
            op("vector", lambda e: e.scalar_tensor_tensor(out=out, in0=in0, scalar=s, in1=in1, op0=op0, op1=op1), R, W)

        def cp(out, in_, R, W, eng="vector"):
            op(eng, lambda e: e.tensor_copy(out=out, in_=in_), R, W)

        def dma(out, in_, R, W, ds, eng="sync"):
            op(eng, lambda e: e.dma_start(out=out, in_=in_), R, W, dsem=ds)

        def memset(ap, val, W, eng="vector"):
            op(eng, lambda e: e.memset(ap, val), [], W)

        def reduce(out, in_, o, R, W):
            op("vector", lambda e: e.tensor_reduce(out=out, in_=in_, axis=AX.X, op=o), R, W)

        def recip(out, in_, R, W):
            op("vector", lambda e: e.reciprocal(out=out, in_=in_), R, W)

        def scan(out, d0, d1, init, R, W):
            op("vector", lambda e: e.tensor_tensor_scan(out=out, data0=d0, data1=d1, initial=init,
                                                        op0=ALU.mult, op1=ALU.add), R, W)

        def count_ge(out, in0, thr, cnt, R, W):
            op("vector", lambda e: e.tensor_scalar(out=out, in0=in0, scalar1=thr, scalar2=None, op0=ALU.is_ge,
                                                   op1=ALU.add, accum_out=cnt), R, W)

        def cpred(out, mask, data, R, W):
            op("vector", lambda e: e.copy_predicated(out=out, mask=mask, data=data), R, W)

        dbg_sem = fw.dsem("dbg")
        dbg_tiles = []

        def dump(name, ap, R):
            if name in dbg_out:
                t = Tile("dbg")
                dma(dbg_out[name], ap, R, [t], dbg_sem, eng="gpsimd")
                dbg_tiles.append(t)
                del dbg_out[name]

        cb = sb("cb", [128, 768], BF16)
        cf = sb("cf", [128, 513], F32)
        dma(cb[:], cb_in, [], [cb], fw.dsem("c0"))
        dma(cf[:], cf_in, [], [cf], fw.dsem("c1"))
        identb = cb[:, 0:128]
        Rm = cb[:, 128:256]
        esel = cb[0:8, 256:768].rearrange("p (c f) -> p c f", c=4)
        identf = cf[:, 0:128]
        negmask = cf[:, 128:256]
        posfill = cf[:, 256:384]
        tril01 = cf[:, 384:512]
        invf = cf[:, 512:513]

        WT = [dict() for _ in range(depth)]
        for l in range(depth):
            wcs = fw.dsem("wcast%d" % l)
            for (nm, src, r0, nr, c0, ncol) in pdefs:
                if src.startswith("w_branch"):
                    s_ap = wsrc["w_branch"][l, int(src[-1]), r0:r0 + nr, c0:c0 + ncol]
                else:
                    s_ap = wsrc[src][l, r0:r0 + nr, c0:c0 + ncol]
                t = Tile("wp")
                WT[l][nm] = t
                step = 512
                for rr in range(0, nr, step):
                    n2 = min(step, nr - rr)
                    dma(Wp[l][nm][rr:rr + n2, :], s_ap[rr:rr + n2, :], [], [t], wcs, eng="gpsimd")
            for t in WT[l].values():
                t.w = {wcs: wcs.n}

        ring = [sb("ring%d" % i, [128, SLOTB], BF16) for i in range(NSLOT)]
        ring_sem = [fw.dsem("ring%d" % i) for i in range(NSLOT)]
        kiT = sb("kiT", [128, L], BF16)
        xt = sb("xt", [128, NSUB, D], F32)
        pt = sb("pt", [128, NSUB, PLE], F32)
        ptb = sb("ptb", [128, NSUB, PLE], BF16)
        posi = sb("posi", [128, T], I32)
        hT = sb("hT", [128, 8, T], BF16)
        pT = sb("pT", [128, 2, T], BF16)
        qT = sb("qT", [128, 4, 2, T], BF16)
        qiT = sb("qiT", [128, 4, T], BF16)
        KTs = sb("KTs", [128, 4, T], BF16)
        Vs = sb("Vs", [128, NSUB, 520], BF16)
        cosT = sb("cosT", [128, T], F32)
        sinT = sb("sinT", [128, T], F32)
        rtmp = sb("rtmp", [128, 3, T], F32)
        xb16 = sb("xb16", [128, T], BF16)
        xrbuf = sb("xrbuf", [128, 4, 3 + T], F32)
        grT = sb("grT", [128, 4, T], BF16)
        zuT = sb("zuT", [128, 4, T], BF16)
        vn = sb("vn", [128, NSUB, 512], BF16)
        hst = sb("hst", [128, 4], F32)
        ybT = sb("ybT", [128, 4, T], BF16)
        ycT = sb("ycT", [128, 4, T], BF16)
        yaT = sb("yaT", [128, 4, T], BF16)
        yaTt = sb("yaTt", [64, T], BF16)
        wis = sb("wis", [128, NSUB, 8], F32)
        diag = sb("diag", [128, 8, 128], BF16)
        small = sb("small", [128, 32], F32)
        smalli = sb("smalli", [128, 4], I32)
        bis = sb("bis", [128, 16], F32)
        bis2 = sb("bis2", [128, 16], F32)
        Tmid = Tile("mid")
        Tcnt = Tile("cnt")
        Tsa = Tile("sa")
        big = sb("big", [128, 6144], F32)
        TS_ = big.T
        TR_ = Tile("bigR")
        score_ap = big[:, 0:L]
        Rrelu = big[:, 4096:6144].bitcast(BF16).rearrange("p (h w) -> p h w", h=8)
        masks = [sb("mask%d" % s, [128, L], BF16) for s in range(NSUB)]
        maskTk = sb("maskTk", [128, 4, T], BF16)
        PT = [sb("PT%d" % i, [128, 2, T], BF16) for i in range(4)]
        xs = sb("xs", [128, D], BF16)
        accm = sb("accm", [128, 8 * T], F32)

        def view(ap, tile):
            v = Buf.__new__(Buf)
            v.t = ap
            v.T = tile
            return v

        acc = view(accm[0:65, :].rearrange("p (h t) -> p h t", h=8), accm.T)
        lt = view(accm[:, 0:5 * T].rearrange("p (k t) -> p k t", k=5), accm.T)
        gz = view(accm[:, 5 * T:5 * T + 512], accm.T)
        xcb = view(accm[:, 5 * T + 512:5 * T + 512 + T // 2].bitcast(BF16), accm.T)
        sc1 = sb("sc1", [128, L], F32)
        rhl2 = [sb("rhl%d" % i, [65, 2, T], BF16) for i in range(2)]
        onesb = sb("onesb", [65, 64], BF16)
        gt = sb("gt", [128, T], F32)
        tmpf = sb("tmpf", [128, 512], F32)
        gsig = sb("gsig", [128, 8, T], BF16)
        merged = big[:, 0:8 * T].rearrange("p (c t) -> p c t", c=8)
        o = 8 * T
        mergedT = big[:, o:o + 4 * T].bitcast(BF16).rearrange("p (c t) -> p c t", c=8)
        o += 4 * T
        fT = [big[:, o + i * 2 * T:o + (i + 1) * 2 * T].bitcast(BF16).rearrange("p (c t) -> p c t", c=4) for i in range(2)]
        o += 4 * T
        assert o <= 4096
        o = 4096
        rl = [big[:, o + i * (T // 2):o + (i + 1) * (T // 2)].bitcast(BF16) for i in range(2)]
        o += T
        sg = big[:, o:o + 512]
        o += 512
        ple_t = big[:, o:o + 1024]
        o += 1024
        assert o <= 6144
        wbdf = big[:, 4096:4096 + 1024].rearrange("p (c j) -> p c j", c=8)
        cols = sb("cols", [128, 48], F32)
        gbv = sb("gbv", [128, 512], F32)
        gcur = sb("gcur", [128, D], F32)
        gsem = fw.dsem("gain")
        wbd = sb("wbd", [128, 8, 128], BF16)
        wspT = sb("wspT", [128, 8, 128], BF16)
        bsp = sb("bsp", [8, 128], F32)
        bsph = sb("bsph", [8, 2, 128], BF16)
        c8 = sb("c8", [128, 8], F32)
        epsc = sb("epsc", [128, 4], F32)
        psem = [fw.dsem("par%d" % i) for i in range(5)]
        xsem = fw.dsem("xload")
        ptsem = fw.dsem("pload")
        possem = fw.dsem("posload")
        ssem = fw.dsem("store")
        kvsem = fw.dsem("kvstore")
        out_tile = Tile("out")

        class Stream:
            def __init__(self):
                self.plan = []
                self.issued = 0
                self.pos = 0

            def add(self, name, loader, deps, hoist=True):
                self.plan.append((name, loader, deps, hoist))

            def next(self, name, look=NSLOT - 1):
                i = self.pos
                assert self.plan[i][0] == name, (self.plan[i][0], name)
                while self.issued < len(self.plan) and (
                        self.issued <= i or (self.issued <= i + look and self.plan[self.issued][3])):
                    k = self.issued
                    _, loader, deps, _ = self.plan[k]
                    loader(ring[k % NSLOT], ring_sem[k % NSLOT], deps)
                    self.issued += 1
                self.pos += 1
                return ring[i % NSLOT]

        stream = Stream()

        def wloader(l, nm, nr, ncol):
            kc = nr // 128

            def f(slot, sem, deps):
                dst = slot[:, 0:kc * ncol].rearrange("p (k w) -> p k w", k=kc)
                src = Wp[l][nm].rearrange("(k p) w -> p k w", p=128)
                dma(dst, src, deps, [slot], sem)
            return f

        KVT = [[Tile("kv") for _ in range((L + KG - 1) // KG)] for _ in range(depth)]

        def kvloader(l, g, wd):
            def f(slot, sem, deps):
                dstk = slot[:, 0:4 * wd].rearrange("p (c w) -> p c w", c=4)
                dma(dstk, KTc[l][:, :, g * KG:g * KG + wd].rearrange("c p w -> p c w"), deps, [slot], sem)
                nb = wd // 128
                dstv = slot[:, 2048:2048 + nb * 520].rearrange("p (b f) -> p b f", b=nb)
                dma(dstv, Vc[l][g * KG:g * KG + wd, :].rearrange("(b p) f -> p b f", p=128), deps, [slot], sem)
            return f

        pinfo = {nm: (nr, ncol) for (nm, _, _, nr, _, ncol) in pdefs}

        def plan_w(l, nm):
            nr, ncol = pinfo[nm]
            stream.add("%d_%s" % (l, nm), wloader(l, nm, nr, ncol), [WT[l][nm]])

        def kv_groups(j):
            nkeys = (j + 1) * T
            out = []
            g = 0
            while g * KG < nkeys:
                out.append((g, min(KG, nkeys - g * KG)))
                g += 1
            return out

        for l in range(depth):
            for j in range(NT):
                for nm in ("q", "k", "qi", "kiwi", "xr", "gr", "zu", "v", "zv"):
                    plan_w(l, nm)
                for n in (2, 1):
                    plan_w(l, "gate%d_0" % n)
                    plan_w(l, "gate%d_1" % n)
                    plan_w(l, "br%d_0" % n)
                    plan_w(l, "br%d_1" % n)
                grps = kv_groups(j)
                for (g, wd) in grps:
                    last = (g == grps[-1][0])
                    stream.add("%d_kv%d_%d" % (l, j, g), kvloader(l, g, wd), [KVT[l][g]], hoist=not last)
                for n in (0,):
                    plan_w(l, "gate%d_0" % n)
                    plan_w(l, "gate%d_1" % n)
                    plan_w(l, "br%d_0" % n)
                    plan_w(l, "br%d_1" % n)
                plan_w(l, "out_0")
                plan_w(l, "out_1")
                plan_w(l, "up_0")
                for g in range(8):
                    if g + 1 < 8:
                        plan_w(l, "up_%d" % (g + 1))
                    plan_w(l, "dn_%d" % g)
                plan_w(l, "ple")
                plan_w(l, "pg_0")
                plan_w(l, "pg_1")

        def wview(slot, nm):
            nr, ncol = pinfo[nm]
            kc = nr // 128
            return slot[:, 0:kc * ncol].rearrange("p (k w) -> p k w", k=kc)

        sm_i = [0]

        def sm():
            i = sm_i[0] % 32
            sm_i[0] += 1
            return small[:, i:i + 1]

        memset(epsc[:, 0:1], EPS, [epsc])
        memset(epsc[:, 1:2], math.pi / 2, [epsc])
        memset(epsc[:, 2:3], 1.0, [epsc])
        memset(epsc[:, 3:4], -BIGM, [epsc])
        memset(Vs[:, :, :], 1.0, [Vs])
        memset(onesb[:, :], 1.0, [onesb])
        memset(qT[:, :, :, :], 0.0, [qT])

        def rstd_from_ss(ss_ap, n):
            a = sm()
            b = sm()
            act(a, ss_ap, AF.Sqrt, [small, epsc], [small], scale=1.0 / n, bias=epsc[:, 0:1])
            recip(b, a, [small], [small])
            return b

        pi = [0]

        def nps():
            pi[0] += 1
            return PS[pi[0] % 4]

        def norm_transpose(gcol0):
            for s in range(NSUB):
                if gcol0 is not None:
                    ss = sm()
                    act(tmpf[:, 0:512], xt[:, s, 0:512], AF.Square, [xt, small], [tmpf, small], accum_out=ss)
                    ss2 = sm()
                    act(tmpf[:, 0:512], xt[:, s, 512:1024], AF.Square, [xt, small], [tmpf, small], accum_out=ss2)
                    ss3 = sm()
                    tt(ss3, ss, ss2, ALU.add, [small], [small])
                    rs = rstd_from_ss(ss3, D)
                    ts(xs[:], xt[:, s, :], rs, None, ALU.mult, None, [xt, small], [xs])
                else:
                    cp(xs[:], xt[:, s, :], [xt], [xs])
                psb = PS[7][:].bitcast(BF16)
                for c in range(8):
                    tr(psb[:, c * 128:(c + 1) * 128], xs[:, c * 128:(c + 1) * 128], identb, [xs, cb], [PS[7]])
                src = psb[:, 0:1024].rearrange("p (c t) -> p c t", c=8)
                dst = hT[:, :, s * 128:(s + 1) * 128]
                if gcol0 is not None:
                    g_ap = cols[:, gcol0:gcol0 + 8].unsqueeze(2).to_broadcast([128, 8, 128])
                    tt(dst, src, g_ap, ALU.mult, [PS[7], cols], [hT])
                else:
                    cp(dst, src, [PS[7]], [hT])

        def post_norm_residual(banks, gcol, s):
            ssa = []
            for hf in range(2):
                a = sm()
                act(tmpf[:, 0:512], banks[hf][:, 0:512], AF.Square, [banks[hf], small], [tmpf, small], accum_out=a)
                ssa.append(a)
            s3 = sm()
            tt(s3, ssa[0], ssa[1], ALU.add, [small], [small])
            rs = rstd_from_ss(s3, D)
            for hf in range(2):
                stt(tmpf[:, 0:512], banks[hf][:, 0:512], rs, gcur[:, hf * 512:(hf + 1) * 512],
                    ALU.mult, ALU.mult, [banks[hf], small, gcur], [tmpf])
                tt(xt[:, s, hf * 512:(hf + 1) * 512], xt[:, s, hf * 512:(hf + 1) * 512], tmpf[:, 0:512], ALU.add,
                   [xt, tmpf], [xt])

        def merge_tile(dst, src, as_write=False):
            for a, b in ((dst.w, src.w), (dst.r, src.r)):
                for k_, v_ in b.items():
                    if a.get(k_, 0) < v_:
                        a[k_] = v_
            if as_write:
                for k_, v_ in src.r.items():
                    if dst.w.get(k_, 0) < v_:
                        dst.w[k_] = v_

        def fm_chunk(panel, pv, c, ps, M=128):
            for kc in range(8):
                mm(ps[0:M, 0:T], pv[:, kc, c * 128:c * 128 + M], hT[:, kc, :], kc == 0, kc == 7, [panel, hT], [ps])

        def rope_evac(ps, dst, W, split=None):
            act(xb16[:], ps[:, 0:T], AF.Copy, [ps], [xb16])
            mm(ps[:, T:2 * T], Rm, xb16[:], True, True, [cb, xb16], [ps])
            tt(rtmp[:, 0, :], ps[:, 0:T], cosT[:], ALU.mult, [ps, cosT], [rtmp])
            tt(rtmp[:, 1, :], ps[:, T:2 * T], sinT[:], ALU.mult, [ps, sinT], [rtmp])
            if split is None:
                tt(dst, rtmp[:, 0, :], rtmp[:, 1, :], ALU.add, [rtmp], W)
            else:
                for hh in range(2):
                    tt(split[hh * 64:(hh + 1) * 64, hh, :], rtmp[hh * 64:(hh + 1) * 64, 0, :], rtmp[hh * 64:(hh + 1) * 64, 1, :],
                       ALU.add, [rtmp], W)

        XTprev = None
        for l in range(depth):
            src_x = x_in if l == 0 else xbuf
            dst_x = y_out if l == depth - 1 else xbuf
            XT = [Tile("xd") for _ in range(NT)] if l < depth - 1 else None
            dma(cols[:], cols_in[l], [], [cols], psem[0])
            dma(gbv[:], gb_in[l, :, 3 * D:3 * D + 512], [], [gbv], psem[1])
            dma(gcur[:], gb_in[l, :, 0:D], [], [gcur], gsem)
            dma(bsp[:], bsp_in[l], [], [bsp], psem[2])
            cp(bsph[:, 0, :], bsp[:], [bsp], [bsph])
            tt(bsp[:], bsp[:], bsph[:, 0, :], ALU.subtract, [bsph], [bsp])
            cp(bsph[:, 1, :], bsp[:], [bsp], [bsph])
            dma(wbdf, wbd_in[l], [], [TR_], psem[3])
            cp(wbd[:], wbdf, [TR_], [wbd])
            dma(wbdf, wsp_in[l], [], [TR_], psem[4])
            tt(wspT[:], wbdf, tril01.unsqueeze(1).to_broadcast([128, 8, 128]), ALU.mult, [TR_, cf], [wspT])
            act(c8[:, 4:8], cols[:, 44:48], AF.Exp, [cols], [c8], scale=-1.0)
            ts(c8[:, 0:4], c8[:, 4:8], -0.25, 1.0 / 3.0, ALU.mult, ALU.add, [c8], [c8])
            tt(c8[:, 0:4], c8[:, 0:4], c8[:, 4:8], ALU.mult, [c8], [c8])
            ts(c8[:, 0:4], c8[:, 0:4], -1.0, 0.5, ALU.mult, ALU.add, [c8], [c8])
            tt(c8[:, 0:4], c8[:, 0:4], c8[:, 4:8], ALU.mult, [c8], [c8])
            ts(c8[:, 0:4], c8[:, 0:4], -1.0, 1.0, ALU.mult, ALU.add, [c8], [c8])
            tt(c8[:, 0:4], c8[:, 0:4], c8[:, 4:8], ALU.mult, [c8], [c8])
            ts(c8[:, 0:4], c8[:, 0:4], -8.0, None, ALU.mult, None, [c8], [c8])
            ts(c8[:, 4:8], c8[:, 0:4], 2.0, None, ALU.mult, None, [c8], [c8])
            memset(xrbuf[:, :, 0:3], 0.0, [xrbuf])
            memset(hst[:], 0.0, [hst])

            for j in range(NT):
                t0 = j * T
                first_tile = (l == 0 and j == 0)
                rd = [XTprev[j]] if (l > 0) else []
                dma(xt[:], src_x[t0:t0 + T, :].rearrange("(s p) d -> p s d", p=128), rd, [xt], xsem)
                dma(pt[:], p_in[l, t0:t0 + T, :].rearrange("(s p) d -> p s d", p=128), [], [pt], ptsem)
                dma(posi[:], pos_in[:, t0:t0 + T], [], [posi], possem)
                ang = rtmp[:, 0, :]
                u = rtmp[:, 1, :]
                rr = rtmp[:, 2, :]
                cp(u, posi[:], [posi], [rtmp])
                ts(ang, u, invf, None, ALU.mult, None, [rtmp, cf], [rtmp])
                ts(u, ang, 1.0 / (2 * math.pi), 12582912.0, ALU.mult, ALU.add, [rtmp], [rtmp])
                ts(u, u, -12582912.0, None, ALU.add, None, [rtmp], [rtmp])
                C1 = 6.28125
                C2 = float(np.float32(2 * math.pi - C1))
                C3 = float(2 * math.pi - C1 - C2)
                stt(rr, u, -C1, ang, ALU.mult, ALU.add, [rtmp], [rtmp])
                stt(rr, u, -C2, rr, ALU.mult, ALU.add, [rtmp], [rtmp])
                stt(rr, u, -C3, rr, ALU.mult, ALU.add, [rtmp], [rtmp])
                act(sinT[:], rr, AF.Sin, [rtmp], [sinT])
                stt(u, rr, -1.0, rr, ALU.mult, ALU.max, [rtmp], [rtmp])
                act(cosT[:], u, AF.Sin, [rtmp, epsc], [cosT], scale=-1.0, bias=epsc[:, 1:2])
                norm_transpose(0)
                if first_tile:
                    dump("hT", hT[:, 0, :], [hT])
                    dump("cosT", cosT[:], [cosT])
                    dump("sinT", sinT[:], [sinT])

                for nm, dstb in (("q", qT), ("k", KTs), ("qi", qiT)):
                    panel = stream.next("%d_%s" % (l, nm))
                    pv = wview(panel, nm)
                    pss = [nps() for _ in range(4)]
                    fm_chunk(panel, pv, 0, pss[0])
                    for c in range(4):
                        if c + 1 < 4:
                            fm_chunk(panel, pv, c + 1, pss[c + 1])
                        if nm == "q":
                            rope_evac(pss[c], None, [dstb], split=qT[:, c, :, :])
                        else:
                            rope_evac(pss[c], dstb[:, c, :], [dstb])
                panel = stream.next("%d_kiwi" % l)
                pv = wview(panel, "kiwi")
                ps = nps()
                for half in range(2):
                    for kc in range(8):
                        mm(ps[half * 64:(half + 1) * 64, 0:T], pv[:, kc, 0:64], hT[:, kc, :], kc == 0, kc == 7,
                           [panel, hT], [ps])
                rope_evac(ps, kiT[:, t0:t0 + T], [kiT])
                for s in range(NSUB):
                    ps = nps()
                    for kc in range(8):
                        mm(ps[:, 0:8], hT[:, kc, s * 128:(s + 1) * 128], pv[:, kc, 64:72], kc == 0, kc == 7,
                           [panel, hT], [ps])
                    cp(wis[:, s, :], ps[:, 0:8], [ps], [wis])
                dma(KTc[l][:, :, t0:t0 + T].rearrange("c p w -> p c w"), KTs[:], [KTs], [KVT[l][t0 // KG]], kvsem, eng="gpsimd")
                if first_tile:
                    dump("qT", qT[:, 0, 0, :], [qT])
                    dump("kiT", kiT[:, 0:T], [kiT])
                    dump("wis", wis[:, 0, :], [wis])
                panel = stream.next("%d_xr" % l)
                pv = wview(panel, "xr")
                for c in range(4):
                    ps = nps()
                    fm_chunk(panel, pv, c, ps)
                    act(xrbuf[:, c, 3:3 + T], ps[:, 0:T], AF.Copy, [ps], [xrbuf])
                for nm, dstb in (("gr", grT), ("zu", zuT)):
                    panel = stream.next("%d_%s" % (l, nm))
                    pv = wview(panel, nm)
                    for c in range(4):
                        ps = nps()
                        fm_chunk(panel, pv, c, ps)
                        act(dstb[:, c, :], ps[:, 0:T], AF.Gelu_apprx_tanh, [ps], [dstb])
                panel = stream.next("%d_v" % l)
                pv = wview(panel, "v")
                for s in range(NSUB):
                    ps = nps()
                    for kc in range(8):
                        mm(ps[:, 0:512], hT[:, kc, s * 128:(s + 1) * 128], pv[:, kc, :], kc == 0, kc == 7, [panel, hT], [ps])
                    dstv = Vs[:, s, :].rearrange("p (h f) -> p h f", h=8)[:, :, 0:64]
                    act(dstv, ps[:, 0:512].rearrange("p (h f) -> p h f", h=8), AF.Copy, [ps], [Vs])
                dma(Vc[l][t0:t0 + T, :].rearrange("(s p) f -> p s f", p=128), Vs[:], [Vs], [KVT[l][t0 // KG]], kvsem, eng="gpsimd")
                panel = stream.next("%d_zv" % l)
                pv = wview(panel, "zv")
                for s in range(NSUB):
                    ps = nps()
                    for kc in range(8):
                        mm(ps[:, 0:512], hT[:, kc, s * 128:(s + 1) * 128], pv[:, kc, :], kc == 0, kc == 7, [panel, hT], [ps])
                    act(gz[:], ps[:, 0:512], AF.Gelu_apprx_tanh, [ps], [gz])
                    ss = sm()
                    act(tmpf[:, 0:512], gz[:], AF.Square, [gz, small], [tmpf, small], accum_out=ss)
                    rs = rstd_from_ss(ss, 512)
                    stt(vn[:, s, :], gz[:], rs, gbv[:], ALU.mult, ALU.mult, [gz, small, gbv], [vn])

                mpi = [0]

                def mps():
                    mpi[0] += 1
                    return PS[5 + mpi[0] % 3]

                def gen_mixC():
                    for s in range(NSUB):
                        for cpair in range(4):
                            ps = mps()
                            for gg in range(2):
                                g = cpair * 2 + gg
                                mm(ps[gg * 64:(gg + 1) * 64, 0:128], vn[:, s, g * 64:(g + 1) * 64], wspT[:, g, :], True, False,
                                   [vn, wspT], [ps])
                            mm(ps[:, 0:128], esel[:, cpair, :], bsph[:, 0, :], False, False, [cb, bsph], [ps])
                            mm(ps[:, 0:128], esel[:, cpair, :], bsph[:, 1, :], False, True, [cb, bsph], [ps])
                            tt(ycT[:, cpair, s * 128:(s + 1) * 128], ps[:, 0:128], zuT[:, cpair, s * 128:(s + 1) * 128], ALU.mult,
                               [ps, zuT], [ycT])
                            yield

                def gen_mixB():
                    for c in range(4):
                        xc = lt[:, 0, :]
                        ts(xc, xrbuf[:, c, 0:T], cols[:, 16 + c:17 + c], cols[:, 32 + c:33 + c], ALU.mult, ALU.add, [xrbuf, cols], [lt])
                        for jj in range(1, 4):
                            stt(xc, xrbuf[:, c, jj:jj + T], cols[:, 16 + jj * 4 + c:17 + jj * 4 + c], xc, ALU.mult, ALU.add,
                                [xrbuf, cols, lt], [lt])
                        yield
                        cp(xrbuf[:, c, 0:3], xrbuf[:, c, T:T + 3], [xrbuf], [xrbuf])
                        act(xcb[:], xc, AF.Copy, [lt], [xcb])
                        ps = mps()
                        mm(ps[:, 0:T], wbd[:, c, :], xcb[:], True, True, [wbd, xcb], [ps])
                        mm(ps[:, T:2 * T], wbd[:, 4 + c, :], xcb[:], True, True, [wbd, xcb], [ps])
                        yield
                        rg = lt[:, 1, :]
                        ig = lt[:, 2, :]
                        av = lt[:, 3, :]
                        sq = lt[:, 4, :]
                        act(rg, ps[:, 0:T], AF.Sigmoid, [ps, cols], [lt], bias=cols[:, 36 + c:37 + c])
                        act(ig, ps[:, T:2 * T], AF.Sigmoid, [ps, cols], [lt], bias=cols[:, 40 + c:41 + c])
                        yield
                        act(av, rg, AF.Exp, [lt, c8], [lt], scale=c8[:, c:c + 1])
                        act(sq, rg, AF.Exp, [lt, c8], [lt], scale=c8[:, 4 + c:5 + c])
                        act(sq, sq, AF.Sqrt, [lt, epsc], [lt], scale=-1.0, bias=epsc[:, 2:3])
                        yield
                        tt(ig, ig, xc, ALU.mult, [lt], [lt])
                        tt(ig, ig, sq, ALU.mult, [lt], [lt])
                        hh = lt[:, 1, :]
                        scan(hh, av, ig, hst[:, c:c + 1], [lt, hst], [lt])
                        yield
                        cp(hst[:, c:c + 1], hh[:, T - 1:T], [lt], [hst])
                        tt(ybT[:, c, :], hh, grT[:, c, :], ALU.mult, [lt, grT], [ybT])
                        yield

                grps = kv_groups(j)
                nkeys = (j + 1) * T
                TRh = [Tile("relu%d" % h) for h in range(8)]

                SC = [(score_ap, TS_), (sc1[:, 0:L], sc1.T)]

                def gen_I(s):
                    N = t0 + 128 * (s + 1)
                    score_s, TSs = SC[s]
                    for h in range(8):
                        merge_tile(TRh[h], TR_, as_write=True)
                        act(diag[:, h, :], identf, AF.Copy, [cf, wis], [diag], scale=wis[:, s, h:h + 1])
                    yield
                    ngrp = (N + KG - 1) // KG
                    for kg in range(ngrp):
                        wd = min(KG, N - kg * KG)
                        for h in range(8):
                            ps = PS[h % 4]
                            r0 = (h % 2) * 64
                            mm(ps[:, 0:wd], qiT[r0:r0 + 64, h // 2, s * 128:(s + 1) * 128],
                               kiT[r0:r0 + 64, kg * KG:kg * KG + wd], True, True, [qiT, kiT], [ps])
                            if h % 2 == 0:
                                act(Rrelu[:, h, 0:wd], ps[:, 0:wd], AF.Relu, [ps], [TRh[h]])
                            else:
                                ts(Rrelu[:, h, 0:wd], ps[:, 0:wd], 0.0, None, ALU.max, None, [ps], [TRh[h]])
                            if h % 2 == 1:
                                yield
                        for h in range(8):
                            mm(PS[4][:, 0:wd], diag[:, h, :], Rrelu[:, h, 0:wd], h == 0, h == 7, [diag, TRh[h]], [PS[4]])
                        if kg == ngrp - 1:
                            if wd > 128:
                                cp(score_s[:, kg * KG:kg * KG + wd - 128], PS[4][:, 0:wd - 128], [PS[4]], [TSs])
                            tt(score_s[:, N - 128:N], PS[4][:, wd - 128:wd], negmask, ALU.add, [PS[4], cf], [TSs])
                            tt(tmpf[:, 0:128], PS[4][:, wd - 128:wd], posfill, ALU.add, [PS[4], cf], [tmpf])
                        else:
                            cp(score_s[:, kg * KG:kg * KG + wd], PS[4][:, 0:wd], [PS[4]], [TSs])
                        yield
                    for h in range(8):
                        merge_tile(TR_, TRh[h])

                def gen_B(s):
                    N = t0 + 128 * (s + 1)
                    score_s, TSs = SC[s]
                    hi0 = bis[:, 0:1]
                    lo = bis[:, 1:2]
                    w0 = bis[:, 2:3]
                    reduce(hi0, score_s[:, 0:N], ALU.max, [TSs, bis], [bis])
                    reduce(lo, tmpf[:, 0:128], ALU.min, [tmpf, bis], [bis])
                    if N > 128:
                        m1 = bis[:, 3:4]
                        reduce(m1, score_s[:, 0:N - 128], ALU.min, [TSs, bis], [bis])
                        tt(lo, lo, m1, ALU.min, [bis], [bis])
                    tt(w0, hi0, lo, ALU.subtract, [bis], [bis])
                    yield
                    mk = masks[s]
                    if N > TOPK:
                        Nd = max(128, (int(N * 0.42) // 128) * 128)
                        Na = N - Nd
                        if s == 0:
                            junkA, TJ = masks[1], masks[1].T
                        else:
                            junkA, TJ = big[:, 4096:6144].bitcast(BF16), TR_
                        for it in range(NBIS):
                            k4 = it % 4
                            mid = bis2[:, k4:k4 + 1]
                            cnt = bis2[:, 4 + k4:5 + k4]
                            vv = bis2[:, 8 + k4:9 + k4]
                            sA = bis2[:, 12 + k4:13 + k4]
                            stt(mid, w0, 0.5 ** (it + 1), lo, ALU.mult, ALU.add, [bis], [Tmid])
                            act(junkA[:, 0:Na], score_s[:, Nd:N], AF.Sign, [TSs, Tmid], [TJ, Tsa], scale=-1.0, bias=mid,
                                accum_out=sA)
                            count_ge(mk[:, 0:Nd], score_s[:, 0:Nd], mid, cnt, [TSs, Tmid], [mk, Tcnt])
                            stt(vv, cnt, 2.0, sA, ALU.mult, ALU.subtract, [Tcnt, Tsa], [Tcnt])
                            ge = smalli[:, k4:k4 + 1]
                            ts(ge, vv, float(2 * TOPK - Na), None, ALU.is_ge, None, [Tcnt], [smalli])
                            cpred(lo, ge, mid, [Tmid, smalli], [bis])
                            yield
                    ts(mk[:, 0:N], score_s[:, 0:N], lo, None, ALU.is_ge, None, [TSs, bis], [mk])
                    if N < nkeys:
                        memset(mk[:, N:nkeys], 0.0, [mk], eng="gpsimd")
                    yield

                def gen_G(items):
                    for ni, n in items:
                        for hf in range(2):
                            gpan = stream.next("%d_gate%d_%d" % (l, n, hf))
                            pv = wview(gpan, "gate0_0")
                            for c in range(4):
                                ps = nps()
                                fm_chunk(gpan, pv, c, ps)
                                act(gsig[:, hf * 4 + c, :], ps[:, 0:T], AF.Sigmoid, [ps], [gsig])
                                yield
                        ysrc = {2: ycT, 1: ybT, 0: yaT}[n]
                        for hf in range(2):
                            bpan = stream.next("%d_br%d_%d" % (l, n, hf))
                            pv = wview(bpan, "br0_0")
                            for c in range(4):
                                dchunk = hf * 4 + c
                                ps = nps()
                                for kc in range(4):
                                    mm(ps[:, 0:T], pv[:, kc, c * 128:(c + 1) * 128], ysrc[:, kc, :], kc == 0, kc == 3,
                                       [bpan, ysrc], [ps])
                                if ni == 0:
                                    tt(merged[:, dchunk, :], ps[:, 0:T], gsig[:, dchunk, :], ALU.mult, [ps, gsig], [TS_])
                                else:
                                    tt(gt[:], ps[:, 0:T], gsig[:, dchunk, :], ALU.mult, [ps, gsig], [gt])
                                    dsto = mergedT if ni == 2 else merged
                                    tt(dsto[:, dchunk, :], merged[:, dchunk, :], gt[:], ALU.add, [TS_, gt], [TS_], eng="gpsimd")
                                yield

                def chain(*gens):
                    for g_ in gens:
                        yield from g_

                def run(*gens):
                    live = list(gens)
                    while live:
                        for g_ in list(live):
                            try:
                                next(g_)
                            except StopIteration:
                                live.remove(g_)

                def run_w(ga, gb_, kb):
                    la = lb = True
                    while la or lb:
                        if la:
                            try:
                                next(ga)
                            except StopIteration:
                                la = False
                        for _ in range(kb):
                            if lb:
                                try:
                                    next(gb_)
                                except StopIteration:
                                    lb = False

                run(chain(gen_mixC(), gen_mixB()), gen_I(0))
                if first_tile:
                    dump("ycT", ycT[:, 0, :], [ycT])
                    dump("ybT", ybT[:, 0, :], [ybT])
                    dump("score", score_ap[:, 0:128], [TS_])
                N1 = t0 + 256
                lenI = 1 + ((N1 + KG - 1) // KG) * 5
                lenB = 2 + (NBIS if (t0 + 128) > TOPK else 0)
                run_w(gen_B(0), gen_I(1), max(1, -(-lenI // lenB)))
                lenB1 = 2 + (NBIS if (t0 + 256) > TOPK else 0)
                run_w(gen_B(1), gen_G([(0, 2), (1, 1)]), max(1, -(-32 // lenB1)))

                first = True
                ucnt = [0]
                for gi, (g, wd) in enumerate(grps):
                    panel = stream.next("%d_kv%d_%d" % (l, j, g))
                    nb = wd // 128
                    KTv = panel[:, 0:4 * wd].rearrange("p (c w) -> p c w", c=4)
                    Vv = panel[:, 2048:2048 + nb * 520].rearrange("p (b f) -> p b f", b=nb)
                    psb = PS[6][:].bitcast(BF16)
                    for s in range(NSUB):
                        for b in range(nb):
                            tr(psb[:, (s * 4 + b) * 128:(s * 4 + b + 1) * 128],
                               masks[s][:, g * KG + b * 128:g * KG + (b + 1) * 128], identb, [masks[s], cb], [PS[6]])
                    for s in range(NSUB):
                        act(maskTk[:, 0:nb, s * 128:(s + 1) * 128],
                            psb[:, s * 512:s * 512 + nb * 128].rearrange("p (b t) -> p b t", b=nb),
                            AF.Copy, [PS[6]], [maskTk])
                    units = [(hp, b) for hp in range(4) for b in range(nb)]

                    def stageA(i):
                        hp, b = units[i]
                        ps = PS[(ucnt[0] + i) % 4]
                        mm(ps[:, 0:2 * T], KTv[:, hp, b * 128:(b + 1) * 128], qT[:, hp, :, :].rearrange("p a t -> p (a t)"),
                           True, True, [panel, qT], [ps])

                    def stageB(i):
                        hp, b = units[i]
                        ps = PS[(ucnt[0] + i) % 4]
                        ptile = PT[(ucnt[0] + i) % len(PT)]
                        act(ptile[:, :, :], ps[:, 0:2 * T].rearrange("p (a t) -> p a t", a=2), AF.Exp,
                            [ps], [ptile], scale=HD ** -0.5)
                        tt(ptile[:, :, :], ptile[:, :, :], maskTk[:, b:b + 1, :].to_broadcast([128, 2, T]), ALU.mult,
                           [ptile, maskTk], [ptile], eng=("vector" if (ucnt[0] + i) % 2 == 0 else "gpsimd"))

                    def stageD(i):
                        hp, b = units[i]
                        ptile = PT[(ucnt[0] + i) % len(PT)]
                        for hh in range(2):
                            h = 2 * hp + hh
                            pacc = PS[4 + hh]
                            mm(pacc[0:65, 0:T], Vv[:, b, h * 65:(h + 1) * 65], ptile[:, hh, :], b == 0, b == nb - 1,
                               [panel, ptile], [pacc])
                            if b == nb - 1:
                                if first:
                                    cp(acc[:, h, :], pacc[0:65, 0:T], [pacc], [acc])
                                else:
                                    tt(acc[:, h, :], acc[:, h, :], pacc[0:65, 0:T], ALU.add, [pacc, acc], [acc])

                    stageA(0)
                    if len(units) > 1:
                        stageA(1)
                    for i in range(len(units)):
                        if i + 2 < len(units):
                            stageA(i + 2)
                        stageB(i)
                        stageD(i)
                    ucnt[0] += len(units)
                    first = False
                accf = accm[0:65, :]
                act(accf[64:65, :], accf[64:65, :], AF.Ln, [acc], [acc])
                act(accf[64:65, :], accf[64:65, :], AF.Exp, [acc], [acc], scale=-1.0)
                for h in range(8):
                    rhl = rhl2[h % 2]
                    cp(rhl[64:65, 0, :], acc[64:65, h, :], [acc], [rhl])
                    tt(rhl[64:65, 1, :], acc[64:65, h, :], rhl[64:65, 0, :], ALU.subtract, [acc, rhl], [rhl])
                    ps = nps()
                    mm(ps[0:64, 0:T], onesb[64:65, 0:64], rhl[64:65, 0, :], True, False, [onesb, rhl], [ps])
                    mm(ps[0:64, 0:T], onesb[64:65, 0:64], rhl[64:65, 1, :], False, True, [onesb, rhl], [ps])
                    if h % 2 == 0:
                        tt(yaT[0:64, h // 2, :], acc[0:64, h, :], ps[0:64, 0:T], ALU.mult, [acc, ps], [yaT])
                    else:
                        tt(yaTt[:, :], acc[0:64, h, :], ps[0:64, 0:T], ALU.mult, [acc, ps], [yaTt])
                        ps2 = nps()
                        mm(ps2[64:128, 0:T], identb[0:64, 0:64], yaTt[:, :], True, True, [cb, yaTt], [ps2])
                        cp(yaT[64:128, h // 2, :], ps2[64:128, 0:T], [ps2], [yaT])
                if first_tile:
                    dump("yaT", yaT[:, 0, :], [yaT])
                if l == 0 and j == 1:
                    dump("yaT1", yaT[:, 0, :], [yaT])
                    dump("acc1", acc[:, 0, :], [acc])

                run(gen_G([(2, 0)]))
                op_ = [stream.next("%d_out_%d" % (l, hf), look=NSLOT - 1 - hf) for hf in range(2)]
                for s in range(NSUB):
                    banks = [PS[4 + 2 * (s % 2)], PS[5 + 2 * (s % 2)]]
                    for hf in range(2):
                        pv = wview(op_[hf], "out_0")
                        for kc in range(8):
                            mm(banks[hf][:, 0:512], mergedT[:, kc, s * 128:(s + 1) * 128], pv[:, kc, :], kc == 0, kc == 7,
                               [op_[hf], TS_], [banks[hf]])
                    post_norm_residual(banks, 0, s)
                dma(gcur[:], gb_in[l, :, D:2 * D], [], [gcur], gsem, eng="gpsimd")
                if first_tile:
                    dump("x1", xt[:, 0, :], [xt])
                norm_transpose(8)
                dbanks = [[PS[4], PS[5]], [PS[6], PS[7]]]
                TF = [Tile("fT0"), Tile("fT1")]
                Trl = [Tile("rl0"), Tile("rl1")]

                for i_ in range(2):
                    merge_tile(TF[i_], TS_, as_write=True)
                    merge_tile(Trl[i_], TR_, as_write=True)

                def ffn_up(g):
                    up = stream.next("%d_up_%d" % (l, g))
                    pv = wview(up, "up_0")
                    fTg = fT[g % 2]
                    for c in range(4):
                        ps = nps()
                        fm_chunk(up, pv, c, ps)
                        rlb = rl[c % 2]
                        act(rlb, ps[:, 0:T], AF.Relu, [ps], [Trl[c % 2]])
                        tt(fTg[:, c, :], rlb, rlb, ALU.mult, [Trl[c % 2]], [TF[g % 2]], eng="gpsimd")

                def ffn_dn(g):
                    dn = stream.next("%d_dn_%d" % (l, g))
                    dv = wview(dn, "dn_0")
                    fTg = fT[g % 2]
                    for s in range(NSUB):
                        for hf in range(2):
                            for c in range(4):
                                mm(dbanks[s][hf][:, 0:512], fTg[:, c, s * 128:(s + 1) * 128], dv[:, c, hf * 512:(hf + 1) * 512],
                                   g == 0 and c == 0, g == 7 and c == 3, [dn, TF[g % 2]], [dbanks[s][hf]])

                ffn_up(0)
                for g in range(8):
                    if g + 1 < 8:
                        ffn_up(g + 1)
                    ffn_dn(g)
                for i_ in range(2):
                    merge_tile(TS_, TF[i_])
                    merge_tile(TR_, Trl[i_])
                for s in range(NSUB):
                    post_norm_residual(dbanks[s], D, s)
                dma(gcur[:], gb_in[l, :, 2 * D:3 * D], [], [gcur], gsem, eng="gpsimd")
                if first_tile:
                    dump("x2", xt[:, 0, :], [xt])
                norm_transpose(None)
                for s in range(NSUB):
                    cp(ptb[:, s, :], pt[:, s, :], [pt], [ptb])
                    psb = PS[3][:].bitcast(BF16)
                    for c in range(2):
                        tr(psb[:, c * 128:(c + 1) * 128], ptb[:, s, c * 128:(c + 1) * 128], identb, [ptb, cb], [PS[3]])
                    cp(pT[:, :, s * 128:(s + 1) * 128], psb[:, 0:256].rearrange("p (c t) -> p c t", c=2), [PS[3]], [pT])
                plp = stream.next("%d_ple" % l)
                plv = wview(plp, "ple")
                pg = [stream.next("%d_pg_%d" % (l, hf), look=NSLOT - 2 - hf) for hf in range(2)]
                ple2 = big[:, 0:2 * D].rearrange("p (s d) -> p s d", s=2)
                sspa = []
                for s in range(NSUB):
                    ssp = []
                    for hf in range(2):
                        pa = PS[4 * (s % 2) + hf]
                        pgb = PS[4 * (s % 2) + 2 + hf]
                        for kc in range(2):
                            mm(pa[:, 0:512], pT[:, kc, s * 128:(s + 1) * 128], plv[:, kc, hf * 512:(hf + 1) * 512], kc == 0, kc == 1,
                               [plp, pT], [pa])
                        gv = wview(pg[hf], "pg_0")
                        for kc in range(8):
                            mm(pgb[:, 0:512], hT[:, kc, s * 128:(s + 1) * 128], gv[:, kc, :], kc == 0, kc == 7, [pg[hf], hT], [pgb])
                        act(sg, pgb[:, 0:512], AF.Sigmoid, [pgb], [TR_])
                        tt(ple2[:, s, hf * 512:(hf + 1) * 512], pa[:, 0:512], sg, ALU.mult, [pa, TR_], [TS_])
                        a = sm()
                        act(tmpf[:, 0:512], ple2[:, s, hf * 512:(hf + 1) * 512], AF.Square, [TS_, small], [tmpf, small], accum_out=a)
                        ssp.append(a)
                    sspa.append(ssp)
                for s in range(NSUB):
                    s3 = sm()
                    tt(s3, sspa[s][0], sspa[s][1], ALU.add, [small], [small])
                    rs = rstd_from_ss(s3, D)
                    for hf in range(2):
                        stt(tmpf[:, 0:512], ple2[:, s, hf * 512:(hf + 1) * 512], rs, gcur[:, hf * 512:(hf + 1) * 512],
                            ALU.mult, ALU.mult, [TS_, small, gcur], [tmpf])
                        tt(xt[:, s, hf * 512:(hf + 1) * 512], xt[:, s, hf * 512:(hf + 1) * 512], tmpf[:, 0:512], ALU.add,
                           [xt, tmpf], [xt])
                if j + 1 < NT:
                    dma(gcur[:], gb_in[l, :, 0:D], [], [gcur], gsem, eng="gpsimd")
                wt = [XT[j]] if XT is not None else [out_tile]
                dma(dst_x[t0:t0 + T, :].rearrange("(s p) d -> p s d", p=128), xt[:], [xt], wt, ssem, eng="gpsimd")
            XTprev = XT

        fw.finish("sync", [out_tile] + dbg_tiles)
        fw.emit()
    return nc, fw


def _consts():
    bf = ml_dtypes.bfloat16
    ident = np.eye(128, dtype=np.float32)
    Rm = np.zeros((128, 128), np.float32)
    for base in (0, 64):
        for d in range(8):
            Rm[base + d + 8, base + d] = -1.0
            Rm[base + d, base + d + 8] = 1.0
    esel = np.zeros((128, 4, 128), np.float32)
    for g in range(8):
        esel[g, g // 2, (g % 2) * 64:(g % 2 + 1) * 64] = 1.0
    cb = np.concatenate([ident, Rm, esel.reshape(128, 512)], axis=1).astype(bf)
    tt_, ss_ = np.meshgrid(np.arange(128), np.arange(128), indexing="ij")
    negmask = np.where(ss_ > tt_, np.float32(NEG), np.float32(0.0))
    posfill = np.where(ss_ > tt_, np.float32(-2 * NEG), np.float32(0.0))
    tril01 = (tt_ <= ss_).astype(np.float32)
    half = 8
    inv_freq = (np.float32(500000.0) ** (-np.arange(half, dtype=np.float32) * np.float32(2.0) / np.float32(16))).astype(np.float32)
    invf = np.zeros((128, 1), np.float32)
    for f in range(128):
        d = f % 64
        if d < 16:
            invf[f, 0] = inv_freq[d % 8]
    cf = np.concatenate([ident, negmask, posfill, tril01, invf], axis=1).astype(np.float32)
    return cb, cf


def _layout_params(inp, depth):
    f = np.float32
    cols = np.zeros((depth, 128, 48), f)
    gb = np.zeros((depth, 128, 3 * D + 512), f)
    wbd = np.zeros((depth, 128, 8, 128), f)
    wsp = np.zeros((depth, 128, 8, 128), f)
    for l in range(depth):
        cols[l, :, 0:8] = np.asarray(inp["g_pre_mix"][l], f).reshape(8, 128).T
        cols[l, :, 8:16] = np.asarray(inp["g_pre_ffn"][l], f).reshape(8, 128).T
        cw = np.asarray(inp["conv_w"][l], f)
        for jj in range(4):
            cols[l, :, 16 + jj * 4:20 + jj * 4] = cw[jj].reshape(4, 128).T
        cols[l, :, 32:36] = np.asarray(inp["conv_b"][l], f).reshape(4, 128).T
        cols[l, :, 36:40] = np.asarray(inp["b_rg_a"][l], f).reshape(4, 128).T
        cols[l, :, 40:44] = np.asarray(inp["b_rg_x"][l], f).reshape(4, 128).T
        cols[l, :, 44:48] = np.asarray(inp["lru_lambda"][l], f).reshape(4, 128).T
        row = np.concatenate([np.asarray(inp["g_post_mix"][l], f), np.asarray(inp["g_post_ffn"][l], f),
                              np.asarray(inp["g_post_ple"][l], f), np.asarray(inp["g_gmlp_v"][l], f)])
        gb[l] = np.broadcast_to(row[None, :], (128, row.size))
        for gi, key in enumerate(("w_rg_a", "w_rg_x")):
            w = np.asarray(inp[key][l], f)
            for c in range(4):
                for hh in range(2):
                    wbd[l, hh * 64:(hh + 1) * 64, gi * 4 + c, hh * 64:(hh + 1) * 64] = w[c * 2 + hh]
        ws = np.asarray(inp["w_spatial"][l], f)
        wsp[l] = np.transpose(ws, (2, 0, 1))
    bsp = np.ascontiguousarray(np.asarray(inp["b_spatial"], f))
    return cols, gb, wbd, wsp, bsp


_CACHE = {}


def kernel(**inputs):
    depth = DEPTH
    x = np.asarray(inputs["x"], np.float32)
    B, L, _ = x.shape
    p = np.asarray(inputs["p"], np.float32)
    pos = np.asarray(inputs["positions"], np.int32)
    cb, cf = _consts()
    cols, gb, wbd, wsp, bsp = _layout_params(inputs, depth)
    shared = {
        "w_in": np.ascontiguousarray(np.asarray(inputs["w_in"], np.float32)),
        "w_branch": np.ascontiguousarray(np.asarray(inputs["w_branch"], np.float32)),
        "w_out": np.ascontiguousarray(np.asarray(inputs["w_out"], np.float32)),
        "w_ffn_up": np.ascontiguousarray(np.asarray(inputs["w_ffn_up"], np.float32)),
        "w_ffn_down": np.ascontiguousarray(np.asarray(inputs["w_ffn_down"], np.float32)),
        "w_ple": np.ascontiguousarray(np.asarray(inputs["w_ple"], np.float32)),
        "w_ple_gate": np.ascontiguousarray(np.asarray(inputs["w_ple_gate"], np.float32)),
        "cols": cols, "gb": gb, "wbd": wbd, "wsp": wsp, "bsp": bsp, "cb": cb, "cf": cf,
    }
    if L not in _CACHE:
        _CACHE[L] = build_program(L, depth)[0]
    nc = _CACHE[L]
    in_maps = []
    for b in range(B):
        m = dict(shared)
        m["x"] = np.ascontiguousarray(x[b])
        m["p"] = np.ascontiguousarray(p[:, b])
        m["pos"] = np.ascontiguousarray(np.broadcast_to(pos[b][None, :], (128, L)))
        in_maps.append(m)
    res = run_bass_kernel_spmd(nc, in_maps, core_ids=list(range(B)))
    return np.stack([np.asarray(r["y"], np.float32) for r in res.results], axis=0)
```

```python
import math
from contextlib import ExitStack
import numpy as np
import ml_dtypes
import concourse.bass as bass
import concourse.mybir as mybir
from concourse.bass_utils import run_bass_kernel_spmd

F32 = mybir.dt.float32
BF16 = mybir.dt.bfloat16
I32 = mybir.dt.int32
AF = mybir.ActivationFunctionType
ALU = mybir.AluOpType
AX = mybir.AxisListType

D = 1024
NH = 8
HD = 64
TOPK = 256
FFN = 4096
PLE = 256
EPS = 1e-6
DEPTH = 2
T = 256
NSUB = T // 128
KG = 512
NSLOT = 3
SLOTB = 4224
NBIS = 12
BIGM = 30000.0
NEG = -1.0e30
IN_OFF = dict(q=0, k=512, v=1024, qi=1536, kiwi=2048, xr=2120, gr=2632, zu=3144, zv=3656, gate=4168)


class Sem:
    def __init__(self, h, name):
        self.h = h
        self.n = 0
        self.name = name


class Tile:
    __slots__ = ("w", "r", "name")

    def __init__(self, name=""):
        self.w = {}
        self.r = {}
        self.name = name


class Buf:
    def __init__(self, t, name=""):
        self.t = t
        self.T = Tile(name)

    def __getitem__(self, k):
        return self.t[k]


class Engine:
    def __init__(self, name, sem):
        self.name = name
        self.sem = sem
        self.ops = []
        self.seen = {}


class FW:
    def __init__(self, nc, stack):
        self.nc = nc
        self.stack = stack
        self.engs = {}
        for n in ("tensor", "vector", "scalar", "gpsimd", "sync"):
            s = Sem(stack.enter_context(nc.semaphore("sem_" + n)), n)
            self.engs[n] = Engine(n, s)
        self.nops = 0

    def dsem(self, name):
        return Sem(self.stack.enter_context(self.nc.semaphore("dsem_" + name)), name)

    def op(self, eng, fn, reads=(), writes=(), dsem=None):
        E = self.engs[eng]
        need = {}
        for b in reads:
            t = b.T if isinstance(b, Buf) else b
            for s, v in t.w.items():
                if need.get(s, 0) < v:
                    need[s] = v
        for b in writes:
            t = b.T if isinstance(b, Buf) else b
            for s, v in t.w.items():
                if need.get(s, 0) < v:
                    need[s] = v
            for s, v in t.r.items():
                if need.get(s, 0) < v:
                    need[s] = v
        raw_self = 0
        for b in reads:
            t = b.T if isinstance(b, Buf) else b
            raw_self = max(raw_self, t.w.get(E.sem, 0))
        waits = []
        for s, v in need.items():
            if s is E.sem:
                if eng != "tensor" and raw_self > E.seen.get(s, 0):
                    E.seen[s] = raw_self
                    waits.append((s, raw_self))
                continue
            if E.seen.get(s, 0) >= v:
                continue
            E.seen[s] = v
            waits.append((s, v))
        if dsem is not None:
            dsem.n += 16
            sig = (dsem, dsem.n, 16)
        else:
            E.sem.n += 1
            sig = (E.sem, E.sem.n, 1)
        E.ops.append((waits, fn, sig))
        self.nops += 1
        s, v = sig[0], sig[1]
        for b in reads:
            t = b.T if isinstance(b, Buf) else b
            if t.r.get(s, 0) < v:
                t.r[s] = v
        for b in writes:
            t = b.T if isinstance(b, Buf) else b
            if t.w.get(s, 0) < v:
                t.w[s] = v

    def finish(self, eng, tiles):
        E = self.engs[eng]
        need = {}
        for b in tiles:
            t = b.T if isinstance(b, Buf) else b
            for d in (t.w, t.r):
                for s, v in d.items():
                    if need.get(s, 0) < v:
                        need[s] = v
        E.ops.append(([(s, v) for s, v in need.items() if s is not E.sem], None, None))

    def emit(self):
        with self.nc.Block() as block:
            for n, E in self.engs.items():
                def body(e, E=E):
                    for waits, fn, sig in E.ops:
                        for s, v in waits:
                            e.wait_ge(s.h, v)
                        if fn is None:
                            continue
                        fn(e).then_inc(sig[0].h, sig[2])
                getattr(block, n)(body)


def panel_defs():
    P = []
    for nm in ("q", "k", "qi"):
        P.append((nm, "w_in", 0, 1024, IN_OFF[nm], 512))
    P.append(("kiwi", "w_in", 0, 1024, IN_OFF["kiwi"], 72))
    for nm in ("xr", "gr", "zu", "v", "zv"):
        P.append((nm, "w_in", 0, 1024, IN_OFF[nm], 512))
    for n in range(3):
        for hf in range(2):
            P.append(("gate%d_%d" % (n, hf), "w_in", 0, 1024, IN_OFF["gate"] + n * 1024 + hf * 512, 512))
    for n in range(3):
        for hf in range(2):
            P.append(("br%d_%d" % (n, hf), "w_branch%d" % n, 0, 512, hf * 512, 512))
    for hf in range(2):
        P.append(("out_%d" % hf, "w_out", 0, 1024, hf * 512, 512))
    for g in range(8):
        P.append(("up_%d" % g, "w_ffn_up", 0, 1024, g * 512, 512))
        P.append(("dn_%d" % g, "w_ffn_down", g * 512, 512, 0, 1024))
    P.append(("ple", "w_ple", 0, 256, 0, 1024))
    for hf in range(2):
        P.append(("pg_%d" % hf, "w_ple_gate", 0, 1024, hf * 512, 512))
    return P


def build_program(L, depth=DEPTH, dbg=None):
    NT = L // T
    nc = bass.Bass("TRN2", target_bir_lowering=False)
    dram = lambda name, shape, dt, kind="ExternalInput": nc.dram_tensor(name, shape, dt, kind=kind).ap()
    x_in = dram("x", [L, D], F32)
    p_in = dram("p", [depth, L, PLE], F32)
    pos_in = dram("pos", [128, L], I32)
    wsrc = {
        "w_in": dram("w_in", [depth, D, 7240], F32),
        "w_branch": dram("w_branch", [depth, 3, 512, D], F32),
        "w_out": dram("w_out", [depth, D, D], F32),
        "w_ffn_up": dram("w_ffn_up", [depth, D, FFN], F32),
        "w_ffn_down": dram("w_ffn_down", [depth, FFN, D], F32),
        "w_ple": dram("w_ple", [depth, PLE, D], F32),
        "w_ple_gate": dram("w_ple_gate", [depth, D, D], F32),
    }
    cols_in = dram("cols", [depth, 128, 48], F32)
    gb_in = dram("gb", [depth, 128, 3 * D + 512], F32)
    wbd_in = dram("wbd", [depth, 128, 8, 128], F32)
    wsp_in = dram("wsp", [depth, 128, 8, 128], F32)
    bsp_in = dram("bsp", [depth, 8, 128], F32)
    cb_in = dram("cb", [128, 256 + 512], BF16)
    cf_in = dram("cf", [128, 4 * 128 + 1], F32)
    y_out = dram("y", [L, D], F32, kind="ExternalOutput")
    xbuf = dram("xbuf", [L, D], F32, kind="Internal")
    KTc = [dram("ktc%d" % l, [4, 128, L], BF16, kind="Internal") for l in range(depth)]
    Vc = [dram("vc%d" % l, [L, 520], BF16, kind="Internal") for l in range(depth)]
    pdefs = panel_defs()
    Wp = [{nm: dram("wp%d_%s" % (l, nm), [nr, ncol], BF16, kind="Internal") for (nm, _, _, nr, _, ncol) in pdefs}
          for l in range(depth)]
    dbg_out = {}
    if dbg:
        for k, shp in dbg.items():
            dbg_out[k] = dram("dbg_" + k, list(shp), F32, kind="ExternalOutput")

    with ExitStack() as st:
        fw = FW(nc, st)
        op = fw.op

        def sb(name, shape, dt):
            return Buf(st.enter_context(nc.sbuf_tensor("s_" + name, shape, dt)), name)

        PS = [Buf(st.enter_context(nc.psum_tensor("ps%d" % i, [128, 512], F32)), "ps%d" % i) for i in range(8)]

        def mm(out, lhsT, rhs, start, stop, R, W):
            op("tensor", lambda e: e.matmul(out, lhsT=lhsT, rhs=rhs, start=start, stop=stop), R, W)

        def tr(out, in_, ident, R, W):
            op("tensor", lambda e: e.transpose(out=out, in_=in_, identity=ident), R, W)

        def act(out, in_, func, R, W, **kw):
            op("scalar", lambda e: e.activation(out=out, in_=in_, func=func, **kw), R, W)

        def ts(out, in0, s1, s2, op0, op1, R, W, eng="vector"):
            if op1 is None:
                op(eng, lambda e: e.tensor_scalar(out=out, in0=in0, scalar1=s1, scalar2=None, op0=op0), R, W)
            else:
                op(eng, lambda e: e.tensor_scalar(out=out, in0=in0, scalar1=s1, scalar2=s2, op0=op0, op1=op1), R, W)

        def tt(out, in0, in1, o, R, W, eng="vector"):
            op(eng, lambda e: e.tensor_tensor(out=out, in0=in0, in1=in1, op=o), R, W)

        def stt(out, in0, s, in1, op0, op1, R, W):
            op("vector", lambda e: e.scalar_tensor_tensor(out=out, in0=in0, scalar=s, in1=in1, op0=op0, op1=op1), R, W)

        def cp(out, in_, R, W, eng="vector"):
            op(eng, lambda e: e.tensor_copy(out=out, in_=in_), R, W)

        def dma(out, in_, R, W, ds, eng="sync"):
            op(eng, lambda e: e.dma_start(out=out, in_=in_), R, W, dsem=ds)

        def memset(ap, val, W, eng="vector"):
            op(eng, lambda e: e.memset(ap, val), [], W)

        def reduce(out, in_, o, R, W):
            op("vector", lambda e: e.tensor_reduce(out=out, in_=in_, axis=AX.X, op=o), R, W)

        def recip(out, in_, R, W):
            op("vector", lambda e: e.reciprocal(out=out, in_=in_), R, W)

        def scan(out, d0, d1, init, R, W):
            op("vector", lambda e: e.tensor_tensor_scan(out=out, data0=d0, data1=d1, initial=init,
                                                        op0=ALU.mult, op1=ALU.add), R, W)

        def count_ge(out, in0, thr, cnt, R, W):
            op("vector", lambda e: e.tensor_scalar(out=out, in0=in0, scalar1=thr, scalar2=None, op0=ALU.is_ge,
                                                   op1=ALU.add, accum_out=cnt), R, W)

        def cpred(out, mask, data, R, W):
            op("vector", lambda e: e.copy_predicated(out=out, mask=mask, data=data), R, W)

        dbg_sem = fw.dsem("dbg")
        dbg_tiles = []

        def dump(name, ap, R):
            if name in dbg_out:
                t = Tile("dbg")
                dma(dbg_out[name], ap, R, [t], dbg_sem, eng="gpsimd")
                dbg_tiles.append(t)
                del dbg_out[name]

        cb = sb("cb", [128, 768], BF16)
        cf = sb("cf", [128, 513], F32)
        dma(cb[:], cb_in, [], [cb], fw.dsem("c0"))
        dma(cf[:], cf_in, [], [cf], fw.dsem("c1"))
        identb = cb[:, 0:128]
        Rm = cb[:, 128:256]
        esel = cb[0:8, 256:768].rearrange("p (c f) -> p c f", c=4)
        identf = cf[:, 0:128]
        negmask = cf[:, 128:256]
        posfill = cf[:, 256:384]
        tril01 = cf[:, 384:512]
        invf = cf[:, 512:513]

        WT = [dict() for _ in range(depth)]
        for l in range(depth):
            wcs = fw.dsem("wcast%d" % l)
            for (nm, src, r0, nr, c0, ncol) in pdefs:
                if src.startswith("w_branch"):
                    s_ap = wsrc["w_branch"][l, int(src[-1]), r0:r0 + nr, c0:c0 + ncol]
                else:
                    s_ap = wsrc[src][l, r0:r0 + nr, c0:c0 + ncol]
                t = Tile("wp")
                WT[l][nm] = t
                step = 512
                for rr in range(0, nr, step):
                    n2 = min(step, nr - rr)
                    dma(Wp[l][nm][rr:rr + n2, :], s_ap[rr:rr + n2, :], [], [t], wcs, eng="gpsimd")
            for t in WT[l].values():
                t.w = {wcs: wcs.n}

        ring = [sb("ring%d" % i, [128, SLOTB], BF16) for i in range(NSLOT)]
        ring_sem = [fw.dsem("ring%d" % i) for i in range(NSLOT)]
        kiT = sb("kiT", [128, L], BF16)
        xt = sb("xt", [128, NSUB, D], F32)
        pt = sb("pt", [128, NSUB, PLE], F32)
        ptb = sb("ptb", [128, NSUB, PLE], BF16)
        posi = sb("posi", [128, T], I32)
        hT = sb("hT", [128, 8, T], BF16)
        pT = sb("pT", [128, 2, T], BF16)
        qT = sb("qT", [128, 4, 2, T], BF16)
        qiT = sb("qiT", [128, 4, T], BF16)
        KTs = sb("KTs", [128, 4, T], BF16)
        Vs = sb("Vs", [128, NSUB, 520], BF16)
        cosT = sb("cosT", [128, T], F32)
        sinT = sb("sinT", [128, T], F32)
        rtmp = sb("rtmp", [128, 3, T], F32)
        xb16 = sb("xb16", [128, T], BF16)
        xrbuf = sb("xrbuf", [128, 4, 3 + T], F32)
        grT = sb("grT", [128, 4, T], BF16)
        zuT = sb("zuT", [128, 4, T], BF16)
        vn = sb("vn", [128, NSUB, 512], BF16)
        hst = sb("hst", [128, 4], F32)
        ybT = sb("ybT", [128, 4, T], BF16)
        ycT = sb("ycT", [128, 4, T], BF16)
        yaT = sb("yaT", [128, 4, T], BF16)
        yaTt = sb("yaTt", [64, T], BF16)
        wis = sb("wis", [128, NSUB, 8], F32)
        diag = sb("diag", [128, 8, 128], BF16)
        small = sb("small", [128, 32], F32)
        smalli = sb("smalli", [128, 4], I32)
        bis = sb("bis", [128, 16], F32)
        bis2 = sb("bis2", [128, 16], F32)
        Tmid = Tile("mid")
        Tcnt = Tile("cnt")
        Tsa = Tile("sa")
        big = sb("big", [128, 6144], F32)
        TS_ = big.T
        TR_ = Tile("bigR")
        score_ap = big[:, 0:L]
        Rrelu = big[:, 4096:6144].bitcast(BF16).rearrange("p (h w) -> p h w", h=8)
        masks = [sb("mask%d" % s, [128, L], BF16) for s in range(NSUB)]
        maskTk = sb("maskTk", [128, 4, T], BF16)
        PT = [sb("PT%d" % i, [128, 2, T], BF16) for i in range(4)]
        xs = sb("xs", [128, D], BF16)
        accm = sb("accm", [128, 8 * T], F32)

        def view(ap, tile):
            v = Buf.__new__(Buf)
            v.t = ap
            v.T = tile
            return v

        acc = view(accm[0:65, :].rearrange("p (h t) -> p h t", h=8), accm.T)
        lt = view(accm[:, 0:5 * T].rearrange("p (k t) -> p k t", k=5), accm.T)
        gz = view(accm[:, 5 * T:5 * T + 512], accm.T)
        xcb = view(accm[:, 5 * T + 512:5 * T + 512 + T // 2].bitcast(BF16), accm.T)
        sc1 = sb("sc1", [128, L], F32)
        rhl2 = [sb("rhl%d" % i, [65, 2, T], BF16) for i in range(2)]
        onesb = sb("onesb", [65, 64], BF16)
        gt = sb("gt", [128, T], F32)
        tmpf = sb("tmpf", [128, 512], F32)
        gsig = sb("gsig", [128, 8, T], BF16)
        merged = big[:, 0:8 * T].rearrange("p (c t) -> p c t", c=8)
        o = 8 * T
        mergedT = big[:, o:o + 4 * T].bitcast(BF16).rearrange("p (c t) -> p c t", c=8)
        o += 4 * T
        fT = [big[:, o + i * 2 * T:o + (i + 1) * 2 * T].bitcast(BF16).rearrange("p (c t) -> p c t", c=4) for i in range(2)]
        o += 4 * T
        assert o <= 4096
        o = 4096
        rl = [big[:, o + i * (T // 2):o + (i + 1) * (T // 2)].bitcast(BF16) for i in range(2)]
        o += T
        sg = big[:, o:o + 512]
        o += 512
        ple_t = big[:, o:o + 1024]
        o += 1024
        assert o <= 6144
        wbdf = big[:, 4096:4096 + 1024].rearrange("p (c j) -> p c j", c=8)
        cols = sb("cols", [128, 48], F32)
        gbv = sb("gbv", [128, 512], F32)
        gcur = sb("gcur", [128, D], F32)
        gsem = fw.dsem("gain")
        wbd = sb("wbd", [128, 8, 128], BF16)
        wspT = sb("wspT", [128, 8, 128], BF16)
        bsp = sb("bsp", [8, 128], F32)
        bsph = sb("bsph", [8, 2, 128], BF16)
        c8 = sb("c8", [128, 8], F32)
        epsc = sb("epsc", [128, 4], F32)
        psem = [fw.dsem("par%d" % i) for i in range(5)]
        xsem = fw.dsem("xload")
        ptsem = fw.dsem("pload")
        possem = fw.dsem("posload")
        ssem = fw.dsem("store")
        kvsem = fw.dsem("kvstore")
        out_tile = Tile("out")

        class Stream:
            def __init__(self):
                self.plan = []
                self.issued = 0
                self.pos = 0

            def add(self, name, loader, deps, hoist=True):
                self.plan.append((name, loader, deps, hoist))

            def next(self, name, look=NSLOT - 1):
                i = self.pos
                assert self.plan[i][0] == name, (self.plan[i][0], name)
                while self.issued < len(self.plan) and (
                        self.issued <= i or (self.issued <= i + look and self.plan[self.issued][3])):
                    k = self.issued
                    _, loader, deps, _ = self.plan[k]
                    loader(ring[k % NSLOT], ring_sem[k % NSLOT], deps)
                    self.issued += 1
                self.pos += 1
                return ring[i % NSLOT]

        stream = Stream()

        def wloader(l, nm, nr, ncol):
            kc = nr // 128

            def f(slot, sem, deps):
                dst = slot[:, 0:kc * ncol].rearrange("p (k w) -> p k w", k=kc)
                src = Wp[l][nm].rearrange("(k p) w -> p k w", p=128)
                dma(dst, src, deps, [slot], sem)
            return f

        KVT = [[Tile("kv") for _ in range((L + KG - 1) // KG)] for _ in range(depth)]

        def kvloader(l, g, wd):
            def f(slot, sem, deps):
                dstk = slot[:, 0:4 * wd].rearrange("p (c w) -> p c w", c=4)
                dma(dstk, KTc[l][:, :, g * KG:g * KG + wd].rearrange("c p w -> p c w"), deps, [slot], sem)
                nb = wd // 128
                dstv = slot[:, 2048:2048 + nb * 520].rearrange("p (b f) -> p b f", b=nb)
                dma(dstv, Vc[l][g * KG:g * KG + wd, :].rearrange("(b p) f -> p b f", p=128), deps, [slot], sem)
            return f

        pinfo = {nm: (nr, ncol) for (nm, _, _, nr, _, ncol) in pdefs}

        def plan_w(l, nm):
            nr, ncol = pinfo[nm]
            stream.add("%d_%s" % (l, nm), wloader(l, nm, nr, ncol), [WT[l][nm]])

        def kv_groups(j):
            nkeys = (j + 1) * T
            out = []
            g = 0
            while g * KG < nkeys:
                out.append((g, min(KG, nkeys - g * KG)))
                g += 1
            return out

        for l in range(depth):
            for j in range(NT):
                for nm in ("q", "k", "qi", "kiwi", "xr", "gr", "zu", "v", "zv"):
                    plan_w(l, nm)
                for n in (2, 1):
                    plan_w(l, "gate%d_0" % n)
                    plan_w(l, "gate%d_1" % n)
                    plan_w(l, "br%d_0" % n)
                    plan_w(l, "br%d_1" % n)
                grps = kv_groups(j)
                for (g, wd) in grps:
                    last = (g == grps[-1][0])
                    stream.add("%d_kv%d_%d" % (l, j, g), kvloader(l, g, wd), [KVT[l][g]], hoist=not last)
                for n in (0,):
                    plan_w(l, "gate%d_0" % n)
                    plan_w(l, "gate%d_1" % n)
                    plan_w(l, "br%d_0" % n)
                    plan_w(l, "br%d_1" % n)
                plan_w(l, "out_0")
                plan_w(l, "out_1")
                plan_w(l, "up_0")
                for g in range(8):
                    if g + 1 < 8:
                        plan_w(l, "up_%d" % (g + 1))
                    plan_w(l, "dn_%d" % g)
                plan_w(l, "ple")
                plan_w(l, "pg_0")
                plan_w(l, "pg_1")

        def wview(slot, nm):
            nr, ncol = pinfo[nm]
            kc = nr // 128
            return slot[:, 0:kc * ncol].rearrange("p (k w) -> p k w", k=kc)

        sm_i = [0]

        def sm():
            i = sm_i[0] % 32
            sm_i[0] += 1
            return small[:, i:i + 1]

        memset(epsc[:, 0:1], EPS, [epsc])
        memset(epsc[:, 1:2], math.pi / 2, [epsc])
        memset(epsc[:, 2:3], 1.0, [epsc])
        memset(epsc[:, 3:4], -BIGM, [epsc])
        memset(Vs[:, :, :], 1.0, [Vs])
        memset(onesb[:, :], 1.0, [onesb])
        memset(qT[:, :, :, :], 0.0, [qT])

        def rstd_from_ss(ss_ap, n):
            a = sm()
            b = sm()
            act(a, ss_ap, AF.Sqrt, [small, epsc], [small], scale=1.0 / n, bias=epsc[:, 0:1])
            recip(b, a, [small], [small])
            return b

        pi = [0]

        def nps():
            pi[0] += 1
            return PS[pi[0] % 4]

        def norm_transpose(gcol0):
            for s in range(NSUB):
                if gcol0 is not None:
                    ss = sm()
                    act(tmpf[:, 0:512], xt[:, s, 0:512], AF.Square, [xt, small], [tmpf, small], accum_out=ss)
                    ss2 = sm()
                    act(tmpf[:, 0:512], xt[:, s, 512:1024], AF.Square, [xt, small], [tmpf, small], accum_out=ss2)
                    ss3 = sm()
                    tt(ss3, ss, ss2, ALU.add, [small], [small])
                    rs = rstd_from_ss(ss3, D)
                    ts(xs[:], xt[:, s, :], rs, None, ALU.mult, None, [xt, small], [xs])
                else:
                    cp(xs[:], xt[:, s, :], [xt], [xs])
                psb = PS[7][:].bitcast(BF16)
                for c in range(8):
                    tr(psb[:, c * 128:(c + 1) * 128], xs[:, c * 128:(c + 1) * 128], identb, [xs, cb], [PS[7]])
                src = psb[:, 0:1024].rearrange("p (c t) -> p c t", c=8)
                dst = hT[:, :, s * 128:(s + 1) * 128]
                if gcol0 is not None:
                    g_ap = cols[:, gcol0:gcol0 + 8].unsqueeze(2).to_broadcast([128, 8, 128])
                    tt(dst, src, g_ap, ALU.mult, [PS[7], cols], [hT])
                else:
                    cp(dst, src, [PS[7]], [hT])

        def post_norm_residual(banks, gcol, s):
            ssa = []
            for hf in range(2):
                a = sm()
                act(tmpf[:, 0:512], banks[hf][:, 0:512], AF.Square, [banks[hf], small], [tmpf, small], accum_out=a)
                ssa.append(a)
            s3 = sm()
            tt(s3, ssa[0], ssa[1], ALU.add, [small], [small])
            rs = rstd_from_ss(s3, D)
            for hf in range(2):
                stt(tmpf[:, 0:512], banks[hf][:, 0:512], rs, gcur[:, hf * 512:(hf + 1) * 512],
                    ALU.mult, ALU.mult, [banks[hf], small, gcur], [tmpf])
                tt(xt[:, s, hf * 512:(hf + 1) * 512], xt[:, s, hf * 512:(hf + 1) * 512], tmpf[:, 0:512], ALU.add,
                   [xt, tmpf], [xt])

        def merge_tile(dst, src, as_write=False):
            for a, b in ((dst.w, src.w), (dst.r, src.r)):
                for k_, v_ in b.items():
                    if a.get(k_, 0) < v_:
                        a[k_] = v_
            if as_write:
                for k_, v_ in src.r.items():
                    if dst.w.get(k_, 0) < v_:
                        dst.w[k_] = v_

        def fm_chunk(panel, pv, c, ps, M=128):
            for kc in range(8):
                mm(ps[0:M, 0:T], pv[:, kc, c * 128:c * 128 + M], hT[:, kc, :], kc == 0, kc == 7, [panel, hT], [ps])

        def rope_evac(ps, dst, W, split=None):
            act(xb16[:], ps[:, 0:T], AF.Copy, [ps], [xb16])
            mm(ps[:, T:2 * T], Rm, xb16[:], True, True, [cb, xb16], [ps])
            tt(rtmp[:, 0, :], ps[:, 0:T], cosT[:], ALU.mult, [ps, cosT], [rtmp])
            tt(rtmp[:, 1, :], ps[:, T:2 * T], sinT[:], ALU.mult, [ps, sinT], [rtmp])
            if split is None:
                tt(dst, rtmp[:, 0, :], rtmp[:, 1, :], ALU.add, [rtmp], W)
            else:
                for hh in range(2):
                    tt(split[hh * 64:(hh + 1) * 64, hh, :], rtmp[hh * 64:(hh + 1) * 64, 0, :], rtmp[hh * 64:(hh + 1) * 64, 1, :],
                       ALU.add, [rtmp], W)

        XTprev = None
        for l in range(depth):
            src_x = x_in if l == 0 else xbuf
            dst_x = y_out if l == depth - 1 else xbuf
            XT = [Tile("xd") for _ in range(NT)] if l < depth - 1 else None
            dma(cols[:], cols_in[l], [], [cols], psem[0])
            dma(gbv[:], gb_in[l, :, 3 * D:3 * D + 512], [], [gbv], psem[1])
            dma(gcur[:], gb_in[l, :, 0:D], [], [gcur], gsem)
            dma(bsp[:], bsp_in[l], [], [bsp], psem[2])
            cp(bsph[:, 0, :], bsp[:], [bsp], [bsph])
            tt(bsp[:], bsp[:], bsph[:, 0, :], ALU.subtract, [bsph], [bsp])
            cp(bsph[:, 1, :], bsp[:], [bsp], [bsph])
            dma(wbdf, wbd_in[l], [], [TR_], psem[3])
            cp(wbd[:], wbdf, [TR_], [wbd])
            dma(wbdf, wsp_in[l], [], [TR_], psem[4])
            tt(wspT[:], wbdf, tril01.unsqueeze(1).to_broadcast([128, 8, 128]), ALU.mult, [TR_, cf], [wspT])
            act(c8[:, 4:8], cols[:, 44:48], AF.Exp, [cols], [c8], scale=-1.0)
            ts(c8[:, 0:4], c8[:, 4:8], -0.25, 1.0 / 3.0, ALU.mult, ALU.add, [c8], [c8])
            tt(c8[:, 0:4], c8[:, 0:4], c8[:, 4:8], ALU.mult, [c8], [c8])
            ts(c8[:, 0:4], c8[:, 0:4], -1.0, 0.5, ALU.mult, ALU.add, [c8], [c8])
            tt(c8[:, 0:4], c8[:, 0:4], c8[:, 4:8], ALU.mult, [c8], [c8])
            ts(c8[:, 0:4], c8[:, 0:4], -1.0, 1.0, ALU.mult, ALU.add, [c8], [c8])
            tt(c8[:, 0:4], c8[:, 0:4], c8[:, 4:8], ALU.mult, [c8], [c8])
            ts(c8[:, 0:4], c8[:, 0:4], -8.0, None, ALU.mult, None, [c8], [c8])
            ts(c8[:, 4:8], c8[:, 0:4], 2.0, None, ALU.mult, None, [c8], [c8])
            memset(xrbuf[:, :, 0:3], 0.0, [xrbuf])
            memset(hst[:], 0.0, [hst])

            for j in range(NT):
                t0 = j * T
                first_tile = (l == 0 and j == 0)
                rd = [XTprev[j]] if (l > 0) else []
                dma(xt[:], src_x[t0:t0 + T, :].rearrange("(s p) d -> p s d", p=128), rd, [xt], xsem)
                dma(pt[:], p_in[l, t0:t0 + T, :].rearrange("(s p) d -> p s d", p=128), [], [pt], ptsem)
                dma(posi[:], pos_in[:, t0:t0 + T], [], [posi], possem)
                ang = rtmp[:, 0, :]
                u = rtmp[:, 1, :]
                rr = rtmp[:, 2, :]
                cp(u, posi[:], [posi], [rtmp])
                ts(ang, u, invf, None, ALU.mult, None, [rtmp, cf], [rtmp])
                ts(u, ang, 1.0 / (2 * math.pi), 12582912.0, ALU.mult, ALU.add, [rtmp], [rtmp])
                ts(u, u, -12582912.0, None, ALU.add, None, [rtmp], [rtmp])
                C1 = 6.28125
                C2 = float(np.float32(2 * math.pi - C1))
                C3 = float(2 * math.pi - C1 - C2)
                stt(rr, u, -C1, ang, ALU.mult, ALU.add, [rtmp], [rtmp])
                stt(rr, u, -C2, rr, ALU.mult, ALU.add, [rtmp], [rtmp])
                stt(rr, u, -C3, rr, ALU.mult, ALU.add, [rtmp], [rtmp])
                act(sinT[:], rr, AF.Sin, [rtmp], [sinT])
                stt(u, rr, -1.0, rr, ALU.mult, ALU.max, [rtmp], [rtmp])
                act(cosT[:], u, AF.Sin, [rtmp, epsc], [cosT], scale=-1.0, bias=epsc[:, 1:2])
                norm_transpose(0)
                if first_tile:
                    dump("hT", hT[:, 0, :], [hT])
                    dump("cosT", cosT[:], [cosT])
                    dump("sinT", sinT[:], [sinT])

                for nm, dstb in (("q", qT), ("k", KTs), ("qi", qiT)):
                    panel = stream.next("%d_%s" % (l, nm))
                    pv = wview(panel, nm)
                    pss = [nps() for _ in range(4)]
                    fm_chunk(panel, pv, 0, pss[0])
                    for c in range(4):
                        if c + 1 < 4:
                            fm_chunk(panel, pv, c + 1, pss[c + 1])
                        if nm == "q":
                            rope_evac(pss[c], None, [dstb], split=qT[:, c, :, :])
                        else:
                            rope_evac(pss[c], dstb[:, c, :], [dstb])
                panel = stream.next("%d_kiwi" % l)
                pv = wview(panel, "kiwi")
                ps = nps()
                for half in range(2):
                    for kc in range(8):
                        mm(ps[half * 64:(half + 1) * 64, 0:T], pv[:, kc, 0:64], hT[:, kc, :], kc == 0, kc == 7,
                           [panel, hT], [ps])
                rope_evac(ps, kiT[:, t0:t0 + T], [kiT])
                for s in range(NSUB):
                    ps = nps()
                    for kc in range(8):
                        mm(ps[:, 0:8], hT[:, kc, s * 128:(s + 1) * 128], pv[:, kc, 64:72], kc == 0, kc == 7,
                           [panel, hT], [ps])
                    cp(wis[:, s, :], ps[:, 0:8], [ps], [wis])
                dma(KTc[l][:, :, t0:t0 + T].rearrange("c p w -> p c w"), KTs[:], [KTs], [KVT[l][t0 // KG]], kvsem, eng="gpsimd")
                if first_tile:
                    dump("qT", qT[:, 0, 0, :], [qT])
                    dump("kiT", kiT[:, 0:T], [kiT])
                    dump("wis", wis[:, 0, :], [wis])
                panel = stream.next("%d_xr" % l)
                pv = wview(panel, "xr")
                for c in range(4):
                    ps = nps()
                    fm_chunk(panel, pv, c, ps)
                    act(xrbuf[:, c, 3:3 + T], ps[:, 0:T], AF.Copy, [ps], [xrbuf])
                for nm, dstb in (("gr", grT), ("zu", zuT)):
                    panel = stream.next("%d_%s" % (l, nm))
                    pv = wview(panel, nm)
                    for c in range(4):
                        ps = nps()
                        fm_chunk(panel, pv, c, ps)
                        act(dstb[:, c, :], ps[:, 0:T], AF.Gelu_apprx_tanh, [ps], [dstb])
                panel = stream.next("%d_v" % l)
                pv = wview(panel, "v")
                for s in range(NSUB):
                    ps = nps()
                    for kc in range(8):
                        mm(ps[:, 0:512], hT[:, kc, s * 128:(s + 1) * 128], pv[:, kc, :], kc == 0, kc == 7, [panel, hT], [ps])
                    dstv = Vs[:, s, :].rearrange("p (h f) -> p h f", h=8)[:, :, 0:64]
                    act(dstv, ps[:, 0:512].rearrange("p (h f) -> p h f", h=8), AF.Copy, [ps], [Vs])
                dma(Vc[l][t0:t0 + T, :].rearrange("(s p) f -> p s f", p=128), Vs[:], [Vs], [KVT[l][t0 // KG]], kvsem, eng="gpsimd")
                panel = stream.next("%d_zv" % l)
                pv = wview(panel, "zv")
                for s in range(NSUB):
                    ps = nps()
                    for kc in range(8):
                        mm(ps[:, 0:512], hT[:, kc, s * 128:(s + 1) * 128], pv[:, kc, :], kc == 0, kc == 7, [panel, hT], [ps])
                    act(gz[:], ps[:, 0:512], AF.Gelu_apprx_tanh, [ps], [gz])
                    ss = sm()
                    act(tmpf[:, 0:512], gz[:], AF.Square, [gz, small], [tmpf, small], accum_out=ss)
                    rs = rstd_from_ss(ss, 512)
                    stt(vn[:, s, :], gz[:], rs, gbv[:], ALU.mult, ALU.mult, [gz, small, gbv], [vn])

                mpi = [0]

                def mps():
                    mpi[0] += 1
                    return PS[5 + mpi[0] % 3]

                def gen_mixC():
                    for s in range(NSUB):
                        for cpair in range(4):
                            ps = mps()
                            for gg in range(2):
                                g = cpair * 2 + gg
                                mm(ps[gg * 64:(gg + 1) * 64, 0:128], vn[:, s, g * 64:(g + 1) * 64], wspT[:, g, :], True, False,
                                   [vn, wspT], [ps])
                            mm(ps[:, 0:128], esel[:, cpair, :], bsph[:, 0, :], False, False, [cb, bsph], [ps])
                            mm(ps[:, 0:128], esel[:, cpair, :], bsph[:, 1, :], False, True, [cb, bsph], [ps])
                            tt(ycT[:, cpair, s * 128:(s + 1) * 128], ps[:, 0:128], zuT[:, cpair, s * 128:(s + 1) * 128], ALU.mult,
                               [ps, zuT], [ycT])
                            yield

                def gen_mixB():
                    for c in range(4):
                        xc = lt[:, 0, :]
                        ts(xc, xrbuf[:, c, 0:T], cols[:, 16 + c:17 + c], cols[:, 32 + c:33 + c], ALU.mult, ALU.add, [xrbuf, cols], [lt])
                        for jj in range(1, 4):
                            stt(xc, xrbuf[:, c, jj:jj + T], cols[:, 16 + jj * 4 + c:17 + jj * 4 + c], xc, ALU.mult, ALU.add,
                                [xrbuf, cols, lt], [lt])
                        yield
                        cp(xrbuf[:, c, 0:3], xrbuf[:, c, T:T + 3], [xrbuf], [xrbuf])
                        act(xcb[:], xc, AF.Copy, [lt], [xcb])
                        ps = mps()
                        mm(ps[:, 0:T], wbd[:, c, :], xcb[:], True, True, [wbd, xcb], [ps])
                        mm(ps[:, T:2 * T], wbd[:, 4 + c, :], xcb[:], True, True, [wbd, xcb], [ps])
                        yield
                        rg = lt[:, 1, :]
                        ig = lt[:, 2, :]
                        av = lt[:, 3, :]
                        sq = lt[:, 4, :]
                        act(rg, ps[:, 0:T], AF.Sigmoid, [ps, cols], [lt], bias=cols[:, 36 + c:37 + c])
                        act(ig, ps[:, T:2 * T], AF.Sigmoid, [ps, cols], [lt], bias=cols[:, 40 + c:41 + c])
                        yield
                        act(av, rg, AF.Exp, [lt, c8], [lt], scale=c8[:, c:c + 1])
                        act(sq, rg, AF.Exp, [lt, c8], [lt], scale=c8[:, 4 + c:5 + c])
                        act(sq, sq, AF.Sqrt, [lt, epsc], [lt], scale=-1.0, bias=epsc[:, 2:3])
                        yield
                        tt(ig, ig, xc, ALU.mult, [lt], [lt])
                        tt(ig, ig, sq, ALU.mult, [lt], [lt])
                        hh = lt[:, 1, :]
                        scan(hh, av, ig, hst[:, c:c + 1], [lt, hst], [lt])
                        yield
                        cp(hst[:, c:c + 1], hh[:, T - 1:T], [lt], [hst])
                        tt(ybT[:, c, :], hh, grT[:, c, :], ALU.mult, [lt, grT], [ybT])
                        yield

                grps = kv_groups(j)
                nkeys = (j + 1) * T
                TRh = [Tile("relu%d" % h) for h in range(8)]

                SC = [(score_ap, TS_), (sc1[:, 0:L], sc1.T)]

                def gen_I(s):
                    N = t0 + 128 * (s + 1)
                    score_s, TSs = SC[s]
                    for h in range(8):
                        merge_tile(TRh[h], TR_, as_write=True)
                        act(diag[:, h, :], identf, AF.Copy, [cf, wis], [diag], scale=wis[:, s, h:h + 1])
                    yield
                    ngrp = (N + KG - 1) // KG
                    for kg in range(ngrp):
                        wd = min(KG, N - kg * KG)
                        for h in range(8):
                            ps = PS[h % 4]
                            r0 = (h % 2) * 64
                            mm(ps[:, 0:wd], qiT[r0:r0 + 64, h // 2, s * 128:(s + 1) * 128],
                               kiT[r0:r0 + 64, kg * KG:kg * KG + wd], True, True, [qiT, kiT], [ps])
                            if h % 2 == 0:
                                act(Rrelu[:, h, 0:wd], ps[:, 0:wd], AF.Relu, [ps], [TRh[h]])
                            else:
                                ts(Rrelu[:, h, 0:wd], ps[:, 0:wd], 0.0, None, ALU.max, None, [ps], [TRh[h]])
                            if h % 2 == 1:
                                yield
                        for h in range(8):
                            mm(PS[4][:, 0:wd], diag[:, h, :], Rrelu[:, h, 0:wd], h == 0, h == 7, [diag, TRh[h]], [PS[4]])
                        if kg == ngrp - 1:
                            if wd > 128:
                                cp(score_s[:, kg * KG:kg * KG + wd - 128], PS[4][:, 0:wd - 128], [PS[4]], [TSs])
                            tt(score_s[:, N - 128:N], PS[4][:, wd - 128:wd], negmask, ALU.add, [PS[4], cf], [TSs])
                            tt(tmpf[:, 0:128], PS[4][:, wd - 128:wd], posfill, ALU.add, [PS[4], cf], [tmpf])
                        else:
                            cp(score_s[:, kg * KG:kg * KG + wd], PS[4][:, 0:wd], [PS[4]], [TSs])
                        yield
                    for h in range(8):
                        merge_tile(TR_, TRh[h])

                def gen_B(s):
                    N = t0 + 128 * (s + 1)
                    score_s, TSs = SC[s]
                    hi0 = bis[:, 0:1]
                    lo = bis[:, 1:2]
                    w0 = bis[:, 2:3]
                    reduce(hi0, score_s[:, 0:N], ALU.max, [TSs, bis], [bis])
                    reduce(lo, tmpf[:, 0:128], ALU.min, [tmpf, bis], [bis])
                    if N > 128:
                        m1 = bis[:, 3:4]
                        reduce(m1, score_s[:, 0:N - 128], ALU.min, [TSs, bis], [bis])
                        tt(lo, lo, m1, ALU.min, [bis], [bis])
                    tt(w0, hi0, lo, ALU.subtract, [bis], [bis])
                    yield
                    mk = masks[s]
                    if N > TOPK:
                        Nd = max(128, (int(N * 0.42) // 128) * 128)
                        Na = N - Nd
                        if s == 0:
                            junkA, TJ = masks[1], masks[1].T
                        else:
                            junkA, TJ = big[:, 4096:6144].bitcast(BF16), TR_
                        for it in range(NBIS):
                            k4 = it % 4
                            mid = bis2[:, k4:k4 + 1]
                            cnt = bis2[:, 4 + k4:5 + k4]
                            vv = bis2[:, 8 + k4:9 + k4]
                            sA = bis2[:, 12 + k4:13 + k4]
                            stt(mid, w0, 0.5 ** (it + 1), lo, ALU.mult, ALU.add, [bis], [Tmid])
                            act(junkA[:, 0:Na], score_s[:, Nd:N], AF.Sign, [TSs, Tmid], [TJ, Tsa], scale=-1.0, bias=mid,
                                accum_out=sA)
                            count_ge(mk[:, 0:Nd], score_s[:, 0:Nd], mid, cnt, [TSs, Tmid], [mk, Tcnt])
                            stt(vv, cnt, 2.0, sA, ALU.mult, ALU.subtract, [Tcnt, Tsa], [Tcnt])
                            ge = smalli[:, k4:k4 + 1]
                            ts(ge, vv, float(2 * TOPK - Na), None, ALU.is_ge, None, [Tcnt], [smalli])
                            cpred(lo, ge, mid, [Tmid, smalli], [bis])
                            yield
                    ts(mk[:, 0:N], score_s[:, 0:N], lo, None, ALU.is_ge, None, [TSs, bis], [mk])
                    if N < nkeys:
                        memset(mk[:, N:nkeys], 0.0, [mk], eng="gpsimd")
                    yield

                def gen_G(items):
                    for ni, n in items:
                        for hf in range(2):
                            gpan = stream.next("%d_gate%d_%d" % (l, n, hf))
                            pv = wview(gpan, "gate0_0")
                            for c in range(4):
                                ps = nps()
                                fm_chunk(gpan, pv, c, ps)
                                act(gsig[:, hf * 4 + c, :], ps[:, 0:T], AF.Sigmoid, [ps], [gsig])
                                yield
                        ysrc = {2: ycT, 1: ybT, 0: yaT}[n]
                        for hf in range(2):
                            bpan = stream.next("%d_br%d_%d" % (l, n, hf))
                            pv = wview(bpan, "br0_0")
                            for c in range(4):
                                dchunk = hf * 4 + c
                                ps = nps()
                                for kc in range(4):
                                    mm(ps[:, 0:T], pv[:, kc, c * 128:(c + 1) * 128], ysrc[:, kc, :], kc == 0, kc == 3,
                                       [bpan, ysrc], [ps])
                                if ni == 0:
                                    tt(merged[:, dchunk, :], ps[:, 0:T], gsig[:, dchunk, :], ALU.mult, [ps, gsig], [TS_])
                                else:
                                    tt(gt[:], ps[:, 0:T], gsig[:, dchunk, :], ALU.mult, [ps, gsig], [gt])
                                    dsto = mergedT if ni == 2 else merged
                                    tt(dsto[:, dchunk, :], merged[:, dchunk, :], gt[:], ALU.add, [TS_, gt], [TS_], eng="gpsimd")
                                yield

                def chain(*gens):
                    for g_ in gens:
                        yield from g_

                def run(*gens):
                    live = list(gens)
                    while live:
                        for g_ in list(live):
                            try:
                                next(g_)
                            except StopIteration:
                                live.remove(g_)

                def run_w(ga, gb_, kb):
                    la = lb = True
                    while la or lb:
                        if la:
                            try:
                                next(ga)
                            except StopIteration:
                                la = False
                        for _ in range(kb):
                            if lb:
                                try:
                                    next(gb_)
                                except StopIteration:
                                    lb = False

                run(chain(gen_mixC(), gen_mixB()), gen_I(0))
                if first_tile:
                    dump("ycT", ycT[:, 0, :], [ycT])
                    dump("ybT", ybT[:, 0, :], [ybT])
                    dump("score", score_ap[:, 0:128], [TS_])
                N1 = t0 + 256
                lenI = 1 + ((N1 + KG - 1) // KG) * 5
                lenB = 2 + (NBIS if (t0 + 128) > TOPK else 0)
                run_w(gen_B(0), gen_I(1), max(1, -(-lenI // lenB)))
                lenB1 = 2 + (NBIS if (t0 + 256) > TOPK else 0)
                run_w(gen_B(1), gen_G([(0, 2), (1, 1)]), max(1, -(-32 // lenB1)))

                first = True
                ucnt = [0]
                for gi, (g, wd) in enumerate(grps):
                    panel = stream.next("%d_kv%d_%d" % (l, j, g))
                    nb = wd // 128
                    KTv = panel[:, 0:4 * wd].rearrange("p (c w) -> p c w", c=4)
                    Vv = panel[:, 2048:2048 + nb * 520].rearrange("p (b f) -> p b f", b=nb)
                    psb = PS[6][:].bitcast(BF16)
                    for s in range(NSUB):
                        for b in range(nb):
                            tr(psb[:, (s * 4 + b) * 128:(s * 4 + b + 1) * 128],
                               masks[s][:, g * KG + b * 128:g * KG + (b + 1) * 128], identb, [masks[s], cb], [PS[6]])
                    for s in range(NSUB):
                        act(maskTk[:, 0:nb, s * 128:(s + 1) * 128],
                            psb[:, s * 512:s * 512 + nb * 128].rearrange("p (b t) -> p b t", b=nb),
                            AF.Copy, [PS[6]], [maskTk])
                    units = [(hp, b) for hp in range(4) for b in range(nb)]

                    def stageA(i):
                        hp, b = units[i]
                        ps = PS[(ucnt[0] + i) % 4]
                        mm(ps[:, 0:2 * T], KTv[:, hp, b * 128:(b + 1) * 128], qT[:, hp, :, :].rearrange("p a t -> p (a t)"),
                           True, True, [panel, qT], [ps])

                    def stageB(i):
                        hp, b = units[i]
                        ps = PS[(ucnt[0] + i) % 4]
                        ptile = PT[(ucnt[0] + i) % len(PT)]
                        act(ptile[:, :, :], ps[:, 0:2 * T].rearrange("p (a t) -> p a t", a=2), AF.Exp,
                            [ps], [ptile], scale=HD ** -0.5)
                        tt(ptile[:, :, :], ptile[:, :, :], maskTk[:, b:b + 1, :].to_broadcast([128, 2, T]), ALU.mult,
                           [ptile, maskTk], [ptile], eng=("gpsimd" if (ucnt[0] + i) % 4 == 3 else "vector"))

                    def stageD(i):
                        hp, b = units[i]
                        ptile = PT[(ucnt[0] + i) % len(PT)]
                        for hh in range(2):
                            h = 2 * hp + hh
                            pacc = PS[4 + hh]
                            mm(pacc[0:65, 0:T], Vv[:, b, h * 65:(h + 1) * 65], ptile[:, hh, :], b == 0, b == nb - 1,
                               [panel, ptile], [pacc])
                            if b == nb - 1:
                                if first:
                                    cp(acc[:, h, :], pacc[0:65, 0:T], [pacc], [acc])
                                else:
                                    tt(acc[:, h, :], acc[:, h, :], pacc[0:65, 0:T], ALU.add, [pacc, acc], [acc])

                    for i in range(min(3, len(units))):
                        stageA(i)
                    for i in range(len(units)):
                        if i + 3 < len(units):
                            stageA(i + 3)
                        stageB(i)
                        stageD(i)
                    ucnt[0] += len(units)
                    first = False
                accf = accm[0:65, :]
                act(accf[64:65, :], accf[64:65, :], AF.Ln, [acc], [acc])
                act(accf[64:65, :], accf[64:65, :], AF.Exp, [acc], [acc], scale=-1.0)
                for h in range(8):
                    rhl = rhl2[h % 2]
                    cp(rhl[64:65, 0, :], acc[64:65, h, :], [acc], [rhl])
                    tt(rhl[64:65, 1, :], acc[64:65, h, :], rhl[64:65, 0, :], ALU.subtract, [acc, rhl], [rhl])
                    ps = nps()
                    mm(ps[0:64, 0:T], onesb[64:65, 0:64], rhl[64:65, 0, :], True, False, [onesb, rhl], [ps])
                    mm(ps[0:64, 0:T], onesb[64:65, 0:64], rhl[64:65, 1, :], False, True, [onesb, rhl], [ps])
                    if h % 2 == 0:
                        tt(yaT[0:64, h // 2, :], acc[0:64, h, :], ps[0:64, 0:T], ALU.mult, [acc, ps], [yaT])
                    else:
                        tt(yaTt[:, :], acc[0:64, h, :], ps[0:64, 0:T], ALU.mult, [acc, ps], [yaTt])
                        ps2 = nps()
                        mm(ps2[64:128, 0:T], identb[0:64, 0:64], yaTt[:, :], True, True, [cb, yaTt], [ps2])
                        cp(yaT[64:128, h // 2, :], ps2[64:128, 0:T], [ps2], [yaT])
                if first_tile:
                    dump("yaT", yaT[:, 0, :], [yaT])
                if l == 0 and j == 1:
                    dump("yaT1", yaT[:, 0, :], [yaT])
                    dump("acc1", acc[:, 0, :], [acc])

                run(gen_G([(2, 0)]))
                op_ = [stream.next("%d_out_%d" % (l, hf), look=NSLOT - 1 - hf) for hf in range(2)]
                for s in range(NSUB):
                    banks = [PS[4 + 2 * (s % 2)], PS[5 + 2 * (s % 2)]]
                    for hf in range(2):
                        pv = wview(op_[hf], "out_0")
                        for kc in range(8):
                            mm(banks[hf][:, 0:512], mergedT[:, kc, s * 128:(s + 1) * 128], pv[:, kc, :], kc == 0, kc == 7,
                               [op_[hf], TS_], [banks[hf]])
                    post_norm_residual(banks, 0, s)
                dma(gcur[:], gb_in[l, :, D:2 * D], [], [gcur], gsem, eng="gpsimd")
                if first_tile:
                    dump("x1", xt[:, 0, :], [xt])
                norm_transpose(8)
                dbanks = [[PS[4], PS[5]], [PS[6], PS[7]]]
                TF = [Tile("fT0"), Tile("fT1")]
                Trl = [Tile("rl0"), Tile("rl1")]

                for i_ in range(2):
                    merge_tile(TF[i_], TS_, as_write=True)
                    merge_tile(Trl[i_], TR_, as_write=True)

                def ffn_up(g):
                    up = stream.next("%d_up_%d" % (l, g))
                    pv = wview(up, "up_0")
                    fTg = fT[g % 2]
                    for c in range(4):
                        ps = nps()
                        fm_chunk(up, pv, c, ps)
                        rlb = rl[c % 2]
                        act(rlb, ps[:, 0:T], AF.Relu, [ps], [Trl[c % 2]])
                        tt(fTg[:, c, :], rlb, rlb, ALU.mult, [Trl[c % 2]], [TF[g % 2]], eng="gpsimd")

                def ffn_dn(g):
                    dn = stream.next("%d_dn_%d" % (l, g))
                    dv = wview(dn, "dn_0")
                    fTg = fT[g % 2]
                    for s in range(NSUB):
                        for hf in range(2):
                            for c in range(4):
                                mm(dbanks[s][hf][:, 0:512], fTg[:, c, s * 128:(s + 1) * 128], dv[:, c, hf * 512:(hf + 1) * 512],
                                   g == 0 and c == 0, g == 7 and c == 3, [dn, TF[g % 2]], [dbanks[s][hf]])

                ffn_up(0)
                for g in range(8):
                    if g + 1 < 8:
                        ffn_up(g + 1)
                    ffn_dn(g)
                for i_ in range(2):
                    merge_tile(TS_, TF[i_])
                    merge_tile(TR_, Trl[i_])
                for s in range(NSUB):
                    post_norm_residual(dbanks[s], D, s)
                dma(gcur[:], gb_in[l, :, 2 * D:3 * D], [], [gcur], gsem, eng="gpsimd")
                if first_tile:
                    dump("x2", xt[:, 0, :], [xt])
                norm_transpose(None)
                for s in range(NSUB):
                    cp(ptb[:, s, :], pt[:, s, :], [pt], [ptb])
                    psb = PS[3][:].bitcast(BF16)
                    for c in range(2):
                        tr(psb[:, c * 128:(c + 1) * 128], ptb[:, s, c * 128:(c + 1) * 128], identb, [ptb, cb], [PS[3]])
                    cp(pT[:, :, s * 128:(s + 1) * 128], psb[:, 0:256].rearrange("p (c t) -> p c t", c=2), [PS[3]], [pT])
                plp = stream.next("%d_ple" % l)
                plv = wview(plp, "ple")
                pg = [stream.next("%d_pg_%d" % (l, hf), look=NSLOT - 2 - hf) for hf in range(2)]
                ple2 = big[:, 0:2 * D].rearrange("p (s d) -> p s d", s=2)
                sspa = []
                for s in range(NSUB):
                    ssp = []
                    for hf in range(2):
                        pa = PS[4 * (s % 2) + hf]
                        pgb = PS[4 * (s % 2) + 2 + hf]
                        for kc in range(2):
                            mm(pa[:, 0:512], pT[:, kc, s * 128:(s + 1) * 128], plv[:, kc, hf * 512:(hf + 1) * 512], kc == 0, kc == 1,
                               [plp, pT], [pa])
                        gv = wview(pg[hf], "pg_0")
                        for kc in range(8):
                            mm(pgb[:, 0:512], hT[:, kc, s * 128:(s + 1) * 128], gv[:, kc, :], kc == 0, kc == 7, [pg[hf], hT], [pgb])
                        act(sg, pgb[:, 0:512], AF.Sigmoid, [pgb], [TR_])
                        tt(ple2[:, s, hf * 512:(hf + 1) * 512], pa[:, 0:512], sg, ALU.mult, [pa, TR_], [TS_])
                        a = sm()
                        act(tmpf[:, 0:512], ple2[:, s, hf * 512:(hf + 1) * 512], AF.Square, [TS_, small], [tmpf, small], accum_out=a)
                        ssp.append(a)
                    sspa.append(ssp)
                for s in range(NSUB):
                    s3 = sm()
                    tt(s3, sspa[s][0], sspa[s][1], ALU.add, [small], [small])
                    rs = rstd_from_ss(s3, D)
                    for hf in range(2):
                        stt(tmpf[:, 0:512], ple2[:, s, hf * 512:(hf + 1) * 512], rs, gcur[:, hf * 512:(hf + 1) * 512],
                            ALU.mult, ALU.mult, [TS_, small, gcur], [tmpf])
                        tt(xt[:, s, hf * 512:(hf + 1) * 512], xt[:, s, hf * 512:(hf + 1) * 512], tmpf[:, 0:512], ALU.add,
                           [xt, tmpf], [xt])
                if j + 1 < NT:
                    dma(gcur[:], gb_in[l, :, 0:D], [], [gcur], gsem, eng="gpsimd")
                wt = [XT[j]] if XT is not None else [out_tile]
                dma(dst_x[t0:t0 + T, :].rearrange("(s p) d -> p s d", p=128), xt[:], [xt], wt, ssem, eng="gpsimd")
            XTprev = XT

        fw.finish("sync", [out_tile] + dbg_tiles)
        fw.emit()
    return nc, fw


def _consts():
    bf = ml_dtypes.bfloat16
    ident = np.eye(128, dtype=np.float32)
    Rm = np.zeros((128, 128), np.float32)
    for base in (0, 64):
        for d in range(8):
            Rm[base + d + 8, base + d] = -1.0
            Rm[base + d, base + d + 8] = 1.0
    esel = np.zeros((128, 4, 128), np.float32)
    for g in range(8):
        esel[g, g // 2, (g % 2) * 64:(g % 2 + 1) * 64] = 1.0
    cb = np.concatenate([ident, Rm, esel.reshape(128, 512)], axis=1).astype(bf)
    tt_, ss_ = np.meshgrid(np.arange(128), np.arange(128), indexing="ij")
    negmask = np.where(ss_ > tt_, np.float32(NEG), np.float32(0.0))
    posfill = np.where(ss_ > tt_, np.float32(-2 * NEG), np.float32(0.0))
    tril01 = (tt_ <= ss_).astype(np.float32)
    half = 8
    inv_freq = (np.float32(500000.0) ** (-np.arange(half, dtype=np.float32) * np.float32(2.0) / np.float32(16))).astype(np.float32)
    invf = np.zeros((128, 1), np.float32)
    for f in range(128):
        d = f % 64
        if d < 16:
            invf[f, 0] = inv_freq[d % 8]
    cf = np.concatenate([ident, negmask, posfill, tril01, invf], axis=1).astype(np.float32)
    return cb, cf


def _layout_params(inp, depth):
    f = np.float32
    cols = np.zeros((depth, 128, 48), f)
    gb = np.zeros((depth, 128, 3 * D + 512), f)
    wbd = np.zeros((depth, 128, 8, 128), f)
    wsp = np.zeros((depth, 128, 8, 128), f)
    for l in range(depth):
        cols[l, :, 0:8] = np.asarray(inp["g_pre_mix"][l], f).reshape(8, 128).T
        cols[l, :, 8:16] = np.asarray(inp["g_pre_ffn"][l], f).reshape(8, 128).T
        cw = np.asarray(inp["conv_w"][l], f)
        for jj in range(4):
            cols[l, :, 16 + jj * 4:20 + jj * 4] = cw[jj].reshape(4, 128).T
        cols[l, :, 32:36] = np.asarray(inp["conv_b"][l], f).reshape(4, 128).T
        cols[l, :, 36:40] = np.asarray(inp["b_rg_a"][l], f).reshape(4, 128).T
        cols[l, :, 40:44] = np.asarray(inp["b_rg_x"][l], f).reshape(4, 128).T
        cols[l, :, 44:48] = np.asarray(inp["lru_lambda"][l], f).reshape(4, 128).T
        row = np.concatenate([np.asarray(inp["g_post_mix"][l], f), np.asarray(inp["g_post_ffn"][l], f),
                              np.asarray(inp["g_post_ple"][l], f), np.asarray(inp["g_gmlp_v"][l], f)])
        gb[l] = np.broadcast_to(row[None, :], (128, row.size))
        for gi, key in enumerate(("w_rg_a", "w_rg_x")):
            w = np.asarray(inp[key][l], f)
            for c in range(4):
                for hh in range(2):
                    wbd[l, hh * 64:(hh + 1) * 64, gi * 4 + c, hh * 64:(hh + 1) * 64] = w[c * 2 + hh]
        ws = np.asarray(inp["w_spatial"][l], f)
        wsp[l] = np.transpose(ws, (2, 0, 1))
    bsp = np.ascontiguousarray(np.asarray(inp["b_spatial"], f))
    return cols, gb, wbd, wsp, bsp


_CACHE = {}


def kernel(**inputs):
    depth = DEPTH
    x = np.asarray(inputs["x"], np.float32)
    B, L, _ = x.shape
    p = np.asarray(inputs["p"], np.float32)
    pos = np.asarray(inputs["positions"], np.int32)
    cb, cf = _consts()
    cols, gb, wbd, wsp, bsp = _layout_params(inputs, depth)
    shared = {
        "w_in": np.ascontiguousarray(np.asarray(inputs["w_in"], np.float32)),
        "w_branch": np.ascontiguousarray(np.asarray(inputs["w_branch"], np.float32)),
        "w_out": np.ascontiguousarray(np.asarray(inputs["w_out"], np.float32)),
        "w_ffn_up": np.ascontiguousarray(np.asarray(inputs["w_ffn_up"], np.float32)),
        "w_ffn_down": np.ascontiguousarray(np.asarray(inputs["w_ffn_down"], np.float32)),
        "w_ple": np.ascontiguousarray(np.asarray(inputs["w_ple"], np.float32)),
        "w_ple_gate": np.ascontiguousarray(np.asarray(inputs["w_ple_gate"], np.float32)),
        "cols": cols, "gb": gb, "wbd": wbd, "wsp": wsp, "bsp": bsp, "cb": cb, "cf": cf,
    }
    if L not in _CACHE:
        _CACHE[L] = build_program(L, depth)[0]
    nc = _CACHE[L]
    in_maps = []
    for b in range(B):
        m = dict(shared)
        m["x"] = np.ascontiguousarray(x[b])
        m["p"] = np.ascontiguousarray(p[:, b])
        m["pos"] = np.ascontiguousarray(np.broadcast_to(pos[b][None, :], (128, L)))
        in_maps.append(m)
    res = run_bass_kernel_spmd(nc, in_maps, core_ids=list(range(B)))
    return np.stack([np.asarray(r["y"], np.float32) for r in res.results], axis=0)
```

```python
import math
from contextlib import ExitStack
import numpy as np
import ml_dtypes
import concourse.bass as bass
import concourse.mybir as mybir
from concourse.bass_utils import run_bass_kernel_spmd

F32 = mybir.dt.float32
BF16 = mybir.dt.bfloat16
I32 = mybir.dt.int32
AF = mybir.ActivationFunctionType
ALU = mybir.AluOpType
AX = mybir.AxisListType

D = 1024
NH = 8
HD = 64
TOPK = 256
FFN = 4096
PLE = 256
EPS = 1e-6
DEPTH = 2
T = 256
NSUB = T // 128
KG = 512
NSLOT = 3
SLOTB = 4224
NBIS = 12
BIGM = 30000.0
NEG = -1.0e30
IN_OFF = dict(q=0, k=512, v=1024, qi=1536, kiwi=2048, xr=2120, gr=2632, zu=3144, zv=3656, gate=4168)


class Sem:
    def __init__(self, h, name):
        self.h = h
        self.n = 0
        self.name = name


class Tile:
    __slots__ = ("w", "r", "name")

    def __init__(self, name=""):
        self.w = {}
        self.r = {}
        self.name = name


class Buf:
    def __init__(self, t, name=""):
        self.t = t
        self.T = Tile(name)

    def __getitem__(self, k):
        return self.t[k]


class Engine:
    def __init__(self, name, sem):
        self.name = name
        self.sem = sem
        self.ops = []
        self.seen = {}


class FW:
    def __init__(self, nc, stack):
        self.nc = nc
        self.stack = stack
        self.engs = {}
        for n in ("tensor", "vector", "scalar", "gpsimd", "sync"):
            s = Sem(stack.enter_context(nc.semaphore("sem_" + n)), n)
            self.engs[n] = Engine(n, s)
        self.nops = 0

    def dsem(self, name):
        return Sem(self.stack.enter_context(self.nc.semaphore("dsem_" + name)), name)

    def op(self, eng, fn, reads=(), writes=(), dsem=None):
        E = self.engs[eng]
        need = {}
        for b in reads:
            t = b.T if isinstance(b, Buf) else b
            for s, v in t.w.items():
                if need.get(s, 0) < v:
                    need[s] = v
        for b in writes:
            t = b.T if isinstance(b, Buf) else b
            for s, v in t.w.items():
                if need.get(s, 0) < v:
                    need[s] = v
            for s, v in t.r.items():
                if need.get(s, 0) < v:
                    need[s] = v
        raw_self = 0
        for b in reads:
            t = b.T if isinstance(b, Buf) else b
            raw_self = max(raw_self, t.w.get(E.sem, 0))
        waits = []
        for s, v in need.items():
            if s is E.sem:
                if eng != "tensor" and raw_self > E.seen.get(s, 0):
                    E.seen[s] = raw_self
                    waits.append((s, raw_self))
                continue
            if E.seen.get(s, 0) >= v:
                continue
            E.seen[s] = v
            waits.append((s, v))
        if dsem is not None:
            dsem.n += 16
            sig = (dsem, dsem.n, 16)
        else:
            E.sem.n += 1
            sig = (E.sem, E.sem.n, 1)
        E.ops.append((waits, fn, sig))
        self.nops += 1
        s, v = sig[0], sig[1]
        for b in reads:
            t = b.T if isinstance(b, Buf) else b
            if t.r.get(s, 0) < v:
                t.r[s] = v
        for b in writes:
            t = b.T if isinstance(b, Buf) else b
            if t.w.get(s, 0) < v:
                t.w[s] = v

    def finish(self, eng, tiles):
        E = self.engs[eng]
        need = {}
        for b in tiles:
            t = b.T if isinstance(b, Buf) else b
            for d in (t.w, t.r):
                for s, v in d.items():
                    if need.get(s, 0) < v:
                        need[s] = v
        E.ops.append(([(s, v) for s, v in need.items() if s is not E.sem], None, None))

    def emit(self):
        with self.nc.Block() as block:
            for n, E in self.engs.items():
                def body(e, E=E):
                    for waits, fn, sig in E.ops:
                        for s, v in waits:
                            e.wait_ge(s.h, v)
                        if fn is None:
                            continue
                        fn(e).then_inc(sig[0].h, sig[2])
                getattr(block, n)(body)


def panel_defs():
    P = []
    for nm in ("q", "k", "qi"):
        P.append((nm, "w_in", 0, 1024, IN_OFF[nm], 512))
    P.append(("kiwi", "w_in", 0, 1024, IN_OFF["kiwi"], 72))
    for nm in ("xr", "gr", "zu", "v", "zv"):
        P.append((nm, "w_in", 0, 1024, IN_OFF[nm], 512))
    for n in range(3):
        for hf in range(2):
            P.append(("gate%d_%d" % (n, hf), "w_in", 0, 1024, IN_OFF["gate"] + n * 1024 + hf * 512, 512))
    for n in range(3):
        for hf in range(2):
            P.append(("br%d_%d" % (n, hf), "w_branch%d" % n, 0, 512, hf * 512, 512))
    for hf in range(2):
        P.append(("out_%d" % hf, "w_out", 0, 1024, hf * 512, 512))
    for g in range(8):
        P.append(("up_%d" % g, "w_ffn_up", 0, 1024, g * 512, 512))
        P.append(("dn_%d" % g, "w_ffn_down", g * 512, 512, 0, 1024))
    P.append(("ple", "w_ple", 0, 256, 0, 1024))
    for hf in range(2):
        P.append(("pg_%d" % hf, "w_ple_gate", 0, 1024, hf * 512, 512))
    return P


def build_program(L, depth=DEPTH, dbg=None):
    NT = L // T
    nc = bass.Bass("TRN2", target_bir_lowering=False)
    dram = lambda name, shape, dt, kind="ExternalInput": nc.dram_tensor(name, shape, dt, kind=kind).ap()
    x_in = dram("x", [L, D], F32)
    p_in = dram("p", [depth, L, PLE], F32)
    pos_in = dram("pos", [128, L], I32)
    wsrc = {
        "w_in": dram("w_in", [depth, D, 7240], F32),
        "w_branch": dram("w_branch", [depth, 3, 512, D], F32),
        "w_out": dram("w_out", [depth, D, D], F32),
        "w_ffn_up": dram("w_ffn_up", [depth, D, FFN], F32),
        "w_ffn_down": dram("w_ffn_down", [depth, FFN, D], F32),
        "w_ple": dram("w_ple", [depth, PLE, D], F32),
        "w_ple_gate": dram("w_ple_gate", [depth, D, D], F32),
    }
    cols_in = dram("cols", [depth, 128, 48], F32)
    gb_in = dram("gb", [depth, 128, 3 * D + 512], F32)
    wbd_in = dram("wbd", [depth, 128, 8, 128], F32)
    wsp_in = dram("wsp", [depth, 128, 8, 128], F32)
    bsp_in = dram("bsp", [depth, 8, 128], F32)
    cb_in = dram("cb", [128, 256 + 512], BF16)
    cf_in = dram("cf", [128, 4 * 128 + 1], F32)
    y_out = dram("y", [L, D], F32, kind="ExternalOutput")
    xbuf = dram("xbuf", [L, D], F32, kind="Internal")
    KTc = [dram("ktc%d" % l, [4, 128, L], BF16, kind="Internal") for l in range(depth)]
    Vc = [dram("vc%d" % l, [L, 520], BF16, kind="Internal") for l in range(depth)]
    pdefs = panel_defs()
    Wp = [{nm: dram("wp%d_%s" % (l, nm), [nr, ncol], BF16, kind="Internal") for (nm, _, _, nr, _, ncol) in pdefs}
          for l in range(depth)]
    dbg_out = {}
    if dbg:
        for k, shp in dbg.items():
            dbg_out[k] = dram("dbg_" + k, list(shp), F32, kind="ExternalOutput")

    with ExitStack() as st:
        fw = FW(nc, st)
        op = fw.op

        def sb(name, shape, dt):
            return Buf(st.enter_context(nc.sbuf_tensor("s_" + name, shape, dt)), name)

        PS = [Buf(st.enter_context(nc.psum_tensor("ps%d" % i, [128, 512], F32)), "ps%d" % i) for i in range(8)]

        def mm(out, lhsT, rhs, start, stop, R, W):
            op("tensor", lambda e: e.matmul(out, lhsT=lhsT, rhs=rhs, start=start, stop=stop), R, W)

        def tr(out, in_, ident, R, W):
            op("tensor", lambda e: e.transpose(out=out, in_=in_, identity=ident), R, W)

        def act(out, in_, func, R, W, **kw):
            op("scalar", lambda e: e.activation(out=out, in_=in_, func=func, **kw), R, W)

        def ts(out, in0, s1, s2, op0, op1, R, W, eng="vector"):
            if op1 is None:
                op(eng, lambda e: e.tensor_scalar(out=out, in0=in0, scalar1=s1, scalar2=None, op0=op0), R, W)
            else:
                op(eng, lambda e: e.tensor_scalar(out=out, in0=in0, scalar1=s1, scalar2=s2, op0=op0, op1=op1), R, W)

        def tt(out, in0, in1, o, R, W, eng="vector"):
            op(eng, lambda e: e.tensor_tensor(out=out, in0=in0, in1=in1, op=o), R, W)

        def stt(out, in0, s, in1, op0, op1, R, W):
            op("vector", lambda e: e.scalar_tensor_tensor(out=out, in0=in0, scalar=s, in1=in1, op0=op0, op1=op1), R, W)

        def cp(out, in_, R, W, eng="vector"):
            op(eng, lambda e: e.tensor_copy(out=out, in_=in_), R, W)

        def dma(out, in_, R, W, ds, eng="sync"):
            op(eng, lambda e: e.dma_start(out=out, in_=in_), R, W, dsem=ds)

        def memset(ap, val, W, eng="vector"):
            op(eng, lambda e: e.memset(ap, val), [], W)

        def reduce(out, in_, o, R, W):
            op("vector", lambda e: e.tensor_reduce(out=out, in_=in_, axis=AX.X, op=o), R, W)

        def recip(out, in_, R, W):
            op("vector", lambda e: e.reciprocal(out=out, in_=in_), R, W)

        def scan(out, d0, d1, init, R, W):
            op("vector", lambda e: e.tensor_tensor_scan(out=out, data0=d0, data1=d1, initial=init,
                                                        op0=ALU.mult, op1=ALU.add), R, W)

        def count_ge(out, in0, thr, cnt, R, W):
            op("vector", lambda e: e.tensor_scalar(out=out, in0=in0, scalar1=thr, scalar2=None, op0=ALU.is_ge,
                                                   op1=ALU.add, accum_out=cnt), R, W)

        def cpred(out, mask, data, R, W):
            op("vector", lambda e: e.copy_predicated(out=out, mask=mask, data=data), R, W)

        dbg_sem = fw.dsem("dbg")
        dbg_tiles = []

        def dump(name, ap, R):
            if name in dbg_out:
                t = Tile("dbg")
                dma(dbg_out[name], ap, R, [t], dbg_sem, eng="gpsimd")
                dbg_tiles.append(t)
                del dbg_out[name]

        cb = sb("cb", [128, 768], BF16)
        cf = sb("cf", [128, 513], F32)
        dma(cb[:], cb_in, [], [cb], fw.dsem("c0"))
        dma(cf[:], cf_in, [], [cf], fw.dsem("c1"))
        identb = cb[:, 0:128]
        Rm = cb[:, 128:256]
        esel = cb[0:8, 256:768].rearrange("p (c f) -> p c f", c=4)
        identf = cf[:, 0:128]
        negmask = cf[:, 128:256]
        posfill = cf[:, 256:384]
        tril01 = cf[:, 384:512]
        invf = cf[:, 512:513]

        WT = [dict() for _ in range(depth)]
        for l in range(depth):
            wcs = fw.dsem("wcast%d" % l)
            for (nm, src, r0, nr, c0, ncol) in pdefs:
                if src.startswith("w_branch"):
                    s_ap = wsrc["w_branch"][l, int(src[-1]), r0:r0 + nr, c0:c0 + ncol]
                else:
                    s_ap = wsrc[src][l, r0:r0 + nr, c0:c0 + ncol]
                t = Tile("wp")
                WT[l][nm] = t
                step = 512
                for rr in range(0, nr, step):
                    n2 = min(step, nr - rr)
                    dma(Wp[l][nm][rr:rr + n2, :], s_ap[rr:rr + n2, :], [], [t], wcs, eng="gpsimd")
            for t in WT[l].values():
                t.w = {wcs: wcs.n}

        ring = [sb("ring%d" % i, [128, SLOTB], BF16) for i in range(NSLOT)]
        ring_sem = [fw.dsem("ring%d" % i) for i in range(NSLOT)]
        kiT = sb("kiT", [128, L], BF16)
        xt = sb("xt", [128, NSUB, D], F32)
        pt = sb("pt", [128, NSUB, PLE], F32)
        ptb = sb("ptb", [128, NSUB, PLE], BF16)
        posi = sb("posi", [128, T], I32)
        hT = sb("hT", [128, 8, T], BF16)
        pT = sb("pT", [128, 2, T], BF16)
        qT = sb("qT", [128, 4, 2, T], BF16)
        qiT = sb("qiT", [128, 4, T], BF16)
        KTs = sb("KTs", [128, 4, T], BF16)
        Vs = sb("Vs", [128, NSUB, 520], BF16)
        cosT = sb("cosT", [128, T], F32)
        sinT = sb("sinT", [128, T], F32)
        rtmp = sb("rtmp", [128, 3, T], F32)
        xb16 = sb("xb16", [128, T], BF16)
        xrbuf = sb("xrbuf", [128, 4, 3 + T], F32)
        grT = sb("grT", [128, 4, T], BF16)
        zuT = sb("zuT", [128, 4, T], BF16)
        vn = sb("vn", [128, NSUB, 512], BF16)
        hst = sb("hst", [128, 4], F32)
        ybT = sb("ybT", [128, 4, T], BF16)
        ycT = sb("ycT", [128, 4, T], BF16)
        yaT = sb("yaT", [128, 4, T], BF16)
        yaTt = sb("yaTt", [64, T], BF16)
        wis = sb("wis", [128, NSUB, 8], F32)
        diag = sb("diag", [128, 8, 128], BF16)
        small = sb("small", [128, 32], F32)
        smalli = sb("smalli", [128, 4], I32)
        bis = sb("bis", [128, 16], F32)
        bis2 = sb("bis2", [128, 16], F32)
        Tmid = Tile("mid")
        Tcnt = Tile("cnt")
        Tsa = Tile("sa")
        big = sb("big", [128, 6144], F32)
        TS_ = big.T
        TR_ = Tile("bigR")
        score_ap = big[:, 0:L]
        Rrelu = big[:, 4096:6144].bitcast(BF16).rearrange("p (h w) -> p h w", h=8)
        masks = [sb("mask%d" % s, [128, L], BF16) for s in range(NSUB)]
        maskTk = sb("maskTk", [128, 4, T], BF16)
        PT = [sb("PT%d" % i, [128, 2, T], BF16) for i in range(4)]
        xs = sb("xs", [128, D], BF16)
        accm = sb("accm", [128, 8 * T], F32)

        def view(ap, tile):
            v = Buf.__new__(Buf)
            v.t = ap
            v.T = tile
            return v

        acc = view(accm[0:65, :].rearrange("p (h t) -> p h t", h=8), accm.T)
        lt = view(accm[:, 0:5 * T].rearrange("p (k t) -> p k t", k=5), accm.T)
        gz = view(accm[:, 5 * T:5 * T + 512], accm.T)
        xcb = view(accm[:, 5 * T + 512:5 * T + 512 + T // 2].bitcast(BF16), accm.T)
        sc1 = sb("sc1", [128, L], F32)
        rhl2 = [sb("rhl%d" % i, [65, 2, T], BF16) for i in range(2)]
        onesb = sb("onesb", [65, 64], BF16)
        gt = sb("gt", [128, T], F32)
        tmpf = sb("tmpf", [128, 512], F32)
        gsig = sb("gsig", [128, 8, T], BF16)
        merged = big[:, 0:8 * T].rearrange("p (c t) -> p c t", c=8)
        o = 8 * T
        mergedT = big[:, o:o + 4 * T].bitcast(BF16).rearrange("p (c t) -> p c t", c=8)
        o += 4 * T
        fT = [big[:, o + i * 2 * T:o + (i + 1) * 2 * T].bitcast(BF16).rearrange("p (c t) -> p c t", c=4) for i in range(2)]
        o += 4 * T
        assert o <= 4096
        o = 4096
        rl = [big[:, o + i * (T // 2):o + (i + 1) * (T // 2)].bitcast(BF16) for i in range(2)]
        o += T
        sg = big[:, o:o + 512]
        o += 512
        ple_t = big[:, o:o + 1024]
        o += 1024
        assert o <= 6144
        wbdf = big[:, 4096:4096 + 1024].rearrange("p (c j) -> p c j", c=8)
        cols = sb("cols", [128, 48], F32)
        gbv = sb("gbv", [128, 512], F32)
        gcur = sb("gcur", [128, D], F32)
        gsem = fw.dsem("gain")
        wbd = sb("wbd", [128, 8, 128], BF16)
        wspT = sb("wspT", [128, 8, 128], BF16)
        bsp = sb("bsp", [8, 128], F32)
        bsph = sb("bsph", [8, 2, 128], BF16)
        c8 = sb("c8", [128, 8], F32)
        epsc = sb("epsc", [128, 4], F32)
        psem = [fw.dsem("par%d" % i) for i in range(5)]
        xsem = fw.dsem("xload")
        ptsem = fw.dsem("pload")
        possem = fw.dsem("posload")
        ssem = fw.dsem("store")
        kvsem = fw.dsem("kvstore")
        out_tile = Tile("out")

        class Stream:
            def __init__(self):
                self.plan = []
                self.issued = 0
                self.pos = 0

            def add(self, name, loader, deps, hoist=True):
                self.plan.append((name, loader, deps, hoist))

            def next(self, name, look=NSLOT - 1):
                i = self.pos
                assert self.plan[i][0] == name, (self.plan[i][0], name)
                while self.issued < len(self.plan) and (
                        self.issued <= i or (self.issued <= i + look and self.plan[self.issued][3])):
                    k = self.issued
                    _, loader, deps, _ = self.plan[k]
                    loader(ring[k % NSLOT], ring_sem[k % NSLOT], deps)
                    self.issued += 1
                self.pos += 1
                return ring[i % NSLOT]

        stream = Stream()

        def wloader(l, nm, nr, ncol):
            kc = nr // 128

            def f(slot, sem, deps):
                dst = slot[:, 0:kc * ncol].rearrange("p (k w) -> p k w", k=kc)
                src = Wp[l][nm].rearrange("(k p) w -> p k w", p=128)
                dma(dst, src, deps, [slot], sem)
            return f

        KVT = [[Tile("kv") for _ in range((L + KG - 1) // KG)] for _ in range(depth)]

        def kvloader(l, g, wd):
            def f(slot, sem, deps):
                dstk = slot[:, 0:4 * wd].rearrange("p (c w) -> p c w", c=4)
                dma(dstk, KTc[l][:, :, g * KG:g * KG + wd].rearrange("c p w -> p c w"), deps, [slot], sem)
                nb = wd // 128
                dstv = slot[:, 2048:2048 + nb * 520].rearrange("p (b f) -> p b f", b=nb)
                dma(dstv, Vc[l][g * KG:g * KG + wd, :].rearrange("(b p) f -> p b f", p=128), deps, [slot], sem)
            return f

        pinfo = {nm: (nr, ncol) for (nm, _, _, nr, _, ncol) in pdefs}

        def plan_w(l, nm):
            nr, ncol = pinfo[nm]
            stream.add("%d_%s" % (l, nm), wloader(l, nm, nr, ncol), [WT[l][nm]])

        def kv_groups(j):
            nkeys = (j + 1) * T
            out = []
            g = 0
            while g * KG < nkeys:
                out.append((g, min(KG, nkeys - g * KG)))
                g += 1
            return out

        for l in range(depth):
            for j in range(NT):
                for nm in ("q", "k", "qi", "kiwi", "xr", "gr", "zu", "v", "zv"):
                    plan_w(l, nm)
                for n in (2, 1):
                    plan_w(l, "gate%d_0" % n)
                    plan_w(l, "gate%d_1" % n)
                    plan_w(l, "br%d_0" % n)
                    plan_w(l, "br%d_1" % n)
                plan_w(l, "gate0_0")
                plan_w(l, "gate0_1")
                grps = kv_groups(j)
                for (g, wd) in grps:
                    last = (g == grps[-1][0])
                    stream.add("%d_kv%d_%d" % (l, j, g), kvloader(l, g, wd), [KVT[l][g]], hoist=not last)
                plan_w(l, "br0_0")
                plan_w(l, "br0_1")
                plan_w(l, "out_0")
                plan_w(l, "out_1")
                plan_w(l, "up_0")
                for g in range(8):
                    if g + 1 < 8:
                        plan_w(l, "up_%d" % (g + 1))
                    plan_w(l, "dn_%d" % g)
                plan_w(l, "ple")
                plan_w(l, "pg_0")
                plan_w(l, "pg_1")

        def wview(slot, nm):
            nr, ncol = pinfo[nm]
            kc = nr // 128
            return slot[:, 0:kc * ncol].rearrange("p (k w) -> p k w", k=kc)

        sm_i = [0]

        def sm():
            i = sm_i[0] % 32
            sm_i[0] += 1
            return small[:, i:i + 1]

        memset(epsc[:, 0:1], EPS, [epsc])
        memset(epsc[:, 1:2], math.pi / 2, [epsc])
        memset(epsc[:, 2:3], 1.0, [epsc])
        memset(epsc[:, 3:4], -BIGM, [epsc])
        memset(Vs[:, :, :], 1.0, [Vs])
        memset(onesb[:, :], 1.0, [onesb])
        memset(qT[:, :, :, :], 0.0, [qT])

        def rstd_from_ss(ss_ap, n):
            a = sm()
            b = sm()
            act(a, ss_ap, AF.Sqrt, [small, epsc], [small], scale=1.0 / n, bias=epsc[:, 0:1])
            recip(b, a, [small], [small])
            return b

        pi = [0]

        def nps():
            pi[0] += 1
            return PS[pi[0] % 4]

        def norm_transpose(gcol0):
            for s in range(NSUB):
                if gcol0 is not None:
                    ss = sm()
                    act(tmpf[:, 0:512], xt[:, s, 0:512], AF.Square, [xt, small], [tmpf, small], accum_out=ss)
                    ss2 = sm()
                    act(tmpf[:, 0:512], xt[:, s, 512:1024], AF.Square, [xt, small], [tmpf, small], accum_out=ss2)
                    ss3 = sm()
                    tt(ss3, ss, ss2, ALU.add, [small], [small])
                    rs = rstd_from_ss(ss3, D)
                    ts(xs[:], xt[:, s, :], rs, None, ALU.mult, None, [xt, small], [xs])
                else:
                    cp(xs[:], xt[:, s, :], [xt], [xs])
                psb = PS[7][:].bitcast(BF16)
                for c in range(8):
                    tr(psb[:, c * 128:(c + 1) * 128], xs[:, c * 128:(c + 1) * 128], identb, [xs, cb], [PS[7]])
                src = psb[:, 0:1024].rearrange("p (c t) -> p c t", c=8)
                dst = hT[:, :, s * 128:(s + 1) * 128]
                if gcol0 is not None:
                    g_ap = cols[:, gcol0:gcol0 + 8].unsqueeze(2).to_broadcast([128, 8, 128])
                    tt(dst, src, g_ap, ALU.mult, [PS[7], cols], [hT])
                else:
                    cp(dst, src, [PS[7]], [hT])

        def post_norm_residual(banks, gcol, s):
            ssa = []
            for hf in range(2):
                a = sm()
                act(tmpf[:, 0:512], banks[hf][:, 0:512], AF.Square, [banks[hf], small], [tmpf, small], accum_out=a)
                ssa.append(a)
            s3 = sm()
            tt(s3, ssa[0], ssa[1], ALU.add, [small], [small])
            rs = rstd_from_ss(s3, D)
            for hf in range(2):
                stt(tmpf[:, 0:512], banks[hf][:, 0:512], rs, gcur[:, hf * 512:(hf + 1) * 512],
                    ALU.mult, ALU.mult, [banks[hf], small, gcur], [tmpf])
                tt(xt[:, s, hf * 512:(hf + 1) * 512], xt[:, s, hf * 512:(hf + 1) * 512], tmpf[:, 0:512], ALU.add,
                   [xt, tmpf], [xt])

        def merge_tile(dst, src, as_write=False):
            for a, b in ((dst.w, src.w), (dst.r, src.r)):
                for k_, v_ in b.items():
                    if a.get(k_, 0) < v_:
                        a[k_] = v_
            if as_write:
                for k_, v_ in src.r.items():
                    if dst.w.get(k_, 0) < v_:
                        dst.w[k_] = v_

        def fm_chunk(panel, pv, c, ps, M=128):
            for kc in range(8):
                mm(ps[0:M, 0:T], pv[:, kc, c * 128:c * 128 + M], hT[:, kc, :], kc == 0, kc == 7, [panel, hT], [ps])

        def rope_evac(ps, dst, W, split=None):
            act(xb16[:], ps[:, 0:T], AF.Copy, [ps], [xb16])
            mm(ps[:, T:2 * T], Rm, xb16[:], True, True, [cb, xb16], [ps])
            tt(rtmp[:, 0, :], ps[:, 0:T], cosT[:], ALU.mult, [ps, cosT], [rtmp])
            tt(rtmp[:, 1, :], ps[:, T:2 * T], sinT[:], ALU.mult, [ps, sinT], [rtmp])
            if split is None:
                tt(dst, rtmp[:, 0, :], rtmp[:, 1, :], ALU.add, [rtmp], W)
            else:
                for hh in range(2):
                    tt(split[hh * 64:(hh + 1) * 64, hh, :], rtmp[hh * 64:(hh + 1) * 64, 0, :], rtmp[hh * 64:(hh + 1) * 64, 1, :],
                       ALU.add, [rtmp], W)

        XTprev = None
        for l in range(depth):
            src_x = x_in if l == 0 else xbuf
            dst_x = y_out if l == depth - 1 else xbuf
            XT = [Tile("xd") for _ in range(NT)] if l < depth - 1 else None
            dma(cols[:], cols_in[l], [], [cols], psem[0])
            dma(gbv[:], gb_in[l, :, 3 * D:3 * D + 512], [], [gbv], psem[1])
            dma(gcur[:], gb_in[l, :, 0:D], [], [gcur], gsem)
            dma(bsp[:], bsp_in[l], [], [bsp], psem[2])
            cp(bsph[:, 0, :], bsp[:], [bsp], [bsph])
            tt(bsp[:], bsp[:], bsph[:, 0, :], ALU.subtract, [bsph], [bsp])
            cp(bsph[:, 1, :], bsp[:], [bsp], [bsph])
            dma(wbdf, wbd_in[l], [], [TR_], psem[3])
            cp(wbd[:], wbdf, [TR_], [wbd])
            dma(wbdf, wsp_in[l], [], [TR_], psem[4])
            tt(wspT[:], wbdf, tril01.unsqueeze(1).to_broadcast([128, 8, 128]), ALU.mult, [TR_, cf], [wspT])
            act(c8[:, 4:8], cols[:, 44:48], AF.Exp, [cols], [c8], scale=-1.0)
            ts(c8[:, 0:4], c8[:, 4:8], -0.25, 1.0 / 3.0, ALU.mult, ALU.add, [c8], [c8])
            tt(c8[:, 0:4], c8[:, 0:4], c8[:, 4:8], ALU.mult, [c8], [c8])
            ts(c8[:, 0:4], c8[:, 0:4], -1.0, 0.5, ALU.mult, ALU.add, [c8], [c8])
            tt(c8[:, 0:4], c8[:, 0:4], c8[:, 4:8], ALU.mult, [c8], [c8])
            ts(c8[:, 0:4], c8[:, 0:4], -1.0, 1.0, ALU.mult, ALU.add, [c8], [c8])
            tt(c8[:, 0:4], c8[:, 0:4], c8[:, 4:8], ALU.mult, [c8], [c8])
            ts(c8[:, 0:4], c8[:, 0:4], -8.0, None, ALU.mult, None, [c8], [c8])
            ts(c8[:, 4:8], c8[:, 0:4], 2.0, None, ALU.mult, None, [c8], [c8])
            memset(xrbuf[:, :, 0:3], 0.0, [xrbuf])
            memset(hst[:], 0.0, [hst])

            for j in range(NT):
                t0 = j * T
                first_tile = (l == 0 and j == 0)
                rd = [XTprev[j]] if (l > 0) else []
                dma(xt[:], src_x[t0:t0 + T, :].rearrange("(s p) d -> p s d", p=128), rd, [xt], xsem)
                dma(pt[:], p_in[l, t0:t0 + T, :].rearrange("(s p) d -> p s d", p=128), [], [pt], ptsem)
                dma(posi[:], pos_in[:, t0:t0 + T], [], [posi], possem)
                ang = rtmp[:, 0, :]
                u = rtmp[:, 1, :]
                rr = rtmp[:, 2, :]
                cp(u, posi[:], [posi], [rtmp])
                ts(ang, u, invf, None, ALU.mult, None, [rtmp, cf], [rtmp])
                ts(u, ang, 1.0 / (2 * math.pi), 12582912.0, ALU.mult, ALU.add, [rtmp], [rtmp])
                ts(u, u, -12582912.0, None, ALU.add, None, [rtmp], [rtmp])
                C1 = 6.28125
                C2 = float(np.float32(2 * math.pi - C1))
                C3 = float(2 * math.pi - C1 - C2)
                stt(rr, u, -C1, ang, ALU.mult, ALU.add, [rtmp], [rtmp])
                stt(rr, u, -C2, rr, ALU.mult, ALU.add, [rtmp], [rtmp])
                stt(rr, u, -C3, rr, ALU.mult, ALU.add, [rtmp], [rtmp])
                act(sinT[:], rr, AF.Sin, [rtmp], [sinT])
                stt(u, rr, -1.0, rr, ALU.mult, ALU.max, [rtmp], [rtmp])
                act(cosT[:], u, AF.Sin, [rtmp, epsc], [cosT], scale=-1.0, bias=epsc[:, 1:2])
                norm_transpose(0)
                if first_tile:
                    dump("hT", hT[:, 0, :], [hT])
                    dump("cosT", cosT[:], [cosT])
                    dump("sinT", sinT[:], [sinT])

                for nm, dstb in (("q", qT), ("k", KTs), ("qi", qiT)):
                    panel = stream.next("%d_%s" % (l, nm))
                    pv = wview(panel, nm)
                    pss = [nps() for _ in range(4)]
                    fm_chunk(panel, pv, 0, pss[0])
                    for c in range(4):
                        if c + 1 < 4:
                            fm_chunk(panel, pv, c + 1, pss[c + 1])
                        if nm == "q":
                            rope_evac(pss[c], None, [dstb], split=qT[:, c, :, :])
                        else:
                            rope_evac(pss[c], dstb[:, c, :], [dstb])
                panel = stream.next("%d_kiwi" % l)
                pv = wview(panel, "kiwi")
                ps = nps()
                for half in range(2):
                    for kc in range(8):
                        mm(ps[half * 64:(half + 1) * 64, 0:T], pv[:, kc, 0:64], hT[:, kc, :], kc == 0, kc == 7,
                           [panel, hT], [ps])
                rope_evac(ps, kiT[:, t0:t0 + T], [kiT])
                for s in range(NSUB):
                    ps = nps()
                    for kc in range(8):
                        mm(ps[:, 0:8], hT[:, kc, s * 128:(s + 1) * 128], pv[:, kc, 64:72], kc == 0, kc == 7,
                           [panel, hT], [ps])
                    cp(wis[:, s, :], ps[:, 0:8], [ps], [wis])
                dma(KTc[l][:, :, t0:t0 + T].rearrange("c p w -> p c w"), KTs[:], [KTs], [KVT[l][t0 // KG]], kvsem, eng="gpsimd")
                if first_tile:
                    dump("qT", qT[:, 0, 0, :], [qT])
                    dump("kiT", kiT[:, 0:T], [kiT])
                    dump("wis", wis[:, 0, :], [wis])
                panel = stream.next("%d_xr" % l)
                pv = wview(panel, "xr")
                for c in range(4):
                    ps = nps()
                    fm_chunk(panel, pv, c, ps)
                    act(xrbuf[:, c, 3:3 + T], ps[:, 0:T], AF.Copy, [ps], [xrbuf])
                for nm, dstb in (("gr", grT), ("zu", zuT)):
                    panel = stream.next("%d_%s" % (l, nm))
                    pv = wview(panel, nm)
                    for c in range(4):
                        ps = nps()
                        fm_chunk(panel, pv, c, ps)
                        act(dstb[:, c, :], ps[:, 0:T], AF.Gelu_apprx_tanh, [ps], [dstb])
                panel = stream.next("%d_v" % l)
                pv = wview(panel, "v")
                for s in range(NSUB):
                    ps = nps()
                    for kc in range(8):
                        mm(ps[:, 0:512], hT[:, kc, s * 128:(s + 1) * 128], pv[:, kc, :], kc == 0, kc == 7, [panel, hT], [ps])
                    dstv = Vs[:, s, :].rearrange("p (h f) -> p h f", h=8)[:, :, 0:64]
                    act(dstv, ps[:, 0:512].rearrange("p (h f) -> p h f", h=8), AF.Copy, [ps], [Vs])
                dma(Vc[l][t0:t0 + T, :].rearrange("(s p) f -> p s f", p=128), Vs[:], [Vs], [KVT[l][t0 // KG]], kvsem, eng="gpsimd")
                panel = stream.next("%d_zv" % l)
                pv = wview(panel, "zv")
                for s in range(NSUB):
                    ps = nps()
                    for kc in range(8):
                        mm(ps[:, 0:512], hT[:, kc, s * 128:(s + 1) * 128], pv[:, kc, :], kc == 0, kc == 7, [panel, hT], [ps])
                    act(gz[:], ps[:, 0:512], AF.Gelu_apprx_tanh, [ps], [gz])
                    ss = sm()
                    act(tmpf[:, 0:512], gz[:], AF.Square, [gz, small], [tmpf, small], accum_out=ss)
                    rs = rstd_from_ss(ss, 512)
                    stt(vn[:, s, :], gz[:], rs, gbv[:], ALU.mult, ALU.mult, [gz, small, gbv], [vn])

                mpi = [0]

                def mps():
                    mpi[0] += 1
                    return PS[5 + mpi[0] % 3]

                def gen_mixC():
                    for s in range(NSUB):
                        for cpair in range(4):
                            ps = mps()
                            for gg in range(2):
                                g = cpair * 2 + gg
                                mm(ps[gg * 64:(gg + 1) * 64, 0:128], vn[:, s, g * 64:(g + 1) * 64], wspT[:, g, :], True, False,
                                   [vn, wspT], [ps])
                            mm(ps[:, 0:128], esel[:, cpair, :], bsph[:, 0, :], False, False, [cb, bsph], [ps])
                            mm(ps[:, 0:128], esel[:, cpair, :], bsph[:, 1, :], False, True, [cb, bsph], [ps])
                            tt(ycT[:, cpair, s * 128:(s + 1) * 128], ps[:, 0:128], zuT[:, cpair, s * 128:(s + 1) * 128], ALU.mult,
                               [ps, zuT], [ycT])
                            yield

                def gen_mixB():
                    for c in range(4):
                        xc = lt[:, 0, :]
                        ts(xc, xrbuf[:, c, 0:T], cols[:, 16 + c:17 + c], cols[:, 32 + c:33 + c], ALU.mult, ALU.add, [xrbuf, cols], [lt])
                        for jj in range(1, 4):
                            stt(xc, xrbuf[:, c, jj:jj + T], cols[:, 16 + jj * 4 + c:17 + jj * 4 + c], xc, ALU.mult, ALU.add,
                                [xrbuf, cols, lt], [lt])
                        yield
                        cp(xrbuf[:, c, 0:3], xrbuf[:, c, T:T + 3], [xrbuf], [xrbuf])
                        act(xcb[:], xc, AF.Copy, [lt], [xcb])
                        ps = mps()
                        mm(ps[:, 0:T], wbd[:, c, :], xcb[:], True, True, [wbd, xcb], [ps])
                        mm(ps[:, T:2 * T], wbd[:, 4 + c, :], xcb[:], True, True, [wbd, xcb], [ps])
                        yield
                        rg = lt[:, 1, :]
                        ig = lt[:, 2, :]
                        av = lt[:, 3, :]
                        sq = lt[:, 4, :]
                        act(rg, ps[:, 0:T], AF.Sigmoid, [ps, cols], [lt], bias=cols[:, 36 + c:37 + c])
                        act(ig, ps[:, T:2 * T], AF.Sigmoid, [ps, cols], [lt], bias=cols[:, 40 + c:41 + c])
                        yield
                        act(av, rg, AF.Exp, [lt, c8], [lt], scale=c8[:, c:c + 1])
                        act(sq, rg, AF.Exp, [lt, c8], [lt], scale=c8[:, 4 + c:5 + c])
                        act(sq, sq, AF.Sqrt, [lt, epsc], [lt], scale=-1.0, bias=epsc[:, 2:3])
                        yield
                        tt(ig, ig, xc, ALU.mult, [lt], [lt])
                        tt(ig, ig, sq, ALU.mult, [lt], [lt])
                        hh = lt[:, 1, :]
                        scan(hh, av, ig, hst[:, c:c + 1], [lt, hst], [lt])
                        yield
                        cp(hst[:, c:c + 1], hh[:, T - 1:T], [lt], [hst])
                        tt(ybT[:, c, :], hh, grT[:, c, :], ALU.mult, [lt, grT], [ybT])
                        yield

                grps = kv_groups(j)
                nkeys = (j + 1) * T
                TRh = [Tile("relu%d" % h) for h in range(8)]

                SC = [(score_ap, TS_), (sc1[:, 0:L], sc1.T)]

                def gen_I(s):
                    N = t0 + 128 * (s + 1)
                    score_s, TSs = SC[s]
                    for h in range(8):
                        merge_tile(TRh[h], TR_, as_write=True)
                        act(diag[:, h, :], identf, AF.Copy, [cf, wis], [diag], scale=wis[:, s, h:h + 1])
                    yield
                    ngrp = (N + KG - 1) // KG
                    for kg in range(ngrp):
                        wd = min(KG, N - kg * KG)
                        for h in range(8):
                            ps = PS[h % 4]
                            r0 = (h % 2) * 64
                            mm(ps[:, 0:wd], qiT[r0:r0 + 64, h // 2, s * 128:(s + 1) * 128],
                               kiT[r0:r0 + 64, kg * KG:kg * KG + wd], True, True, [qiT, kiT], [ps])
                            if h % 2 == 0:
                                act(Rrelu[:, h, 0:wd], ps[:, 0:wd], AF.Relu, [ps], [TRh[h]])
                            else:
                                ts(Rrelu[:, h, 0:wd], ps[:, 0:wd], 0.0, None, ALU.max, None, [ps], [TRh[h]])
                            if h % 2 == 1:
                                yield
                        for h in range(8):
                            mm(PS[4][:, 0:wd], diag[:, h, :], Rrelu[:, h, 0:wd], h == 0, h == 7, [diag, TRh[h]], [PS[4]])
                        if kg == ngrp - 1:
                            if wd > 128:
                                cp(score_s[:, kg * KG:kg * KG + wd - 128], PS[4][:, 0:wd - 128], [PS[4]], [TSs])
                            tt(score_s[:, N - 128:N], PS[4][:, wd - 128:wd], negmask, ALU.add, [PS[4], cf], [TSs])
                            tt(tmpf[:, 0:128], PS[4][:, wd - 128:wd], posfill, ALU.add, [PS[4], cf], [tmpf])
                        else:
                            cp(score_s[:, kg * KG:kg * KG + wd], PS[4][:, 0:wd], [PS[4]], [TSs])
                        yield
                    for h in range(8):
                        merge_tile(TR_, TRh[h])

                def gen_B(s):
                    N = t0 + 128 * (s + 1)
                    score_s, TSs = SC[s]
                    hi0 = bis[:, 0:1]
                    lo = bis[:, 1:2]
                    w0 = bis[:, 2:3]
                    reduce(hi0, score_s[:, 0:N], ALU.max, [TSs, bis], [bis])
                    reduce(lo, tmpf[:, 0:128], ALU.min, [tmpf, bis], [bis])
                    if N > 128:
                        m1 = bis[:, 3:4]
                        reduce(m1, score_s[:, 0:N - 128], ALU.min, [TSs, bis], [bis])
                        tt(lo, lo, m1, ALU.min, [bis], [bis])
                    tt(w0, hi0, lo, ALU.subtract, [bis], [bis])
                    yield
                    mk = masks[s]
                    if N > TOPK:
                        Nd = max(128, (int(N * 0.42) // 128) * 128)
                        Na = N - Nd
                        if s == 0:
                            junkA, TJ = masks[1], masks[1].T
                        else:
                            junkA, TJ = big[:, 4096:6144].bitcast(BF16), TR_
                        for it in range(NBIS):
                            k4 = it % 4
                            mid = bis2[:, k4:k4 + 1]
                            cnt = bis2[:, 4 + k4:5 + k4]
                            vv = bis2[:, 8 + k4:9 + k4]
                            sA = bis2[:, 12 + k4:13 + k4]
                            stt(mid, w0, 0.5 ** (it + 1), lo, ALU.mult, ALU.add, [bis], [Tmid])
                            act(junkA[:, 0:Na], score_s[:, Nd:N], AF.Sign, [TSs, Tmid], [TJ, Tsa], scale=-1.0, bias=mid,
                                accum_out=sA)
                            count_ge(mk[:, 0:Nd], score_s[:, 0:Nd], mid, cnt, [TSs, Tmid], [mk, Tcnt])
                            stt(vv, cnt, 2.0, sA, ALU.mult, ALU.subtract, [Tcnt, Tsa], [Tcnt])
                            ge = smalli[:, k4:k4 + 1]
                            ts(ge, vv, float(2 * TOPK - Na), None, ALU.is_ge, None, [Tcnt], [smalli])
                            cpred(lo, ge, mid, [Tmid, smalli], [bis])
                            yield
                    ts(mk[:, 0:N], score_s[:, 0:N], lo, None, ALU.is_ge, None, [TSs, bis], [mk])
                    if N < nkeys:
                        memset(mk[:, N:nkeys], 0.0, [mk], eng="gpsimd")
                    yield

                def gen_G(items):
                    for ni, n, do_gate, do_branch in items:
                        if do_gate:
                            for hf in range(2):
                                gpan = stream.next("%d_gate%d_%d" % (l, n, hf))
                                pv = wview(gpan, "gate0_0")
                                for c in range(4):
                                    ps = nps()
                                    fm_chunk(gpan, pv, c, ps)
                                    act(gsig[:, hf * 4 + c, :], ps[:, 0:T], AF.Sigmoid, [ps], [gsig])
                                    yield
                        if not do_branch:
                            continue
                        ysrc = {2: ycT, 1: ybT, 0: yaT}[n]
                        for hf in range(2):
                            bpan = stream.next("%d_br%d_%d" % (l, n, hf))
                            pv = wview(bpan, "br0_0")
                            for c in range(4):
                                dchunk = hf * 4 + c
                                ps = nps()
                                for kc in range(4):
                                    mm(ps[:, 0:T], pv[:, kc, c * 128:(c + 1) * 128], ysrc[:, kc, :], kc == 0, kc == 3,
                                       [bpan, ysrc], [ps])
                                if ni == 0:
                                    tt(merged[:, dchunk, :], ps[:, 0:T], gsig[:, dchunk, :], ALU.mult, [ps, gsig], [TS_])
                                else:
                                    tt(gt[:], ps[:, 0:T], gsig[:, dchunk, :], ALU.mult, [ps, gsig], [gt])
                                    dsto = mergedT if ni == 2 else merged
                                    tt(dsto[:, dchunk, :], merged[:, dchunk, :], gt[:], ALU.add, [TS_, gt], [TS_], eng="gpsimd")
                                yield

                def chain(*gens):
                    for g_ in gens:
                        yield from g_

                def run(*gens):
                    live = list(gens)
                    while live:
                        for g_ in list(live):
                            try:
                                next(g_)
                            except StopIteration:
                                live.remove(g_)

                def run_w(ga, gb_, kb):
                    la = lb = True
                    while la or lb:
                        if la:
                            try:
                                next(ga)
                            except StopIteration:
                                la = False
                        for _ in range(kb):
                            if lb:
                                try:
                                    next(gb_)
                                except StopIteration:
                                    lb = False

                run(gen_mixB(), gen_mixC(), gen_I(0))
                if first_tile:
                    dump("ycT", ycT[:, 0, :], [ycT])
                    dump("ybT", ybT[:, 0, :], [ybT])
                    dump("score", score_ap[:, 0:128], [TS_])
                N1 = t0 + 256
                lenI = 1 + ((N1 + KG - 1) // KG) * 5
                lenB = 2 + (NBIS if (t0 + 128) > TOPK else 0)
                run_w(gen_B(0), gen_I(1), max(1, -(-lenI // lenB)))
                lenB1 = 2 + (NBIS if (t0 + 256) > TOPK else 0)
                run_w(gen_B(1), gen_G([(0, 2, True, True), (1, 1, True, True), (2, 0, True, False)]), max(1, -(-40 // lenB1)))

                first = True
                ucnt = [0]
                for gi, (g, wd) in enumerate(grps):
                    panel = stream.next("%d_kv%d_%d" % (l, j, g))
                    nb = wd // 128
                    KTv = panel[:, 0:4 * wd].rearrange("p (c w) -> p c w", c=4)
                    Vv = panel[:, 2048:2048 + nb * 520].rearrange("p (b f) -> p b f", b=nb)
                    psb = PS[6][:].bitcast(BF16)
                    for s in range(NSUB):
                        for b in range(nb):
                            tr(psb[:, (s * 4 + b) * 128:(s * 4 + b + 1) * 128],
                               masks[s][:, g * KG + b * 128:g * KG + (b + 1) * 128], identb, [masks[s], cb], [PS[6]])
                    for s in range(NSUB):
                        act(maskTk[:, 0:nb, s * 128:(s + 1) * 128],
                            psb[:, s * 512:s * 512 + nb * 128].rearrange("p (b t) -> p b t", b=nb),
                            AF.Copy, [PS[6]], [maskTk])
                    units = [(hp, b) for hp in range(4) for b in range(nb)]

                    def stageA(i):
                        hp, b = units[i]
                        ps = PS[(ucnt[0] + i) % 4]
                        mm(ps[:, 0:2 * T], KTv[:, hp, b * 128:(b + 1) * 128], qT[:, hp, :, :].rearrange("p a t -> p (a t)"),
                           True, True, [panel, qT], [ps])

                    def stageB(i):
                        hp, b = units[i]
                        ps = PS[(ucnt[0] + i) % 4]
                        ptile = PT[(ucnt[0] + i) % len(PT)]
                        act(ptile[:, :, :], ps[:, 0:2 * T].rearrange("p (a t) -> p a t", a=2), AF.Exp,
                            [ps], [ptile], scale=HD ** -0.5)
                        tt(ptile[:, :, :], ptile[:, :, :], maskTk[:, b:b + 1, :].to_broadcast([128, 2, T]), ALU.mult,
                           [ptile, maskTk], [ptile], eng=("gpsimd" if (ucnt[0] + i) % 4 == 3 else "vector"))

                    def stageD(i):
                        hp, b = units[i]
                        ptile = PT[(ucnt[0] + i) % len(PT)]
                        for hh in range(2):
                            h = 2 * hp + hh
                            pacc = PS[4 + hh]
                            mm(pacc[0:65, 0:T], Vv[:, b, h * 65:(h + 1) * 65], ptile[:, hh, :], b == 0, b == nb - 1,
                               [panel, ptile], [pacc])
                            if b == nb - 1:
                                if first:
                                    cp(acc[:, h, :], pacc[0:65, 0:T], [pacc], [acc])
                                else:
                                    tt(acc[:, h, :], acc[:, h, :], pacc[0:65, 0:T], ALU.add, [pacc, acc], [acc])

                    for i in range(min(3, len(units))):
                        stageA(i)
                    for i in range(len(units)):
                        if i + 3 < len(units):
                            stageA(i + 3)
                        stageB(i)
                        stageD(i)
                    ucnt[0] += len(units)
                    first = False
                accf = accm[0:65, :]
                act(accf[64:65, :], accf[64:65, :], AF.Ln, [acc], [acc])
                act(accf[64:65, :], accf[64:65, :], AF.Exp, [acc], [acc], scale=-1.0)
                for h in range(8):
                    rhl = rhl2[h % 2]
                    cp(rhl[64:65, 0, :], acc[64:65, h, :], [acc], [rhl])
                    tt(rhl[64:65, 1, :], acc[64:65, h, :], rhl[64:65, 0, :], ALU.subtract, [acc, rhl], [rhl])
                    ps = nps()
                    mm(ps[0:64, 0:T], onesb[64:65, 0:64], rhl[64:65, 0, :], True, False, [onesb, rhl], [ps])
                    mm(ps[0:64, 0:T], onesb[64:65, 0:64], rhl[64:65, 1, :], False, True, [onesb, rhl], [ps])
                    if h % 2 == 0:
                        tt(yaT[0:64, h // 2, :], acc[0:64, h, :], ps[0:64, 0:T], ALU.mult, [acc, ps], [yaT])
                    else:
                        tt(yaTt[:, :], acc[0:64, h, :], ps[0:64, 0:T], ALU.mult, [acc, ps], [yaTt])
                        ps2 = nps()
                        mm(ps2[64:128, 0:T], identb[0:64, 0:64], yaTt[:, :], True, True, [cb, yaTt], [ps2])
                        cp(yaT[64:128, h // 2, :], ps2[64:128, 0:T], [ps2], [yaT])
                if first_tile:
                    dump("yaT", yaT[:, 0, :], [yaT])
                if l == 0 and j == 1:
                    dump("yaT1", yaT[:, 0, :], [yaT])
                    dump("acc1", acc[:, 0, :], [acc])

                run(gen_G([(2, 0, False, True)]))
                op_ = [stream.next("%d_out_%d" % (l, hf), look=NSLOT - 1 - hf) for hf in range(2)]
                for s in range(NSUB):
                    banks = [PS[4 + 2 * (s % 2)], PS[5 + 2 * (s % 2)]]
                    for hf in range(2):
                        pv = wview(op_[hf], "out_0")
                        for kc in range(8):
                            mm(banks[hf][:, 0:512], mergedT[:, kc, s * 128:(s + 1) * 128], pv[:, kc, :], kc == 0, kc == 7,
                               [op_[hf], TS_], [banks[hf]])
                    post_norm_residual(banks, 0, s)
                dma(gcur[:], gb_in[l, :, D:2 * D], [], [gcur], gsem, eng="gpsimd")
                if first_tile:
                    dump("x1", xt[:, 0, :], [xt])
                norm_transpose(8)
                for s in range(NSUB):
                    cp(ptb[:, s, :], pt[:, s, :], [pt], [ptb])
                    psb = PS[3][:].bitcast(BF16)
                    for c in range(2):
                        tr(psb[:, c * 128:(c + 1) * 128], ptb[:, s, c * 128:(c + 1) * 128], identb, [ptb, cb], [PS[3]])
                    cp(pT[:, :, s * 128:(s + 1) * 128], psb[:, 0:256].rearrange("p (c t) -> p c t", c=2), [PS[3]], [pT])
                dbanks = [[PS[4], PS[5]], [PS[6], PS[7]]]
                TF = [Tile("fT0"), Tile("fT1")]
                Trl = [Tile("rl0"), Tile("rl1")]

                for i_ in range(2):
                    merge_tile(TF[i_], TS_, as_write=True)
                    merge_tile(Trl[i_], TR_, as_write=True)

                def ffn_up(g):
                    up = stream.next("%d_up_%d" % (l, g))
                    pv = wview(up, "up_0")
                    fTg = fT[g % 2]
                    for c in range(4):
                        ps = nps()
                        fm_chunk(up, pv, c, ps)
                        rlb = rl[c % 2]
                        act(rlb, ps[:, 0:T], AF.Relu, [ps], [Trl[c % 2]])
                        tt(fTg[:, c, :], rlb, rlb, ALU.mult, [Trl[c % 2]], [TF[g % 2]], eng="gpsimd")

                def ffn_dn(g):
                    dn = stream.next("%d_dn_%d" % (l, g))
                    dv = wview(dn, "dn_0")
                    fTg = fT[g % 2]
                    for s in range(NSUB):
                        for hf in range(2):
                            for c in range(4):
                                mm(dbanks[s][hf][:, 0:512], fTg[:, c, s * 128:(s + 1) * 128], dv[:, c, hf * 512:(hf + 1) * 512],
                                   g == 0 and c == 0, g == 7 and c == 3, [dn, TF[g % 2]], [dbanks[s][hf]])

                ffn_up(0)
                for g in range(8):
                    if g + 1 < 8:
                        ffn_up(g + 1)
                    ffn_dn(g)
                for i_ in range(2):
                    merge_tile(TS_, TF[i_])
                    merge_tile(TR_, Trl[i_])
                for s in range(NSUB):
                    post_norm_residual(dbanks[s], D, s)
                dma(gcur[:], gb_in[l, :, 2 * D:3 * D], [], [gcur], gsem, eng="gpsimd")
                if first_tile:
                    dump("x2", xt[:, 0, :], [xt])
                norm_transpose(None)
                plp = stream.next("%d_ple" % l)
                plv = wview(plp, "ple")
                pg = [stream.next("%d_pg_%d" % (l, hf), look=NSLOT - 2 - hf) for hf in range(2)]
                ple2 = big[:, 0:2 * D].rearrange("p (s d) -> p s d", s=2)
                sspa = []
                for s in range(NSUB):
                    ssp = []
                    for hf in range(2):
                        pa = PS[4 * (s % 2) + hf]
                        pgb = PS[4 * (s % 2) + 2 + hf]
                        for kc in range(2):
                            mm(pa[:, 0:512], pT[:, kc, s * 128:(s + 1) * 128], plv[:, kc, hf * 512:(hf + 1) * 512], kc == 0, kc == 1,
                               [plp, pT], [pa])
                        gv = wview(pg[hf], "pg_0")
                        for kc in range(8):
                            mm(pgb[:, 0:512], hT[:, kc, s * 128:(s + 1) * 128], gv[:, kc, :], kc == 0, kc == 7, [pg[hf], hT], [pgb])
                        act(sg, pgb[:, 0:512], AF.Sigmoid, [pgb], [TR_])
                        tt(ple2[:, s, hf * 512:(hf + 1) * 512], pa[:, 0:512], sg, ALU.mult, [pa, TR_], [TS_])
                        a = sm()
                        act(tmpf[:, 0:512], ple2[:, s, hf * 512:(hf + 1) * 512], AF.Square, [TS_, small], [tmpf, small], accum_out=a)
                        ssp.append(a)
                    sspa.append(ssp)
                for s in range(NSUB):
                    s3 = sm()
                    tt(s3, sspa[s][0], sspa[s][1], ALU.add, [small], [small])
                    rs = rstd_from_ss(s3, D)
                    for hf in range(2):
                        stt(tmpf[:, 0:512], ple2[:, s, hf * 512:(hf + 1) * 512], rs, gcur[:, hf * 512:(hf + 1) * 512],
                            ALU.mult, ALU.mult, [TS_, small, gcur], [tmpf])
                        tt(xt[:, s, hf * 512:(hf + 1) * 512], xt[:, s, hf * 512:(hf + 1) * 512], tmpf[:, 0:512], ALU.add,
                           [xt, tmpf], [xt])
                if j + 1 < NT:
                    dma(gcur[:], gb_in[l, :, 0:D], [], [gcur], gsem, eng="gpsimd")
                wt = [XT[j]] if XT is not None else [out_tile]
                dma(dst_x[t0:t0 + T, :].rearrange("(s p) d -> p s d", p=128), xt[:], [xt], wt, ssem, eng="gpsimd")
            XTprev = XT

        fw.finish("sync", [out_tile] + dbg_tiles)
        fw.emit()
    return nc, fw


def _consts():
    bf = ml_dtypes.bfloat16
    ident = np.eye(128, dtype=np.float32)
    Rm = np.zeros((128, 128), np.float32)
    for base in (0, 64):
        for d in range(8):
            Rm[base + d + 8, base + d] = -1.0
            Rm[base + d, base + d + 8] = 1.0
    esel = np.zeros((128, 4, 128), np.float32)
    for g in range(8):
        esel[g, g // 2, (g % 2) * 64:(g % 2 + 1) * 64] = 1.0
    cb = np.concatenate([ident, Rm, esel.reshape(128, 512)], axis=1).astype(bf)
    tt_, ss_ = np.meshgrid(np.arange(128), np.arange(128), indexing="ij")
    negmask = np.where(ss_ > tt_, np.float32(NEG), np.float32(0.0))
    posfill = np.where(ss_ > tt_, np.float32(-2 * NEG), np.float32(0.0))
    tril01 = (tt_ <= ss_).astype(np.float32)
    half = 8
    inv_freq = (np.float32(500000.0) ** (-np.arange(half, dtype=np.float32) * np.float32(2.0) / np.float32(16))).astype(np.float32)
    invf = np.zeros((128, 1), np.float32)
    for f in range(128):
        d = f % 64
        if d < 16:
            invf[f, 0] = inv_freq[d % 8]
    cf = np.concatenate([ident, negmask, posfill, tril01, invf], axis=1).astype(np.float32)
    return cb, cf


def _layout_params(inp, depth):
    f = np.float32
    cols = np.zeros((depth, 128, 48), f)
    gb = np.zeros((depth, 128, 3 * D + 512), f)
    wbd = np.zeros((depth, 128, 8, 128), f)
    wsp = np.zeros((depth, 128, 8, 128), f)
    for l in range(depth):
        cols[l, :, 0:8] = np.asarray(inp["g_pre_mix"][l], f).reshape(8, 128).T
        cols[l, :, 8:16] = np.asarray(inp["g_pre_ffn"][l], f).reshape(8, 128).T
        cw = np.asarray(inp["conv_w"][l], f)
        for jj in range(4):
            cols[l, :, 16 + jj * 4:20 + jj * 4] = cw[jj].reshape(4, 128).T
        cols[l, :, 32:36] = np.asarray(inp["conv_b"][l], f).reshape(4, 128).T
        cols[l, :, 36:40] = np.asarray(inp["b_rg_a"][l], f).reshape(4, 128).T
        cols[l, :, 40:44] = np.asarray(inp["b_rg_x"][l], f).reshape(4, 128).T
        cols[l, :, 44:48] = np.asarray(inp["lru_lambda"][l], f).reshape(4, 128).T
        row = np.concatenate([np.asarray(inp["g_post_mix"][l], f), np.asarray(inp["g_post_ffn"][l], f),
                              np.asarray(inp["g_post_ple"][l], f), np.asarray(inp["g_gmlp_v"][l], f)])
        gb[l] = np.broadcast_to(row[None, :], (128, row.size))
        for gi, key in enumerate(("w_rg_a", "w_rg_x")):
            w = np.asarray(inp[key][l], f)
            for c in range(4):
                for hh in range(2):
                    wbd[l, hh * 64:(hh + 1) * 64, gi * 4 + c, hh * 64:(hh + 1) * 64] = w[c * 2 + hh]
        ws = np.asarray(inp["w_spatial"][l], f)
        wsp[l] = np.transpose(ws, (2, 0, 1))
    bsp = np.ascontiguousarray(np.asarray(inp["b_spatial"], f))
    return cols, gb, wbd, wsp, bsp


_CACHE = {}


def kernel(**inputs):
    depth = DEPTH
    x = np.asarray(inputs["x"], np.float32)
    B, L, _ = x.shape
    p = np.asarray(inputs["p"], np.float32)
    pos = np.asarray(inputs["positions"], np.int32)
    cb, cf = _consts()
    cols, gb, wbd, wsp, bsp = _layout_params(inputs, depth)
    shared = {
        "w_in": np.ascontiguousarray(np.asarray(inputs["w_in"], np.float32)),
        "w_branch": np.ascontiguousarray(np.asarray(inputs["w_branch"], np.float32)),
        "w_out": np.ascontiguousarray(np.asarray(inputs["w_out"], np.float32)),
        "w_ffn_up": np.ascontiguousarray(np.asarray(inputs["w_ffn_up"], np.float32)),
        "w_ffn_down": np.ascontiguousarray(np.asarray(inputs["w_ffn_down"], np.float32)),
        "w_ple": np.ascontiguousarray(np.asarray(inputs["w_ple"], np.float32)),
        "w_ple_gate": np.ascontiguousarray(np.asarray(inputs["w_ple_gate"], np.float32)),
        "cols": cols, "gb": gb, "wbd": wbd, "wsp": wsp, "bsp": bsp, "cb": cb, "cf": cf,
    }
    if L not in _CACHE:
        _CACHE[L] = build_program(L, depth)[0]
    nc = _CACHE[L]
    in_maps = []
    for b in range(B):
        m = dict(shared)
        m["x"] = np.ascontiguousarray(x[b])
        m["p"] = np.ascontiguousarray(p[:, b])
        m["pos"] = np.ascontiguousarray(np.broadcast_to(pos[b][None, :], (128, L)))
        in_maps.append(m)
    res = run_bass_kernel_spmd(nc, in_maps, core_ids=list(range(B)))
    return np.stack([np.asarray(r["y"], np.float32) for r in res.results], axis=0)
```

```python
import math
from contextlib import ExitStack
import numpy as np
import ml_dtypes
import concourse.bass as bass
import concourse.mybir as mybir
from concourse.bass_utils import run_bass_kernel_spmd

F32 = mybir.dt.float32
BF16 = mybir.dt.bfloat16
I32 = mybir.dt.int32
AF = mybir.ActivationFunctionType
ALU = mybir.AluOpType
AX = mybir.AxisListType

D = 1024
NH = 8
HD = 64
TOPK = 256
FFN = 4096
PLE = 256
EPS = 1e-6
DEPTH = 2
T = 256
NSUB = T // 128
KG = 512
NSLOT = 3
SLOTB = 4224
NBIS = 12
BIGM = 30000.0
NEG = -1.0e30
IN_OFF = dict(q=0, k=512, v=1024, qi=1536, kiwi=2048, xr=2120, gr=2632, zu=3144, zv=3656, gate=4168)


class Sem:
    def __init__(self, h, name):
        self.h = h
        self.n = 0
        self.name = name


class Tile:
    __slots__ = ("w", "r", "name")

    def __init__(self, name=""):
        self.w = {}
        self.r = {}
        self.name = name


class Buf:
    def __init__(self, t, name=""):
        self.t = t
        self.T = Tile(name)

    def __getitem__(self, k):
        return self.t[k]


class Engine:
    def __init__(self, name, sem):
        self.name = name
        self.sem = sem
        self.ops = []
        self.seen = {}


class FW:
    def __init__(self, nc, stack):
        self.nc = nc
        self.stack = stack
        self.engs = {}
        for n in ("tensor", "vector", "scalar", "gpsimd", "sync"):
            s = Sem(stack.enter_context(nc.semaphore("sem_" + n)), n)
            self.engs[n] = Engine(n, s)
        self.nops = 0

    def dsem(self, name):
        return Sem(self.stack.enter_context(self.nc.semaphore("dsem_" + name)), name)

    def op(self, eng, fn, reads=(), writes=(), dsem=None):
        E = self.engs[eng]
        need = {}
        for b in reads:
            t = b.T if isinstance(b, Buf) else b
            for s, v in t.w.items():
                if need.get(s, 0) < v:
                    need[s] = v
        for b in writes:
            t = b.T if isinstance(b, Buf) else b
            for s, v in t.w.items():
                if need.get(s, 0) < v:
                    need[s] = v
            for s, v in t.r.items():
                if need.get(s, 0) < v:
                    need[s] = v
        raw_self = 0
        for b in reads:
            t = b.T if isinstance(b, Buf) else b
            raw_self = max(raw_self, t.w.get(E.sem, 0))
        waits = []
        for s, v in need.items():
            if s is E.sem:
                if eng != "tensor" and raw_self > E.seen.get(s, 0):
                    E.seen[s] = raw_self
                    waits.append((s, raw_self))
                continue
            if E.seen.get(s, 0) >= v:
                continue
            E.seen[s] = v
            waits.append((s, v))
        if dsem is not None:
            dsem.n += 16
            sig = (dsem, dsem.n, 16)
        else:
            E.sem.n += 1
            sig = (E.sem, E.sem.n, 1)
        E.ops.append((waits, fn, sig))
        self.nops += 1
        s, v = sig[0], sig[1]
        for b in reads:
            t = b.T if isinstance(b, Buf) else b
            if t.r.get(s, 0) < v:
                t.r[s] = v
        for b in writes:
            t = b.T if isinstance(b, Buf) else b
            if t.w.get(s, 0) < v:
                t.w[s] = v

    def finish(self, eng, tiles):
        E = self.engs[eng]
        need = {}
        for b in tiles:
            t = b.T if isinstance(b, Buf) else b
            for d in (t.w, t.r):
                for s, v in d.items():
                    if need.get(s, 0) < v:
                        need[s] = v
        E.ops.append(([(s, v) for s, v in need.items() if s is not E.sem], None, None))

    def emit(self):
        with self.nc.Block() as block:
            for n, E in self.engs.items():
                def body(e, E=E):
                    for waits, fn, sig in E.ops:
                        for s, v in waits:
                            e.wait_ge(s.h, v)
                        if fn is None:
                            continue
                        fn(e).then_inc(sig[0].h, sig[2])
                getattr(block, n)(body)


def panel_defs():
    P = []
    for nm in ("q", "k", "qi"):
        P.append((nm, "w_in", 0, 1024, IN_OFF[nm], 512))
    P.append(("kiwi", "w_in", 0, 1024, IN_OFF["kiwi"], 72))
    for nm in ("xr", "gr", "zu", "v", "zv"):
        P.append((nm, "w_in", 0, 1024, IN_OFF[nm], 512))
    for n in range(3):
        for hf in range(2):
            P.append(("gate%d_%d" % (n, hf), "w_in", 0, 1024, IN_OFF["gate"] + n * 1024 + hf * 512, 512))
    for n in range(3):
        for hf in range(2):
            P.append(("br%d_%d" % (n, hf), "w_branch%d" % n, 0, 512, hf * 512, 512))
    for hf in range(2):
        P.append(("out_%d" % hf, "w_out", 0, 1024, hf * 512, 512))
    for g in range(8):
        P.append(("up_%d" % g, "w_ffn_up", 0, 1024, g * 512, 512))
        P.append(("dn_%d" % g, "w_ffn_down", g * 512, 512, 0, 1024))
    P.append(("ple", "w_ple", 0, 256, 0, 1024))
    for hf in range(2):
        P.append(("pg_%d" % hf, "w_ple_gate", 0, 1024, hf * 512, 512))
    return P


def build_program(L, depth=DEPTH, dbg=None):
    NT = L // T
    nc = bass.Bass("TRN2", target_bir_lowering=False)
    dram = lambda name, shape, dt, kind="ExternalInput": nc.dram_tensor(name, shape, dt, kind=kind).ap()
    x_in = dram("x", [L, D], F32)
    p_in = dram("p", [depth, L, PLE], F32)
    pos_in = dram("pos", [128, L], I32)
    wsrc = {
        "w_in": dram("w_in", [depth, D, 7240], F32),
        "w_branch": dram("w_branch", [depth, 3, 512, D], F32),
        "w_out": dram("w_out", [depth, D, D], F32),
        "w_ffn_up": dram("w_ffn_up", [depth, D, FFN], F32),
        "w_ffn_down": dram("w_ffn_down", [depth, FFN, D], F32),
        "w_ple": dram("w_ple", [depth, PLE, D], F32),
        "w_ple_gate": dram("w_ple_gate", [depth, D, D], F32),
    }
    cols_in = dram("cols", [depth, 128, 48], F32)
    gb_in = dram("gb", [depth, 128, 3 * D + 512], F32)
    wbd_in = dram("wbd", [depth, 128, 8, 128], F32)
    wsp_in = dram("wsp", [depth, 128, 8, 128], F32)
    bsp_in = dram("bsp", [depth, 8, 128], F32)
    cb_in = dram("cb", [128, 256 + 512], BF16)
    cf_in = dram("cf", [128, 4 * 128 + 1], F32)
    y_out = dram("y", [L, D], F32, kind="ExternalOutput")
    xbuf = dram("xbuf", [L, D], F32, kind="Internal")
    KTc = [dram("ktc%d" % l, [4, 128, L], BF16, kind="Internal") for l in range(depth)]
    Vc = [dram("vc%d" % l, [L, 520], BF16, kind="Internal") for l in range(depth)]
    pdefs = panel_defs()
    Wp = [{nm: dram("wp%d_%s" % (l, nm), [nr, ncol], BF16, kind="Internal") for (nm, _, _, nr, _, ncol) in pdefs}
          for l in range(depth)]
    dbg_out = {}
    if dbg:
        for k, shp in dbg.items():
            dbg_out[k] = dram("dbg_" + k, list(shp), F32, kind="ExternalOutput")

    with ExitStack() as st:
        fw = FW(nc, st)
        op = fw.op

        def sb(name, shape, dt):
            return Buf(st.enter_context(nc.sbuf_tensor("s_" + name, shape, dt)), name)

        PS = [Buf(st.enter_context(nc.psum_tensor("ps%d" % i, [128, 512], F32)), "ps%d" % i) for i in range(8)]

        def mm(out, lhsT, rhs, start, stop, R, W):
            op("tensor", lambda e: e.matmul(out, lhsT=lhsT, rhs=rhs, start=start, stop=stop), R, W)

        def tr(out, in_, ident, R, W):
            op("tensor", lambda e: e.transpose(out=out, in_=in_, identity=ident), R, W)

        def act(out, in_, func, R, W, **kw):
            op("scalar", lambda e: e.activation(out=out, in_=in_, func=func, **kw), R, W)

        def ts(out, in0, s1, s2, op0, op1, R, W, eng="vector"):
            if op1 is None:
                op(eng, lambda e: e.tensor_scalar(out=out, in0=in0, scalar1=s1, scalar2=None, op0=op0), R, W)
            else:
                op(eng, lambda e: e.tensor_scalar(out=out, in0=in0, scalar1=s1, scalar2=s2, op0=op0, op1=op1), R, W)

        def tt(out, in0, in1, o, R, W, eng="vector"):
            op(eng, lambda e: e.tensor_tensor(out=out, in0=in0, in1=in1, op=o), R, W)

        def stt(out, in0, s, in1, op0, op1, R, W):
            op("vector", lambda e: e.scalar_tensor_tensor(out=out, in0=in0, scalar=s, in1=in1, op0=op0, op1=op1), R, W)

        def cp(out, in_, R, W, eng="vector"):
            op(eng, lambda e: e.tensor_copy(out=out, in_=in_), R, W)

        def dma(out, in_, R, W, ds, eng="sync"):
            op(eng, lambda e: e.dma_start(out=out, in_=in_), R, W, dsem=ds)

        def memset(ap, val, W, eng="vector"):
            op(eng, lambda e: e.memset(ap, val), [], W)

        def reduce(out, in_, o, R, W):
            op("vector", lambda e: e.tensor_reduce(out=out, in_=in_, axis=AX.X, op=o), R, W)

        def recip(out, in_, R, W):
            op("vector", lambda e: e.reciprocal(out=out, in_=in_), R, W)

        def scan(out, d0, d1, init, R, W):
            op("vector", lambda e: e.tensor_tensor_scan(out=out, data0=d0, data1=d1, initial=init,
                                                        op0=ALU.mult, op1=ALU.add), R, W)

        def count_ge(out, in0, thr, cnt, R, W):
            op("vector", lambda e: e.tensor_scalar(out=out, in0=in0, scalar1=thr, scalar2=None, op0=ALU.is_ge,
                                                   op1=ALU.add, accum_out=cnt), R, W)

        def cpred(out, mask, data, R, W):
            op("vector", lambda e: e.copy_predicated(out=out, mask=mask, data=data), R, W)

        dbg_sem = fw.dsem("dbg")
        dbg_tiles = []

        def dump(name, ap, R):
            if name in dbg_out:
                t = Tile("dbg")
                dma(dbg_out[name], ap, R, [t], dbg_sem, eng="gpsimd")
                dbg_tiles.append(t)
                del dbg_out[name]

        cb = sb("cb", [128, 768], BF16)
        cf = sb("cf", [128, 513], F32)
        dma(cb[:], cb_in, [], [cb], fw.dsem("c0"))
        dma(cf[:], cf_in, [], [cf], fw.dsem("c1"))
        identb = cb[:, 0:128]
        Rm = cb[:, 128:256]
        esel = cb[0:8, 256:768].rearrange("p (c f) -> p c f", c=4)
        identf = cf[:, 0:128]
        negmask = cf[:, 128:256]
        posfill = cf[:, 256:384]
        tril01 = cf[:, 384:512]
        invf = cf[:, 512:513]

        WT = [dict() for _ in range(depth)]
        for l in range(depth):
            wcs = fw.dsem("wcast%d" % l)
            for (nm, src, r0, nr, c0, ncol) in pdefs:
                if src.startswith("w_branch"):
                    s_ap = wsrc["w_branch"][l, int(src[-1]), r0:r0 + nr, c0:c0 + ncol]
                else:
                    s_ap = wsrc[src][l, r0:r0 + nr, c0:c0 + ncol]
                t = Tile("wp")
                WT[l][nm] = t
                step = 512
                for rr in range(0, nr, step):
                    n2 = min(step, nr - rr)
                    dma(Wp[l][nm][rr:rr + n2, :], s_ap[rr:rr + n2, :], [], [t], wcs, eng="gpsimd")
            for t in WT[l].values():
                t.w = {wcs: wcs.n}

        ring = [sb("ring%d" % i, [128, SLOTB], BF16) for i in range(NSLOT)]
        ring_sem = [fw.dsem("ring%d" % i) for i in range(NSLOT)]
        kiT = sb("kiT", [128, L], BF16)
        xts = [sb("xt%d" % i, [128, NSUB, D], F32) for i in range(2)]
        xt = xts[0]
        pt = sb("pt", [128, NSUB, PLE], F32)
        ptb = sb("ptb", [128, NSUB, PLE], BF16)
        posi = sb("posi", [128, T], I32)
        hT = sb("hT", [128, 8, T], BF16)
        hTb = sb("hTb", [128, 8, T], BF16)
        pT = sb("pT", [128, 2, T], BF16)
        qT = sb("qT", [128, 4, 2, T], BF16)
        qiT = sb("qiT", [128, 4, T], BF16)
        KTs = sb("KTs", [128, 4, T], BF16)
        Vs = sb("Vs", [128, NSUB, 520], BF16)
        cosT = sb("cosT", [128, T], F32)
        sinT = sb("sinT", [128, T], F32)
        rtmp = sb("rtmp", [128, 3, T], F32)
        xb16 = sb("xb16", [128, T], BF16)
        xrbuf = sb("xrbuf", [128, 4, 3 + T], F32)
        grT = sb("grT", [128, 4, T], BF16)
        zuT = sb("zuT", [128, 4, T], BF16)
        vn = sb("vn", [128, NSUB, 512], BF16)
        hst = sb("hst", [128, 4], F32)
        ybT = sb("ybT", [128, 4, T], BF16)
        ycT = sb("ycT", [128, 4, T], BF16)
        yaT = sb("yaT", [128, 4, T], BF16)
        yaTt = sb("yaTt", [64, T], BF16)
        wis = sb("wis", [128, NSUB, 8], F32)
        diag = sb("diag", [128, 8, 128], BF16)
        small = sb("small", [128, 32], F32)
        smalli = sb("smalli", [128, 4], I32)
        bis = sb("bis", [128, 16], F32)
        bis2 = sb("bis2", [128, 16], F32)
        Tmid = Tile("mid")
        Tcnt = Tile("cnt")
        Tsa = Tile("sa")
        big = sb("big", [128, 6144], F32)
        TS_ = big.T
        TR_ = Tile("bigR")
        score_ap = big[:, 0:L]
        Rrelu = big[:, 4096:6144].bitcast(BF16).rearrange("p (h w) -> p h w", h=8)
        masks = [sb("mask%d" % s, [128, L], BF16) for s in range(NSUB)]
        maskTk = sb("maskTk", [128, 4, T], BF16)
        PT = [sb("PT%d" % i, [128, 2, T], BF16) for i in range(4)]
        xs = sb("xs", [128, D], BF16)
        accm = sb("accm", [128, 8 * T], F32)

        def view(ap, tile):
            v = Buf.__new__(Buf)
            v.t = ap
            v.T = tile
            return v

        acc = view(accm[0:65, :].rearrange("p (h t) -> p h t", h=8), accm.T)
        lt = view(accm[:, 0:5 * T].rearrange("p (k t) -> p k t", k=5), accm.T)
        gz = view(accm[:, 5 * T:5 * T + 512], accm.T)
        xcb = view(accm[:, 5 * T + 512:5 * T + 512 + T // 2].bitcast(BF16), accm.T)
        sc1 = sb("sc1", [128, L], F32)
        rhl2 = [sb("rhl%d" % i, [65, 2, T], BF16) for i in range(2)]
        onesb = sb("onesb", [65, 64], BF16)
        gt = sb("gt", [128, T], F32)
        tmpf = sb("tmpf", [128, 512], F32)
        gsig = sb("gsig", [128, 8, T], BF16)
        merged = big[:, 0:8 * T].rearrange("p (c t) -> p c t", c=8)
        o = 8 * T
        mergedT = big[:, o:o + 4 * T].bitcast(BF16).rearrange("p (c t) -> p c t", c=8)
        o += 4 * T
        fT = [big[:, o + i * 2 * T:o + (i + 1) * 2 * T].bitcast(BF16).rearrange("p (c t) -> p c t", c=4) for i in range(2)]
        o += 4 * T
        assert o <= 4096
        o = 4096
        rl = [big[:, o + i * (T // 2):o + (i + 1) * (T // 2)].bitcast(BF16) for i in range(2)]
        o += T
        sg = big[:, o:o + 512]
        o += 512
        ple_t = big[:, o:o + 1024]
        o += 1024
        assert o <= 6144
        wbdf = big[:, 4096:4096 + 1024].rearrange("p (c j) -> p c j", c=8)
        cols = sb("cols", [128, 48], F32)
        gbv = sb("gbv", [128, 512], F32)
        gcur = sb("gcur", [128, D], F32)
        gsem = fw.dsem("gain")
        wbd = sb("wbd", [128, 8, 128], BF16)
        wspT = sb("wspT", [128, 8, 128], BF16)
        bsp = sb("bsp", [8, 128], F32)
        bsph = sb("bsph", [8, 2, 128], BF16)
        c8 = sb("c8", [128, 8], F32)
        epsc = sb("epsc", [128, 4], F32)
        psem = [fw.dsem("par%d" % i) for i in range(5)]
        xsem = fw.dsem("xload")
        ptsem = fw.dsem("pload")
        possem = fw.dsem("posload")
        ssem = fw.dsem("store")
        kvsem = fw.dsem("kvstore")
        out_tile = Tile("out")

        class Stream:
            def __init__(self):
                self.plan = []
                self.issued = 0
                self.pos = 0

            def add(self, name, loader, deps, hoist=True):
                self.plan.append((name, loader, deps, hoist))

            def next(self, name, look=NSLOT - 1):
                i = self.pos
                assert self.plan[i][0] == name, (self.plan[i][0], name)
                while self.issued < len(self.plan) and (
                        self.issued <= i or (self.issued <= i + look and self.plan[self.issued][3])):
                    k = self.issued
                    _, loader, deps, _ = self.plan[k]
                    loader(ring[k % NSLOT], ring_sem[k % NSLOT], deps)
                    self.issued += 1
                self.pos += 1
                return ring[i % NSLOT]

        stream = Stream()

        def wloader(l, nm, nr, ncol):
            kc = nr // 128

            def f(slot, sem, deps):
                dst = slot[:, 0:kc * ncol].rearrange("p (k w) -> p k w", k=kc)
                src = Wp[l][nm].rearrange("(k p) w -> p k w", p=128)
                dma(dst, src, deps, [slot], sem)
            return f

        KVT = [[Tile("kv") for _ in range((L + KG - 1) // KG)] for _ in range(depth)]

        def kvloader(l, g, wd):
            def f(slot, sem, deps):
                dstk = slot[:, 0:4 * wd].rearrange("p (c w) -> p c w", c=4)
                dma(dstk, KTc[l][:, :, g * KG:g * KG + wd].rearrange("c p w -> p c w"), deps, [slot], sem)
                nb = wd // 128
                dstv = slot[:, 2048:2048 + nb * 520].rearrange("p (b f) -> p b f", b=nb)
                dma(dstv, Vc[l][g * KG:g * KG + wd, :].rearrange("(b p) f -> p b f", p=128), deps, [slot], sem)
            return f

        pinfo = {nm: (nr, ncol) for (nm, _, _, nr, _, ncol) in pdefs}

        def plan_w(l, nm):
            nr, ncol = pinfo[nm]
            stream.add("%d_%s" % (l, nm), wloader(l, nm, nr, ncol), [WT[l][nm]])

        def kv_groups(j):
            nkeys = (j + 1) * T
            out = []
            g = 0
            while g * KG < nkeys:
                out.append((g, min(KG, nkeys - g * KG)))
                g += 1
            return out

        for l in range(depth):
            for j in range(NT):
                for nm in ("q", "k", "qi", "kiwi", "xr", "gr", "zu", "v", "zv"):
                    plan_w(l, nm)
                for n in (2, 1):
                    plan_w(l, "gate%d_0" % n)
                    plan_w(l, "gate%d_1" % n)
                    plan_w(l, "br%d_0" % n)
                    plan_w(l, "br%d_1" % n)
                plan_w(l, "gate0_0")
                plan_w(l, "gate0_1")
                grps = kv_groups(j)
                for (g, wd) in grps:
                    last = (g == grps[-1][0])
                    stream.add("%d_kv%d_%d" % (l, j, g), kvloader(l, g, wd), [KVT[l][g]], hoist=not last)
                plan_w(l, "br0_0")
                plan_w(l, "br0_1")
                plan_w(l, "out_0")
                plan_w(l, "out_1")
                plan_w(l, "up_0")
                for g in range(8):
                    if g + 1 < 8:
                        plan_w(l, "up_%d" % (g + 1))
                    plan_w(l, "dn_%d" % g)
                plan_w(l, "ple")
                plan_w(l, "pg_0")
                plan_w(l, "pg_1")

        def wview(slot, nm):
            nr, ncol = pinfo[nm]
            kc = nr // 128
            return slot[:, 0:kc * ncol].rearrange("p (k w) -> p k w", k=kc)

        sm_i = [0]

        def sm():
            i = sm_i[0] % 32
            sm_i[0] += 1
            return small[:, i:i + 1]

        memset(epsc[:, 0:1], EPS, [epsc])
        memset(epsc[:, 1:2], math.pi / 2, [epsc])
        memset(epsc[:, 2:3], 1.0, [epsc])
        memset(epsc[:, 3:4], -BIGM, [epsc])
        memset(Vs[:, :, :], 1.0, [Vs])
        memset(onesb[:, :], 1.0, [onesb])
        memset(qT[:, :, :, :], 0.0, [qT])

        def rstd_from_ss(ss_ap, n):
            a = sm()
            b = sm()
            act(a, ss_ap, AF.Sqrt, [small, epsc], [small], scale=1.0 / n, bias=epsc[:, 0:1])
            recip(b, a, [small], [small])
            return b

        pi = [0]

        def nps():
            pi[0] += 1
            return PS[pi[0] % 4]

        def norm_transpose(gcol0, hT, xt):
            for s in range(NSUB):
                if gcol0 is not None:
                    ss = sm()
                    act(tmpf[:, 0:512], xt[:, s, 0:512], AF.Square, [xt, small], [tmpf, small], accum_out=ss)
                    ss2 = sm()
                    act(tmpf[:, 0:512], xt[:, s, 512:1024], AF.Square, [xt, small], [tmpf, small], accum_out=ss2)
                    ss3 = sm()
                    tt(ss3, ss, ss2, ALU.add, [small], [small])
                    rs = rstd_from_ss(ss3, D)
                    ts(xs[:], xt[:, s, :], rs, None, ALU.mult, None, [xt, small], [xs])
                else:
                    cp(xs[:], xt[:, s, :], [xt], [xs])
                psb = PS[7][:].bitcast(BF16)
                for c in range(8):
                    tr(psb[:, c * 128:(c + 1) * 128], xs[:, c * 128:(c + 1) * 128], identb, [xs, cb], [PS[7]])
                src = psb[:, 0:1024].rearrange("p (c t) -> p c t", c=8)
                dst = hT[:, :, s * 128:(s + 1) * 128]
                if gcol0 is not None:
                    g_ap = cols[:, gcol0:gcol0 + 8].unsqueeze(2).to_broadcast([128, 8, 128])
                    tt(dst, src, g_ap, ALU.mult, [PS[7], cols], [hT])
                else:
                    cp(dst, src, [PS[7]], [hT])

        def post_norm_residual(banks, gcol, s):
            ssa = []
            for hf in range(2):
                a = sm()
                act(tmpf[:, 0:512], banks[hf][:, 0:512], AF.Square, [banks[hf], small], [tmpf, small], accum_out=a)
                ssa.append(a)
            s3 = sm()
            tt(s3, ssa[0], ssa[1], ALU.add, [small], [small])
            rs = rstd_from_ss(s3, D)
            for hf in range(2):
                stt(tmpf[:, 0:512], banks[hf][:, 0:512], rs, gcur[:, hf * 512:(hf + 1) * 512],
                    ALU.mult, ALU.mult, [banks[hf], small, gcur], [tmpf])
                tt(xt[:, s, hf * 512:(hf + 1) * 512], xt[:, s, hf * 512:(hf + 1) * 512], tmpf[:, 0:512], ALU.add,
                   [xt, tmpf], [xt])

        def merge_tile(dst, src, as_write=False):
            for a, b in ((dst.w, src.w), (dst.r, src.r)):
                for k_, v_ in b.items():
                    if a.get(k_, 0) < v_:
                        a[k_] = v_
            if as_write:
                for k_, v_ in src.r.items():
                    if dst.w.get(k_, 0) < v_:
                        dst.w[k_] = v_

        def fm_chunk(panel, pv, c, ps, M=128, hsrc=None):
            hsrc = hT if hsrc is None else hsrc
            for kc in range(8):
                mm(ps[0:M, 0:T], pv[:, kc, c * 128:c * 128 + M], hsrc[:, kc, :], kc == 0, kc == 7, [panel, hsrc], [ps])

        def rope_evac(ps, dst, W, split=None):
            act(xb16[:], ps[:, 0:T], AF.Copy, [ps], [xb16])
            mm(ps[:, T:2 * T], Rm, xb16[:], True, True, [cb, xb16], [ps])
            tt(rtmp[:, 0, :], ps[:, 0:T], cosT[:], ALU.mult, [ps, cosT], [rtmp])
            tt(rtmp[:, 1, :], ps[:, T:2 * T], sinT[:], ALU.mult, [ps, sinT], [rtmp])
            if split is None:
                tt(dst, rtmp[:, 0, :], rtmp[:, 1, :], ALU.add, [rtmp], W)
            else:
                for hh in range(2):
                    tt(split[hh * 64:(hh + 1) * 64, hh, :], rtmp[hh * 64:(hh + 1) * 64, 0, :], rtmp[hh * 64:(hh + 1) * 64, 1, :],
                       ALU.add, [rtmp], W)

        XTprev = None
        for l in range(depth):
            src_x = x_in if l == 0 else xbuf
            dst_x = y_out if l == depth - 1 else xbuf
            XT = [Tile("xd") for _ in range(NT)] if l < depth - 1 else None
            dma(cols[:], cols_in[l], [], [cols], psem[0])
            dma(gbv[:], gb_in[l, :, 3 * D:3 * D + 512], [], [gbv], psem[1])
            dma(gcur[:], gb_in[l, :, 0:D], [], [gcur], gsem)
            dma(bsp[:], bsp_in[l], [], [bsp], psem[2])
            cp(bsph[:, 0, :], bsp[:], [bsp], [bsph])
            tt(bsp[:], bsp[:], bsph[:, 0, :], ALU.subtract, [bsph], [bsp])
            cp(bsph[:, 1, :], bsp[:], [bsp], [bsph])
            dma(wbdf, wbd_in[l], [], [TR_], psem[3])
            cp(wbd[:], wbdf, [TR_], [wbd])
            dma(wbdf, wsp_in[l], [], [TR_], psem[4])
            tt(wspT[:], wbdf, tril01.unsqueeze(1).to_broadcast([128, 8, 128]), ALU.mult, [TR_, cf], [wspT])
            act(c8[:, 4:8], cols[:, 44:48], AF.Exp, [cols], [c8], scale=-1.0)
            ts(c8[:, 0:4], c8[:, 4:8], -0.25, 1.0 / 3.0, ALU.mult, ALU.add, [c8], [c8])
            tt(c8[:, 0:4], c8[:, 0:4], c8[:, 4:8], ALU.mult, [c8], [c8])
            ts(c8[:, 0:4], c8[:, 0:4], -1.0, 0.5, ALU.mult, ALU.add, [c8], [c8])
            tt(c8[:, 0:4], c8[:, 0:4], c8[:, 4:8], ALU.mult, [c8], [c8])
            ts(c8[:, 0:4], c8[:, 0:4], -1.0, 1.0, ALU.mult, ALU.add, [c8], [c8])
            tt(c8[:, 0:4], c8[:, 0:4], c8[:, 4:8], ALU.mult, [c8], [c8])
            ts(c8[:, 0:4], c8[:, 0:4], -8.0, None, ALU.mult, None, [c8], [c8])
            ts(c8[:, 4:8], c8[:, 0:4], 2.0, None, ALU.mult, None, [c8], [c8])
            memset(xrbuf[:, :, 0:3], 0.0, [xrbuf])
            memset(hst[:], 0.0, [hst])

            for j in range(NT):
                t0 = j * T
                first_tile = (l == 0 and j == 0)
                def load_x(jj):
                    rd_ = [XTprev[jj]] if (l > 0) else []
                    dma(xts[jj % 2][:], src_x[jj * T:(jj + 1) * T, :].rearrange("(s p) d -> p s d", p=128), rd_, [xts[jj % 2]], xsem)

                def prologue(jj):
                    t0 = jj * T
                    xt = xts[jj % 2]
                    dma(pt[:], p_in[l, t0:t0 + T, :].rearrange("(s p) d -> p s d", p=128), [], [pt], ptsem)
                    dma(posi[:], pos_in[:, t0:t0 + T], [], [posi], possem)
                    ang = rtmp[:, 0, :]
                    u = rtmp[:, 1, :]
                    rr = rtmp[:, 2, :]
                    cp(u, posi[:], [posi], [rtmp])
                    ts(ang, u, invf, None, ALU.mult, None, [rtmp, cf], [rtmp])
                    ts(u, ang, 1.0 / (2 * math.pi), 12582912.0, ALU.mult, ALU.add, [rtmp], [rtmp])
                    ts(u, u, -12582912.0, None, ALU.add, None, [rtmp], [rtmp])
                    C1 = 6.28125
                    C2 = float(np.float32(2 * math.pi - C1))
                    C3 = float(2 * math.pi - C1 - C2)
                    stt(rr, u, -C1, ang, ALU.mult, ALU.add, [rtmp], [rtmp])
                    stt(rr, u, -C2, rr, ALU.mult, ALU.add, [rtmp], [rtmp])
                    stt(rr, u, -C3, rr, ALU.mult, ALU.add, [rtmp], [rtmp])
                    act(sinT[:], rr, AF.Sin, [rtmp], [sinT])
                    stt(u, rr, -1.0, rr, ALU.mult, ALU.max, [rtmp], [rtmp])
                    act(cosT[:], u, AF.Sin, [rtmp, epsc], [cosT], scale=-1.0, bias=epsc[:, 1:2])
                    norm_transpose(0, hT, xt)

                xt = xts[j % 2]
                if j == 0:
                    load_x(0)
                    prologue(0)
                if j + 1 < NT:
                    load_x(j + 1)
                if first_tile:
                    dump("hT", hT[:, 0, :], [hT])
                    dump("cosT", cosT[:], [cosT])
                    dump("sinT", sinT[:], [sinT])

                for nm, dstb in (("q", qT), ("k", KTs), ("qi", qiT)):
                    panel = stream.next("%d_%s" % (l, nm))
                    pv = wview(panel, nm)
                    pss = [nps() for _ in range(4)]
                    fm_chunk(panel, pv, 0, pss[0])
                    for c in range(4):
                        if c + 1 < 4:
                            fm_chunk(panel, pv, c + 1, pss[c + 1])
                        if nm == "q":
                            rope_evac(pss[c], None, [dstb], split=qT[:, c, :, :])
                        else:
                            rope_evac(pss[c], dstb[:, c, :], [dstb])
                panel = stream.next("%d_kiwi" % l)
                pv = wview(panel, "kiwi")
                ps = nps()
                for half in range(2):
                    for kc in range(8):
                        mm(ps[half * 64:(half + 1) * 64, 0:T], pv[:, kc, 0:64], hT[:, kc, :], kc == 0, kc == 7,
                           [panel, hT], [ps])
                rope_evac(ps, kiT[:, t0:t0 + T], [kiT])
                for s in range(NSUB):
                    ps = nps()
                    for kc in range(8):
                        mm(ps[:, 0:8], hT[:, kc, s * 128:(s + 1) * 128], pv[:, kc, 64:72], kc == 0, kc == 7,
                           [panel, hT], [ps])
                    cp(wis[:, s, :], ps[:, 0:8], [ps], [wis])
                dma(KTc[l][:, :, t0:t0 + T].rearrange("c p w -> p c w"), KTs[:], [KTs], [KVT[l][t0 // KG]], kvsem, eng="gpsimd")
                if first_tile:
                    dump("qT", qT[:, 0, 0, :], [qT])
                    dump("kiT", kiT[:, 0:T], [kiT])
                    dump("wis", wis[:, 0, :], [wis])
                panel = stream.next("%d_xr" % l)
                pv = wview(panel, "xr")
                for c in range(4):
                    ps = nps()
                    fm_chunk(panel, pv, c, ps)
                    act(xrbuf[:, c, 3:3 + T], ps[:, 0:T], AF.Copy, [ps], [xrbuf])
                for nm, dstb in (("gr", grT), ("zu", zuT)):
                    panel = stream.next("%d_%s" % (l, nm))
                    pv = wview(panel, nm)
                    for c in range(4):
                        ps = nps()
                        fm_chunk(panel, pv, c, ps)
                        act(dstb[:, c, :], ps[:, 0:T], AF.Gelu_apprx_tanh, [ps], [dstb])
                panel = stream.next("%d_v" % l)
                pv = wview(panel, "v")
                for s in range(NSUB):
                    ps = nps()
                    for kc in range(8):
                        mm(ps[:, 0:512], hT[:, kc, s * 128:(s + 1) * 128], pv[:, kc, :], kc == 0, kc == 7, [panel, hT], [ps])
                    dstv = Vs[:, s, :].rearrange("p (h f) -> p h f", h=8)[:, :, 0:64]
                    act(dstv, ps[:, 0:512].rearrange("p (h f) -> p h f", h=8), AF.Copy, [ps], [Vs])
                dma(Vc[l][t0:t0 + T, :].rearrange("(s p) f -> p s f", p=128), Vs[:], [Vs], [KVT[l][t0 // KG]], kvsem, eng="gpsimd")
                panel = stream.next("%d_zv" % l)
                pv = wview(panel, "zv")
                for s in range(NSUB):
                    ps = nps()
                    for kc in range(8):
                        mm(ps[:, 0:512], hT[:, kc, s * 128:(s + 1) * 128], pv[:, kc, :], kc == 0, kc == 7, [panel, hT], [ps])
                    act(gz[:], ps[:, 0:512], AF.Gelu_apprx_tanh, [ps], [gz])
                    ss = sm()
                    act(tmpf[:, 0:512], gz[:], AF.Square, [gz, small], [tmpf, small], accum_out=ss)
                    rs = rstd_from_ss(ss, 512)
                    stt(vn[:, s, :], gz[:], rs, gbv[:], ALU.mult, ALU.mult, [gz, small, gbv], [vn])

                mpi = [0]

                def mps():
                    mpi[0] += 1
                    return PS[5 + mpi[0] % 3]

                def gen_mixC():
                    for s in range(NSUB):
                        for cpair in range(4):
                            ps = mps()
                            for gg in range(2):
                                g = cpair * 2 + gg
                                mm(ps[gg * 64:(gg + 1) * 64, 0:128], vn[:, s, g * 64:(g + 1) * 64], wspT[:, g, :], True, False,
                                   [vn, wspT], [ps])
                            mm(ps[:, 0:128], esel[:, cpair, :], bsph[:, 0, :], False, False, [cb, bsph], [ps])
                            mm(ps[:, 0:128], esel[:, cpair, :], bsph[:, 1, :], False, True, [cb, bsph], [ps])
                            tt(ycT[:, cpair, s * 128:(s + 1) * 128], ps[:, 0:128], zuT[:, cpair, s * 128:(s + 1) * 128], ALU.mult,
                               [ps, zuT], [ycT])
                            yield

                def gen_mixB():
                    for c in range(4):
                        xc = lt[:, 0, :]
                        ts(xc, xrbuf[:, c, 0:T], cols[:, 16 + c:17 + c], cols[:, 32 + c:33 + c], ALU.mult, ALU.add, [xrbuf, cols], [lt])
                        for jj in range(1, 4):
                            stt(xc, xrbuf[:, c, jj:jj + T], cols[:, 16 + jj * 4 + c:17 + jj * 4 + c], xc, ALU.mult, ALU.add,
                                [xrbuf, cols, lt], [lt])
                        yield
                        cp(xrbuf[:, c, 0:3], xrbuf[:, c, T:T + 3], [xrbuf], [xrbuf])
                        act(xcb[:], xc, AF.Copy, [lt], [xcb])
                        ps = mps()
                        mm(ps[:, 0:T], wbd[:, c, :], xcb[:], True, True, [wbd, xcb], [ps])
                        mm(ps[:, T:2 * T], wbd[:, 4 + c, :], xcb[:], True, True, [wbd, xcb], [ps])
                        yield
                        rg = lt[:, 1, :]
                        ig = lt[:, 2, :]
                        av = lt[:, 3, :]
                        sq = lt[:, 4, :]
                        act(rg, ps[:, 0:T], AF.Sigmoid, [ps, cols], [lt], bias=cols[:, 36 + c:37 + c])
                        act(ig, ps[:, T:2 * T], AF.Sigmoid, [ps, cols], [lt], bias=cols[:, 40 + c:41 + c])
                        yield
                        act(av, rg, AF.Exp, [lt, c8], [lt], scale=c8[:, c:c + 1])
                        act(sq, rg, AF.Exp, [lt, c8], [lt], scale=c8[:, 4 + c:5 + c])
                        act(sq, sq, AF.Sqrt, [lt, epsc], [lt], scale=-1.0, bias=epsc[:, 2:3])
                        yield
                        tt(ig, ig, xc, ALU.mult, [lt], [lt])
                        tt(ig, ig, sq, ALU.mult, [lt], [lt])
                        hh = lt[:, 1, :]
                        scan(hh, av, ig, hst[:, c:c + 1], [lt, hst], [lt])
                        yield
                        cp(hst[:, c:c + 1], hh[:, T - 1:T], [lt], [hst])
                        tt(ybT[:, c, :], hh, grT[:, c, :], ALU.mult, [lt, grT], [ybT])
                        yield

                grps = kv_groups(j)
                nkeys = (j + 1) * T
                TRh = [Tile("relu%d" % h) for h in range(8)]

                SC = [(score_ap, TS_), (sc1[:, 0:L], sc1.T)]

                def gen_I(s):
                    N = t0 + 128 * (s + 1)
                    score_s, TSs = SC[s]
                    for h in range(8):
                        merge_tile(TRh[h], TR_, as_write=True)
                        act(diag[:, h, :], identf, AF.Copy, [cf, wis], [diag], scale=wis[:, s, h:h + 1])
                    yield
                    ngrp = (N + KG - 1) // KG
                    for kg in range(ngrp):
                        wd = min(KG, N - kg * KG)
                        for h in range(8):
                            ps = PS[h % 4]
                            r0 = (h % 2) * 64
                            mm(ps[:, 0:wd], qiT[r0:r0 + 64, h // 2, s * 128:(s + 1) * 128],
                               kiT[r0:r0 + 64, kg * KG:kg * KG + wd], True, True, [qiT, kiT], [ps])
                            if h % 2 == 0:
                                act(Rrelu[:, h, 0:wd], ps[:, 0:wd], AF.Relu, [ps], [TRh[h]])
                            else:
                                ts(Rrelu[:, h, 0:wd], ps[:, 0:wd], 0.0, None, ALU.max, None, [ps], [TRh[h]])
                            if h % 2 == 1:
                                yield
                        for h in range(8):
                            mm(PS[4][:, 0:wd], diag[:, h, :], Rrelu[:, h, 0:wd], h == 0, h == 7, [diag, TRh[h]], [PS[4]])
                        if kg == ngrp - 1:
                            if wd > 128:
                                cp(score_s[:, kg * KG:kg * KG + wd - 128], PS[4][:, 0:wd - 128], [PS[4]], [TSs])
                            tt(score_s[:, N - 128:N], PS[4][:, wd - 128:wd], negmask, ALU.add, [PS[4], cf], [TSs])
                            tt(tmpf[:, 0:128], PS[4][:, wd - 128:wd], posfill, ALU.add, [PS[4], cf], [tmpf])
                        else:
                            cp(score_s[:, kg * KG:kg * KG + wd], PS[4][:, 0:wd], [PS[4]], [TSs])
                        yield
                    for h in range(8):
                        merge_tile(TR_, TRh[h])

                def gen_B(s):
                    N = t0 + 128 * (s + 1)
                    score_s, TSs = SC[s]
                    hi0 = bis[:, 0:1]
                    lo = bis[:, 1:2]
                    w0 = bis[:, 2:3]
                    reduce(hi0, score_s[:, 0:N], ALU.max, [TSs, bis], [bis])
                    reduce(lo, tmpf[:, 0:128], ALU.min, [tmpf, bis], [bis])
                    if N > 128:
                        m1 = bis[:, 3:4]
                        reduce(m1, score_s[:, 0:N - 128], ALU.min, [TSs, bis], [bis])
                        tt(lo, lo, m1, ALU.min, [bis], [bis])
                    tt(w0, hi0, lo, ALU.subtract, [bis], [bis])
                    yield
                    mk = masks[s]
                    if N > TOPK:
                        Nd = max(128, (int(N * 0.42) // 128) * 128)
                        Na = N - Nd
                        if s == 0:
                            junkA, TJ = masks[1], masks[1].T
                        else:
                            junkA, TJ = big[:, 4096:6144].bitcast(BF16), TR_
                        for it in range(NBIS):
                            k4 = it % 4
                            mid = bis2[:, k4:k4 + 1]
                            cnt = bis2[:, 4 + k4:5 + k4]
                            vv = bis2[:, 8 + k4:9 + k4]
                            sA = bis2[:, 12 + k4:13 + k4]
                            stt(mid, w0, 0.5 ** (it + 1), lo, ALU.mult, ALU.add, [bis], [Tmid])
                            act(junkA[:, 0:Na], score_s[:, Nd:N], AF.Sign, [TSs, Tmid], [TJ, Tsa], scale=-1.0, bias=mid,
                                accum_out=sA)
                            count_ge(mk[:, 0:Nd], score_s[:, 0:Nd], mid, cnt, [TSs, Tmid], [mk, Tcnt])
                            stt(vv, cnt, 2.0, sA, ALU.mult, ALU.subtract, [Tcnt, Tsa], [Tcnt])
                            ge = smalli[:, k4:k4 + 1]
                            ts(ge, vv, float(2 * TOPK - Na), None, ALU.is_ge, None, [Tcnt], [smalli])
                            cpred(lo, ge, mid, [Tmid, smalli], [bis])
                            yield
                    ts(mk[:, 0:N], score_s[:, 0:N], lo, None, ALU.is_ge, None, [TSs, bis], [mk])
                    if N < nkeys:
                        memset(mk[:, N:nkeys], 0.0, [mk], eng="gpsimd")
                    yield

                def gen_G(items):
                    for ni, n, do_gate, do_branch in items:
                        if do_gate:
                            for hf in range(2):
                                gpan = stream.next("%d_gate%d_%d" % (l, n, hf))
                                pv = wview(gpan, "gate0_0")
                                for c in range(4):
                                    ps = nps()
                                    fm_chunk(gpan, pv, c, ps)
                                    act(gsig[:, hf * 4 + c, :], ps[:, 0:T], AF.Sigmoid, [ps], [gsig])
                                    yield
                        if not do_branch:
                            continue
                        ysrc = {2: ycT, 1: ybT, 0: yaT}[n]
                        for hf in range(2):
                            bpan = stream.next("%d_br%d_%d" % (l, n, hf))
                            pv = wview(bpan, "br0_0")
                            for c in range(4):
                                dchunk = hf * 4 + c
                                ps = nps()
                                for kc in range(4):
                                    mm(ps[:, 0:T], pv[:, kc, c * 128:(c + 1) * 128], ysrc[:, kc, :], kc == 0, kc == 3,
                                       [bpan, ysrc], [ps])
                                if ni == 0:
                                    tt(merged[:, dchunk, :], ps[:, 0:T], gsig[:, dchunk, :], ALU.mult, [ps, gsig], [TS_])
                                else:
                                    tt(gt[:], ps[:, 0:T], gsig[:, dchunk, :], ALU.mult, [ps, gsig], [gt])
                                    dsto = mergedT if ni == 2 else merged
                                    tt(dsto[:, dchunk, :], merged[:, dchunk, :], gt[:], ALU.add, [TS_, gt], [TS_], eng="gpsimd")
                                yield

                def chain(*gens):
                    for g_ in gens:
                        yield from g_

                def run(*gens):
                    live = list(gens)
                    while live:
                        for g_ in list(live):
                            try:
                                next(g_)
                            except StopIteration:
                                live.remove(g_)

                def run_w(ga, gb_, kb):
                    la = lb = True
                    while la or lb:
                        if la:
                            try:
                                next(ga)
                            except StopIteration:
                                la = False
                        for _ in range(kb):
                            if lb:
                                try:
                                    next(gb_)
                                except StopIteration:
                                    lb = False

                run(gen_mixB(), gen_mixC(), gen_I(0))
                if first_tile:
                    dump("ycT", ycT[:, 0, :], [ycT])
                    dump("ybT", ybT[:, 0, :], [ybT])
                    dump("score", score_ap[:, 0:128], [TS_])
                N1 = t0 + 256
                lenI = 1 + ((N1 + KG - 1) // KG) * 5
                lenB = 2 + (NBIS if (t0 + 128) > TOPK else 0)
                run_w(gen_B(0), gen_I(1), max(1, -(-lenI // lenB)))
                lenB1 = 2 + (NBIS if (t0 + 256) > TOPK else 0)
                run_w(gen_B(1), gen_G([(0, 2, True, True), (1, 1, True, True), (2, 0, True, False)]), max(1, -(-40 // lenB1)))

                first = True
                ucnt = [0]
                for gi, (g, wd) in enumerate(grps):
                    panel = stream.next("%d_kv%d_%d" % (l, j, g))
                    nb = wd // 128
                    KTv = panel[:, 0:4 * wd].rearrange("p (c w) -> p c w", c=4)
                    Vv = panel[:, 2048:2048 + nb * 520].rearrange("p (b f) -> p b f", b=nb)
                    psb = PS[6][:].bitcast(BF16)
                    for s in range(NSUB):
                        for b in range(nb):
                            tr(psb[:, (s * 4 + b) * 128:(s * 4 + b + 1) * 128],
                               masks[s][:, g * KG + b * 128:g * KG + (b + 1) * 128], identb, [masks[s], cb], [PS[6]])
                    for s in range(NSUB):
                        act(maskTk[:, 0:nb, s * 128:(s + 1) * 128],
                            psb[:, s * 512:s * 512 + nb * 128].rearrange("p (b t) -> p b t", b=nb),
                            AF.Copy, [PS[6]], [maskTk])
                    units = [(hp, b) for hp in range(4) for b in range(nb)]

                    def stageA(i):
                        hp, b = units[i]
                        ps = PS[(ucnt[0] + i) % 4]
                        mm(ps[:, 0:2 * T], KTv[:, hp, b * 128:(b + 1) * 128], qT[:, hp, :, :].rearrange("p a t -> p (a t)"),
                           True, True, [panel, qT], [ps])

                    def stageB(i):
                        hp, b = units[i]
                        ps = PS[(ucnt[0] + i) % 4]
                        ptile = PT[(ucnt[0] + i) % len(PT)]
                        act(ptile[:, :, :], ps[:, 0:2 * T].rearrange("p (a t) -> p a t", a=2), AF.Exp,
                            [ps], [ptile], scale=HD ** -0.5)
                        tt(ptile[:, :, :], ptile[:, :, :], maskTk[:, b:b + 1, :].to_broadcast([128, 2, T]), ALU.mult,
                           [ptile, maskTk], [ptile], eng=("gpsimd" if (ucnt[0] + i) % 4 == 3 else "vector"))

                    def stageD(i):
                        hp, b = units[i]
                        ptile = PT[(ucnt[0] + i) % len(PT)]
                        for hh in range(2):
                            h = 2 * hp + hh
                            pacc = PS[4 + hh]
                            mm(pacc[0:65, 0:T], Vv[:, b, h * 65:(h + 1) * 65], ptile[:, hh, :], b == 0, b == nb - 1,
                               [panel, ptile], [pacc])
                            if b == nb - 1:
                                if first:
                                    cp(acc[:, h, :], pacc[0:65, 0:T], [pacc], [acc])
                                else:
                                    tt(acc[:, h, :], acc[:, h, :], pacc[0:65, 0:T], ALU.add, [pacc, acc], [acc])

                    for i in range(min(3, len(units))):
                        stageA(i)
                    for i in range(len(units)):
                        if i + 3 < len(units):
                            stageA(i + 3)
                        stageB(i)
                        stageD(i)
                    ucnt[0] += len(units)
                    first = False
                accf = accm[0:65, :]
                act(accf[64:65, :], accf[64:65, :], AF.Ln, [acc], [acc])
                act(accf[64:65, :], accf[64:65, :], AF.Exp, [acc], [acc], scale=-1.0)
                for h in range(8):
                    rhl = rhl2[h % 2]
                    cp(rhl[64:65, 0, :], acc[64:65, h, :], [acc], [rhl])
                    tt(rhl[64:65, 1, :], acc[64:65, h, :], rhl[64:65, 0, :], ALU.subtract, [acc, rhl], [rhl])
                    ps = nps()
                    mm(ps[0:64, 0:T], onesb[64:65, 0:64], rhl[64:65, 0, :], True, False, [onesb, rhl], [ps])
                    mm(ps[0:64, 0:T], onesb[64:65, 0:64], rhl[64:65, 1, :], False, True, [onesb, rhl], [ps])
                    if h % 2 == 0:
                        tt(yaT[0:64, h // 2, :], acc[0:64, h, :], ps[0:64, 0:T], ALU.mult, [acc, ps], [yaT])
                    else:
                        tt(yaTt[:, :], acc[0:64, h, :], ps[0:64, 0:T], ALU.mult, [acc, ps], [yaTt])
                        ps2 = nps()
                        mm(ps2[64:128, 0:T], identb[0:64, 0:64], yaTt[:, :], True, True, [cb, yaTt], [ps2])
                        cp(yaT[64:128, h // 2, :], ps2[64:128, 0:T], [ps2], [yaT])
                if first_tile:
                    dump("yaT", yaT[:, 0, :], [yaT])
                if l == 0 and j == 1:
                    dump("yaT1", yaT[:, 0, :], [yaT])
                    dump("acc1", acc[:, 0, :], [acc])

                run(gen_G([(2, 0, False, True)]))
                op_ = [stream.next("%d_out_%d" % (l, hf), look=NSLOT - 1 - hf) for hf in range(2)]
                for s in range(NSUB):
                    banks = [PS[4 + 2 * (s % 2)], PS[5 + 2 * (s % 2)]]
                    for hf in range(2):
                        pv = wview(op_[hf], "out_0")
                        for kc in range(8):
                            mm(banks[hf][:, 0:512], mergedT[:, kc, s * 128:(s + 1) * 128], pv[:, kc, :], kc == 0, kc == 7,
                               [op_[hf], TS_], [banks[hf]])
                    post_norm_residual(banks, 0, s)
                dma(gcur[:], gb_in[l, :, D:2 * D], [], [gcur], gsem, eng="gpsimd")
                if first_tile:
                    dump("x1", xt[:, 0, :], [xt])
                norm_transpose(8, hTb, xt)
                for s in range(NSUB):
                    cp(ptb[:, s, :], pt[:, s, :], [pt], [ptb])
                    psb = PS[3][:].bitcast(BF16)
                    for c in range(2):
                        tr(psb[:, c * 128:(c + 1) * 128], ptb[:, s, c * 128:(c + 1) * 128], identb, [ptb, cb], [PS[3]])
                    cp(pT[:, :, s * 128:(s + 1) * 128], psb[:, 0:256].rearrange("p (c t) -> p c t", c=2), [PS[3]], [pT])
                dbanks = [[PS[4], PS[5]], [PS[6], PS[7]]]
                TF = [Tile("fT0"), Tile("fT1")]
                Trl = [Tile("rl0"), Tile("rl1")]

                for i_ in range(2):
                    merge_tile(TF[i_], TS_, as_write=True)
                    merge_tile(Trl[i_], TR_, as_write=True)

                def ffn_up(g):
                    up = stream.next("%d_up_%d" % (l, g))
                    pv = wview(up, "up_0")
                    fTg = fT[g % 2]
                    for c in range(4):
                        ps = nps()
                        fm_chunk(up, pv, c, ps, hsrc=hTb)
                        rlb = rl[c % 2]
                        act(rlb, ps[:, 0:T], AF.Relu, [ps], [Trl[c % 2]])
                        tt(fTg[:, c, :], rlb, rlb, ALU.mult, [Trl[c % 2]], [TF[g % 2]], eng="gpsimd")

                def ffn_dn(g):
                    dn = stream.next("%d_dn_%d" % (l, g))
                    dv = wview(dn, "dn_0")
                    fTg = fT[g % 2]
                    for s in range(NSUB):
                        for hf in range(2):
                            for c in range(4):
                                mm(dbanks[s][hf][:, 0:512], fTg[:, c, s * 128:(s + 1) * 128], dv[:, c, hf * 512:(hf + 1) * 512],
                                   g == 0 and c == 0, g == 7 and c == 3, [dn, TF[g % 2]], [dbanks[s][hf]])

                if j + 1 < NT:
                    prologue(j + 1)
                ffn_up(0)
                for g in range(8):
                    if g + 1 < 8:
                        ffn_up(g + 1)
                    ffn_dn(g)
                for i_ in range(2):
                    merge_tile(TS_, TF[i_])
                    merge_tile(TR_, Trl[i_])
                for s in range(NSUB):
                    post_norm_residual(dbanks[s], D, s)
                dma(gcur[:], gb_in[l, :, 2 * D:3 * D], [], [gcur], gsem, eng="gpsimd")
                if first_tile:
                    dump("x2", xt[:, 0, :], [xt])
                norm_transpose(None, hTb, xt)
                plp = stream.next("%d_ple" % l)
                plv = wview(plp, "ple")
                pg = [stream.next("%d_pg_%d" % (l, hf), look=NSLOT - 2 - hf) for hf in range(2)]
                ple2 = big[:, 0:2 * D].rearrange("p (s d) -> p s d", s=2)
                sspa = []
                for s in range(NSUB):
                    ssp = []
                    for hf in range(2):
                        pa = PS[4 * (s % 2) + hf]
                        pgb = PS[4 * (s % 2) + 2 + hf]
                        for kc in range(2):
                            mm(pa[:, 0:512], pT[:, kc, s * 128:(s + 1) * 128], plv[:, kc, hf * 512:(hf + 1) * 512], kc == 0, kc == 1,
                               [plp, pT], [pa])
                        gv = wview(pg[hf], "pg_0")
                        for kc in range(8):
                            mm(pgb[:, 0:512], hTb[:, kc, s * 128:(s + 1) * 128], gv[:, kc, :], kc == 0, kc == 7, [pg[hf], hTb], [pgb])
                        act(sg, pgb[:, 0:512], AF.Sigmoid, [pgb], [TR_])
                        tt(ple2[:, s, hf * 512:(hf + 1) * 512], pa[:, 0:512], sg, ALU.mult, [pa, TR_], [TS_])
                        a = sm()
                        act(tmpf[:, 0:512], ple2[:, s, hf * 512:(hf + 1) * 512], AF.Square, [TS_, small], [tmpf, small], accum_out=a)
                        ssp.append(a)
                    sspa.append(ssp)
                for s in range(NSUB):
                    s3 = sm()
                    tt(s3, sspa[s][0], sspa[s][1], ALU.add, [small], [small])
                    rs = rstd_from_ss(s3, D)
                    for hf in range(2):
                        stt(tmpf[:, 0:512], ple2[:, s, hf * 512:(hf + 1) * 512], rs, gcur[:, hf * 512:(hf + 1) * 512],
                            ALU.mult, ALU.mult, [TS_, small, gcur], [tmpf])
                        tt(xt[:, s, hf * 512:(hf + 1) * 512], xt[:, s, hf * 512:(hf + 1) * 512], tmpf[:, 0:512], ALU.add,
                           [xt, tmpf], [xt])
                if j + 1 < NT:
                    dma(gcur[:], gb_in[l, :, 0:D], [], [gcur], gsem, eng="gpsimd")
                wt = [XT[j]] if XT is not None else [out_tile]
                dma(dst_x[t0:t0 + T, :].rearrange("(s p) d -> p s d", p=128), xt[:], [xt], wt, ssem, eng="gpsimd")
            XTprev = XT

        fw.finish("sync", [out_tile] + dbg_tiles)
        fw.emit()
    return nc, fw


def _consts():
    bf = ml_dtypes.bfloat16
    ident = np.eye(128, dtype=np.float32)
    Rm = np.zeros((128, 128), np.float32)
    for base in (0, 64):
        for d in range(8):
            Rm[base + d + 8, base + d] = -1.0
            Rm[base + d, base + d + 8] = 1.0
    esel = np.zeros((128, 4, 128), np.float32)
    for g in range(8):
        esel[g, g // 2, (g % 2) * 64:(g % 2 + 1) * 64] = 1.0
    cb = np.concatenate([ident, Rm, esel.reshape(128, 512)], axis=1).astype(bf)
    tt_, ss_ = np.meshgrid(np.arange(128), np.arange(128), indexing="ij")
    negmask = np.where(ss_ > tt_, np.float32(NEG), np.float32(0.0))
    posfill = np.where(ss_ > tt_, np.float32(-2 * NEG), np.float32(0.0))
    tril01 = (tt_ <= ss_).astype(np.float32)
    half = 8
    inv_freq = (np.float32(500000.0) ** (-np.arange(half, dtype=np.float32) * np.float32(2.0) / np.float32(16))).astype(np.float32)
    invf = np.zeros((128, 1), np.float32)
    for f in range(128):
        d = f % 64
        if d < 16:
            invf[f, 0] = inv_freq[d % 8]
    cf = np.concatenate([ident, negmask, posfill, tril01, invf], axis=1).astype(np.float32)
    return cb, cf


def _layout_params(inp, depth):
    f = np.float32
    cols = np.zeros((depth, 128, 48), f)
    gb = np.zeros((depth, 128, 3 * D + 512), f)
    wbd = np.zeros((depth, 128, 8, 128), f)
    wsp = np.zeros((depth, 128, 8, 128), f)
    for l in range(depth):
        cols[l, :, 0:8] = np.asarray(inp["g_pre_mix"][l], f).reshape(8, 128).T
        cols[l, :, 8:16] = np.asarray(inp["g_pre_ffn"][l], f).reshape(8, 128).T
        cw = np.asarray(inp["conv_w"][l], f)
        for jj in range(4):
            cols[l, :, 16 + jj * 4:20 + jj * 4] = cw[jj].reshape(4, 128).T
        cols[l, :, 32:36] = np.asarray(inp["conv_b"][l], f).reshape(4, 128).T
        cols[l, :, 36:40] = np.asarray(inp["b_rg_a"][l], f).reshape(4, 128).T
        cols[l, :, 40:44] = np.asarray(inp["b_rg_x"][l], f).reshape(4, 128).T
        cols[l, :, 44:48] = np.asarray(inp["lru_lambda"][l], f).reshape(4, 128).T
        row = np.concatenate([np.asarray(inp["g_post_mix"][l], f), np.asarray(inp["g_post_ffn"][l], f),
                              np.asarray(inp["g_post_ple"][l], f), np.asarray(inp["g_gmlp_v"][l], f)])
        gb[l] = np.broadcast_to(row[None, :], (128, row.size))
        for gi, key in enumerate(("w_rg_a", "w_rg_x")):
            w = np.asarray(inp[key][l], f)
            for c in range(4):
                for hh in range(2):
                    wbd[l, hh * 64:(hh + 1) * 64, gi * 4 + c, hh * 64:(hh + 1) * 64] = w[c * 2 + hh]
        ws = np.asarray(inp["w_spatial"][l], f)
        wsp[l] = np.transpose(ws, (2, 0, 1))
    bsp = np.ascontiguousarray(np.asarray(inp["b_spatial"], f))
    return cols, gb, wbd, wsp, bsp


_CACHE = {}


def kernel(**inputs):
    depth = DEPTH
    x = np.asarray(inputs["x"], np.float32)
    B, L, _ = x.shape
    p = np.asarray(inputs["p"], np.float32)
    pos = np.asarray(inputs["positions"], np.int32)
    cb, cf = _consts()
    cols, gb, wbd, wsp, bsp = _layout_params(inputs, depth)
    shared = {
        "w_in": np.ascontiguousarray(np.asarray(inputs["w_in"], np.float32)),
        "w_branch": np.ascontiguousarray(np.asarray(inputs["w_branch"], np.float32)),
        "w_out": np.ascontiguousarray(np.asarray(inputs["w_out"], np.float32)),
        "w_ffn_up": np.ascontiguousarray(np.asarray(inputs["w_ffn_up"], np.float32)),
        "w_ffn_down": np.ascontiguousarray(np.asarray(inputs["w_ffn_down"], np.float32)),
        "w_ple": np.ascontiguousarray(np.asarray(inputs["w_ple"], np.float32)),
        "w_ple_gate": np.ascontiguousarray(np.asarray(inputs["w_ple_gate"], np.float32)),
        "cols": cols, "gb": gb, "wbd": wbd, "wsp": wsp, "bsp": bsp, "cb": cb, "cf": cf,
    }
    if L not in _CACHE:
        _CACHE[L] = build_program(L, depth)[0]
    nc = _CACHE[L]
    in_maps = []
    for b in range(B):
        m = dict(shared)
        m["x"] = np.ascontiguousarray(x[b])
        m["p"] = np.ascontiguousarray(p[:, b])
        m["pos"] = np.ascontiguousarray(np.broadcast_to(pos[b][None, :], (128, L)))
        in_maps.append(m)
    res = run_bass_kernel_spmd(nc, in_maps, core_ids=list(range(B)))
    return np.stack([np.asarray(r["y"], np.float32) for r in res.results], axis=0)
```

```python
import math
from contextlib import ExitStack
import numpy as np
import ml_dtypes
import concourse.bass as bass
import concourse.mybir as mybir
from concourse.bass_utils import run_bass_kernel_spmd

F32 = mybir.dt.float32
BF16 = mybir.dt.bfloat16
I32 = mybir.dt.int32
AF = mybir.ActivationFunctionType
ALU = mybir.AluOpType
AX = mybir.AxisListType

D = 1024
NH = 8
HD = 64
TOPK = 256
FFN = 4096
PLE = 256
EPS = 1e-6
DEPTH = 2
T = 256
NSUB = T // 128
KG = 512
NSLOT = 3
SLOTB = 4224
NBIS = 12
BIGM = 30000.0
NEG = -1.0e30
IN_OFF = dict(q=0, k=512, v=1024, qi=1536, kiwi=2048, xr=2120, gr=2632, zu=3144, zv=3656, gate=4168)


class Sem:
    def __init__(self, h, name):
        self.h = h
        self.n = 0
        self.name = name


class Tile:
    __slots__ = ("w", "r", "name")

    def __init__(self, name=""):
        self.w = {}
        self.r = {}
        self.name = name


class Buf:
    def __init__(self, t, name=""):
        self.t = t
        self.T = Tile(name)

    def __getitem__(self, k):
        return self.t[k]


class Engine:
    def __init__(self, name, sem):
        self.name = name
        self.sem = sem
        self.ops = []
        self.seen = {}


class FW:
    def __init__(self, nc, stack):
        self.nc = nc
        self.stack = stack
        self.engs = {}
        for n in ("tensor", "vector", "scalar", "gpsimd", "sync"):
            s = Sem(stack.enter_context(nc.semaphore("sem_" + n)), n)
            self.engs[n] = Engine(n, s)
        self.nops = 0

    def dsem(self, name):
        return Sem(self.stack.enter_context(self.nc.semaphore("dsem_" + name)), name)

    def op(self, eng, fn, reads=(), writes=(), dsem=None):
        E = self.engs[eng]
        need = {}
        for b in reads:
            t = b.T if isinstance(b, Buf) else b
            for s, v in t.w.items():
                if need.get(s, 0) < v:
                    need[s] = v
        for b in writes:
            t = b.T if isinstance(b, Buf) else b
            for s, v in t.w.items():
                if need.get(s, 0) < v:
                    need[s] = v
            for s, v in t.r.items():
                if need.get(s, 0) < v:
                    need[s] = v
        raw_self = 0
        for b in reads:
            t = b.T if isinstance(b, Buf) else b
            raw_self = max(raw_self, t.w.get(E.sem, 0))
        waits = []
        for s, v in need.items():
            if s is E.sem:
                if eng != "tensor" and raw_self > E.seen.get(s, 0):
                    E.seen[s] = raw_self
                    waits.append((s, raw_self))
                continue
            if E.seen.get(s, 0) >= v:
                continue
            E.seen[s] = v
            waits.append((s, v))
        if dsem is not None:
            dsem.n += 16
            sig = (dsem, dsem.n, 16)
        else:
            E.sem.n += 1
            sig = (E.sem, E.sem.n, 1)
        E.ops.append((waits, fn, sig))
        self.nops += 1
        s, v = sig[0], sig[1]
        for b in reads:
            t = b.T if isinstance(b, Buf) else b
            if t.r.get(s, 0) < v:
                t.r[s] = v
        for b in writes:
            t = b.T if isinstance(b, Buf) else b
            if t.w.get(s, 0) < v:
                t.w[s] = v

    def finish(self, eng, tiles):
        E = self.engs[eng]
        need = {}
        for b in tiles:
            t = b.T if isinstance(b, Buf) else b
            for d in (t.w, t.r):
                for s, v in d.items():
                    if need.get(s, 0) < v:
                        need[s] = v
        E.ops.append(([(s, v) for s, v in need.items() if s is not E.sem], None, None))

    def emit(self):
        with self.nc.Block() as block:
            for n, E in self.engs.items():
                def body(e, E=E):
                    for waits, fn, sig in E.ops:
                        for s, v in waits:
                            e.wait_ge(s.h, v)
                        if fn is None:
                            continue
                        fn(e).then_inc(sig[0].h, sig[2])
                getattr(block, n)(body)


def panel_defs():
    P = []
    for nm in ("q", "k", "qi"):
        P.append((nm, "w_in", 0, 1024, IN_OFF[nm], 512))
    P.append(("kiwi", "w_in", 0, 1024, IN_OFF["kiwi"], 72))
    for nm in ("xr", "gr", "zu", "v", "zv"):
        P.append((nm, "w_in", 0, 1024, IN_OFF[nm], 512))
    for n in range(3):
        for hf in range(2):
            P.append(("gate%d_%d" % (n, hf), "w_in", 0, 1024, IN_OFF["gate"] + n * 1024 + hf * 512, 512))
    for n in range(3):
        for hf in range(2):
            P.append(("br%d_%d" % (n, hf), "w_branch%d" % n, 0, 512, hf * 512, 512))
    for hf in range(2):
        P.append(("out_%d" % hf, "w_out", 0, 1024, hf * 512, 512))
    for g in range(8):
        P.append(("up_%d" % g, "w_ffn_up", 0, 1024, g * 512, 512))
        P.append(("dn_%d" % g, "w_ffn_down", g * 512, 512, 0, 1024))
    P.append(("ple", "w_ple", 0, 256, 0, 1024))
    for hf in range(2):
        P.append(("pg_%d" % hf, "w_ple_gate", 0, 1024, hf * 512, 512))
    return P


def build_program(L, depth=DEPTH, dbg=None):
    NT = L // T
    nc = bass.Bass("TRN2", target_bir_lowering=False)
    dram = lambda name, shape, dt, kind="ExternalInput": nc.dram_tensor(name, shape, dt, kind=kind).ap()
    x_in = dram("x", [L, D], F32)
    p_in = dram("p", [depth, L, PLE], F32)
    pos_in = dram("pos", [128, L], I32)
    wsrc = {
        "w_in": dram("w_in", [depth, D, 7240], F32),
        "w_branch": dram("w_branch", [depth, 3, 512, D], F32),
        "w_out": dram("w_out", [depth, D, D], F32),
        "w_ffn_up": dram("w_ffn_up", [depth, D, FFN], F32),
        "w_ffn_down": dram("w_ffn_down", [depth, FFN, D], F32),
        "w_ple": dram("w_ple", [depth, PLE, D], F32),
        "w_ple_gate": dram("w_ple_gate", [depth, D, D], F32),
    }
    cols_in = dram("cols", [depth, 128, 48], F32)
    gb_in = dram("gb", [depth, 128, 3 * D + 512], F32)
    wbd_in = dram("wbd", [depth, 128, 8, 128], F32)
    wsp_in = dram("wsp", [depth, 128, 8, 128], F32)
    bsp_in = dram("bsp", [depth, 8, 128], F32)
    cb_in = dram("cb", [128, 256 + 512], BF16)
    cf_in = dram("cf", [128, 4 * 128 + 1], F32)
    y_out = dram("y", [L, D], F32, kind="ExternalOutput")
    xbuf = dram("xbuf", [L, D], F32, kind="Internal")
    KTc = [dram("ktc%d" % l, [4, 128, L], BF16, kind="Internal") for l in range(depth)]
    Vc = [dram("vc%d" % l, [L, 520], BF16, kind="Internal") for l in range(depth)]
    pdefs = panel_defs()
    Wp = [{nm: dram("wp%d_%s" % (l, nm), [nr, ncol], BF16, kind="Internal") for (nm, _, _, nr, _, ncol) in pdefs}
          for l in range(depth)]
    dbg_out = {}
    if dbg:
        for k, shp in dbg.items():
            dbg_out[k] = dram("dbg_" + k, list(shp), F32, kind="ExternalOutput")

    with ExitStack() as st:
        fw = FW(nc, st)
        op = fw.op

        def sb(name, shape, dt):
            return Buf(st.enter_context(nc.sbuf_tensor("s_" + name, shape, dt)), name)

        PS = [Buf(st.enter_context(nc.psum_tensor("ps%d" % i, [128, 512], F32)), "ps%d" % i) for i in range(8)]

        def mm(out, lhsT, rhs, start, stop, R, W):
            op("tensor", lambda e: e.matmul(out, lhsT=lhsT, rhs=rhs, start=start, stop=stop), R, W)

        def tr(out, in_, ident, R, W):
            op("tensor", lambda e: e.transpose(out=out, in_=in_, identity=ident), R, W)

        def act(out, in_, func, R, W, **kw):
            op("scalar", lambda e: e.activation(out=out, in_=in_, func=func, **kw), R, W)

        def ts(out, in0, s1, s2, op0, op1, R, W, eng="vector"):
            if op1 is None:
                op(eng, lambda e: e.tensor_scalar(out=out, in0=in0, scalar1=s1, scalar2=None, op0=op0), R, W)
            else:
                op(eng, lambda e: e.tensor_scalar(out=out, in0=in0, scalar1=s1, scalar2=s2, op0=op0, op1=op1), R, W)

        def tt(out, in0, in1, o, R, W, eng="vector"):
            op(eng, lambda e: e.tensor_tensor(out=out, in0=in0, in1=in1, op=o), R, W)

        def stt(out, in0, s, in1, op0, op1, R, W):
            op("vector", lambda e: e.scalar_tensor_tensor(out=out, in0=in0, scalar=s, in1=in1, op0=op0, op1=op1), R, W)

        def cp(out, in_, R, W, eng="vector"):
            op(eng, lambda e: e.tensor_copy(out=out, in_=in_), R, W)

        def dma(out, in_, R, W, ds, eng="sync"):
            op(eng, lambda e: e.dma_start(out=out, in_=in_), R, W, dsem=ds)

        def memset(ap, val, W, eng="vector"):
            op(eng, lambda e: e.memset(ap, val), [], W)

        def reduce(out, in_, o, R, W):
            op("vector", lambda e: e.tensor_reduce(out=out, in_=in_, axis=AX.X, op=o), R, W)

        def recip(out, in_, R, W):
            op("vector", lambda e: e.reciprocal(out=out, in_=in_), R, W)

        def scan(out, d0, d1, init, R, W):
            op("vector", lambda e: e.tensor_tensor_scan(out=out, data0=d0, data1=d1, initial=init,
                                                        op0=ALU.mult, op1=ALU.add), R, W)

        def count_ge(out, in0, thr, cnt, R, W):
            op("vector", lambda e: e.tensor_scalar(out=out, in0=in0, scalar1=thr, scalar2=None, op0=ALU.is_ge,
                                                   op1=ALU.add, accum_out=cnt), R, W)

        def cpred(out, mask, data, R, W):
            op("vector", lambda e: e.copy_predicated(out=out, mask=mask, data=data), R, W)

        dbg_sem = fw.dsem("dbg")
        dbg_tiles = []

        def dump(name, ap, R):
            if name in dbg_out:
                t = Tile("dbg")
                dma(dbg_out[name], ap, R, [t], dbg_sem, eng="gpsimd")
                dbg_tiles.append(t)
                del dbg_out[name]

        cb = sb("cb", [128, 768], BF16)
        cf = sb("cf", [128, 513], F32)
        dma(cb[:], cb_in, [], [cb], fw.dsem("c0"))
        dma(cf[:], cf_in, [], [cf], fw.dsem("c1"))
        identb = cb[:, 0:128]
        Rm = cb[:, 128:256]
        esel = cb[0:8, 256:768].rearrange("p (c f) -> p c f", c=4)
        identf = cf[:, 0:128]
        negmask = cf[:, 128:256]
        posfill = cf[:, 256:384]
        tril01 = cf[:, 384:512]
        invf = cf[:, 512:513]

        WT = [dict() for _ in range(depth)]
        for l in range(depth):
            wcs = fw.dsem("wcast%d" % l)
            for (nm, src, r0, nr, c0, ncol) in pdefs:
                if src.startswith("w_branch"):
                    s_ap = wsrc["w_branch"][l, int(src[-1]), r0:r0 + nr, c0:c0 + ncol]
                else:
                    s_ap = wsrc[src][l, r0:r0 + nr, c0:c0 + ncol]
                t = Tile("wp")
                WT[l][nm] = t
                step = 512
                for rr in range(0, nr, step):
                    n2 = min(step, nr - rr)
                    dma(Wp[l][nm][rr:rr + n2, :], s_ap[rr:rr + n2, :], [], [t], wcs, eng="gpsimd")
            for t in WT[l].values():
                t.w = {wcs: wcs.n}

        ring = [sb("ring%d" % i, [128, SLOTB], BF16) for i in range(NSLOT)]
        ring_sem = [fw.dsem("ring%d" % i) for i in range(NSLOT)]
        kiT = sb("kiT", [128, L], BF16)
        xts = [sb("xt%d" % i, [128, NSUB, D], F32) for i in range(2)]
        xt = xts[0]
        pt = sb("pt", [128, NSUB, PLE], F32)
        ptb = sb("ptb", [128, NSUB, PLE], BF16)
        posi = sb("posi", [128, T], I32)
        hT = sb("hT", [128, 8, T], BF16)
        hTb = sb("hTb", [128, 8, T], BF16)
        pT = sb("pT", [128, 2, T], BF16)
        qT = sb("qT", [128, 4, 2, T], BF16)
        qiT = sb("qiT", [128, 4, T], BF16)
        KTs = sb("KTs", [128, 4, T], BF16)
        Vs = sb("Vs", [128, NSUB, 520], BF16)
        cosT = sb("cosT", [128, T], F32)
        sinT = sb("sinT", [128, T], F32)
        rtmp = sb("rtmp", [128, 3, T], F32)
        xb16 = sb("xb16", [128, T], BF16)
        xrbuf = sb("xrbuf", [128, 4, 3 + T], F32)
        grT = sb("grT", [128, 4, T], BF16)
        zuT = sb("zuT", [128, 4, T], BF16)
        vn = sb("vn", [128, NSUB, 512], BF16)
        hst = sb("hst", [128, 4], F32)
        ybT = sb("ybT", [128, 4, T], BF16)
        ycT = sb("ycT", [128, 4, T], BF16)
        yaT = sb("yaT", [128, 4, T], BF16)
        yaTt = sb("yaTt", [64, T], BF16)
        wis = sb("wis", [128, NSUB, 8], F32)
        diag = sb("diag", [128, 8, 128], BF16)
        small = sb("small", [128, 32], F32)
        smalli = sb("smalli", [128, 4], I32)
        bis = sb("bis", [128, 16], F32)
        bis2 = sb("bis2", [128, 16], F32)
        Tmid = Tile("mid")
        Tcnt = Tile("cnt")
        Tsa = Tile("sa")
        big = sb("big", [128, 6144], F32)
        TS_ = big.T
        TR_ = Tile("bigR")
        score_ap = big[:, 0:L]
        Rrelu = big[:, 4096:6144].bitcast(BF16).rearrange("p (h w) -> p h w", h=8)
        masks = [sb("mask%d" % s, [128, L], BF16) for s in range(NSUB)]
        maskTk = sb("maskTk", [128, 4, T], BF16)
        PT = [sb("PT%d" % i, [128, 2, T], BF16) for i in range(4)]
        xs = sb("xs", [128, D], BF16)
        accm = sb("accm", [128, 8 * T], F32)

        def view(ap, tile):
            v = Buf.__new__(Buf)
            v.t = ap
            v.T = tile
            return v

        acc = view(accm[0:65, :].rearrange("p (h t) -> p h t", h=8), accm.T)
        lt = view(accm[:, 0:5 * T].rearrange("p (k t) -> p k t", k=5), accm.T)
        gz = view(accm[:, 5 * T:5 * T + 512], accm.T)
        xcb = view(accm[:, 5 * T + 512:5 * T + 512 + T // 2].bitcast(BF16), accm.T)
        sc1 = sb("sc1", [128, L], F32)
        rhl2 = [sb("rhl%d" % i, [65, 2, T], BF16) for i in range(2)]
        onesb = sb("onesb", [65, 64], BF16)
        gt = sb("gt", [128, T], F32)
        tmpf = sb("tmpf", [128, 512], F32)
        gsig = sb("gsig", [128, 8, T], BF16)
        merged = big[:, 0:8 * T].rearrange("p (c t) -> p c t", c=8)
        o = 8 * T
        mergedT = big[:, o:o + 4 * T].bitcast(BF16).rearrange("p (c t) -> p c t", c=8)
        o += 4 * T
        fT = [big[:, o + i * 2 * T:o + (i + 1) * 2 * T].bitcast(BF16).rearrange("p (c t) -> p c t", c=4) for i in range(2)]
        o += 4 * T
        assert o <= 4096
        o = 4096
        rl = [big[:, o + i * (T // 2):o + (i + 1) * (T // 2)].bitcast(BF16) for i in range(2)]
        o += T
        sg = big[:, o:o + 512]
        o += 512
        ple_t = big[:, o:o + 1024]
        o += 1024
        assert o <= 6144
        wbdf = big[:, 4096:4096 + 1024].rearrange("p (c j) -> p c j", c=8)
        cols = sb("cols", [128, 48], F32)
        gbv = sb("gbv", [128, 512], F32)
        gcur = sb("gcur", [128, D], F32)
        gsem = fw.dsem("gain")
        wbd = sb("wbd", [128, 8, 128], BF16)
        wspT = sb("wspT", [128, 8, 128], BF16)
        bsp = sb("bsp", [8, 128], F32)
        bsph = sb("bsph", [8, 2, 128], BF16)
        c8 = sb("c8", [128, 8], F32)
        epsc = sb("epsc", [128, 4], F32)
        psem = [fw.dsem("par%d" % i) for i in range(5)]
        xsems = [fw.dsem("xload%d" % i) for i in range(2)]
        ptsem = fw.dsem("pload")
        possem = fw.dsem("posload")
        ssem = fw.dsem("store")
        kvsem = fw.dsem("kvstore")
        out_tile = Tile("out")

        class Stream:
            def __init__(self):
                self.plan = []
                self.issued = 0
                self.pos = 0

            def add(self, name, loader, deps, hoist=True):
                self.plan.append((name, loader, deps, hoist))

            def next(self, name, look=NSLOT - 1):
                i = self.pos
                assert self.plan[i][0] == name, (self.plan[i][0], name)
                while self.issued < len(self.plan) and (
                        self.issued <= i or (self.issued <= i + look and self.plan[self.issued][3])):
                    k = self.issued
                    _, loader, deps, _ = self.plan[k]
                    loader(ring[k % NSLOT], ring_sem[k % NSLOT], deps)
                    self.issued += 1
                self.pos += 1
                return ring[i % NSLOT]

        stream = Stream()

        def wloader(l, nm, nr, ncol):
            kc = nr // 128

            def f(slot, sem, deps):
                dst = slot[:, 0:kc * ncol].rearrange("p (k w) -> p k w", k=kc)
                src = Wp[l][nm].rearrange("(k p) w -> p k w", p=128)
                dma(dst, src, deps, [slot], sem)
            return f

        KVT = [[Tile("kv") for _ in range((L + KG - 1) // KG)] for _ in range(depth)]

        def kvloader(l, g, wd):
            def f(slot, sem, deps):
                dstk = slot[:, 0:4 * wd].rearrange("p (c w) -> p c w", c=4)
                dma(dstk, KTc[l][:, :, g * KG:g * KG + wd].rearrange("c p w -> p c w"), deps, [slot], sem)
                nb = wd // 128
                dstv = slot[:, 2048:2048 + nb * 520].rearrange("p (b f) -> p b f", b=nb)
                dma(dstv, Vc[l][g * KG:g * KG + wd, :].rearrange("(b p) f -> p b f", p=128), deps, [slot], sem)
            return f

        pinfo = {nm: (nr, ncol) for (nm, _, _, nr, _, ncol) in pdefs}

        def plan_w(l, nm):
            nr, ncol = pinfo[nm]
            stream.add("%d_%s" % (l, nm), wloader(l, nm, nr, ncol), [WT[l][nm]])

        def kv_groups(j):
            nkeys = (j + 1) * T
            out = []
            g = 0
            while g * KG < nkeys:
                out.append((g, min(KG, nkeys - g * KG)))
                g += 1
            return out

        for l in range(depth):
            for j in range(NT):
                for nm in ("q", "k", "qi", "kiwi", "xr", "gr", "zu", "v", "zv"):
                    plan_w(l, nm)
                for n in (2, 1):
                    plan_w(l, "gate%d_0" % n)
                    plan_w(l, "gate%d_1" % n)
                    plan_w(l, "br%d_0" % n)
                    plan_w(l, "br%d_1" % n)
                plan_w(l, "gate0_0")
                plan_w(l, "gate0_1")
                grps = kv_groups(j)
                for (g, wd) in grps:
                    last = (g == grps[-1][0])
                    stream.add("%d_kv%d_%d" % (l, j, g), kvloader(l, g, wd), [KVT[l][g]], hoist=not last)
                plan_w(l, "br0_0")
                plan_w(l, "br0_1")
                plan_w(l, "out_0")
                plan_w(l, "out_1")
                plan_w(l, "up_0")
                for g in range(8):
                    if g + 1 < 8:
                        plan_w(l, "up_%d" % (g + 1))
                    plan_w(l, "dn_%d" % g)
                plan_w(l, "ple")
                plan_w(l, "pg_0")
                plan_w(l, "pg_1")

        def wview(slot, nm):
            nr, ncol = pinfo[nm]
            kc = nr // 128
            return slot[:, 0:kc * ncol].rearrange("p (k w) -> p k w", k=kc)

        sm_i = [0]

        def sm():
            i = sm_i[0] % 32
            sm_i[0] += 1
            return small[:, i:i + 1]

        memset(epsc[:, 0:1], EPS, [epsc])
        memset(epsc[:, 1:2], math.pi / 2, [epsc])
        memset(epsc[:, 2:3], 1.0, [epsc])
        memset(epsc[:, 3:4], -BIGM, [epsc])
        memset(Vs[:, :, :], 1.0, [Vs])
        memset(onesb[:, :], 1.0, [onesb])
        memset(qT[:, :, :, :], 0.0, [qT])

        def rstd_from_ss(ss_ap, n):
            a = sm()
            b = sm()
            act(a, ss_ap, AF.Sqrt, [small, epsc], [small], scale=1.0 / n, bias=epsc[:, 0:1])
            recip(b, a, [small], [small])
            return b

        pi = [0]

        def nps():
            pi[0] += 1
            return PS[pi[0] % 4]

        def norm_transpose(gcol0, hT, xt):
            for s in range(NSUB):
                if gcol0 is not None:
                    ss = sm()
                    act(tmpf[:, 0:512], xt[:, s, 0:512], AF.Square, [xt, small], [tmpf, small], accum_out=ss)
                    ss2 = sm()
                    act(tmpf[:, 0:512], xt[:, s, 512:1024], AF.Square, [xt, small], [tmpf, small], accum_out=ss2)
                    ss3 = sm()
                    tt(ss3, ss, ss2, ALU.add, [small], [small])
                    rs = rstd_from_ss(ss3, D)
                    ts(xs[:], xt[:, s, :], rs, None, ALU.mult, None, [xt, small], [xs])
                else:
                    cp(xs[:], xt[:, s, :], [xt], [xs])
                psb = PS[7][:].bitcast(BF16)
                for c in range(8):
                    tr(psb[:, c * 128:(c + 1) * 128], xs[:, c * 128:(c + 1) * 128], identb, [xs, cb], [PS[7]])
                src = psb[:, 0:1024].rearrange("p (c t) -> p c t", c=8)
                dst = hT[:, :, s * 128:(s + 1) * 128]
                if gcol0 is not None:
                    g_ap = cols[:, gcol0:gcol0 + 8].unsqueeze(2).to_broadcast([128, 8, 128])
                    tt(dst, src, g_ap, ALU.mult, [PS[7], cols], [hT])
                else:
                    cp(dst, src, [PS[7]], [hT])

        def post_norm_residual(banks, gcol, s):
            ssa = []
            for hf in range(2):
                a = sm()
                act(tmpf[:, 0:512], banks[hf][:, 0:512], AF.Square, [banks[hf], small], [tmpf, small], accum_out=a)
                ssa.append(a)
            s3 = sm()
            tt(s3, ssa[0], ssa[1], ALU.add, [small], [small])
            rs = rstd_from_ss(s3, D)
            for hf in range(2):
                stt(tmpf[:, 0:512], banks[hf][:, 0:512], rs, gcur[:, hf * 512:(hf + 1) * 512],
                    ALU.mult, ALU.mult, [banks[hf], small, gcur], [tmpf])
                tt(xt[:, s, hf * 512:(hf + 1) * 512], xt[:, s, hf * 512:(hf + 1) * 512], tmpf[:, 0:512], ALU.add,
                   [xt, tmpf], [xt])

        def merge_tile(dst, src, as_write=False):
            for a, b in ((dst.w, src.w), (dst.r, src.r)):
                for k_, v_ in b.items():
                    if a.get(k_, 0) < v_:
                        a[k_] = v_
            if as_write:
                for k_, v_ in src.r.items():
                    if dst.w.get(k_, 0) < v_:
                        dst.w[k_] = v_

        def fm_chunk(panel, pv, c, ps, M=128, hsrc=None):
            hsrc = hT if hsrc is None else hsrc
            for kc in range(8):
                mm(ps[0:M, 0:T], pv[:, kc, c * 128:c * 128 + M], hsrc[:, kc, :], kc == 0, kc == 7, [panel, hsrc], [ps])

        def rope_evac(ps, dst, W, split=None):
            act(xb16[:], ps[:, 0:T], AF.Copy, [ps], [xb16])
            mm(ps[:, T:2 * T], Rm, xb16[:], True, True, [cb, xb16], [ps])
            tt(rtmp[:, 0, :], ps[:, 0:T], cosT[:], ALU.mult, [ps, cosT], [rtmp])
            tt(rtmp[:, 1, :], ps[:, T:2 * T], sinT[:], ALU.mult, [ps, sinT], [rtmp])
            if split is None:
                tt(dst, rtmp[:, 0, :], rtmp[:, 1, :], ALU.add, [rtmp], W)
            else:
                for hh in range(2):
                    tt(split[hh * 64:(hh + 1) * 64, hh, :], rtmp[hh * 64:(hh + 1) * 64, 0, :], rtmp[hh * 64:(hh + 1) * 64, 1, :],
                       ALU.add, [rtmp], W)

        XTprev = None
        for l in range(depth):
            src_x = x_in if l == 0 else xbuf
            dst_x = y_out if l == depth - 1 else xbuf
            XT = [Tile("xd") for _ in range(NT)] if l < depth - 1 else None
            dma(cols[:], cols_in[l], [], [cols], psem[0])
            dma(gbv[:], gb_in[l, :, 3 * D:3 * D + 512], [], [gbv], psem[1])
            dma(gcur[:], gb_in[l, :, 0:D], [], [gcur], gsem)
            dma(bsp[:], bsp_in[l], [], [bsp], psem[2])
            cp(bsph[:, 0, :], bsp[:], [bsp], [bsph])
            tt(bsp[:], bsp[:], bsph[:, 0, :], ALU.subtract, [bsph], [bsp])
            cp(bsph[:, 1, :], bsp[:], [bsp], [bsph])
            dma(wbdf, wbd_in[l], [], [TR_], psem[3])
            cp(wbd[:], wbdf, [TR_], [wbd])
            dma(wbdf, wsp_in[l], [], [TR_], psem[4])
            tt(wspT[:], wbdf, tril01.unsqueeze(1).to_broadcast([128, 8, 128]), ALU.mult, [TR_, cf], [wspT])
            act(c8[:, 4:8], cols[:, 44:48], AF.Exp, [cols], [c8], scale=-1.0)
            ts(c8[:, 0:4], c8[:, 4:8], -0.25, 1.0 / 3.0, ALU.mult, ALU.add, [c8], [c8])
            tt(c8[:, 0:4], c8[:, 0:4], c8[:, 4:8], ALU.mult, [c8], [c8])
            ts(c8[:, 0:4], c8[:, 0:4], -1.0, 0.5, ALU.mult, ALU.add, [c8], [c8])
            tt(c8[:, 0:4], c8[:, 0:4], c8[:, 4:8], ALU.mult, [c8], [c8])
            ts(c8[:, 0:4], c8[:, 0:4], -1.0, 1.0, ALU.mult, ALU.add, [c8], [c8])
            tt(c8[:, 0:4], c8[:, 0:4], c8[:, 4:8], ALU.mult, [c8], [c8])
            ts(c8[:, 0:4], c8[:, 0:4], -8.0, None, ALU.mult, None, [c8], [c8])
            ts(c8[:, 4:8], c8[:, 0:4], 2.0, None, ALU.mult, None, [c8], [c8])
            memset(xrbuf[:, :, 0:3], 0.0, [xrbuf])
            memset(hst[:], 0.0, [hst])

            for j in range(NT):
                t0 = j * T
                first_tile = (l == 0 and j == 0)
                def load_x(jj):
                    rd_ = [XTprev[jj]] if (l > 0) else []
                    dma(xts[jj % 2][:], src_x[jj * T:(jj + 1) * T, :].rearrange("(s p) d -> p s d", p=128), rd_, [xts[jj % 2]], xsems[jj % 2])

                def prologue(jj):
                    t0 = jj * T
                    xt = xts[jj % 2]
                    dma(pt[:], p_in[l, t0:t0 + T, :].rearrange("(s p) d -> p s d", p=128), [], [pt], ptsem)
                    dma(posi[:], pos_in[:, t0:t0 + T], [], [posi], possem)
                    ang = rtmp[:, 0, :]
                    u = rtmp[:, 1, :]
                    rr = rtmp[:, 2, :]
                    cp(u, posi[:], [posi], [rtmp])
                    ts(ang, u, invf, None, ALU.mult, None, [rtmp, cf], [rtmp])
                    ts(u, ang, 1.0 / (2 * math.pi), 12582912.0, ALU.mult, ALU.add, [rtmp], [rtmp])
                    ts(u, u, -12582912.0, None, ALU.add, None, [rtmp], [rtmp])
                    C1 = 6.28125
                    C2 = float(np.float32(2 * math.pi - C1))
                    C3 = float(2 * math.pi - C1 - C2)
                    stt(rr, u, -C1, ang, ALU.mult, ALU.add, [rtmp], [rtmp])
                    stt(rr, u, -C2, rr, ALU.mult, ALU.add, [rtmp], [rtmp])
                    stt(rr, u, -C3, rr, ALU.mult, ALU.add, [rtmp], [rtmp])
                    act(sinT[:], rr, AF.Sin, [rtmp], [sinT])
                    stt(u, rr, -1.0, rr, ALU.mult, ALU.max, [rtmp], [rtmp])
                    act(cosT[:], u, AF.Sin, [rtmp, epsc], [cosT], scale=-1.0, bias=epsc[:, 1:2])
                    norm_transpose(0, hT, xt)

                xt = xts[j % 2]
                if j == 0:
                    load_x(0)
                    prologue(0)
                if j + 1 < NT:
                    load_x(j + 1)
                if first_tile:
                    dump("hT", hT[:, 0, :], [hT])
                    dump("cosT", cosT[:], [cosT])
                    dump("sinT", sinT[:], [sinT])

                for nm, dstb in (("q", qT), ("k", KTs), ("qi", qiT)):
                    panel = stream.next("%d_%s" % (l, nm))
                    pv = wview(panel, nm)
                    pss = [nps() for _ in range(4)]
                    fm_chunk(panel, pv, 0, pss[0])
                    for c in range(4):
                        if c + 1 < 4:
                            fm_chunk(panel, pv, c + 1, pss[c + 1])
                        if nm == "q":
                            rope_evac(pss[c], None, [dstb], split=qT[:, c, :, :])
                        else:
                            rope_evac(pss[c], dstb[:, c, :], [dstb])
                panel = stream.next("%d_kiwi" % l)
                pv = wview(panel, "kiwi")
                ps = nps()
                for half in range(2):
                    for kc in range(8):
                        mm(ps[half * 64:(half + 1) * 64, 0:T], pv[:, kc, 0:64], hT[:, kc, :], kc == 0, kc == 7,
                           [panel, hT], [ps])
                rope_evac(ps, kiT[:, t0:t0 + T], [kiT])
                for s in range(NSUB):
                    ps = nps()
                    for kc in range(8):
                        mm(ps[:, 0:8], hT[:, kc, s * 128:(s + 1) * 128], pv[:, kc, 64:72], kc == 0, kc == 7,
                           [panel, hT], [ps])
                    cp(wis[:, s, :], ps[:, 0:8], [ps], [wis])
                dma(KTc[l][:, :, t0:t0 + T].rearrange("c p w -> p c w"), KTs[:], [KTs], [KVT[l][t0 // KG]], kvsem, eng="gpsimd")
                if first_tile:
                    dump("qT", qT[:, 0, 0, :], [qT])
                    dump("kiT", kiT[:, 0:T], [kiT])
                    dump("wis", wis[:, 0, :], [wis])
                panel = stream.next("%d_xr" % l)
                pv = wview(panel, "xr")
                for c in range(4):
                    ps = nps()
                    fm_chunk(panel, pv, c, ps)
                    act(xrbuf[:, c, 3:3 + T], ps[:, 0:T], AF.Copy, [ps], [xrbuf])
                for nm, dstb in (("gr", grT), ("zu", zuT)):
                    panel = stream.next("%d_%s" % (l, nm))
                    pv = wview(panel, nm)
                    for c in range(4):
                        ps = nps()
                        fm_chunk(panel, pv, c, ps)
                        act(dstb[:, c, :], ps[:, 0:T], AF.Gelu_apprx_tanh, [ps], [dstb])
                panel = stream.next("%d_v" % l)
                pv = wview(panel, "v")
                for s in range(NSUB):
                    ps = nps()
                    for kc in range(8):
                        mm(ps[:, 0:512], hT[:, kc, s * 128:(s + 1) * 128], pv[:, kc, :], kc == 0, kc == 7, [panel, hT], [ps])
                    dstv = Vs[:, s, :].rearrange("p (h f) -> p h f", h=8)[:, :, 0:64]
                    act(dstv, ps[:, 0:512].rearrange("p (h f) -> p h f", h=8), AF.Copy, [ps], [Vs])
                dma(Vc[l][t0:t0 + T, :].rearrange("(s p) f -> p s f", p=128), Vs[:], [Vs], [KVT[l][t0 // KG]], kvsem, eng="gpsimd")
                panel = stream.next("%d_zv" % l)
                pv = wview(panel, "zv")
                for s in range(NSUB):
                    ps = nps()
                    for kc in range(8):
                        mm(ps[:, 0:512], hT[:, kc, s * 128:(s + 1) * 128], pv[:, kc, :], kc == 0, kc == 7, [panel, hT], [ps])
                    act(gz[:], ps[:, 0:512], AF.Gelu_apprx_tanh, [ps], [gz])
                    ss = sm()
                    act(tmpf[:, 0:512], gz[:], AF.Square, [gz, small], [tmpf, small], accum_out=ss)
                    rs = rstd_from_ss(ss, 512)
                    stt(vn[:, s, :], gz[:], rs, gbv[:], ALU.mult, ALU.mult, [gz, small, gbv], [vn])

                mpi = [0]

                def mps():
                    mpi[0] += 1
                    return PS[5 + mpi[0] % 3]

                def gen_mixC():
                    for s in range(NSUB):
                        for cpair in range(4):
                            ps = mps()
                            for gg in range(2):
                                g = cpair * 2 + gg
                                mm(ps[gg * 64:(gg + 1) * 64, 0:128], vn[:, s, g * 64:(g + 1) * 64], wspT[:, g, :], True, False,
                                   [vn, wspT], [ps])
                            mm(ps[:, 0:128], esel[:, cpair, :], bsph[:, 0, :], False, False, [cb, bsph], [ps])
                            mm(ps[:, 0:128], esel[:, cpair, :], bsph[:, 1, :], False, True, [cb, bsph], [ps])
                            tt(ycT[:, cpair, s * 128:(s + 1) * 128], ps[:, 0:128], zuT[:, cpair, s * 128:(s + 1) * 128], ALU.mult,
                               [ps, zuT], [ycT])
                            yield

                def gen_mixB():
                    for c in range(4):
                        xc = lt[:, 0, :]
                        ts(xc, xrbuf[:, c, 0:T], cols[:, 16 + c:17 + c], cols[:, 32 + c:33 + c], ALU.mult, ALU.add, [xrbuf, cols], [lt])
                        for jj in range(1, 4):
                            stt(xc, xrbuf[:, c, jj:jj + T], cols[:, 16 + jj * 4 + c:17 + jj * 4 + c], xc, ALU.mult, ALU.add,
                                [xrbuf, cols, lt], [lt])
                        yield
                        cp(xrbuf[:, c, 0:3], xrbuf[:, c, T:T + 3], [xrbuf], [xrbuf])
                        act(xcb[:], xc, AF.Copy, [lt], [xcb])
                        ps = mps()
                        mm(ps[:, 0:T], wbd[:, c, :], xcb[:], True, True, [wbd, xcb], [ps])
                        mm(ps[:, T:2 * T], wbd[:, 4 + c, :], xcb[:], True, True, [wbd, xcb], [ps])
                        yield
                        rg = lt[:, 1, :]
                        ig = lt[:, 2, :]
                        av = lt[:, 3, :]
                        sq = lt[:, 4, :]
                        act(rg, ps[:, 0:T], AF.Sigmoid, [ps, cols], [lt], bias=cols[:, 36 + c:37 + c])
                        act(ig, ps[:, T:2 * T], AF.Sigmoid, [ps, cols], [lt], bias=cols[:, 40 + c:41 + c])
                        yield
                        act(av, rg, AF.Exp, [lt, c8], [lt], scale=c8[:, c:c + 1])
                        act(sq, rg, AF.Exp, [lt, c8], [lt], scale=c8[:, 4 + c:5 + c])
                        act(sq, sq, AF.Sqrt, [lt, epsc], [lt], scale=-1.0, bias=epsc[:, 2:3])
                        yield
                        tt(ig, ig, xc, ALU.mult, [lt], [lt])
                        tt(ig, ig, sq, ALU.mult, [lt], [lt])
                        hh = lt[:, 1, :]
                        scan(hh, av, ig, hst[:, c:c + 1], [lt, hst], [lt])
                        yield
                        cp(hst[:, c:c + 1], hh[:, T - 1:T], [lt], [hst])
                        tt(ybT[:, c, :], hh, grT[:, c, :], ALU.mult, [lt, grT], [ybT])
                        yield

                grps = kv_groups(j)
                nkeys = (j + 1) * T
                TRh = [Tile("relu%d" % h) for h in range(8)]

                SC = [(score_ap, TS_), (sc1[:, 0:L], sc1.T)]

                def gen_I(s):
                    N = t0 + 128 * (s + 1)
                    score_s, TSs = SC[s]
                    for h in range(8):
                        merge_tile(TRh[h], TR_, as_write=True)
                        act(diag[:, h, :], identf, AF.Copy, [cf, wis], [diag], scale=wis[:, s, h:h + 1])
                    yield
                    ngrp = (N + KG - 1) // KG
                    for kg in range(ngrp):
                        wd = min(KG, N - kg * KG)
                        for h in range(8):
                            ps = PS[h % 4]
                            r0 = (h % 2) * 64
                            mm(ps[:, 0:wd], qiT[r0:r0 + 64, h // 2, s * 128:(s + 1) * 128],
                               kiT[r0:r0 + 64, kg * KG:kg * KG + wd], True, True, [qiT, kiT], [ps])
                            if h % 2 == 0:
                                act(Rrelu[:, h, 0:wd], ps[:, 0:wd], AF.Relu, [ps], [TRh[h]])
                            else:
                                ts(Rrelu[:, h, 0:wd], ps[:, 0:wd], 0.0, None, ALU.max, None, [ps], [TRh[h]])
                            if h % 2 == 1:
                                yield
                        for h in range(8):
                            mm(PS[4][:, 0:wd], diag[:, h, :], Rrelu[:, h, 0:wd], h == 0, h == 7, [diag, TRh[h]], [PS[4]])
                        if kg == ngrp - 1:
                            if wd > 128:
                                cp(score_s[:, kg * KG:kg * KG + wd - 128], PS[4][:, 0:wd - 128], [PS[4]], [TSs])
                            tt(score_s[:, N - 128:N], PS[4][:, wd - 128:wd], negmask, ALU.add, [PS[4], cf], [TSs])
                            tt(tmpf[:, 0:128], PS[4][:, wd - 128:wd], posfill, ALU.add, [PS[4], cf], [tmpf])
                        else:
                            cp(score_s[:, kg * KG:kg * KG + wd], PS[4][:, 0:wd], [PS[4]], [TSs])
                        yield
                    for h in range(8):
                        merge_tile(TR_, TRh[h])

                def gen_B(s):
                    N = t0 + 128 * (s + 1)
                    score_s, TSs = SC[s]
                    hi0 = bis[:, 0:1]
                    lo = bis[:, 1:2]
                    w0 = bis[:, 2:3]
                    reduce(hi0, score_s[:, 0:N], ALU.max, [TSs, bis], [bis])
                    reduce(lo, tmpf[:, 0:128], ALU.min, [tmpf, bis], [bis])
                    if N > 128:
                        m1 = bis[:, 3:4]
                        reduce(m1, score_s[:, 0:N - 128], ALU.min, [TSs, bis], [bis])
                        tt(lo, lo, m1, ALU.min, [bis], [bis])
                    tt(w0, hi0, lo, ALU.subtract, [bis], [bis])
                    yield
                    mk = masks[s]
                    if N > TOPK:
                        Nd = max(128, (int(N * 0.42) // 128) * 128)
                        Na = N - Nd
                        if s == 0:
                            junkA, TJ = masks[1], masks[1].T
                        else:
                            junkA, TJ = big[:, 4096:6144].bitcast(BF16), TR_
                        for it in range(NBIS):
                            k4 = it % 4
                            mid = bis2[:, k4:k4 + 1]
                            cnt = bis2[:, 4 + k4:5 + k4]
                            vv = bis2[:, 8 + k4:9 + k4]
                            sA = bis2[:, 12 + k4:13 + k4]
                            stt(mid, w0, 0.5 ** (it + 1), lo, ALU.mult, ALU.add, [bis], [Tmid])
                            act(junkA[:, 0:Na], score_s[:, Nd:N], AF.Sign, [TSs, Tmid], [TJ, Tsa], scale=-1.0, bias=mid,
                                accum_out=sA)
                            count_ge(mk[:, 0:Nd], score_s[:, 0:Nd], mid, cnt, [TSs, Tmid], [mk, Tcnt])
                            stt(vv, cnt, 2.0, sA, ALU.mult, ALU.subtract, [Tcnt, Tsa], [Tcnt])
                            ge = smalli[:, k4:k4 + 1]
                            ts(ge, vv, float(2 * TOPK - Na), None, ALU.is_ge, None, [Tcnt], [smalli])
                            cpred(lo, ge, mid, [Tmid, smalli], [bis])
                            yield
                    ts(mk[:, 0:N], score_s[:, 0:N], lo, None, ALU.is_ge, None, [TSs, bis], [mk])
                    if N < nkeys:
                        memset(mk[:, N:nkeys], 0.0, [mk], eng="gpsimd")
                    yield

                def gen_G(items):
                    for ni, n, do_gate, do_branch in items:
                        if do_gate:
                            for hf in range(2):
                                gpan = stream.next("%d_gate%d_%d" % (l, n, hf))
                                pv = wview(gpan, "gate0_0")
                                for c in range(4):
                                    ps = nps()
                                    fm_chunk(gpan, pv, c, ps)
                                    act(gsig[:, hf * 4 + c, :], ps[:, 0:T], AF.Sigmoid, [ps], [gsig])
                                    yield
                        if not do_branch:
                            continue
                        ysrc = {2: ycT, 1: ybT, 0: yaT}[n]
                        for hf in range(2):
                            bpan = stream.next("%d_br%d_%d" % (l, n, hf))
                            pv = wview(bpan, "br0_0")
                            for c in range(4):
                                dchunk = hf * 4 + c
                                ps = nps()
                                for kc in range(4):
                                    mm(ps[:, 0:T], pv[:, kc, c * 128:(c + 1) * 128], ysrc[:, kc, :], kc == 0, kc == 3,
                                       [bpan, ysrc], [ps])
                                if ni == 0:
                                    tt(merged[:, dchunk, :], ps[:, 0:T], gsig[:, dchunk, :], ALU.mult, [ps, gsig], [TS_])
                                else:
                                    tt(gt[:], ps[:, 0:T], gsig[:, dchunk, :], ALU.mult, [ps, gsig], [gt])
                                    dsto = mergedT if ni == 2 else merged
                                    tt(dsto[:, dchunk, :], merged[:, dchunk, :], gt[:], ALU.add, [TS_, gt], [TS_], eng="gpsimd")
                                yield

                def chain(*gens):
                    for g_ in gens:
                        yield from g_

                def run(*gens):
                    live = list(gens)
                    while live:
                        for g_ in list(live):
                            try:
                                next(g_)
                            except StopIteration:
                                live.remove(g_)

                def run_w(ga, gb_, kb):
                    la = lb = True
                    while la or lb:
                        if la:
                            try:
                                next(ga)
                            except StopIteration:
                                la = False
                        for _ in range(kb):
                            if lb:
                                try:
                                    next(gb_)
                                except StopIteration:
                                    lb = False

                run(gen_mixB(), gen_mixC(), gen_I(0))
                if first_tile:
                    dump("ycT", ycT[:, 0, :], [ycT])
                    dump("ybT", ybT[:, 0, :], [ybT])
                    dump("score", score_ap[:, 0:128], [TS_])
                N1 = t0 + 256
                lenI = 1 + ((N1 + KG - 1) // KG) * 5
                lenB = 2 + (NBIS if (t0 + 128) > TOPK else 0)
                run_w(gen_B(0), gen_I(1), max(1, -(-lenI // lenB)))
                lenB1 = 2 + (NBIS if (t0 + 256) > TOPK else 0)
                run_w(gen_B(1), gen_G([(0, 2, True, True), (1, 1, True, True), (2, 0, True, False)]), max(1, -(-40 // lenB1)))

                first = True
                ucnt = [0]
                for gi, (g, wd) in enumerate(grps):
                    panel = stream.next("%d_kv%d_%d" % (l, j, g))
                    nb = wd // 128
                    KTv = panel[:, 0:4 * wd].rearrange("p (c w) -> p c w", c=4)
                    Vv = panel[:, 2048:2048 + nb * 520].rearrange("p (b f) -> p b f", b=nb)
                    psb = PS[6][:].bitcast(BF16)
                    for s in range(NSUB):
                        for b in range(nb):
                            tr(psb[:, (s * 4 + b) * 128:(s * 4 + b + 1) * 128],
                               masks[s][:, g * KG + b * 128:g * KG + (b + 1) * 128], identb, [masks[s], cb], [PS[6]])
                    for s in range(NSUB):
                        act(maskTk[:, 0:nb, s * 128:(s + 1) * 128],
                            psb[:, s * 512:s * 512 + nb * 128].rearrange("p (b t) -> p b t", b=nb),
                            AF.Copy, [PS[6]], [maskTk])
                    units = [(hp, b) for hp in range(4) for b in range(nb)]

                    def stageA(i):
                        hp, b = units[i]
                        ps = PS[(ucnt[0] + i) % 4]
                        mm(ps[:, 0:2 * T], KTv[:, hp, b * 128:(b + 1) * 128], qT[:, hp, :, :].rearrange("p a t -> p (a t)"),
                           True, True, [panel, qT], [ps])

                    def stageB(i):
                        hp, b = units[i]
                        ps = PS[(ucnt[0] + i) % 4]
                        ptile = PT[(ucnt[0] + i) % len(PT)]
                        act(ptile[:, :, :], ps[:, 0:2 * T].rearrange("p (a t) -> p a t", a=2), AF.Exp,
                            [ps], [ptile], scale=HD ** -0.5)
                        tt(ptile[:, :, :], ptile[:, :, :], maskTk[:, b:b + 1, :].to_broadcast([128, 2, T]), ALU.mult,
                           [ptile, maskTk], [ptile], eng=("gpsimd" if (ucnt[0] + i) % 4 == 3 else "vector"))

                    def stageD(i):
                        hp, b = units[i]
                        ptile = PT[(ucnt[0] + i) % len(PT)]
                        for hh in range(2):
                            h = 2 * hp + hh
                            pacc = PS[4 + hh]
                            mm(pacc[0:65, 0:T], Vv[:, b, h * 65:(h + 1) * 65], ptile[:, hh, :], b == 0, b == nb - 1,
                               [panel, ptile], [pacc])
                            if b == nb - 1:
                                if first:
                                    cp(acc[:, h, :], pacc[0:65, 0:T], [pacc], [acc])
                                else:
                                    tt(acc[:, h, :], acc[:, h, :], pacc[0:65, 0:T], ALU.add, [pacc, acc], [acc])

                    for i in range(min(3, len(units))):
                        stageA(i)
                    for i in range(len(units)):
                        if i + 3 < len(units):
                            stageA(i + 3)
                        stageB(i)
                        stageD(i)
                    ucnt[0] += len(units)
                    first = False
                accf = accm[0:65, :]
                act(accf[64:65, :], accf[64:65, :], AF.Ln, [acc], [acc])
                act(accf[64:65, :], accf[64:65, :], AF.Exp, [acc], [acc], scale=-1.0)
                for h in range(8):
                    rhl = rhl2[h % 2]
                    cp(rhl[64:65, 0, :], acc[64:65, h, :], [acc], [rhl])
                    tt(rhl[64:65, 1, :], acc[64:65, h, :], rhl[64:65, 0, :], ALU.subtract, [acc, rhl], [rhl])
                    ps = nps()
                    mm(ps[0:64, 0:T], onesb[64:65, 0:64], rhl[64:65, 0, :], True, False, [onesb, rhl], [ps])
                    mm(ps[0:64, 0:T], onesb[64:65, 0:64], rhl[64:65, 1, :], False, True, [onesb, rhl], [ps])
                    if h % 2 == 0:
                        tt(yaT[0:64, h // 2, :], acc[0:64, h, :], ps[0:64, 0:T], ALU.mult, [acc, ps], [yaT])
                    else:
                        tt(yaTt[:, :], acc[0:64, h, :], ps[0:64, 0:T], ALU.mult, [acc, ps], [yaTt])
                        ps2 = nps()
                        mm(ps2[64:128, 0:T], identb[0:64, 0:64], yaTt[:, :], True, True, [cb, yaTt], [ps2])
                        cp(yaT[64:128, h // 2, :], ps2[64:128, 0:T], [ps2], [yaT])
                if first_tile:
                    dump("yaT", yaT[:, 0, :], [yaT])
                if l == 0 and j == 1:
                    dump("yaT1", yaT[:, 0, :], [yaT])
                    dump("acc1", acc[:, 0, :], [acc])

                run(gen_G([(2, 0, False, True)]))
                op_ = [stream.next("%d_out_%d" % (l, hf), look=NSLOT - 1 - hf) for hf in range(2)]
                for s in range(NSUB):
                    banks = [PS[4 + 2 * (s % 2)], PS[5 + 2 * (s % 2)]]
                    for hf in range(2):
                        pv = wview(op_[hf], "out_0")
                        for kc in range(8):
                            mm(banks[hf][:, 0:512], mergedT[:, kc, s * 128:(s + 1) * 128], pv[:, kc, :], kc == 0, kc == 7,
                               [op_[hf], TS_], [banks[hf]])
                    post_norm_residual(banks, 0, s)
                dma(gcur[:], gb_in[l, :, D:2 * D], [], [gcur], gsem, eng="gpsimd")
                if first_tile:
                    dump("x1", xt[:, 0, :], [xt])
                norm_transpose(8, hTb, xt)
                for s in range(NSUB):
                    cp(ptb[:, s, :], pt[:, s, :], [pt], [ptb])
                    psb = PS[3][:].bitcast(BF16)
                    for c in range(2):
                        tr(psb[:, c * 128:(c + 1) * 128], ptb[:, s, c * 128:(c + 1) * 128], identb, [ptb, cb], [PS[3]])
                    cp(pT[:, :, s * 128:(s + 1) * 128], psb[:, 0:256].rearrange("p (c t) -> p c t", c=2), [PS[3]], [pT])
                dbanks = [[PS[4], PS[5]], [PS[6], PS[7]]]
                TF = [Tile("fT0"), Tile("fT1")]
                Trl = [Tile("rl0"), Tile("rl1")]

                for i_ in range(2):
                    merge_tile(TF[i_], TS_, as_write=True)
                    merge_tile(Trl[i_], TR_, as_write=True)

                def ffn_up(g):
                    up = stream.next("%d_up_%d" % (l, g))
                    pv = wview(up, "up_0")
                    fTg = fT[g % 2]
                    for c in range(4):
                        ps = nps()
                        fm_chunk(up, pv, c, ps, hsrc=hTb)
                        rlb = rl[c % 2]
                        act(rlb, ps[:, 0:T], AF.Relu, [ps], [Trl[c % 2]])
                        tt(fTg[:, c, :], rlb, rlb, ALU.mult, [Trl[c % 2]], [TF[g % 2]], eng="gpsimd")

                def ffn_dn(g):
                    dn = stream.next("%d_dn_%d" % (l, g))
                    dv = wview(dn, "dn_0")
                    fTg = fT[g % 2]
                    for s in range(NSUB):
                        for hf in range(2):
                            for c in range(4):
                                mm(dbanks[s][hf][:, 0:512], fTg[:, c, s * 128:(s + 1) * 128], dv[:, c, hf * 512:(hf + 1) * 512],
                                   g == 0 and c == 0, g == 7 and c == 3, [dn, TF[g % 2]], [dbanks[s][hf]])

                if j + 1 < NT:
                    prologue(j + 1)
                ffn_up(0)
                for g in range(8):
                    if g + 1 < 8:
                        ffn_up(g + 1)
                    ffn_dn(g)
                for i_ in range(2):
                    merge_tile(TS_, TF[i_])
                    merge_tile(TR_, Trl[i_])
                for s in range(NSUB):
                    post_norm_residual(dbanks[s], D, s)
                dma(gcur[:], gb_in[l, :, 2 * D:3 * D], [], [gcur], gsem, eng="gpsimd")
                if first_tile:
                    dump("x2", xt[:, 0, :], [xt])
                norm_transpose(None, hTb, xt)
                plp = stream.next("%d_ple" % l)
                plv = wview(plp, "ple")
                pg = [stream.next("%d_pg_%d" % (l, hf), look=NSLOT - 2 - hf) for hf in range(2)]
                ple2 = big[:, 0:2 * D].rearrange("p (s d) -> p s d", s=2)
                sspa = []
                for s in range(NSUB):
                    ssp = []
                    for hf in range(2):
                        pa = PS[4 * (s % 2) + hf]
                        pgb = PS[4 * (s % 2) + 2 + hf]
                        for kc in range(2):
                            mm(pa[:, 0:512], pT[:, kc, s * 128:(s + 1) * 128], plv[:, kc, hf * 512:(hf + 1) * 512], kc == 0, kc == 1,
                               [plp, pT], [pa])
                        gv = wview(pg[hf], "pg_0")
                        for kc in range(8):
                            mm(pgb[:, 0:512], hTb[:, kc, s * 128:(s + 1) * 128], gv[:, kc, :], kc == 0, kc == 7, [pg[hf], hTb], [pgb])
                        act(sg, pgb[:, 0:512], AF.Sigmoid, [pgb], [TR_])
                        tt(ple2[:, s, hf * 512:(hf + 1) * 512], pa[:, 0:512], sg, ALU.mult, [pa, TR_], [TS_])
                        a = sm()
                        act(tmpf[:, 0:512], ple2[:, s, hf * 512:(hf + 1) * 512], AF.Square, [TS_, small], [tmpf, small], accum_out=a)
                        ssp.append(a)
                    sspa.append(ssp)
                for s in range(NSUB):
                    s3 = sm()
                    tt(s3, sspa[s][0], sspa[s][1], ALU.add, [small], [small])
                    rs = rstd_from_ss(s3, D)
                    for hf in range(2):
                        stt(tmpf[:, 0:512], ple2[:, s, hf * 512:(hf + 1) * 512], rs, gcur[:, hf * 512:(hf + 1) * 512],
                            ALU.mult, ALU.mult, [TS_, small, gcur], [tmpf])
                        tt(xt[:, s, hf * 512:(hf + 1) * 512], xt[:, s, hf * 512:(hf + 1) * 512], tmpf[:, 0:512], ALU.add,
                           [xt, tmpf], [xt])
                if j + 1 < NT:
                    dma(gcur[:], gb_in[l, :, 0:D], [], [gcur], gsem, eng="gpsimd")
                wt = [XT[j]] if XT is not None else [out_tile]
                dma(dst_x[t0:t0 + T, :].rearrange("(s p) d -> p s d", p=128), xt[:], [xt], wt, ssem, eng="gpsimd")
            XTprev = XT

        fw.finish("sync", [out_tile] + dbg_tiles)
        fw.emit()
    return nc, fw


def _consts():
    bf = ml_dtypes.bfloat16
    ident = np.eye(128, dtype=np.float32)
    Rm = np.zeros((128, 128), np.float32)
    for base in (0, 64):
        for d in range(8):
            Rm[base + d + 8, base + d] = -1.0
            Rm[base + d, base + d + 8] = 1.0
    esel = np.zeros((128, 4, 128), np.float32)
    for g in range(8):
        esel[g, g // 2, (g % 2) * 64:(g % 2 + 1) * 64] = 1.0
    cb = np.concatenate([ident, Rm, esel.reshape(128, 512)], axis=1).astype(bf)
    tt_, ss_ = np.meshgrid(np.arange(128), np.arange(128), indexing="ij")
    negmask = np.where(ss_ > tt_, np.float32(NEG), np.float32(0.0))
    posfill = np.where(ss_ > tt_, np.float32(-2 * NEG), np.float32(0.0))
    tril01 = (tt_ <= ss_).astype(np.float32)
    half = 8
    inv_freq = (np.float32(500000.0) ** (-np.arange(half, dtype=np.float32) * np.float32(2.0) / np.float32(16))).astype(np.float32)
    invf = np.zeros((128, 1), np.float32)
    for f in range(128):
        d = f % 64
        if d < 16:
            invf[f, 0] = inv_freq[d % 8]
    cf = np.concatenate([ident, negmask, posfill, tril01, invf], axis=1).astype(np.float32)
    return cb, cf


def _layout_params(inp, depth):
    f = np.float32
    cols = np.zeros((depth, 128, 48), f)
    gb = np.zeros((depth, 128, 3 * D + 512), f)
    wbd = np.zeros((depth, 128, 8, 128), f)
    wsp = np.zeros((depth, 128, 8, 128), f)
    for l in range(depth):
        cols[l, :, 0:8] = np.asarray(inp["g_pre_mix"][l], f).reshape(8, 128).T
        cols[l, :, 8:16] = np.asarray(inp["g_pre_ffn"][l], f).reshape(8, 128).T
        cw = np.asarray(inp["conv_w"][l], f)
        for jj in range(4):
            cols[l, :, 16 + jj * 4:20 + jj * 4] = cw[jj].reshape(4, 128).T
        cols[l, :, 32:36] = np.asarray(inp["conv_b"][l], f).reshape(4, 128).T
        cols[l, :, 36:40] = np.asarray(inp["b_rg_a"][l], f).reshape(4, 128).T
        cols[l, :, 40:44] = np.asarray(inp["b_rg_x"][l], f).reshape(4, 128).T
        cols[l, :, 44:48] = np.asarray(inp["lru_lambda"][l], f).reshape(4, 128).T
        row = np.concatenate([np.asarray(inp["g_post_mix"][l], f), np.asarray(inp["g_post_ffn"][l], f),
                              np.asarray(inp["g_post_ple"][l], f), np.asarray(inp["g_gmlp_v"][l], f)])
        gb[l] = np.broadcast_to(row[None, :], (128, row.size))
        for gi, key in enumerate(("w_rg_a", "w_rg_x")):
            w = np.asarray(inp[key][l], f)
            for c in range(4):
                for hh in range(2):
                    wbd[l, hh * 64:(hh + 1) * 64, gi * 4 + c, hh * 64:(hh + 1) * 64] = w[c * 2 + hh]
        ws = np.asarray(inp["w_spatial"][l], f)
        wsp[l] = np.transpose(ws, (2, 0, 1))
    bsp = np.ascontiguousarray(np.asarray(inp["b_spatial"], f))
    return cols, gb, wbd, wsp, bsp


_CACHE = {}


def kernel(**inputs):
    depth = DEPTH
    x = np.asarray(inputs["x"], np.float32)
    B, L, _ = x.shape
    p = np.asarray(inputs["p"], np.float32)
    pos = np.asarray(inputs["positions"], np.int32)
    cb, cf = _consts()
    cols, gb, wbd, wsp, bsp = _layout_params(inputs, depth)
    shared = {
        "w_in": np.ascontiguousarray(np.asarray(inputs["w_in"], np.float32)),
        "w_branch": np.ascontiguousarray(np.asarray(inputs["w_branch"], np.float32)),
        "w_out": np.ascontiguousarray(np.asarray(inputs["w_out"], np.float32)),
        "w_ffn_up": np.ascontiguousarray(np.asarray(inputs["w_ffn_up"], np.float32)),
        "w_ffn_down": np.ascontiguousarray(np.asarray(inputs["w_ffn_down"], np.float32)),
        "w_ple": np.ascontiguousarray(np.asarray(inputs["w_ple"], np.float32)),
        "w_ple_gate": np.ascontiguousarray(np.asarray(inputs["w_ple_gate"], np.float32)),
        "cols": cols, "gb": gb, "wbd": wbd, "wsp": wsp, "bsp": bsp, "cb": cb, "cf": cf,
    }
    if L not in _CACHE:
        _CACHE[L] = build_program(L, depth)[0]
    nc = _CACHE[L]
    in_maps = []
    for b in range(B):
        m = dict(shared)
        m["x"] = np.ascontiguousarray(x[b])
        m["p"] = np.ascontiguousarray(p[:, b])
        m["pos"] = np.ascontiguousarray(np.broadcast_to(pos[b][None, :], (128, L)))
        in_maps.append(m)
    res = run_bass_kernel_spmd(nc, in_maps, core_ids=list(range(B)))
    return np.stack([np.asarray(r["y"], np.float32) for r in res.results], axis=0)
```
